# Optimizing a Trainium2 kernel written in Bass

```python
import jax, jax.numpy as jnp
from jax import lax
import numpy as np

D_MODEL = 1024
BATCH = 4
SEQ = 4096
DEPTH = 1

CTX_LEN = 256
GRID_W = 64
D_MIX = D_MODEL
HEAD_DIM = 64
ATT_HEADS = 8
ATT_KV_HEADS = 2
GQA_GROUP = ATT_HEADS // ATT_KV_HEADS
D_ATT = ATT_HEADS * HEAD_DIM
D_ATT_KV = ATT_KV_HEADS * HEAD_DIM
ATT_SCALE = HEAD_DIM ** -0.5
ROPE_THETA = 10000.0
ROPE_PAIRS = HEAD_DIM // 4
Q_BLOCK = 128
RWKV_HEADS = 8
RWKV_HEAD = 64
D_RWKV = RWKV_HEADS * RWKV_HEAD
D_DECAY_LORA = 64
D_AAA_LORA = 64
D_GATE_LORA = 128
D_RWKV_IN = 3 * D_RWKV + D_DECAY_LORA + D_AAA_LORA + D_GATE_LORA
D_IN = D_ATT + 2 * D_ATT_KV + D_RWKV_IN
ATT_CUTS = (D_ATT, D_ATT + D_ATT_KV, D_ATT + 2 * D_ATT_KV)
RWKV_CUTS = (D_RWKV, 2 * D_RWKV, 3 * D_RWKV, 3 * D_RWKV + D_DECAY_LORA,
             3 * D_RWKV + D_DECAY_LORA + D_AAA_LORA)
N_EXPERTS = 16
CAPACITY_FACTOR = 2
D_EXPERT = D_MODEL
N_MOD = 6
LN_EPS = 1e-5
RMS_EPS = 1e-6
GN_EPS = 64e-5
L2_EPS = 1e-12
ALPHA = (2.0 * DEPTH) ** 0.25
BETA = (8.0 * DEPTH) ** -0.25

kernel_name = 'hybrid_gqa_rwkv7_ec_dit_layer'


def layer_norm(x):
    xf = x.astype(jnp.float32)
    mu = jnp.mean(xf, -1, keepdims=True)
    var = jnp.mean(jnp.square(xf - mu), -1, keepdims=True)
    return ((xf - mu) * lax.rsqrt(var + LN_EPS)).astype(x.dtype)


def layer_norm_affine(x, g, b):
    return layer_norm(x) * g + b


def modulate(h, shift, scale):
    return h * (1.0 + scale) + shift


def to_heads(t, n):
    return t.reshape(t.shape[:-1] + (n, t.shape[-1] // n))


def head_rms_norm(t, gain):
    tf = t.astype(jnp.float32)
    return (tf * lax.rsqrt(jnp.mean(tf * tf, -1, keepdims=True) + RMS_EPS) * gain).astype(t.dtype)


def axial_rope_tables(n_tokens):
    rows = n_tokens // GRID_W
    row = jnp.repeat(jnp.arange(rows), GRID_W)
    col = jnp.tile(jnp.arange(GRID_W), rows)
    inv = ROPE_THETA ** (-jnp.arange(ROPE_PAIRS, dtype=jnp.float32) / ROPE_PAIRS)
    ang = jnp.stack([row, col], -1).astype(jnp.float32)[:, :, None] * inv
    return jnp.cos(ang), jnp.sin(ang)


def apply_axial_rope(t, cos, sin):
    B, L, H, _ = t.shape
    tf = t.astype(jnp.float32).reshape(B, L, H, 2, 2, ROPE_PAIRS)
    t1, t2 = tf[..., 0, :], tf[..., 1, :]
    cs, sn = cos[None, :, None], sin[None, :, None]
    out = jnp.stack([t1 * cs - t2 * sn, t2 * cs + t1 * sn], axis=-2)
    return out.reshape(B, L, H, HEAD_DIM).astype(t.dtype)


def gqa_attend(q, k, v):
    B, Tq = q.shape[:2]
    qg = q.reshape(B, Tq, ATT_KV_HEADS, GQA_GROUP, HEAD_DIM)
    s = jnp.einsum('bqkgd,bskd->bkgqs', qg, k).astype(jnp.float32) * ATT_SCALE
    p = jax.nn.softmax(s, axis=-1).astype(v.dtype)
    o = jnp.einsum('bkgqs,bskd->bqkgd', p, v)
    return o.reshape(B, Tq, D_ATT)


def token_shift(p, mu):
    pad = jnp.pad(p, ((0, 0), (1, 1), (0, 0)))
    return p + (0.5 * (pad[:, :-2] + pad[:, 2:]) - p) * mu


def rwkv_inputs(rw, decay_w0, decay_up, iclr_a0, iclr_up, gate_up, k_k, k_a):
    f32 = jnp.float32
    r, k, v, wl, al, gl = jnp.split(rw, RWKV_CUTS, axis=-1)
    g = jax.nn.sigmoid(gl) @ gate_up
    kk = to_heads(k * k_k, RWKV_HEADS).astype(f32)
    kk = kk / jnp.maximum(jnp.sqrt(jnp.sum(kk * kk, -1, keepdims=True)), L2_EPS)
    wt = jnp.tanh(wl)
    dirs = []
    for d in range(2):
        w = -jax.nn.softplus(-(decay_w0[d] + wt @ decay_up[d])) - 0.5
        decay = jnp.exp(-jnp.exp(to_heads(w, RWKV_HEADS).astype(f32)))
        a = jax.nn.sigmoid(iclr_a0[d] + al @ iclr_up[d])
        kd = to_heads(k * (1.0 + (a - 1.0) * k_a), RWKV_HEADS).astype(f32)
        a_h = to_heads(a, RWKV_HEADS).astype(f32)
        dirs.append((decay, kd, -kk, kk * a_h))
    r_h = to_heads(r, RWKV_HEADS).astype(f32)
    k_h = to_heads(k, RWKV_HEADS).astype(f32)
    v_h = to_heads(v, RWKV_HEADS).astype(f32)
    return r_h, k_h, v_h, g, dirs


def wkv7_scan(r, decay, k, v, a, b, s0, reverse, return_y):
    xs = tuple(jnp.moveaxis(t, 1, 0) for t in (r, decay, k, v, a, b))

    def step(s, inp):
        rt, wt, kt, vt, at, bt = inp
        sa = jnp.einsum('bhvk,bhk->bhv', s, at)
        s = s * wt[:, :, None, :] + sa[..., None] * bt[:, :, None, :] + vt[..., None] * kt[:, :, None, :]
        y = jnp.einsum('bhvk,bhk->bhv', s, rt) if return_y else None
        return s, y

    s_fin, ys = lax.scan(step, s0, xs, reverse=reverse)
    return s_fin, (jnp.moveaxis(ys, 0, 1) if return_y else None)


def rwkv_output(y, r, k, v, g, r_k, lnx_g, lnx_b):
    B, T = y.shape[:2]
    mu = jnp.mean(y, -1, keepdims=True)
    var = jnp.mean(jnp.square(y - mu), -1, keepdims=True)
    yn = ((y - mu) * lax.rsqrt(var + GN_EPS)).reshape(B, T, D_RWKV) * lnx_g + lnx_b
    bonus = (jnp.sum(r * k * r_k, -1, keepdims=True) * v).reshape(B, T, D_RWKV)
    return (yn + bonus) * g


def token_mixer(h_lat, h_ctx, w_in, q_gain, k_gain, tshift_mu, decay_w0, decay_up, iclr_a0, iclr_up,
                gate_up, k_k, k_a, r_k, lnx_g, lnx_b, w_out, with_ctx_out):
    B, L, _ = h_lat.shape
    q_l, k_l, v_l, rw_l = jnp.split(h_lat @ w_in, ATT_CUTS, axis=-1)
    q_c, k_c, v_c, rw_c = jnp.split(h_ctx @ w_in, ATT_CUTS, axis=-1)

    cos, sin = axial_rope_tables(L)
    q_l = apply_axial_rope(head_rms_norm(to_heads(q_l, ATT_HEADS), q_gain), cos, sin)
    k_l = apply_axial_rope(head_rms_norm(to_heads(k_l, ATT_KV_HEADS), k_gain), cos, sin)
    k_c = head_rms_norm(to_heads(k_c, ATT_KV_HEADS), k_gain)
    v_l = to_heads(v_l, ATT_KV_HEADS)
    v_c = to_heads(v_c, ATT_KV_HEADS)
    k_all = jnp.concatenate([k_l, k_c], axis=1)
    v_all = jnp.concatenate([v_l, v_c], axis=1)
    n_blk = L // Q_BLOCK
    q_blocks = jnp.moveaxis(q_l.reshape(B, n_blk, Q_BLOCK, ATT_HEADS, HEAD_DIM), 1, 0)
    att_l = lax.map(lambda qb: gqa_attend(qb, k_all, v_all), q_blocks)
    att_l = jnp.moveaxis(att_l, 0, 1).reshape(B, L, D_ATT)

    rwkv_p = (decay_w0, decay_up, iclr_a0, iclr_up, gate_up, k_k, k_a)
    r_c, kb_c, vr_c, g_c, dirs_c = rwkv_inputs(token_shift(rw_c, tshift_mu), *rwkv_p)
    r_l, kb_l, vr_l, g_l, dirs_l = rwkv_inputs(token_shift(rw_l, tshift_mu), *rwkv_p)
    s0 = jnp.zeros((h_ctx.shape[0], RWKV_HEADS, RWKV_HEAD, RWKV_HEAD), jnp.float32)
    y_l, y_c = None, None
    for d, reverse in enumerate((False, True)):
        dc, kc, ac, bc = dirs_c[d]
        s_c, yc = wkv7_scan(r_c, dc, kc, vr_c, ac, bc, s0, reverse, with_ctx_out)
        dl, kl, al, bl = dirs_l[d]
        _, yl = wkv7_scan(r_l, dl, kl, vr_l, al, bl, s_c, reverse, True)
        y_l = yl if y_l is None else y_l + yl
        if with_ctx_out:
            y_c = yc if y_c is None else y_c + yc
    rw_out_l = rwkv_output(y_l, r_l, kb_l, vr_l, g_l, r_k, lnx_g, lnx_b).astype(h_lat.dtype)
    out_l = jnp.concatenate([att_l, rw_out_l], axis=-1) @ w_out
    if not with_ctx_out:
        return out_l, None
    q_c = head_rms_norm(to_heads(q_c, ATT_HEADS), q_gain)
    att_c = gqa_attend(q_c, k_c, v_c)
    rw_out_c = rwkv_output(y_c, r_c, kb_c, vr_c, g_c, r_k, lnx_g, lnx_b).astype(h_ctx.dtype)
    out_c = jnp.concatenate([att_c, rw_out_c], axis=-1) @ w_out
    return out_l, out_c


def expert_choice_ffn(h, router_w, w_gate, w_up, w_down):
    B, T, D = h.shape
    cap = CAPACITY_FACTOR * T // N_EXPERTS
    logits = jnp.einsum('btd,de->bte', h, router_w).astype(jnp.float32)
    aff = jax.nn.softmax(logits, axis=-1)
    gate, idx = lax.top_k(jnp.swapaxes(aff, 1, 2), cap)
    xin = jax.vmap(lambda hb, ib: hb[ib])(h, idx)
    hid = jax.nn.silu(jnp.einsum('becd,edf->becf', xin, w_gate)) * jnp.einsum('becd,edf->becf', xin, w_up)
    y = jnp.einsum('becf,efd->becd', hid, w_down) * gate[..., None].astype(h.dtype)
    return jax.vmap(lambda ib, yb: jnp.zeros((T, D), h.dtype).at[ib.reshape(-1)].add(yb.reshape(-1, D)))(idx, y)


def setup_inputs(seed: int = 0) -> dict:
    key = jax.random.key(seed)
    ks = jax.random.split(key, 32)
    f32 = jnp.float32
    nl = DEPTH

    def nrm(k, shape, s):
        return jax.random.normal(k, shape, f32) * s

    return {
        'x': nrm(ks[0], (BATCH, SEQ, D_MODEL), 1.0),
        'c': nrm(ks[1], (BATCH, D_MODEL), 1.0),
        'ctx': nrm(ks[2], (BATCH, CTX_LEN, D_MODEL), 1.0),
        'c_ctx': nrm(ks[3], (D_MODEL,), 1.0),
        'w_ada': nrm(ks[4], (nl, D_MODEL, N_MOD * D_MODEL), 0.02),
        'b_ada': nrm(ks[5], (nl, N_MOD * D_MODEL), 0.02),
        'w_in': nrm(ks[6], (nl, D_MODEL, D_IN), D_MODEL ** -0.5),
        'q_gain': 1.0 + nrm(ks[7], (nl, HEAD_DIM), 0.1),
        'k_gain': 1.0 + nrm(ks[8], (nl, HEAD_DIM), 0.1),
        'tshift_mu': jax.random.uniform(ks[9], (nl, D_RWKV_IN), f32),
        'decay_w0': jax.random.uniform(ks[10], (nl, 2, D_RWKV), f32, -6.0, -1.0),
        'decay_up': nrm(ks[11], (nl, 2, D_DECAY_LORA, D_RWKV), 0.1 * D_DECAY_LORA ** -0.5),
        'iclr_a0': nrm(ks[12], (nl, 2, D_RWKV), 0.5),
        'iclr_up': nrm(ks[13], (nl, 2, D_AAA_LORA, D_RWKV), D_AAA_LORA ** -0.5),
        'gate_up': nrm(ks[14], (nl, D_GATE_LORA, D_RWKV), D_GATE_LORA ** -0.5),
        'k_k': 0.85 + nrm(ks[15], (nl, D_RWKV), 0.1),
        'k_a': 1.0 + nrm(ks[16], (nl, D_RWKV), 0.1),
        'r_k': nrm(ks[17], (nl, RWKV_HEADS, RWKV_HEAD), 0.1),
        'lnx_g': 1.0 + nrm(ks[18], (nl, D_RWKV), 0.1),
        'lnx_b': nrm(ks[19], (nl, D_RWKV), 0.02),
        'w_out': nrm(ks[20], (nl, D_MIX, D_MODEL), BETA * D_MIX ** -0.5),
        'ln1_g': 1.0 + nrm(ks[21], (nl, D_MODEL), 0.1),
        'ln1_b': nrm(ks[22], (nl, D_MODEL), 0.02),
        'router_w': nrm(ks[23], (nl, D_MODEL, N_EXPERTS), D_MODEL ** -0.5),
        'exp_w_gate': nrm(ks[24], (nl, N_EXPERTS, D_MODEL, D_EXPERT), D_MODEL ** -0.5),
        'exp_w_up': nrm(ks[25], (nl, N_EXPERTS, D_MODEL, D_EXPERT), D_MODEL ** -0.5),
        'exp_w_down': nrm(ks[26], (nl, N_EXPERTS, D_EXPERT, D_MODEL), BETA * D_EXPERT ** -0.5),
        'ln2_g': 1.0 + nrm(ks[27], (nl, D_MODEL), 0.1),
        'ln2_b': nrm(ks[28], (nl, D_MODEL), 0.02),
    }


def reference(x, c, ctx, c_ctx, w_ada, b_ada, w_in, q_gain, k_gain, tshift_mu, decay_w0, decay_up,
              iclr_a0, iclr_up, gate_up, k_k, k_a, r_k, lnx_g, lnx_b, w_out, ln1_g, ln1_b,
              router_w, exp_w_gate, exp_w_up, exp_w_down, ln2_g, ln2_b):
    for li in range(DEPTH):
        last = li == DEPTH - 1
        mod = jax.nn.silu(c) @ w_ada[li] + b_ada[li]
        sh1, sc1, gt1, sh2, sc2, gt2 = jnp.split(mod[:, None, :], N_MOD, axis=-1)
        mc = jnp.split(jax.nn.silu(c_ctx) @ w_ada[li] + b_ada[li], N_MOD, axis=-1)
        h_lat = modulate(layer_norm(x), sh1, sc1)
        h_ctx = modulate(layer_norm(ctx), mc[0], mc[1])
        mix_l, mix_c = token_mixer(h_lat, h_ctx, w_in[li], q_gain[li], k_gain[li], tshift_mu[li],
                                   decay_w0[li], decay_up[li], iclr_a0[li], iclr_up[li], gate_up[li],
                                   k_k[li], k_a[li], r_k[li], lnx_g[li], lnx_b[li], w_out[li],
                                   not last)
        x = layer_norm_affine(ALPHA * x + gt1 * mix_l, ln1_g[li], ln1_b[li])
        h2 = modulate(layer_norm(x), sh2, sc2)
        ffn_l = expert_choice_ffn(h2, router_w[li], exp_w_gate[li], exp_w_up[li], exp_w_down[li])
        x = layer_norm_affine(ALPHA * x + gt2 * ffn_l, ln2_g[li], ln2_b[li])
        if not last:
            ctx = layer_norm_affine(ALPHA * ctx + mc[2] * mix_c, ln1_g[li], ln1_b[li])
            h2c = modulate(layer_norm(ctx), mc[3], mc[4])
            ffn_c = expert_choice_ffn(h2c, router_w[li], exp_w_gate[li], exp_w_up[li], exp_w_down[li])
            ctx = layer_norm_affine(ALPHA * ctx + mc[5] * ffn_c, ln2_g[li], ln2_b[li])
    return x
```

```python
import contextlib
import os
import numpy as np
import concourse.bass as bass
import concourse.mybir as mybir
from concourse.bass_utils import run_bass_kernel_spmd

F32 = mybir.dt.float32
BF16 = mybir.dt.bfloat16
AF = mybir.ActivationFunctionType
ALU = mybir.AluOpType
AX = mybir.AxisListType

D = 1024
TL = 4096
TC = 256
TT = TL + TC
NT = TT // 128
ALPHA = 2.0 ** 0.25
DEC_C = -float(np.exp(-0.5))


class Sched:
    CE = ('pe', 'act', 'dve', 'pool')

    def __init__(self, nc, stack, ndma=32):
        self.nc = nc
        self.ops = {e: [] for e in ('pe', 'act', 'dve', 'pool', 'sp')}
        self.cnt = {e: 0 for e in self.CE}
        self.last_w = {}
        self.readers = {}
        self.waited = {e: {} for e in self.ops}
        self.ndma = ndma
        self.dma_val = [0] * ndma
        self.dma_i = 0
        names = list(self.CE) + ['d%d' % i for i in range(ndma)]
        self.sems = {n: stack.enter_context(nc.semaphore('s_' + n)) for n in names}

    def _deps(self, eng, reads, writes):
        deps = {}

        def add(tok):
            if tok is None:
                return
            s, v = tok
            if deps.get(s, 0) < v:
                deps[s] = v
        for r in reads:
            add(self.last_w.get(r))
        for w in writes:
            add(self.last_w.get(w))
            for t in self.readers.get(w, ()):
                add(t)
        waits = []
        for s, v in deps.items():
            if s == eng and eng == 'pe':
                continue
            if self.waited[eng].get(s, 0) >= v:
                continue
            self.waited[eng][s] = v
            waits.append((s, v))
        return waits

    def _commit(self, tok, reads, writes):
        for r in reads:
            self.readers.setdefault(r, []).append(tok)
        for w in writes:
            self.last_w[w] = tok
            self.readers[w] = []

    def op(self, eng, fn, reads=(), writes=()):
        waits = self._deps(eng, reads, writes)
        self.cnt[eng] += 1
        tok = (eng, self.cnt[eng])
        self.ops[eng].append((waits, fn, (eng, 1)))
        self._commit(tok, reads, writes)

    def dma(self, fn, reads=(), writes=(), q='sp'):
        slot = self.dma_i % self.ndma
        self.dma_i += 1
        s = 'd%d' % slot
        waits = self._deps(q, reads, writes)
        pv = self.dma_val[slot]
        if pv > 0 and self.waited[q].get(s, 0) < pv:
            self.waited[q][s] = pv
            waits.append((s, pv))
        self.dma_val[slot] = pv + 16
        tok = (s, pv + 16)
        self.ops[q].append((waits, fn, (s, 16)))
        self._commit(tok, reads, writes)

    def barrier(self):
        allw = [(e, c) for e, c in self.cnt.items() if c > 0]
        allw += [('d%d' % i, v) for i, v in enumerate(self.dma_val) if v > 0]
        for eng in self.ops:
            waits = []
            for s, v in allw:
                if self.waited[eng].get(s, 0) >= v:
                    continue
                self.waited[eng][s] = v
                waits.append((s, v))
            if waits:
                self.ops[eng].append((waits, None, None))
        self.last_w = {}
        self.readers = {}

    def flush(self):
        nc = self.nc
        sems = self.sems
        ops = self.ops
        self.ops = {e: [] for e in ops}
        with nc.Block() as block:
            def run(engname, engobj):
                for waits, fn, inc in ops[engname]:
                    for ws, wv in waits:
                        engobj.wait_ge(sems[ws], wv)
                    if fn is not None:
                        ins = fn(engobj)
                        ins.then_inc(sems[inc[0]], inc[1])

            @block.sync
            def _(e):
                run('sp', e)

            @block.tensor
            def _(e):
                run('pe', e)

            @block.scalar
            def _(e):
                run('act', e)

            @block.vector
            def _(e):
                run('dve', e)

            @block.gpsimd
            def _(e):
                run('pool', e)


def build_nc(stop_after=99, dbg=False):
    nc = bass.Bass("TRN2", target_bir_lowering=False)

    def din(name, shape, dt=F32):
        return nc.dram_tensor(name, list(shape), dt, kind="ExternalInput").ap()

    def dscr(name, shape, dt=F32):
        return nc.dram_tensor(name, list(shape), dt, kind="Internal").ap()

    x_d = din("x", [TL, D]); ctx_d = din("ctx", [TC, D])
    cc_d = din("cc", [128, 16])
    wada_d = din("w_ada", [D, 6 * D]); bada_d = din("b_ada", [1, 6 * D])
    win_d = din("w_in", [D, 2560])
    qg_d = din("qg", [1, 512]); kg_d = din("kg", [1, 128])
    cos_d = din("cosT", [TL, 512]); sin_d = din("sinS", [TL, 512])
    ident_d = din("ident", [128, 128])
    rp_d = din("rp", [64, 8 * 10])
    lmu_d = din("lmu", [128, 3])
    decup_d = din("decup", [64, 2 * 512]); iclup_d = din("iclup", [64, 2 * 512]); gateup_d = din("gateup", [128, 512])
    mk4_d = din("mk4", [128, 2 * 512]); mn1_d = din("mn1", [128, 2 * 128]); bm_d = din("bm", [128, 512])
    rst_d = din("rst", [64, 1024])
    wout_d = din("w_out", [D, D]); lnx_d = din("lnx", [2, 512])
    ln1_d = din("ln1", [2, D]); ln2_d = din("ln2", [2, D])
    rw_d = din("router_w", [D, 16])
    x1D = dscr("x1D", [TL, D]); h2D = dscr("h2D", [TL, D], BF16)
    wg_d = din("exp_w_gate", [16, D, D]); wu_d = din("exp_w_up", [16, D, D]); wd_d = din("exp_w_down", [16, D, D])
    iot_d = din("iot", [128, 512 + 4]); ustr_d = din("ustr", [128, 128])
    ywD = dscr("ywD", [16, 512, D], BF16)
    ydir = dscr("ydir", [2, TL, 512]); bonD = dscr("bonD", [TL, 512]); gD = dscr("gD", [TL, 512])
    out_d = nc.dram_tensor("out", [TL, D], F32, kind="ExternalOutput").ap()
    modd = dscr("modd", [2, 6 * D])
    rwT = dscr("rwT", [1792, TT])
    dbg_outs = {}

    def dout(name, shape, dt=F32):
        ap = nc.dram_tensor(name, list(shape), dt, kind="ExternalOutput").ap()
        dbg_outs[name] = ap
        return ap

    with contextlib.ExitStack() as top:
        S = Sched(nc, top)

        def sb(stack, name, shape, dt):
            return stack.enter_context(nc.sbuf_tensor("sb_" + name, list(shape), dt))

        PS = top.enter_context(nc.psum_tensor("PS", [128, 8 * 512], F32))
        P = [PS[:, i * 512:(i + 1) * 512] for i in range(8)]
        PK = ['P%d' % i for i in range(8)]
        identf = sb(top, "identf", [128, 128], F32)
        identb = sb(top, "identb", [128, 128], BF16)
        eps5 = sb(top, "eps5", [128, 1], F32)
        eps6 = sb(top, "eps6", [128, 1], F32)
        S.dma(lambda e: e.dma_start(out=identf[:], in_=ident_d[:, :]), writes=['identf'])
        S.op('dve', lambda e: e.tensor_copy(out=identb[:], in_=identf[:]), reads=['identf'], writes=['identb'])
        S.op('pool', lambda e: e.memset(eps5[:], 1e-5), writes=['eps5'])
        S.op('pool', lambda e: e.memset(eps6[:], 1e-6), writes=['eps6'])

        def ln_stats(stack_tiles, src, key_src, eps_t, eps_key):
            stats, mv, rstd, nb, kq = stack_tiles
            for cch in range(2):
                S.op('dve', lambda e, cch=cch: e.bn_stats(out=stats[:, cch, :], in_=src[:, cch * 512:(cch + 1) * 512]),
                     reads=[key_src], writes=[kq + 'stats'])
            S.op('dve', lambda e: e.bn_aggr(out=mv[:], in_=stats[:]), reads=[kq + 'stats'], writes=[kq + 'mv'])
            S.op('act', lambda e: e.activation(out=rstd[:], in_=mv[:, 1:2], func=AF.Ln, bias=eps_t[:], scale=1.0),
                 reads=[kq + 'mv', eps_key], writes=[kq + 'rstd'])
            S.op('act', lambda e: e.activation(out=rstd[:], in_=rstd[:], func=AF.Exp, scale=-0.5),
                 reads=[kq + 'rstd'], writes=[kq + 'rstd'])
            S.op('dve', lambda e: e.scalar_tensor_tensor(out=nb[:], in0=mv[:, 0:1], scalar=-1.0, in1=rstd[:],
                                                        op0=ALU.mult, op1=ALU.mult),
                 reads=[kq + 'mv', kq + 'rstd'], writes=[kq + 'nb'])

        with contextlib.ExitStack() as ph:
            cc = sb(ph, "cc", [128, 8, 2], F32)
            ccs = sb(ph, "ccs", [128, 8, 2], F32)
            wst = [sb(ph, "wst%d" % i, [128, 8, 512], F32) for i in range(2)]
            bada = sb(ph, "bada", [2, 6 * D], F32)
            modrow = sb(ph, "modrow", [2, 6 * D], F32)
            S.dma(lambda e: e.dma_start(out=cc[:].rearrange("p a b -> p (a b)"), in_=cc_d[:, :]), writes=['cc'])
            S.dma(lambda e: e.dma_start(out=bada[:], in_=bada_d.partition_broadcast(2)), writes=['bada'])
            S.op('act', lambda e: e.activation(out=ccs[:], in_=cc[:], func=AF.Silu), reads=['cc'], writes=['ccs'])
            for g in range(12):
                w = wst[g % 2]
                wk = 'wst%d' % (g % 2)
                S.dma(lambda e, w=w, g=g: e.dma_start(
                    out=w[:], in_=wada_d[:, g * 512:(g + 1) * 512].rearrange("(j p) n -> p j n", p=128)), writes=[wk])
                for j in range(8):
                    S.op('pe', lambda e, w=w, j=j: e.matmul(P[0][0:2, :], lhsT=ccs[:, j, :], rhs=w[:, j, :],
                                                           start=(j == 0), stop=(j == 7)),
                         reads=['ccs', wk], writes=['P0'])
                S.op('dve', lambda e, g=g: e.tensor_tensor(out=modrow[:, g * 512:(g + 1) * 512], in0=P[0][0:2, :],
                                                          in1=bada[:, g * 512:(g + 1) * 512], op=ALU.add),
                     reads=['P0', 'bada'], writes=['modrow'])
            for lo in (1024, 4096):
                S.op('dve', lambda e, lo=lo: e.tensor_scalar_add(out=modrow[:, lo:lo + 1024], in0=modrow[:, lo:lo + 1024],
                                                                scalar1=1.0), reads=['modrow'], writes=['modrow'])
            S.dma(lambda e: e.dma_start(out=modd[:, :], in_=modrow[:]), reads=['modrow'], writes=['modd'])
            if dbg:
                o = dout("dbg_mod", [2, 6 * D])
                S.dma(lambda e: e.dma_start(out=o[:, :], in_=modrow[:]), reads=['modrow'])
            S.barrier()
            S.flush()
        if stop_after <= 0:
            return nc, dbg_outs

        def modrow_bc(dst, row, lo):
            S.dma(lambda e: e.dma_start(out=dst[:], in_=modd[row:row + 1, lo:lo + 1024].partition_broadcast(128)),
                  reads=['modd'], writes=[dst.name if hasattr(dst, 'name') else 'x'])

        aff_all = sb(top, "aff_all", [128, 32, 16], F32)
        stAtt = contextlib.ExitStack()
        attT_all = sb(stAtt, "attT_all", [128, 4, TL], BF16)
        with contextlib.ExitStack() as phAB:
            qT_all = sb(phAB, "qT_all", [128, 4, TL], BF16)
            kTd = sb(phAB, "kTd", [128, 2, TT], BF16)
            Vx = sb(phAB, "Vx", [128, NT, 2, 80], BF16)
            with contextlib.ExitStack() as ph:
                winb = sb(ph, "winb", [128, 8, 2560], BF16)
                wstg = [sb(ph, "wstg%d" % i, [128, 640], F32) for i in range(2)]
                sc1p = sb(ph, "sc1p", [128, D], F32); sh1 = sb(ph, "sh1", [128, D], F32)
                csc1p = sb(ph, "csc1p", [128, D], F32); csh1 = sb(ph, "csh1", [128, D], F32)
                qg = sb(ph, "qg", [128, 512], F32); kg = sb(ph, "kg", [128, 128], F32)
                for dst, nm, row, lo in ((sh1, 'sh1', 0, 0), (sc1p, 'sc1p', 0, 1024), (csh1, 'csh1', 1, 0), (csc1p, 'csc1p', 1, 1024)):
                    S.dma(lambda e, dst=dst, row=row, lo=lo: e.dma_start(
                        out=dst[:], in_=modd[row:row + 1, lo:lo + 1024].partition_broadcast(128)), reads=['modd'], writes=[nm])
                S.dma(lambda e: e.dma_start(out=qg[:], in_=qg_d.partition_broadcast(128)), writes=['qg'])
                S.dma(lambda e: e.dma_start(out=kg[:], in_=kg_d.partition_broadcast(128)), writes=['kg'])
                for jj in range(32):
                    j = jj // 4; c0 = (jj % 4) * 640
                    w = wstg[jj % 2]; wk = 'wstg%d' % (jj % 2)
                    S.dma(lambda e, w=w, j=j, c0=c0: e.dma_start(out=w[:], in_=win_d[j * 128:(j + 1) * 128, c0:c0 + 640]), writes=[wk])
                    S.op('pool' if jj % 2 else 'dve', lambda e, w=w, j=j, c0=c0: e.tensor_copy(out=winb[:, j, c0:c0 + 640], in_=w[:]),
                         reads=[wk], writes=['winb'])
                S.op('pool', lambda e: e.memset(Vx[:], 1.0), writes=['Vx'])
                xt = [sb(ph, "xt%d" % i, [128, D], F32) for i in range(2)]
                xn = sb(ph, "xn", [128, D], F32)
                hb = sb(ph, "hb", [128, D], BF16)
                hT = sb(ph, "hT", [128, 8, 512], BF16)
                stats = sb(ph, "stats", [128, 2, 6], F32); mv = sb(ph, "mv", [128, 2], F32)
                rstd = sb(ph, "rstd", [128, 1], F32); nb = sb(ph, "nb", [128, 1], F32)
                lnt = (stats, mv, rstd, nb, 'A')
                rstg = [sb(ph, "rstg%d" % i, [128, 512], F32) for i in range(2)]
                cosT = sb(ph, "cosT", [128, 512], F32); sinS = sb(ph, "sinS", [128, 512], F32)
                sq = sb(ph, "sq", [128, 512], F32)
                ssum = sb(ph, "ssum", [128, 8], F32)
                qn = sb(ph, "qn", [128, 512], F32); qa = sb(ph, "qa", [128, 512], F32)
                qbt = sb(ph, "qbt", [128, 512], F32)
                qo = sb(ph, "qo", [128, 512], BF16)
                ko = sb(ph, "ko", [128, 2, 2, 64], BF16)
                blocks = [(0, 256)] + [(256 + 512 * i, 512) for i in range(8)]
                ti = 0
                for (u0, ntok) in blocks:
                    ntile = ntok // 128
                    is_ctx = (u0 == 0)
                    for tl in range(ntile):
                        u = u0 + tl * 128
                        gt = u // 128
                        X = xt[ti % 2]; xk = 'xt%d' % (ti % 2)
                        ti += 1
                        src = ctx_d[u:u + 128, :] if is_ctx else x_d[u - TC:u - TC + 128, :]
                        S.dma(lambda e, X=X, src=src: e.dma_start(out=X[:], in_=src), writes=[xk])
                        ln_stats(lnt, X, xk, eps5, 'eps5')
                        S.op('act', lambda e, X=X: e.activation(out=xn[:], in_=X[:], func=AF.Identity, bias=nb[:], scale=rstd[:]),
                             reads=[xk, 'Anb', 'Arstd'], writes=['xn'])
                        scp, shh, k1, k2 = (csc1p, csh1, 'csc1p', 'csh1') if is_ctx else (sc1p, sh1, 'sc1p', 'sh1')
                        S.op('pool', lambda e, scp=scp: e.tensor_tensor(out=xn[:], in0=xn[:], in1=scp[:], op=ALU.mult),
                             reads=['xn', k1], writes=['xn'])
                        S.op('dve', lambda e, shh=shh: e.tensor_tensor(out=hb[:], in0=xn[:], in1=shh[:], op=ALU.add),
                             reads=['xn', k2], writes=['hb'])
                        pTb = P[0][:].bitcast(BF16)
                        for j in range(8):
                            S.op('pe', lambda e, j=j, pTb=pTb: e.transpose(out=pTb[:, j * 128:(j + 1) * 128],
                                                                      in_=hb[:, j * 128:(j + 1) * 128], identity=identb[:]),
                                 reads=['hb', 'identb'], writes=['P0'])
                        S.op('act', lambda e, tl=tl, pTb=pTb: e.copy(out=hT[:, :, tl * 128:(tl + 1) * 128],
                                                                 in_=pTb.rearrange("p (j t) -> p j t", j=8)),
                             reads=['P0'], writes=['hT'])
                        for j in range(8):
                            S.op('pe', lambda e, j=j, tl=tl: e.matmul(P[1][:, :], lhsT=hT[:, j, tl * 128:(tl + 1) * 128],
                                                                     rhs=winb[:, j, 0:512], start=(j == 0), stop=(j == 7)),
                                 reads=['hT', 'winb'], writes=['P1'])
                        for j in range(8):
                            S.op('pe', lambda e, j=j, tl=tl: e.matmul(P[2][:, 0:256], lhsT=hT[:, j, tl * 128:(tl + 1) * 128],
                                                                     rhs=winb[:, j, 512:768], start=(j == 0), stop=(j == 7)),
                                 reads=['hT', 'winb'], writes=['P2'])
                        S.op('act', lambda e, gt=gt: e.copy(out=Vx[:, gt, :, 0:64],
                                                          in_=P[2][:, 128:256].rearrange("p (a b) -> p a b", a=2)),
                             reads=['P2'], writes=['Vx'])
                        if not is_ctx:
                            ul = u - TC
                            S.dma(lambda e, ul=ul: e.dma_start(out=cosT[:], in_=cos_d[ul:ul + 128, :]), writes=['cosT'])
                            S.dma(lambda e, ul=ul: e.dma_start(out=sinS[:], in_=sin_d[ul:ul + 128, :]), writes=['sinS'])

                        def normrope(psrc, pk, nh, gain, gk, do_rope, outfn):
                            w_ = nh * 64
                            S.op('act', lambda e: e.activation(out=sq[:, 0:w_], in_=psrc, func=AF.Square),
                                 reads=[pk], writes=['sq'])
                            S.op('dve', lambda e: e.tensor_reduce(out=ssum[:, 0:nh], in_=sq[:, 0:w_].rearrange("p (h d) -> p h d", d=64),
                                                                 axis=AX.X, op=ALU.add), reads=['sq'], writes=['ssum'])
                            S.op('act', lambda e: e.activation(out=ssum[:, 0:nh], in_=ssum[:, 0:nh], func=AF.Ln, bias=eps6[:], scale=1.0 / 64),
                                 reads=['ssum', 'eps6'], writes=['ssum'])
                            S.op('act', lambda e: e.activation(out=ssum[:, 0:nh], in_=ssum[:, 0:nh], func=AF.Exp, scale=-0.5),
                                 reads=['ssum'], writes=['ssum'])
                            S.op('dve', lambda e: e.tensor_tensor(out=qn[:, 0:w_].rearrange("p (h d) -> p h d", d=64),
                                                                 in0=psrc.rearrange("p (h d) -> p h d", d=64),
                                                                 in1=ssum[:, 0:nh].unsqueeze(2).to_broadcast([128, nh, 64]), op=ALU.mult),
                                 reads=[pk, 'ssum'], writes=['qn'])
                            if not do_rope:
                                S.op('pool', lambda e: outfn(e, qn[:, 0:w_], gain[:, 0:w_], ALU.mult), reads=['qn', gk], writes=['qko'])
                                return
                            S.op('pool', lambda e: e.tensor_tensor(out=qn[:, 0:w_], in0=qn[:, 0:w_], in1=gain[:, 0:w_], op=ALU.mult),
                                 reads=['qn', gk], writes=['qn'])
                            S.op('dve', lambda e: e.tensor_tensor(out=qa[:, 0:w_], in0=qn[:, 0:w_], in1=cosT[:, 0:w_], op=ALU.mult),
                                 reads=['qn', 'cosT'], writes=['qa'])
                            qv = qn[:, 0:w_].rearrange("p (g s q) -> p g s q", s=2, q=16)
                            bv = qbt[:, 0:w_].rearrange("p (g s q) -> p g s q", s=2, q=16)
                            sv = sinS[:, 0:w_].rearrange("p (g s q) -> p g s q", s=2, q=16)
                            for s_ in range(2):
                                S.op('pool', lambda e, s_=s_: e.tensor_tensor(out=bv[:, :, s_, :], in0=qv[:, :, 1 - s_, :],
                                                                             in1=sv[:, :, s_, :], op=ALU.mult),
                                     reads=['qn', 'sinS'], writes=['qbt'])
                            S.op('dve', lambda e: outfn(e, qa[:, 0:w_], qbt[:, 0:w_], ALU.add), reads=['qa', 'qbt'], writes=['qko'])

                        if not is_ctx:
                            normrope(P[1][:, :], 'P1', 8, qg, 'qg', True,
                                     lambda e, a, b_, op: e.tensor_tensor(out=qo[:], in0=a, in1=b_, op=op))
                            pq = P[3][:].bitcast(BF16)
                            for hp in range(4):
                                S.op('pe', lambda e, hp=hp, pq=pq: e.transpose(out=pq[:, hp * 128:(hp + 1) * 128],
                                                                          in_=qo[:, hp * 128:(hp + 1) * 128], identity=identb[:]),
                                     reads=['qko', 'identb'], writes=['P3'])
                            S.op('act', lambda e, ul=ul, pq=pq: e.copy(out=qT_all[:, :, ul:ul + 128],
                                                                   in_=pq[:, 0:512].rearrange("p (j t) -> p j t", j=4)),
                                 reads=['P3'], writes=['qT_all'])

                        def kout(e, a, b_, op):
                            return e.tensor_tensor(out=ko[:, :, 0, :], in0=a.rearrange("p (h d) -> p h d", d=64),
                                                   in1=b_.rearrange("p (h d) -> p h d", d=64), op=op)
                        normrope(P[2][:, 0:128], 'P2', 2, kg, 'kg', not is_ctx, kout)
                        S.op('pool', lambda e: e.tensor_copy(out=ko[:, :, 1, :], in_=ko[:, :, 0, :]), reads=['qko'], writes=['qko'])
                        pk_ = P[3][:].bitcast(BF16)
                        for kv in range(2):
                            S.op('pe', lambda e, kv=kv, pk_=pk_: e.transpose(
                                out=pk_[:, 512 + kv * 128:512 + (kv + 1) * 128],
                                in_=ko[:, kv, :, :].rearrange("p a d -> p (a d)"), identity=identb[:]),
                                reads=['qko', 'identb'], writes=['P3'])
                        S.op('act', lambda e, u=u, pk_=pk_: e.copy(out=kTd[:, :, u:u + 128],
                                                               in_=pk_[:, 512:768].rearrange("p (j t) -> p j t", j=2)),
                             reads=['P3'], writes=['kTd'])
                    for g in range(14):
                        pb = P[4 + g % 2]; pbk = 'P%d' % (4 + g % 2)
                        for j in range(8):
                            S.op('pe', lambda e, j=j, g=g, pb=pb, ntok=ntok: e.matmul(pb[:, 0:ntok], lhsT=winb[:, j, 768 + g * 128:768 + (g + 1) * 128],
                                                                         rhs=hT[:, j, 0:ntok], start=(j == 0), stop=(j == 7)),
                                 reads=['hT', 'winb'], writes=[pbk])
                        rs = rstg[g % 2]; rk = 'rstg%d' % (g % 2)
                        S.op('act' if g % 2 else 'dve',
                             (lambda e, rs=rs, pb=pb, ntok=ntok: e.copy(out=rs[:, 0:ntok], in_=pb[:, 0:ntok])) if g % 2 else
                             (lambda e, rs=rs, pb=pb, ntok=ntok: e.tensor_copy(out=rs[:, 0:ntok], in_=pb[:, 0:ntok])),
                             reads=[pbk], writes=[rk])
                        S.dma(lambda e, rs=rs, g=g, u0=u0, ntok=ntok: e.dma_start(out=rwT[g * 128:(g + 1) * 128, u0:u0 + ntok], in_=rs[:, 0:ntok]),
                              reads=[rk], writes=['rwT'])
                if dbg:
                    o1 = dout("dbg_qT", [128, 4 * TL], BF16)
                    S.dma(lambda e: e.dma_start(out=o1[:, :], in_=qT_all[:].rearrange("p a t -> p (a t)")), reads=['qT_all'])
                    o2 = dout("dbg_kTd", [128, 2 * TT], BF16)
                    S.dma(lambda e: e.dma_start(out=o2[:, :], in_=kTd[:].rearrange("p a t -> p (a t)")), reads=['kTd'])
                    o3 = dout("dbg_rwT", [1792, TT])
                    S.dma(lambda e: e.dma_start(out=o3[:, :], in_=rwT[:, :]), reads=['rwT'])
                S.barrier()
                S.flush()
            if stop_after <= 1:
                return nc, dbg_outs
            with contextlib.ExitStack() as ph:
                pT = [sb(ph, "pT%d" % i, [128, 512], BF16) for i in range(3)]
                rec = sb(ph, "rec", [128, 8], F32)
                atok = sb(ph, "atok", [128, 8, 64], BF16)
                pi = 0
                for g in range(int(os.environ.get('K_NG', 16))):
                    q0 = g * 256
                    for st in range(NT):
                        for hp in range(int(os.environ.get('K_NHP', 4))):
                            sb0 = 2 * (hp % 2)
                            scb = PS[:, sb0 * 512:(sb0 + 2) * 512].rearrange("p (b n) -> p b n", b=2)
                            sck = 'P%d' % sb0
                            kvh = hp // 2
                            for hh in range(int(os.environ.get('K_HH0', 0)), int(os.environ.get('K_NHH', 2))):
                                S.op('pe', lambda e, hh=hh, hp=hp, scb=scb, kvh=kvh, st=st, q0=q0: e.matmul(
                                    scb[:, hh, 0:256],
                                    lhsT=kTd[hh * 64:(hh + 1) * 64, kvh, st * 128:(st + 1) * 128],
                                    rhs=qT_all[hh * 64:(hh + 1) * 64, hp, q0:q0 + 256], start=True, stop=True),
                                    reads=['kTd', 'qT_all'], writes=[sck])
                            pt = pT[pi % 3]; ptk = 'pT%d' % (pi % 3)
                            pi += 1
                            if not os.environ.get('K_SKIP_EXP'):
                                S.op('act', lambda e, pt=pt, scb=scb: e.activation(out=pt[:].rearrange('p (b n) -> p b n', b=2), in_=scb[:, :, 0:256], func=AF.Exp, scale=0.125),
                                     reads=[sck], writes=[ptk])
                            for hh in range(2 if not os.environ.get('K_SKIP_PV') else 0):
                                head = 2 * hp + hh
                                for qt in range(2):
                                    ab = P[4 + 2 * qt + head // 4]; abk = 'P%d' % (4 + 2 * qt + head // 4)
                                    c0 = (head % 4) * 65
                                    S.op('pe', lambda e, ab=ab, c0=c0, pt=pt, hh=hh, qt=qt, st=st, kvh=kvh, head=head: e.matmul(
                                        ab[:, c0:c0 + 65], lhsT=pt[:, hh * 256 + qt * 128:hh * 256 + (qt + 1) * 128],
                                        rhs=Vx[:, st, kvh, 0:65], start=(st == 0 and head % 4 == 0), stop=(st == NT - 1 and head % 4 == 3)),
                                        reads=[ptk, 'Vx'], writes=[abk])
                    for qt in range(2 if not os.environ.get('K_SKIP_NORM') else 0):
                        for half in range(2):
                            ab = P[4 + 2 * qt + half]; abk = 'P%d' % (4 + 2 * qt + half)
                            av = ab[:, 0:260].rearrange("p (h c) -> p h c", c=65)
                            S.op('dve', lambda e, av=av, half=half: e.reciprocal(out=rec[:, half * 4:(half + 1) * 4], in_=av[:, :, 64]),
                                 reads=[abk], writes=['rec'])
                            S.op('dve', lambda e, av=av, half=half: e.tensor_tensor(
                                out=atok[:, half * 4:(half + 1) * 4, :], in0=av[:, :, 0:64],
                                in1=rec[:, half * 4:(half + 1) * 4].unsqueeze(2).to_broadcast([128, 4, 64]), op=ALU.mult),
                                reads=[abk, 'rec'], writes=['atok'])
                        pa = P[0][:].bitcast(BF16)
                        if os.environ.get('K_SKIP_TR'):
                            continue
                        for hp in range(4):
                            S.op('pe', lambda e, hp=hp, pa=pa: e.transpose(
                                out=pa[:, hp * 128:(hp + 1) * 128],
                                in_=atok[:, 2 * hp:2 * hp + 2, :].rearrange("p a d -> p (a d)"), identity=identb[:]),
                                reads=['atok', 'identb'], writes=['P0'])
                        if os.environ.get('K_SKIP_CP'):
                            continue
                        S.op('dve', lambda e, pa=pa, q0=q0, qt=qt: e.tensor_copy(
                            out=attT_all[:, :, q0 + qt * 128:q0 + (qt + 1) * 128],
                            in_=pa[:, 0:512].rearrange("p (j t) -> p j t", j=4)), reads=['P0'], writes=['attT_all'])
                if dbg:
                    o1 = dout("dbg_attT", [128, 4 * TL], BF16)
                    S.dma(lambda e: e.dma_start(out=o1[:, :], in_=attT_all[:].rearrange("p a t -> p (a t)")), reads=['attT_all'])
                S.barrier()
                S.flush()
        if stop_after <= 2:
            return nc, dbg_outs

        def TTo(eng, out, a, b_, op, R, W):
            S.op(eng, lambda e: e.tensor_tensor(out=out, in0=a, in1=b_, op=op), reads=R, writes=W)

        def CP(eng, out, in_, R, W):
            if eng == 'act':
                S.op('act', lambda e: e.copy(out=out, in_=in_), reads=R, writes=W)
            else:
                S.op(eng, lambda e: e.tensor_copy(out=out, in_=in_), reads=R, writes=W)

        def ACTF(out, in_, func, R, W, scale=1.0, bias=None):
            if bias is None:
                S.op('act', lambda e: e.activation(out=out, in_=in_, func=func, scale=scale), reads=R, writes=W)
            else:
                S.op('act', lambda e: e.activation(out=out, in_=in_, func=func, scale=scale, bias=bias), reads=R, writes=W)

        def MM(out, lhsT, rhs, R, W, start=True, stop=True):
            S.op('pe', lambda e: e.matmul(out, lhsT=lhsT, rhs=rhs, start=start, stop=stop), reads=R, writes=W)

        def TR(out, in_, idn, R, W):
            S.op('pe', lambda e: e.transpose(out=out, in_=in_, identity=idn), reads=R, writes=W)

        with contextlib.ExitStack() as ph:
            def t32(name, shape=(64, 1024)):
                return sb(ph, name, list(shape), F32)
            rp = t32("rp", (64, 8, 10)); rpd = t32("rpd", (64, 8, 8))
            lmu = t32("lmu", (128, 3)); lmd = t32("lmd", (128, 6))
            wl_st = t32("wl_st", (128, 1024))
            decupb = sb(ph, "decupb", [64, 2, 512], BF16); iclupb = sb(ph, "iclupb", [64, 2, 512], BF16)
            gateupb = sb(ph, "gateupb", [128, 512], BF16)
            mk4 = t32("mk4", (128, 2, 512)); mn1 = t32("mn1", (128, 2, 128)); bm = t32("bm", (128, 512))
            rst = t32("rst"); ones64 = sb(ph, "ones64", [64, 64], BF16); tiny = t32("tiny", (64, 1))
            S.dma(lambda e: e.dma_start(out=rp[:].rearrange("k h n -> k (h n)"), in_=rp_d[:, :]), writes=['rp'])
            S.dma(lambda e: e.dma_start(out=lmu[:], in_=lmu_d[:, :]), writes=['lmu'])
            S.dma(lambda e: e.dma_start(out=mk4[:].rearrange("p a n -> p (a n)"), in_=mk4_d[:, :]), writes=['mk4'])
            S.dma(lambda e: e.dma_start(out=mn1[:].rearrange("p a n -> p (a n)"), in_=mn1_d[:, :]), writes=['mn1'])
            S.dma(lambda e: e.dma_start(out=bm[:], in_=bm_d[:, :]), writes=['bm'])
            S.dma(lambda e: e.dma_start(out=rst[:], in_=rst_d[:, :]), writes=['rst'])
            S.op('pool', lambda e: e.memset(ones64[:], 1.0), writes=['ones64'])
            S.op('pool', lambda e: e.memset(tiny[:], 1e-24), writes=['tiny'])
            for (dst, src, nm, rows) in ((decupb, decup_d, 'decupb', 64), (iclupb, iclup_d, 'iclupb', 64), (gateupb, gateup_d, 'gateupb', 128)):
                ncol = 1024 if rows == 64 else 512
                S.dma(lambda e, src=src, rows=rows, ncol=ncol: e.dma_start(out=wl_st[0:rows, 0:ncol], in_=src[:, :]), writes=['wl_st'])
                dv = dst[:].rearrange("p a n -> p (a n)") if rows == 64 else dst[:]
                CP('dve', dv, wl_st[0:rows, 0:ncol], ['wl_st'], [nm])
            for i in range(3):
                S.op('dve', lambda e, i=i: e.tensor_scalar(out=rpd[:, :, 2 * i], in0=rp[:, :, i], scalar1=0.5, scalar2=None, op0=ALU.mult),
                     reads=['rp'], writes=['rpd'])
                S.op('dve', lambda e, i=i: e.tensor_scalar(out=rpd[:, :, 2 * i + 1], in0=rp[:, :, i], scalar1=-1.0, scalar2=1.0,
                                                          op0=ALU.mult, op1=ALU.add), reads=['rp'], writes=['rpd'])
                S.op('dve', lambda e, i=i: e.tensor_scalar(out=lmd[:, 2 * i:2 * i + 1], in0=lmu[:, i:i + 1], scalar1=0.5, scalar2=None, op0=ALU.mult),
                     reads=['lmu'], writes=['lmd'])
                S.op('dve', lambda e, i=i: e.tensor_scalar(out=lmd[:, 2 * i + 1:2 * i + 2], in0=lmu[:, i:i + 1], scalar1=-1.0, scalar2=1.0,
                                                          op0=ALU.mult, op1=ALU.add), reads=['lmu'], writes=['lmd'])
            S.op('dve', lambda e: e.tensor_scalar(out=rpd[:, :, 6], in0=rp[:, :, 4], scalar1=-1.0, scalar2=1.0, op0=ALU.mult, op1=ALU.add),
                 reads=['rp'], writes=['rpd'])

            def bc(t2):
                return t2.unsqueeze(2).to_broadcast([64, 8, 128])

            pin = [sb(ph, "pin%d" % i, [64, 8, 130], F32) for i in range(3)]
            plo = [sb(ph, "plo%d" % i, [128, 130], F32) for i in range(3)]
            tS = t32("tS"); xr = t32("xr"); xk = t32("xk"); xv = t32("xv")
            xlo = t32("xlo", (128, 3, 128)); twl = sb(ph, "twl", [64, 128], BF16); xalb = sb(ph, "xalb", [64, 128], BF16)
            glsb = sb(ph, "glsb", [128, 128], BF16)
            kkk = t32("kkk"); sqb = sb(ph, "sqb", [64, 1024], BF16); rs = t32("rs"); kk = t32("kk")
            sg = t32("sg"); ad = t32("ad"); cs = t32("cs"); csb = t32("csb"); ex = t32("ex"); E1 = t32("E1"); E2 = t32("E2"); E3 = t32("E3")
            bb = t32("bb"); t1 = t32("t1"); kd = t32("kd"); bt32 = t32("bt32"); kt32 = t32("kt32"); rkb = sb(ph, "rkb", [64, 1024], BF16)
            bv = t32("bv"); bvt = t32("bvt", (128, 512)); gtok = t32("gtok", (128, 512))
            opT = {n: sb(ph, "op_" + n, [64, 1024], BF16) for n in ("a", "r", "b", "k", "bh", "kh", "v")}
            M4 = sb(ph, "M4", [128, 512], BF16); N1 = sb(ph, "N1", [128, 128], BF16); IN1T = sb(ph, "IN1T", [128, 128], BF16)
            N2p = sb(ph, "N2p", [128, 256], BF16); IN2T = sb(ph, "IN2T", [128, 128], BF16)
            N4p = sb(ph, "N4p", [128, 256], BF16); IN4T = sb(ph, "IN4T", [128, 128], BF16); IN8T = sb(ph, "IN8T", [128, 128], BF16)
            TM = sb(ph, "TM", [128, 5, 64], BF16)
            Xs = [sb(ph, "Xs%d" % i, [128, 128], BF16) for i in range(2)]
            Bbd = sb(ph, "Bbd", [128, 512], BF16); Ubd = sb(ph, "Ubd", [128, 512], BF16); Vbd = sb(ph, "Vbd", [128, 512], BF16)
            GTs = sb(ph, "GTs", [64, 512], BF16); Es = t32("Es", (64, 512)); QTs = sb(ph, "QTs", [64, 128], BF16)
            Y0s = t32("Y0s", (128, 64)); ytmp = t32("ytmp", (128, 512)); yc = t32("yc", (128, 64))
            Yblk = t32("Yblk", (128, 8, 64))
            H = t32("H", (64, 512)); Hb = sb(ph, "Hb", [64, 512], BF16); Ht = t32("Ht", (64, 512))

            def view_hct(t):
                return t[:, :]

            def view_out(t):
                return t[:, :]

            def g16(t):
                return t[:, :].rearrange("k (g t) -> k g t", t=16)

            def chv(t, c):
                return t[:, :].rearrange("k (h c t) -> k h c t", h=8, c=8)[:, :, c, :]

            for d in range(2):
                S.op('pool', lambda e: e.memset(H[:], 0.0), writes=['H'])
                S.op('pool', lambda e: e.memset(Hb[:], 0.0), writes=['Hb'])
                cblocks = [0, 1] if d == 0 else [1, 0]
                lblocks = list(range(2, NT)) if d == 0 else list(range(NT - 1, 1, -1))
                nblk = int(os.environ.get('K_RBLK', 99))
                for bi, blk in enumerate((cblocks + lblocks)[:nblk]):
                    u0 = blk * 128
                    is_ctx = blk < 2
                    seq_lo, seq_hi = (0, TC) if is_ctx else (TC, TT)
                    lo = max(u0 - 1, seq_lo); hi = min(u0 + 129, seq_hi)
                    c_lo = lo - (u0 - 1); c_hi = c_lo + (hi - lo)
                    for i in range(3):
                        if c_lo > 0:
                            S.op('pool', lambda e, i=i: e.memset(pin[i][:, :, 0:1], 0.0), writes=['pin%d' % i])
                        if c_hi < 130:
                            S.op('pool', lambda e, i=i: e.memset(pin[i][:, :, 129:130], 0.0), writes=['pin%d' % i])
                        S.dma(lambda e, i=i, lo=lo, hi=hi, c_lo=c_lo, c_hi=c_hi: e.dma_start(
                            out=pin[i][:, :, c_lo:c_hi],
                            in_=rwT[i * 512:(i + 1) * 512, lo:hi].rearrange("(h k) t -> k h t", k=64)), reads=['rwT'], writes=['pin%d' % i])
                    for i, (r0, nr) in enumerate(((1536, 64), (1600, 64), (1664, 128))):
                        if c_lo > 0:
                            S.op('pool', lambda e, i=i: e.memset(plo[i][:, 0:1], 0.0), writes=['plo%d' % i])
                        if c_hi < 130:
                            S.op('pool', lambda e, i=i: e.memset(plo[i][:, 129:130], 0.0), writes=['plo%d' % i])
                        S.dma(lambda e, i=i, r0=r0, nr=nr, lo=lo, hi=hi, c_lo=c_lo, c_hi=c_hi: e.dma_start(
                            out=plo[i][0:nr, c_lo:c_hi], in_=rwT[r0:r0 + nr, lo:hi]), reads=['rwT'], writes=['plo%d' % i])
                    for i, xo in enumerate((xr, xk, xv)):
                        xo3 = xo[:, :].rearrange("k (h t) -> k h t", h=8)
                        ts3 = tS[:, :].rearrange("k (h t) -> k h t", h=8)
                        TTo('pool', ts3, pin[i][:, :, 0:128], pin[i][:, :, 2:130], ALU.add, ['pin%d' % i], ['tS'])
                        TTo('pool', ts3, ts3, bc(rpd[:, :, 2 * i]), ALU.mult, ['tS', 'rpd'], ['tS'])
                        TTo('dve', xo3, pin[i][:, :, 1:129], bc(rpd[:, :, 2 * i + 1]), ALU.mult, ['pin%d' % i, 'rpd'], ['x%d' % i])
                        TTo('dve', xo3, xo3, ts3, ALU.add, ['x%d' % i, 'tS'], ['x%d' % i])
                    for i, nr in enumerate((64, 64, 128)):
                        S.op('pool', lambda e, i=i, nr=nr: e.tensor_tensor(out=tS[0:nr, 0:128] if nr == 64 else wl_st[:, 0:128],
                                                                         in0=plo[i][0:nr, 0:128], in1=plo[i][0:nr, 2:130], op=ALU.add),
                             reads=['plo%d' % i], writes=['tS' if nr == 64 else 'wl_st'])
                        S.op('dve', lambda e, i=i, nr=nr: e.tensor_scalar(out=xlo[0:nr, i, :], in0=plo[i][0:nr, 1:129],
                                                                        scalar1=lmd[0:nr, 2 * i + 1:2 * i + 2], scalar2=None, op0=ALU.mult),
                             reads=['plo%d' % i, 'lmd'], writes=['xlo'])
                        S.op('dve', lambda e, i=i, nr=nr: e.scalar_tensor_tensor(
                            out=xlo[0:nr, i, :], in0=(tS[0:nr, 0:128] if nr == 64 else wl_st[:, 0:128]), scalar=lmd[0:nr, 2 * i:2 * i + 1],
                            in1=xlo[0:nr, i, :], op0=ALU.mult, op1=ALU.add),
                            reads=['tS' if nr == 64 else 'wl_st', 'lmd', 'xlo'], writes=['xlo'])
                    ACTF(twl[:], xlo[0:64, 0, :], AF.Tanh, ['xlo'], ['twl'])
                    CP('pool', xalb[:], xlo[0:64, 1, :], ['xlo'], ['xalb'])
                    k3 = lambda t: t[:, :].rearrange("k (h t) -> k h t", h=8)
                    TTo('pool', k3(kkk), k3(xk), bc(rp[:, :, 3]), ALU.mult, ['x1', 'rp'], ['kkk'])
                    ACTF(sqb[:], kkk[:], AF.Square, ['kkk'], ['sqb'])
                    PP = PS[0:64, 0:1024]
                    for hf in range(2):
                        MM(PS[0:64, hf * 512:(hf + 1) * 512], ones64[:], sqb[:, hf * 512:(hf + 1) * 512], ['ones64', 'sqb'], ['P0'])
                    ACTF(rs[:], PP, AF.Ln, ['P0', 'tiny'], ['rs'], bias=tiny[:])
                    ACTF(rs[:], rs[:], AF.Exp, ['rs'], ['rs'], scale=-0.5)
                    TTo('dve', kk[:], kkk[:], rs[:], ALU.mult, ['kkk', 'rs'], ['kk'])
                    for h in range(8):
                        MM(PS[0:64, h * 128:(h + 1) * 128], decupb[:, d, h * 64:(h + 1) * 64], twl[:], ['decupb', 'twl'], ['P0'])
                    TTo('dve', k3(sg), PP.rearrange("k (h t) -> k h t", h=8), bc(rp[:, :, 6 + d]), ALU.add, ['P0', 'rp'], ['sg'])
                    ACTF(sg[:], sg[:], AF.Sigmoid, ['sg'], ['sg'])
                    for h in range(8):
                        MM(PS[0:64, 1024 + h * 128:1024 + (h + 1) * 128], iclupb[:, d, h * 64:(h + 1) * 64], xalb[:], ['iclupb', 'xalb'], ['P2', 'P3'])
                    TTo('dve', k3(ad), PS[0:64, 1024:2048].rearrange("k (h t) -> k h t", h=8), bc(rp[:, :, 8 + d]), ALU.add, ['P2', 'P3', 'rp'], ['ad'])
                    ACTF(ad[:], ad[:], AF.Sigmoid, ['ad'], ['ad'])
                    S.op('dve', lambda e: e.tensor_tensor_scan(out=cs[:], data0=rst[:], data1=sg[:], initial=0.0, op0=ALU.mult, op1=ALU.add),
                         reads=['rst', 'sg'], writes=['cs'])
                    csf = cs
                    if d == 1:
                        TTo('pool', ex[:], sg[:], cs[:], ALU.subtract, ['sg', 'cs'], ['ex'])
                        csv = cs[:, :].rearrange("k (g t) -> k g t", t=16)
                        TTo('dve', csb[:, :].rearrange("k (g t) -> k g t", t=16), ex[:, :].rearrange("k (g t) -> k g t", t=16),
                           csv[:, :, 15:16].to_broadcast([64, 64, 16]), ALU.add, ['ex', 'cs'], ['csb'])
                        csf = csb
                    ACTF(E2[:], csf[:], AF.Exp, ['cs', 'csb'], ['E2'], scale=DEC_C)
                    TTo('pool', ex[:], csf[:], sg[:], ALU.subtract, ['cs', 'csb', 'sg'], ['ex'])
                    ACTF(E1[:], ex[:], AF.Exp, ['ex'], ['E1'], scale=DEC_C)
                    S.op('dve', lambda e: e.reciprocal(out=E3[:], in_=E2[:]), reads=['E2'], writes=['E3'])
                    tsel = 15 if d == 0 else 0
                    def cm(t, h):
                        return t[:, :].rearrange("k (c h t) -> k c h t", c=8, h=8)[:, :, h, :]

                    def hm(t, h):
                        return t[:, :].rearrange("k (h c t) -> k h c t", h=8, c=8)[:, h, :, :]
                    TTo('pool', bb[:], kk[:], ad[:], ALU.mult, ['kk', 'ad'], ['bb'])
                    TTo('dve', bt32[:], bb[:], E3[:], ALU.mult, ['bb', 'E3'], ['bt32'])
                    TTo('pool', k3(t1), k3(ad), bc(rp[:, :, 4]), ALU.mult, ['ad', 'rp'], ['t1'])
                    TTo('dve', k3(t1), k3(t1), bc(rpd[:, :, 6]), ALU.add, ['t1', 'rpd'], ['t1'])
                    TTo('pool', kd[:], xk[:], t1[:], ALU.mult, ['x1', 't1'], ['kd'])
                    TTo('dve', kt32[:], kd[:], E3[:], ALU.mult, ['kd', 'E3'], ['kt32'])
                    for h in range(8):
                        pch = hm(E2, h)[:, :, tsel:tsel + 1].to_broadcast([64, 8, 16])
                        S.op('dve', lambda e, h=h: e.scalar_tensor_tensor(out=cm(opT['a'], h), in0=hm(kk, h), scalar=-1.0, in1=hm(E1, h),
                                                                         op0=ALU.mult, op1=ALU.mult), reads=['kk', 'E1'], writes=['op_a'])
                        TTo('pool', cm(opT['r'], h), hm(xr, h), hm(E2, h), ALU.mult, ['x0', 'E2'], ['op_r'])
                        CP('act', cm(opT['b'], h), hm(bt32, h), ['bt32'], ['op_b'])
                        TTo('pool', cm(opT['bh'], h), hm(bt32, h), pch, ALU.mult, ['bt32', 'E2'], ['op_bh'])
                        CP('act', cm(opT['k'], h), hm(kt32, h), ['kt32'], ['op_k'])
                        TTo('dve', cm(opT['kh'], h), hm(kt32, h), pch, ALU.mult, ['kt32', 'E2'], ['op_kh'])
                        CP('act', cm(opT['v'], h), hm(xv, h), ['x2'], ['op_v'])
                    if d == 0 and not is_ctx:
                        ul = u0 - TC
                        TTo('pool', k3(t1), k3(xr), bc(rp[:, :, 5]), ALU.mult, ['x0', 'rp'], ['t1'])
                        TTo('dve', rkb[:], t1[:], xk[:], ALU.mult, ['t1', 'x1'], ['rkb'])
                        for hf in range(2):
                            MM(PS[0:64, hf * 512:(hf + 1) * 512], ones64[:], rkb[:, hf * 512:(hf + 1) * 512], ['ones64', 'rkb'], ['P0'])
                        TTo('dve', bv[:], PP, xv[:], ALU.mult, ['P0', 'x2'], ['bv'])
                        for h in range(8):
                            TR(PS[:, 1536 + h * 64:1536 + (h + 1) * 64], bv[:, h * 128:(h + 1) * 128], identf[0:64, 0:64], ['bv', 'identf'], ['P3'])
                        CP('dve', bvt[:], PS[:, 1536:2048], ['P3'], ['bvt'])
                        S.dma(lambda e, ul=ul: e.dma_start(out=bonD[ul:ul + 128, :], in_=bvt[:]), reads=['bvt'], writes=['bonD'])
                        ACTF(glsb[:], xlo[:, 2, :], AF.Sigmoid, ['xlo'], ['glsb'])
                        MM(PS[:, 1536:2048], glsb[:], gateupb[:], ['glsb', 'gateupb'], ['P3'])
                        CP('dve', gtok[:], PS[:, 1536:2048], ['P3'], ['gtok'])
                        S.dma(lambda e, ul=ul: e.dma_start(out=gD[ul:ul + 128, :], in_=gtok[:]), reads=['gtok'], writes=['gD'])
                    B2 = PS[:, 1024:1536]; B3 = PS[:, 1536:2048]; B4 = PS[:, 2048:2560]; B5 = PS[:, 2560:3072]
                    B6 = PS[:, 3072:3584]; B7 = PS[:, 3584:4096]
                    B5b = B5.bitcast(BF16)
                    nch = int(os.environ.get('K_RCH', 8))
                    for c in (list(range(8)) if d == 0 else list(range(7, -1, -1)))[:nch]:
                        csl = slice(c * 128, (c + 1) * 128)
                        aC = opT['a'][:, csl]; rC = opT['r'][:, csl]; bC = opT['b'][:, csl]; kC = opT['k'][:, csl]
                        bhC = opT['bh'][:, csl]; khC = opT['kh'][:, csl]; vC = opT['v'][:, csl]
                        MM(B2[:, 0:128], bC, aC, ['op_b', 'op_a'], ['P2'])
                        MM(B2[:, 128:256], bC, rC, ['op_b', 'op_r'], ['P2'])
                        MM(B2[:, 256:384], kC, aC, ['op_k', 'op_a'], ['P2'])
                        MM(B2[:, 384:512], kC, rC, ['op_k', 'op_r'], ['P2'])
                        MM(B3[:, 0:128], aC, bC, ['op_a', 'op_b'], ['P3'])
                        TTo('dve', M4[:], B2, mk4[:, d, :], ALU.mult, ['P2', 'mk4'], ['M4'])
                        TTo('dve', N1[:], B3[:, 0:128], mn1[:, d, :], ALU.mult, ['P3', 'mn1'], ['N1'])
                        N1T = M4[:, 0:128]; ArbT = M4[:, 128:256]; AakT = M4[:, 256:384]; ArkT = M4[:, 384:512]
                        TTo('pool', IN1T[:], N1T, identb[:], ALU.add, ['M4', 'identb'], ['IN1T'])
                        MM(B3[:, 128:256], N1T, N1[:], ['M4', 'N1'], ['P3'])
                        MM(B3[:, 256:384], N1[:], N1T, ['M4', 'N1'], ['P3'])
                        CP('dve', N2p[:], B3[:, 128:384], ['P3'], ['N2p'])
                        TTo('pool', IN2T[:], N2p[:, 128:256], identb[:], ALU.add, ['N2p', 'identb'], ['IN2T'])
                        MM(B4[:, 0:128], N2p[:, 128:256], N2p[:, 0:128], ['N2p'], ['P4'])
                        MM(B4[:, 128:256], N2p[:, 0:128], N2p[:, 128:256], ['N2p'], ['P4'])
                        CP('dve', N4p[:], B4[:, 0:256], ['P4'], ['N4p'])
                        TTo('pool', IN4T[:], N4p[:, 128:256], identb[:], ALU.add, ['N4p', 'identb'], ['IN4T'])
                        MM(B4[:, 256:384], N4p[:, 0:128], N4p[:, 128:256], ['N4p'], ['P4'])
                        TTo('dve', IN8T[:], B4[:, 256:384], identf[:], ALU.add, ['P4', 'identf'], ['IN8T'])
                        for si, src_, kname in ((0, aC, 'op_a'), (1, vC, 'op_v'), (2, bhC, 'op_bh'), (3, khC, 'op_kh')):
                            TR(B5b[:, si * 64:(si + 1) * 64], src_, identb[0:64, 0:64], [kname, 'identb'], ['P5'])
                        CP('dve', TM[:, 0, :], B5b[:, 0:64], ['P5'], ['TM'])
                        CP('dve', TM[:, 2:5, :], B5b[:, 64:256].rearrange("p (a n) -> p a n", a=3), ['P5'], ['TM'])
                        MM(B5[:, 128:192], AakT, TM[:, 2, :], ['M4', 'TM'], ['P5'])
                        CP('dve', TM[:, 1, :], B5[:, 128:192], ['P5'], ['TM'])
                        Xc = TM[:, 0:2, :].rearrange("p a n -> p (a n)")
                        xk_ = 'TM'
                        for li, (INT, ik) in enumerate(((IN8T, 'IN8T'), (IN4T, 'IN4T'), (IN2T, 'IN2T'), (IN1T, 'IN1T'))):
                            MM(B5[:, 256:384], INT[:], Xc, [ik, xk_], ['P5'])
                            Xn = Xs[li % 2]; xk_ = 'Xs%d' % (li % 2)
                            CP('dve', Xn[:], B5[:, 256:384], ['P5'], [xk_])
                            Xc = Xn[:]
                        Wc = Xc[:, 0:64]; U0 = Xc[:, 64:128]
                        bm3 = bm[:, :].rearrange("p (h n) -> p h n", h=8)
                        for dst_, src_, rk_, wk_ in ((Bbd, TM[:, 3, :], 'TM', 'Bbd'), (Ubd, U0, xk_, 'Ubd'), (Vbd, TM[:, 2, :], 'TM', 'Vbd')):
                            TTo('pool', dst_[:, :].rearrange("p (h n) -> p h n", h=8), src_.unsqueeze(1).to_broadcast([128, 8, 64]), bm3, ALU.mult,
                               [rk_, 'bm'], [wk_])
                        MM(B6[0:64, :], Wc, Bbd[:], [xk_, 'Bbd'], ['P6'])
                        CP('act', GTs[:], B6[0:64, :], ['P6'], ['GTs'])
                        MM(B7[0:64, :], TM[:, 3, :], Ubd[:], ['TM', 'Ubd'], ['P7'], start=True, stop=False)
                        MM(B7[0:64, :], TM[:, 4, :], Vbd[:], ['TM', 'Vbd'], ['P7'], start=False, stop=True)
                        CP('act', Es[:], B7[0:64, :], ['P7'], ['Es'])
                        if not is_ctx:
                            MM(B5[0:64, 384:512], Wc, ArbT, [xk_, 'M4'], ['P5'])
                            TTo('dve', QTs[:], B5[0:64, 384:512], rC, ALU.add, ['P5', 'op_r'], ['QTs'])
                            MM(B3[:, 384:448], ArbT, U0, ['M4', xk_], ['P3'], start=True, stop=False)
                            MM(B3[:, 384:448], ArkT, TM[:, 2, :], ['M4', 'TM'], ['P3'], start=False, stop=True)
                            CP('dve', Y0s[:], B3[:, 384:448], ['P3'], ['Y0s'])
                            MM(B6, QTs[:], Hb[:], ['QTs', 'Hb'], ['P6'])
                            TTo('dve', ytmp[:], B6, bm[:], ALU.mult, ['P6', 'bm'], ['ytmp'])
                            S.op('dve', lambda e: e.tensor_reduce(out=yc[:], in_=ytmp[:, :].rearrange("p (h v) -> p v h", h=8), axis=AX.X, op=ALU.add),
                                 reads=['ytmp'], writes=['yc'])
                            TTo('pool', Yblk[:, c, :], yc[:], Y0s[:], ALU.add, ['yc', 'Y0s'], ['Yblk'])
                        for h in range(8):
                            MM(B7[0:64, h * 64:(h + 1) * 64], GTs[:, h * 64:(h + 1) * 64], Hb[:, h * 64:(h + 1) * 64], ['GTs', 'Hb'], ['P7'])
                        PCc = chv(E2, c)[:, :, tsel:tsel + 1].to_broadcast([64, 8, 64])
                        TTo('pool', Ht[:, :].rearrange("k (h v) -> k h v", h=8), H[:, :].rearrange("k (h v) -> k h v", h=8), PCc, ALU.mult,
                           ['H', 'E2'], ['Ht'])
                        TTo('pool', Ht[:], Ht[:], Es[:], ALU.add, ['Ht', 'Es'], ['Ht'])
                        TTo('dve', H[:], Ht[:], B7[0:64, :], ALU.add, ['Ht', 'P7'], ['H'])
                        CP('act', Hb[:], H[:], ['H'], ['Hb'])
                    if not is_ctx:
                        ul = u0 - TC
                        for h in range(8):
                            S.dma(lambda e, h=h, ul=ul, d=d: e.dma_start(
                                out=ydir[d, ul:ul + 128, h * 64:(h + 1) * 64].rearrange("(c t) v -> t c v", t=16),
                                in_=Yblk[h * 16:(h + 1) * 16, :, :]), reads=['Yblk'], writes=['ydir'])
            if dbg:
                oy = dout("dbg_y", [2 * TL, 512]); ob = dout("dbg_bon", [TL, 512]); og = dout("dbg_g", [TL, 512])
                S.dma(lambda e: e.dma_start(out=oy[:, :], in_=ydir.rearrange("d t n -> (d t) n")), reads=['ydir'])
                S.dma(lambda e: e.dma_start(out=ob[:, :], in_=bonD[:, :]), reads=['bonD'])
                S.dma(lambda e: e.dma_start(out=og[:, :], in_=gD[:, :]), reads=['gD'])
                oH = dout("dbg_H", [64, 512])
                S.dma(lambda e: e.dma_start(out=oH[:, :], in_=H[:]), reads=['H'])
            S.barrier()
            S.flush()
        if stop_after <= 3:
            return nc, dbg_outs

        with contextlib.ExitStack() as ph:
            woutb = sb(ph, "woutb", [128, 8, D], BF16)
            cst = {}
            for nm, src in (("gt1", modd[0:1, 2048:3072]), ("sh2", modd[0:1, 3072:4096]), ("sc2p", modd[0:1, 4096:5120]),
                            ("ln1g", ln1_d[0:1, :]), ("ln1b", ln1_d[1:2, :])):
                cst[nm] = sb(ph, nm, [128, D], F32)
                S.dma(lambda e, nm=nm, src=src: e.dma_start(out=cst[nm][:], in_=src.partition_broadcast(128)), reads=['modd'], writes=[nm])
            for nm, row in (("lnxg", 0), ("lnxb", 1)):
                cst[nm] = sb(ph, nm, [128, 512], F32)
                S.dma(lambda e, nm=nm, row=row: e.dma_start(out=cst[nm][:], in_=lnx_d[row:row + 1, :].partition_broadcast(128)), writes=[nm])
            wst2 = [sb(ph, "wst2_%d" % i, [128, D], F32) for i in range(2)]
            for j in range(8):
                w = wst2[j % 2]; wk = 'wst2_%d' % (j % 2)
                S.dma(lambda e, w=w, j=j: e.dma_start(out=w[:], in_=wout_d[j * 128:(j + 1) * 128, :]), writes=[wk])
                CP('pool' if j % 2 else 'dve', woutb[:, j, :], w[:], [wk], ['woutb'])
            rwf = sb(ph, "rwf", [128, 8, 16], F32)
            S.dma(lambda e: e.dma_start(out=rwf[:], in_=rw_d.rearrange("(j p) n -> p j n", p=128)), writes=['rwf'])
            gneps = sb(ph, "gneps", [128, 1], F32)
            S.op('pool', lambda e: e.memset(gneps[:], 64e-5), writes=['gneps'])
            yf = sb(ph, "yf", [128, 512], F32); yb = sb(ph, "yb", [128, 512], F32)
            bon = sb(ph, "bon", [128, 512], F32); gg = sb(ph, "gg", [128, 512], F32)
            ysum = sb(ph, "ysum", [128, 512], F32); ysq = sb(ph, "ysq", [128, 512], F32)
            gst = sb(ph, "gst", [128, 8], F32); gvar = sb(ph, "gvar", [128, 8], F32)
            rwob = sb(ph, "rwob", [128, 512], BF16); rwoT = sb(ph, "rwoT", [128, 4, 128], BF16)
            xin_t = sb(ph, "xin_t", [128, D], F32); tres = sb(ph, "tres", [128, D], F32)
            x1t = sb(ph, "x1t", [128, D], F32); h2f = sb(ph, "h2f", [128, D], F32); h2b = sb(ph, "h2b", [128, D], BF16)
            h2T = sb(ph, "h2T", [128, 8, 128], F32)
            stats = sb(ph, "statsC", [128, 2, 6], F32); mv = sb(ph, "mvC", [128, 2], F32)
            rstd = sb(ph, "rstdC", [128, 1], F32); nb = sb(ph, "nbC", [128, 1], F32)
            lntC = (stats, mv, rstd, nb, 'C')
            lmax = sb(ph, "lmax", [128, 1], F32); lex = sb(ph, "lex", [128, 16], F32); lsum = sb(ph, "lsum", [128, 1], F32)

            def v8(t):
                return t[:, :].rearrange("p (h v) -> p h v", h=8)

            def b8(t):
                return t[:, :].unsqueeze(2).to_broadcast([128, 8, 64])
            for i in range(int(os.environ.get('K_CT', 32))):
                t0 = i * 128
                S.dma(lambda e, t0=t0: e.dma_start(out=yf[:], in_=ydir[0, t0:t0 + 128, :]), reads=['ydir'], writes=['yf'])
                S.dma(lambda e, t0=t0: e.dma_start(out=yb[:], in_=ydir[1, t0:t0 + 128, :]), reads=['ydir'], writes=['yb'])
                S.dma(lambda e, t0=t0: e.dma_start(out=bon[:], in_=bonD[t0:t0 + 128, :]), reads=['bonD'], writes=['bon'])
                S.dma(lambda e, t0=t0: e.dma_start(out=gg[:], in_=gD[t0:t0 + 128, :]), reads=['gD'], writes=['gg'])
                S.dma(lambda e, t0=t0: e.dma_start(out=xin_t[:], in_=x_d[t0:t0 + 128, :]), writes=['xin_t'])
                TTo('pool', ysum[:], yf[:], yb[:], ALU.add, ['yf', 'yb'], ['ysum'])
                S.op('dve', lambda e: e.tensor_reduce(out=gst[:], in_=v8(ysum), axis=AX.X, op=ALU.add), reads=['ysum'], writes=['gst'])
                S.op('dve', lambda e: e.tensor_scalar(out=gst[:], in0=gst[:], scalar1=-1.0 / 64, scalar2=None, op0=ALU.mult),
                     reads=['gst'], writes=['gst'])
                TTo('dve', v8(ysum), v8(ysum), b8(gst), ALU.add, ['ysum', 'gst'], ['ysum'])
                ACTF(ysq[:], ysum[:], AF.Square, ['ysum'], ['ysq'])
                S.op('dve', lambda e: e.tensor_reduce(out=gvar[:], in_=v8(ysq), axis=AX.X, op=ALU.add), reads=['ysq'], writes=['gvar'])
                ACTF(gvar[:], gvar[:], AF.Ln, ['gvar', 'gneps'], ['gvar'], scale=1.0 / 64, bias=gneps[:])
                ACTF(gvar[:], gvar[:], AF.Exp, ['gvar'], ['gvar'], scale=-0.5)
                TTo('dve', v8(ysum), v8(ysum), b8(gvar), ALU.mult, ['ysum', 'gvar'], ['ysum'])
                TTo('pool', ysum[:], ysum[:], cst['lnxg'][:], ALU.mult, ['ysum', 'lnxg'], ['ysum'])
                TTo('dve', ysum[:], ysum[:], cst['lnxb'][:], ALU.add, ['ysum', 'lnxb'], ['ysum'])
                TTo('pool', ysum[:], ysum[:], bon[:], ALU.add, ['ysum', 'bon'], ['ysum'])
                TTo('dve', rwob[:], ysum[:], gg[:], ALU.mult, ['ysum', 'gg'], ['rwob'])
                pr_ = P[0][:].bitcast(BF16)
                for j in range(4):
                    TR(pr_[:, j * 128:(j + 1) * 128], rwob[:, j * 128:(j + 1) * 128], identb[:], ['rwob', 'identb'], ['P0'])
                CP('dve', rwoT[:], pr_[:, 0:512].rearrange("p (j t) -> p j t", j=4), ['P0'], ['rwoT'])
                for half in range(2):
                    ob = PS[:, 1024 + half * 512:1024 + (half + 1) * 512]
                    for j in range(8):
                        lt = attT_all[:, j, t0:t0 + 128] if j < 4 else rwoT[:, j - 4, :]
                        MM(ob, lt, woutb[:, j, half * 512:(half + 1) * 512], ['attT_all', 'rwoT', 'woutb'], ['P2'], start=(j == 0), stop=(j == 7))
                TTo('dve', tres[:], PS[:, 1024:2048], cst['gt1'][:], ALU.mult, ['P2', 'gt1'], ['tres'])
                S.op('dve', lambda e: e.scalar_tensor_tensor(out=tres[:], in0=xin_t[:], scalar=ALPHA, in1=tres[:], op0=ALU.mult, op1=ALU.add),
                     reads=['xin_t', 'tres'], writes=['tres'])
                ln_stats(lntC, tres, 'tres', eps5, 'eps5')
                S.op('act', lambda e: e.activation(out=x1t[:], in_=tres[:], func=AF.Identity, bias=nb[:], scale=rstd[:]),
                     reads=['tres', 'Cnb', 'Crstd'], writes=['x1t'])
                TTo('pool', x1t[:], x1t[:], cst['ln1g'][:], ALU.mult, ['x1t', 'ln1g'], ['x1t'])
                TTo('dve', x1t[:], x1t[:], cst['ln1b'][:], ALU.add, ['x1t', 'ln1b'], ['x1t'])
                S.dma(lambda e, t0=t0: e.dma_start(out=x1D[t0:t0 + 128, :], in_=x1t[:]), reads=['x1t'], writes=['x1D'])
                ln_stats(lntC, x1t, 'x1t', eps5, 'eps5')
                S.op('act', lambda e: e.activation(out=h2f[:], in_=x1t[:], func=AF.Identity, bias=nb[:], scale=rstd[:]),
                     reads=['x1t', 'Cnb', 'Crstd'], writes=['h2f'])
                TTo('pool', h2f[:], h2f[:], cst['sc2p'][:], ALU.mult, ['h2f', 'sc2p'], ['h2f'])
                TTo('dve', h2f[:], h2f[:], cst['sh2'][:], ALU.add, ['h2f', 'sh2'], ['h2f'])
                CP('pool', h2b[:], h2f[:], ['h2f'], ['h2b'])
                S.dma(lambda e, t0=t0: e.dma_start(out=h2D[t0:t0 + 128, :], in_=h2b[:]), reads=['h2b'], writes=['h2D'])
                for j in range(8):
                    TR(PS[:, 2048 + j * 128:2048 + (j + 1) * 128], h2f[:, j * 128:(j + 1) * 128], identf[:], ['h2f', 'identf'], ['P4'])
                CP('dve', h2T[:].rearrange("p j t -> p (j t)"), PS[:, 2048:3072], ['P4'], ['h2T'])
                for j in range(8):
                    MM(PS[:, 3072:3088], h2T[:, j, :], rwf[:, j, :], ['h2T', 'rwf'], ['P6'], start=(j == 0), stop=(j == 7))
                S.op('dve', lambda e: e.tensor_reduce(out=lmax[:], in_=PS[:, 3072:3088], axis=AX.X, op=ALU.max), reads=['P6'], writes=['lmax'])
                S.op('dve', lambda e: e.tensor_scalar(out=lmax[:], in0=lmax[:], scalar1=-1.0, scalar2=None, op0=ALU.mult), reads=['lmax'], writes=['lmax'])
                ACTF(lex[:], PS[:, 3072:3088], AF.Exp, ['P6', 'lmax'], ['lex'], bias=lmax[:])
                S.op('dve', lambda e: e.tensor_reduce(out=lsum[:], in_=lex[:], axis=AX.X, op=ALU.add), reads=['lex'], writes=['lsum'])
                S.op('dve', lambda e: e.reciprocal(out=lsum[:], in_=lsum[:]), reads=['lsum'], writes=['lsum'])
                S.op('dve', lambda e, i=i: e.tensor_scalar(out=aff_all[:, i, :], in0=lex[:], scalar1=lsum[:], scalar2=None, op0=ALU.mult),
                     reads=['lex', 'lsum'], writes=['aff_all'])
            if dbg:
                o1 = dout("dbg_x1", [TL, D]); o2 = dout("dbg_aff", [128, 512])
                S.dma(lambda e: e.dma_start(out=o1[:, :], in_=x1D[:, :]), reads=['x1D'])
                S.dma(lambda e: e.dma_start(out=o2[:, :], in_=aff_all[:].rearrange("p a b -> p (a b)")), reads=['aff_all'])
            S.barrier()
            S.flush()
        stAtt.close()
        if stop_after <= 4:
            return nc, dbg_outs
        posm = sb(top, "posm", [128, 32, 16], F32)
        gw = sb(top, "gw", [128, 32, 16, 2], BF16)
        onesf = sb(top, "onesf", [128, 128], F32)
        iot = sb(top, "iot", [128, 516], F32)
        S.op('pool', lambda e: e.memset(onesf[:], 1.0), writes=['onesf'])
        S.dma(lambda e: e.dma_start(out=iot[:], in_=iot_d[:, :]), writes=['iot'])

        with contextlib.ExitStack() as ph:
            lo = sb(ph, "lo", [128, 16], F32); hi = sb(ph, "hi", [128, 16], F32); mid = sb(ph, "mid", [128, 16], F32)
            cmpt = sb(ph, "cmpt", [128, 32, 16], F32); cntp = sb(ph, "cntp", [128, 16], F32); ge = sb(ph, "ge", [128, 16], F32)
            dlt = sb(ph, "dlt", [128, 16], F32)
            ustr = sb(ph, "ustr", [128, 128], F32)
            mask = sb(ph, "mask", [128, 32, 16], F32); tot = sb(ph, "tot", [128, 32, 16], F32); cum = sb(ph, "cum", [128, 32, 16], F32)
            glo = sb(ph, "glo", [128, 32, 16], F32); ghi32 = sb(ph, "ghi32", [128, 32, 16], F32)
            S.dma(lambda e: e.dma_start(out=ustr[:], in_=ustr_d[:, :]), writes=['ustr'])
            S.op('pool', lambda e: e.memset(lo[:], 0.0), writes=['lo'])
            S.op('pool', lambda e: e.memset(hi[:], 1.0), writes=['hi'])
            affv = aff_all[:, :, :]
            for it in range(30):
                TTo('dve', mid[:], lo[:], hi[:], ALU.add, ['lo', 'hi'], ['mid'])
                S.op('dve', lambda e: e.tensor_scalar(out=mid[:], in0=mid[:], scalar1=0.5, scalar2=None, op0=ALU.mult), reads=['mid'], writes=['mid'])
                TTo('dve', cmpt[:], affv, mid[:, :].unsqueeze(1).to_broadcast([128, 32, 16]), ALU.is_ge, ['aff_all', 'mid'], ['cmpt'])
                S.op('dve', lambda e: e.tensor_reduce(out=cntp[:], in_=cmpt[:].rearrange("p t e -> p e t"), axis=AX.X, op=ALU.add),
                     reads=['cmpt'], writes=['cntp'])
                MM(PS[:, 0:16], onesf[:], cntp[:], ['onesf', 'cntp'], ['P0'])
                S.op('dve', lambda e: e.tensor_scalar(out=ge[:], in0=PS[:, 0:16], scalar1=511.5, scalar2=None, op0=ALU.is_ge), reads=['P0'], writes=['ge'])
                TTo('dve', dlt[:], mid[:], lo[:], ALU.subtract, ['mid', 'lo'], ['dlt'])
                TTo('dve', dlt[:], dlt[:], ge[:], ALU.mult, ['dlt', 'ge'], ['dlt'])
                TTo('dve', lo[:], lo[:], dlt[:], ALU.add, ['lo', 'dlt'], ['lo'])
                TTo('dve', dlt[:], hi[:], mid[:], ALU.subtract, ['hi', 'mid'], ['dlt'])
                TTo('dve', dlt[:], dlt[:], ge[:], ALU.mult, ['dlt', 'ge'], ['dlt'])
                TTo('dve', hi[:], mid[:], dlt[:], ALU.add, ['mid', 'dlt'], ['hi'])
            TTo('dve', mask[:], affv, lo[:, :].unsqueeze(1).to_broadcast([128, 32, 16]), ALU.is_ge, ['aff_all', 'lo'], ['mask'])
            m2 = mask[:].rearrange("p t e -> p (t e)")
            MM(PS[:, 512:1024], ustr[:], m2, ['ustr', 'mask'], ['P1'])
            MM(PS[:, 1024:1536], onesf[:], m2, ['onesf', 'mask'], ['P2'])
            CP('dve', tot[:].rearrange("p t e -> p (t e)"), PS[:, 1024:1536], ['P2'], ['tot'])
            for e_ in range(16):
                S.op('dve', lambda e, e_=e_: e.tensor_tensor_scan(out=cum[:, :, e_], data0=onesf[:, 0:32], data1=tot[:, :, e_], initial=0.0,
                                                                 op0=ALU.mult, op1=ALU.add), reads=['tot', 'onesf'], writes=['cum'])
            TTo('dve', cum[:], cum[:], tot[:], ALU.subtract, ['cum', 'tot'], ['cum'])
            TTo('dve', cum[:].rearrange("p t e -> p (t e)"), cum[:].rearrange("p t e -> p (t e)"), PS[:, 512:1024], ALU.add, ['cum', 'P1'], ['cum'])
            S.op('dve', lambda e: e.scalar_tensor_tensor(out=posm[:], in0=cum[:], scalar=1.0, in1=mask[:], op0=ALU.add, op1=ALU.mult),
                 reads=['cum', 'mask'], writes=['posm'])
            S.op('dve', lambda e: e.tensor_scalar(out=posm[:], in0=posm[:], scalar1=-1.0, scalar2=None, op0=ALU.add), reads=['posm'], writes=['posm'])
            TTo('dve', glo[:], affv, mask[:], ALU.mult, ['aff_all', 'mask'], ['glo'])
            CP('dve', gw[:, :, :, 0], glo[:], ['glo'], ['gw'])
            CP('dve', ghi32[:], gw[:, :, :, 0], ['gw'], ['ghi32'])
            TTo('dve', gw[:, :, :, 1], glo[:], ghi32[:], ALU.subtract, ['glo', 'ghi32'], ['gw'])
            if dbg:
                o1 = dout("dbg_posm", [128, 512])
                S.dma(lambda e: e.dma_start(out=o1[:, :], in_=posm[:].rearrange("p a b -> p (a b)")), reads=['posm'])
            S.barrier()
            S.flush()
        if stop_after <= 5:
            return nc, dbg_outs

        with contextlib.ExitStack() as ph:
            h2_all = sb(ph, "h2_all", [128, 32, D], BF16)
            for i in range(32):
                S.dma(lambda e, i=i: e.dma_start(out=h2_all[:, i, :], in_=h2D[i * 128:(i + 1) * 128, :]), reads=['h2D'], writes=['h2_all'])
            OH = sb(ph, "OH", [128, 32, 512], BF16)
            xinT = sb(ph, "xinT", [128, 8, 512], BF16); hidT = sb(ph, "hidT", [128, 8, 512], BF16)
            wgb = sb(ph, "wgb", [128, 8, D], BF16); wub = sb(ph, "wub", [128, 8, D], BF16); wdb = sb(ph, "wdb", [128, 8, D], BF16)
            wsg = [sb(ph, "wsg%d" % i, [128, D], F32) for i in range(3)]
            gcs = sb(ph, "gcs", [128, 4], F32); sgt = sb(ph, "sgt", [128, 512], F32)
            ywt = sb(ph, "ywt", [128, 4, D], BF16)
            wi = 0
            for ex_ in range(int(os.environ.get('K_NE', 16))):
                for (wsrc, wdst, wkey) in ((wg_d, wgb, 'wgb'), (wu_d, wub, 'wub'), (wd_d, wdb, 'wdb')):
                    for j in range(8):
                        w = wsg[wi % 3]; wk = 'wsg%d' % (wi % 3)
                        S.dma(lambda e, w=w, wsrc=wsrc, ex_=ex_, j=j: e.dma_start(out=w[:], in_=wsrc[ex_, j * 128:(j + 1) * 128, :]), writes=[wk])
                        CP(('dve', 'pool', 'act')[wi % 3], wdst[:, j, :], w[:], [wk], [wkey])
                        wi += 1
                for i in range(32):
                    S.op('dve' if i % 2 else 'pool', lambda e, i=i, ex_=ex_: e.tensor_scalar(
                        out=OH[:, i, :], in0=iot[:, 0:512], scalar1=posm[:, i, ex_:ex_ + 1], scalar2=None, op0=ALU.is_equal),
                        reads=['iot', 'posm'], writes=['OH'])
                for j in range(8):
                    pb = P[j % 2]; pk = 'P%d' % (j % 2)
                    for i in range(32):
                        MM(pb, h2_all[:, i, j * 128:(j + 1) * 128], OH[:, i, :], ['h2_all', 'OH'], [pk], start=(i == 0), stop=(i == 31))
                    CP('act' if j % 2 else 'dve', xinT[:, j, :], pb, [pk], ['xinT'])
                gcp = PS[:, 1024:1032].rearrange("p (c k) -> p c k", k=2)
                for ct in range(4):
                    for i in range(32):
                        MM(gcp[:, ct, :], OH[:, i, ct * 128:(ct + 1) * 128], gw[:, i, ex_, :], ['OH', 'gw'], ['P2'],
                           start=(i == 0 and ct == 0), stop=(i == 31 and ct == 3))
                TTo('dve', gcs[:], gcp[:, :, 0], gcp[:, :, 1], ALU.add, ['P2'], ['gcs']) if False else None
                CP('dve', sgt[:, 0:8], PS[:, 1024:1032], ['P2'], ['sgt'])
                TTo('dve', gcs[:], sgt[:, 0:8].rearrange("p (c k) -> p c k", k=2)[:, :, 0], sgt[:, 0:8].rearrange("p (c k) -> p c k", k=2)[:, :, 1],
                    ALU.add, ['sgt'], ['gcs'])
                for fc in range(8):
                    for j in range(8):
                        MM(P[4], wgb[:, j, fc * 128:(fc + 1) * 128], xinT[:, j, :], ['wgb', 'xinT'], ['P4'], start=(j == 0), stop=(j == 7))
                    for j in range(8):
                        MM(P[5], wub[:, j, fc * 128:(fc + 1) * 128], xinT[:, j, :], ['wub', 'xinT'], ['P5'], start=(j == 0), stop=(j == 7))
                    ACTF(sgt[:], P[4], AF.Silu, ['P4'], ['sgt'])
                    TTo('dve', hidT[:, fc, :], sgt[:], P[5], ALU.mult, ['sgt', 'P5'], ['hidT'])
                for ct in range(4):
                    for half in range(2):
                        pb = P[6 + half]; pk = 'P%d' % (6 + half)
                        for fc in range(8):
                            MM(pb, hidT[:, fc, ct * 128:(ct + 1) * 128], wdb[:, fc, half * 512:(half + 1) * 512], ['hidT', 'wdb'], [pk],
                               start=(fc == 0), stop=(fc == 7))
                        S.op('act', lambda e, ct=ct, half=half, pb=pb: e.activation(out=ywt[:, ct, half * 512:(half + 1) * 512], in_=pb,
                                                                                  func=AF.Copy, scale=gcs[:, ct:ct + 1]),
                             reads=[pk, 'gcs'], writes=['ywt'])
                S.dma(lambda e, ex_=ex_: e.dma_start(out=ywD[ex_].rearrange("(c p) n -> p c n", p=128), in_=ywt[:]), reads=['ywt'], writes=['ywD'])
            if dbg:
                o1 = dout("dbg_yw", [16 * 512, D], BF16)
                S.dma(lambda e: e.dma_start(out=o1[:, :], in_=ywD.rearrange("e c n -> (e c) n")), reads=['ywD'])
            S.barrier()
            S.flush()
        if stop_after <= 6:
            return nc, dbg_outs

        with contextlib.ExitStack() as ph:
            yw_all = sb(ph, "yw_all", [128, 16, 4, D], BF16)
            for ex_ in range(16):
                S.dma(lambda e, ex_=ex_: e.dma_start(out=yw_all[:, ex_, :, :], in_=ywD[ex_].rearrange("(c p) n -> p c n", p=128)),
                      reads=['ywD'], writes=['yw_all'])
            cst = {}
            for nm, src in (("gt2", modd[0:1, 5120:6144]), ("ln2g", ln2_d[0:1, :]), ("ln2b", ln2_d[1:2, :])):
                cst[nm] = sb(ph, nm, [128, D], F32)
                S.dma(lambda e, nm=nm, src=src: e.dma_start(out=cst[nm][:], in_=src.partition_broadcast(128)), reads=['modd'], writes=[nm])
            dg = sb(ph, "dg", [128, 4, 128], F32)
            OHT = sb(ph, "OHT", [128, 4, 2048], BF16)
            x1l = sb(ph, "x1l", [128, D], F32); tr2 = sb(ph, "tr2", [128, D], F32); xo = sb(ph, "xo", [128, D], F32)
            stats = sb(ph, "statsF", [128, 2, 6], F32); mv = sb(ph, "mvF", [128, 2], F32)
            rstd = sb(ph, "rstdF", [128, 1], F32); nb = sb(ph, "nbF", [128, 1], F32)
            lntF = (stats, mv, rstd, nb, 'F')
            for i in range(int(os.environ.get('K_FT', 32))):
                t0 = i * 128
                S.dma(lambda e, t0=t0: e.dma_start(out=x1l[:], in_=x1D[t0:t0 + 128, :]), reads=['x1D'], writes=['x1l'])
                for eg in range(4):
                    for k_ in range(4):
                        ex_ = eg * 4 + k_
                        S.op('dve' if k_ % 2 else 'pool', lambda e, k_=k_, ex_=ex_, i=i: e.tensor_scalar(
                            out=dg[:, k_, :], in0=identf[:], scalar1=posm[:, i, ex_:ex_ + 1], scalar2=None, op0=ALU.mult),
                            reads=['identf', 'posm'], writes=['dg'])
                    MM(PS[:, eg * 512:(eg + 1) * 512], onesf[:], dg[:].rearrange("p a t -> p (a t)"), ['onesf', 'dg'], ['P%d' % eg])
                for ct in range(4):
                    S.op('dve', lambda e, ct=ct: e.tensor_scalar(out=OHT[:, ct, :], in0=PS[:, 0:2048], scalar1=iot[:, 512 + ct:513 + ct],
                                                                scalar2=None, op0=ALU.is_equal),
                         reads=['P0', 'P1', 'P2', 'P3', 'iot'], writes=['OHT'])
                for half in range(2):
                    pb = P[4 + half]; pk = 'P%d' % (4 + half)
                    n = 0
                    for ex_ in range(16):
                        for ct in range(4):
                            MM(pb, OHT[:, ct, ex_ * 128:(ex_ + 1) * 128], yw_all[:, ex_, ct, half * 512:(half + 1) * 512], ['OHT', 'yw_all'], [pk],
                               start=(n == 0), stop=(n == 63))
                            n += 1
                TTo('dve', tr2[:], PS[:, 2048:3072], cst['gt2'][:], ALU.mult, ['P4', 'P5', 'gt2'], ['tr2'])
                S.op('dve', lambda e: e.scalar_tensor_tensor(out=tr2[:], in0=x1l[:], scalar=ALPHA, in1=tr2[:], op0=ALU.mult, op1=ALU.add),
                     reads=['x1l', 'tr2'], writes=['tr2'])
                ln_stats(lntF, tr2, 'tr2', eps5, 'eps5')
                S.op('act', lambda e: e.activation(out=xo[:], in_=tr2[:], func=AF.Identity, bias=nb[:], scale=rstd[:]),
                     reads=['tr2', 'Fnb', 'Frstd'], writes=['xo'])
                TTo('pool', xo[:], xo[:], cst['ln2g'][:], ALU.mult, ['xo', 'ln2g'], ['xo'])
                TTo('dve', xo[:], xo[:], cst['ln2b'][:], ALU.add, ['xo', 'ln2b'], ['xo'])
                S.dma(lambda e, t0=t0: e.dma_start(out=out_d[t0:t0 + 128, :], in_=xo[:]), reads=['xo'], writes=['out'])
            S.barrier()
            S.flush()
    return nc, dbg_outs


def host_inputs(inp, b):
    f = np.float32
    m = {}
    m["x"] = np.ascontiguousarray(inp["x"][b], dtype=f)
    m["ctx"] = np.ascontiguousarray(inp["ctx"][b], dtype=f)
    m["cc"] = np.ascontiguousarray(np.stack([inp["c"][b].reshape(8, 128).T, inp["c_ctx"].reshape(8, 128).T], -1).reshape(128, 16), dtype=f)
    m["w_ada"] = np.ascontiguousarray(inp["w_ada"][0], dtype=f)
    m["b_ada"] = np.ascontiguousarray(inp["b_ada"][0].reshape(1, -1), dtype=f)
    m["w_in"] = np.ascontiguousarray(inp["w_in"][0], dtype=f)
    m["qg"] = np.ascontiguousarray(np.tile(inp["q_gain"][0], 8).reshape(1, 512), dtype=f)
    m["kg"] = np.ascontiguousarray(np.tile(inp["k_gain"][0], 2).reshape(1, 128), dtype=f)
    t = np.arange(TL)
    pos = np.stack([t // 64, t % 64], -1).astype(np.float32)
    inv = (10000.0 ** (-np.arange(16, dtype=np.float32) / 16)).astype(np.float32)
    ang = pos[:, :, None] * inv[None, None, :]
    cs = np.cos(ang).astype(f); sn = np.sin(ang).astype(f)
    cos2 = np.stack([cs, cs], 2).reshape(TL, 64)
    sin2 = np.stack([-sn, sn], 2).reshape(TL, 64)
    m["cosT"] = np.ascontiguousarray(np.tile(cos2, (1, 8)), dtype=f)
    m["sinS"] = np.ascontiguousarray(np.tile(sin2, (1, 8)), dtype=f)
    m["ident"] = np.eye(128, dtype=f)
    def kh(v):
        return np.asarray(v, dtype=f).reshape(8, 64).T
    mu = inp["tshift_mu"][0]
    cols = [kh(mu[0:512]), kh(mu[512:1024]), kh(mu[1024:1536]), kh(inp["k_k"][0]), kh(inp["k_a"][0]), kh(inp["r_k"][0].reshape(-1)),
            kh(inp["decay_w0"][0, 0]), kh(inp["decay_w0"][0, 1]), kh(inp["iclr_a0"][0, 0]), kh(inp["iclr_a0"][0, 1])]
    m["rp"] = np.ascontiguousarray(np.stack(cols, -1).reshape(64, 80), dtype=f)
    lmu = np.zeros((128, 3), f)
    lmu[0:64, 0] = mu[1536:1600]; lmu[0:64, 1] = mu[1600:1664]; lmu[:, 2] = mu[1664:1792]
    m["lmu"] = lmu
    m["decup"] = np.ascontiguousarray(np.concatenate([inp["decay_up"][0, 0], inp["decay_up"][0, 1]], 1), dtype=f)
    m["iclup"] = np.ascontiguousarray(np.concatenate([inp["iclr_up"][0, 0], inp["iclr_up"][0, 1]], 1), dtype=f)
    m["gateup"] = np.ascontiguousarray(inp["gate_up"][0], dtype=f)
    hh = np.repeat(np.arange(8), 16); tt = np.tile(np.arange(16), 8)
    same = hh[:, None] == hh[None, :]
    msf = (same & (tt[:, None] < tt[None, :])).astype(f); mif = (same & (tt[:, None] <= tt[None, :])).astype(f)
    msb = msf.T.copy(); mib = mif.T.copy()
    m["mk4"] = np.ascontiguousarray(np.concatenate([msf, mif, msf, mif, msb, mib, msb, mib], 1), dtype=f)
    m["mn1"] = np.ascontiguousarray(np.concatenate([msb, msf], 1), dtype=f)
    m["bm"] = np.ascontiguousarray((hh[:, None] == np.repeat(np.arange(8), 64)[None, :]).astype(f))
    rst = np.ones((64, 1024), f); rst[:, ::16] = 0.0
    m["rst"] = rst
    m["w_out"] = np.ascontiguousarray(inp["w_out"][0], dtype=f)
    m["lnx"] = np.ascontiguousarray(np.stack([inp["lnx_g"][0], inp["lnx_b"][0]]), dtype=f)
    m["ln1"] = np.ascontiguousarray(np.stack([inp["ln1_g"][0], inp["ln1_b"][0]]), dtype=f)
    m["ln2"] = np.ascontiguousarray(np.stack([inp["ln2_g"][0], inp["ln2_b"][0]]), dtype=f)
    m["router_w"] = np.ascontiguousarray(inp["router_w"][0], dtype=f)
    m["exp_w_gate"] = np.ascontiguousarray(inp["exp_w_gate"][0], dtype=f)
    m["exp_w_up"] = np.ascontiguousarray(inp["exp_w_up"][0], dtype=f)
    m["exp_w_down"] = np.ascontiguousarray(inp["exp_w_down"][0], dtype=f)
    iot = np.zeros((128, 516), f)
    iot[:, 0:512] = np.arange(512, dtype=f)[None, :]
    iot[:, 512:516] = np.arange(128, dtype=f)[:, None] + 128.0 * np.arange(4, dtype=f)[None, :]
    m["iot"] = iot
    m["ustr"] = np.triu(np.ones((128, 128), f), 1)
    return m


_NC_CACHE = {}


def kernel(**inputs):
    inp = {k: np.asarray(v) for k, v in inputs.items()}
    if "full" not in _NC_CACHE:
        _NC_CACHE["full"] = build_nc()[0]
    nc = _NC_CACHE["full"]
    in_maps = [host_inputs(inp, c // 2) for c in range(8)]
    res = run_bass_kernel_spmd(nc, in_maps, core_ids=list(range(8)))
    out = np.stack([res.results[2 * b]["out"] for b in range(4)], 0).astype(np.float32)
    return out
```

```python
import contextlib
import os
import numpy as np
import concourse.bass as bass
import concourse.mybir as mybir
from concourse.bass_utils import run_bass_kernel_spmd

F32 = mybir.dt.float32
BF16 = mybir.dt.bfloat16
AF = mybir.ActivationFunctionType
ALU = mybir.AluOpType
AX = mybir.AxisListType

D = 1024
TL = 4096
TC = 256
TT = TL + TC
NT = TT // 128
ALPHA = 2.0 ** 0.25
DEC_C = -float(np.exp(-0.5))


class Sched:
    CE = ('pe', 'act', 'dve', 'pool')

    def __init__(self, nc, stack, ndma=32):
        self.nc = nc
        self.ops = {e: [] for e in ('pe', 'act', 'dve', 'pool', 'sp')}
        self.cnt = {e: 0 for e in self.CE}
        self.last_w = {}
        self.readers = {}
        self.waited = {e: {} for e in self.ops}
        self.ndma = ndma
        self.dma_val = [0] * ndma
        self.dma_i = 0
        names = list(self.CE) + ['d%d' % i for i in range(ndma)]
        self.sems = {n: stack.enter_context(nc.semaphore('s_' + n)) for n in names}

    def _deps(self, eng, reads, writes):
        deps = {}

        def add(tok):
            if tok is None:
                return
            s, v = tok
            if deps.get(s, 0) < v:
                deps[s] = v
        for r in reads:
            add(self.last_w.get(r))
        for w in writes:
            add(self.last_w.get(w))
            for t in self.readers.get(w, ()):
                add(t)
        waits = []
        for s, v in deps.items():
            if s == eng and eng == 'pe':
                continue
            if self.waited[eng].get(s, 0) >= v:
                continue
            self.waited[eng][s] = v
            waits.append((s, v))
        return waits

    def _commit(self, tok, reads, writes):
        for r in reads:
            self.readers.setdefault(r, []).append(tok)
        for w in writes:
            self.last_w[w] = tok
            self.readers[w] = []

    def op(self, eng, fn, reads=(), writes=()):
        waits = self._deps(eng, reads, writes)
        self.cnt[eng] += 1
        tok = (eng, self.cnt[eng])
        self.ops[eng].append((waits, fn, (eng, 1)))
        self._commit(tok, reads, writes)

    def dma(self, fn, reads=(), writes=(), q='sp'):
        slot = self.dma_i % self.ndma
        self.dma_i += 1
        s = 'd%d' % slot
        waits = self._deps(q, reads, writes)
        pv = self.dma_val[slot]
        if pv > 0 and self.waited[q].get(s, 0) < pv:
            self.waited[q][s] = pv
            waits.append((s, pv))
        self.dma_val[slot] = pv + 16
        tok = (s, pv + 16)
        self.ops[q].append((waits, fn, (s, 16)))
        self._commit(tok, reads, writes)

    def barrier(self):
        allw = [(e, c) for e, c in self.cnt.items() if c > 0]
        allw += [('d%d' % i, v) for i, v in enumerate(self.dma_val) if v > 0]
        for eng in self.ops:
            waits = []
            for s, v in allw:
                if self.waited[eng].get(s, 0) >= v:
                    continue
                self.waited[eng][s] = v
                waits.append((s, v))
            if waits:
                self.ops[eng].append((waits, None, None))
        self.last_w = {}
        self.readers = {}

    def flush(self):
        nc = self.nc
        sems = self.sems
        ops = self.ops
        self.ops = {e: [] for e in ops}
        with nc.Block() as block:
            def run(engname, engobj):
                for waits, fn, inc in ops[engname]:
                    for ws, wv in waits:
                        engobj.wait_ge(sems[ws], wv)
                    if fn is not None:
                        ins = fn(engobj)
                        ins.then_inc(sems[inc[0]], inc[1])

            @block.sync
            def _(e):
                run('sp', e)

            @block.tensor
            def _(e):
                run('pe', e)

            @block.scalar
            def _(e):
                run('act', e)

            @block.vector
            def _(e):
                run('dve', e)

            @block.gpsimd
            def _(e):
                run('pool', e)


def build_nc(stop_after=99, dbg=False):
    nc = bass.Bass("TRN2", target_bir_lowering=False)

    def din(name, shape, dt=F32):
        return nc.dram_tensor(name, list(shape), dt, kind="ExternalInput").ap()

    def dscr(name, shape, dt=F32):
        return nc.dram_tensor(name, list(shape), dt, kind="Internal").ap()

    x_d = din("x", [TL, D]); ctx_d = din("ctx", [TC, D])
    cc_d = din("cc", [128, 16])
    wada_d = din("w_ada", [D, 6 * D]); bada_d = din("b_ada", [1, 6 * D])
    win_d = din("w_in", [D, 2560])
    qg_d = din("qg", [1, 512]); kg_d = din("kg", [1, 128])
    cos_d = din("cosT", [TL, 512]); sin_d = din("sinS", [TL, 512])
    ident_d = din("ident", [128, 128])
    rp_d = din("rp", [64, 8 * 10])
    lmu_d = din("lmu", [128, 3])
    decup_d = din("decup", [64, 2 * 512]); iclup_d = din("iclup", [64, 2 * 512]); gateup_d = din("gateup", [128, 512])
    mk4_d = din("mk4", [128, 2 * 512]); mn1_d = din("mn1", [128, 2 * 128]); bm_d = din("bm", [128, 512])
    rst_d = din("rst", [64, 1024])
    wout_d = din("w_out", [D, D]); lnx_d = din("lnx", [2, 512])
    ln1_d = din("ln1", [2, D]); ln2_d = din("ln2", [2, D])
    rw_d = din("router_w", [D, 16])
    x1D = dscr("x1D", [TL, D]); h2D = dscr("h2D", [TL, D], BF16)
    wg_d = din("exp_w_gate", [16, D, D]); wu_d = din("exp_w_up", [16, D, D]); wd_d = din("exp_w_down", [16, D, D])
    iot_d = din("iot", [128, 512 + 4]); ustr_d = din("ustr", [128, 128])
    ywD = dscr("ywD", [16, 512, D], BF16)
    ydir = dscr("ydir", [2, TL, 512]); bonD = dscr("bonD", [TL, 512]); gD = dscr("gD", [TL, 512])
    out_d = nc.dram_tensor("out", [TL, D], F32, kind="ExternalOutput").ap()
    modd = dscr("modd", [2, 6 * D])
    rwT = dscr("rwT", [1792, TT])
    dbg_outs = {}

    def dout(name, shape, dt=F32):
        ap = nc.dram_tensor(name, list(shape), dt, kind="ExternalOutput").ap()
        dbg_outs[name] = ap
        return ap

    with contextlib.ExitStack() as top:
        S = Sched(nc, top)

        def sb(stack, name, shape, dt):
            return stack.enter_context(nc.sbuf_tensor("sb_" + name, list(shape), dt))

        PS = top.enter_context(nc.psum_tensor("PS", [128, 8 * 512], F32))
        P = [PS[:, i * 512:(i + 1) * 512] for i in range(8)]
        PK = ['P%d' % i for i in range(8)]
        identf = sb(top, "identf", [128, 128], F32)
        identb = sb(top, "identb", [128, 128], BF16)
        eps5 = sb(top, "eps5", [128, 1], F32)
        eps6 = sb(top, "eps6", [128, 1], F32)
        S.dma(lambda e: e.dma_start(out=identf[:], in_=ident_d[:, :]), writes=['identf'])
        S.op('dve', lambda e: e.tensor_copy(out=identb[:], in_=identf[:]), reads=['identf'], writes=['identb'])
        S.op('pool', lambda e: e.memset(eps5[:], 1e-5), writes=['eps5'])
        S.op('pool', lambda e: e.memset(eps6[:], 1e-6), writes=['eps6'])

        def ln_stats(stack_tiles, src, key_src, eps_t, eps_key):
            stats, mv, rstd, nb, kq = stack_tiles
            for cch in range(2):
                S.op('dve', lambda e, cch=cch: e.bn_stats(out=stats[:, cch, :], in_=src[:, cch * 512:(cch + 1) * 512]),
                     reads=[key_src], writes=[kq + 'stats'])
            S.op('dve', lambda e: e.bn_aggr(out=mv[:], in_=stats[:]), reads=[kq + 'stats'], writes=[kq + 'mv'])
            S.op('act', lambda e: e.activation(out=rstd[:], in_=mv[:, 1:2], func=AF.Ln, bias=eps_t[:], scale=1.0),
                 reads=[kq + 'mv', eps_key], writes=[kq + 'rstd'])
            S.op('act', lambda e: e.activation(out=rstd[:], in_=rstd[:], func=AF.Exp, scale=-0.5),
                 reads=[kq + 'rstd'], writes=[kq + 'rstd'])
            S.op('dve', lambda e: e.scalar_tensor_tensor(out=nb[:], in0=mv[:, 0:1], scalar=-1.0, in1=rstd[:],
                                                        op0=ALU.mult, op1=ALU.mult),
                 reads=[kq + 'mv', kq + 'rstd'], writes=[kq + 'nb'])

        with contextlib.ExitStack() as ph:
            cc = sb(ph, "cc", [128, 8, 2], F32)
            ccs = sb(ph, "ccs", [128, 8, 2], F32)
            wst = [sb(ph, "wst%d" % i, [128, 8, 512], F32) for i in range(2)]
            bada = sb(ph, "bada", [2, 6 * D], F32)
            modrow = sb(ph, "modrow", [2, 6 * D], F32)
            S.dma(lambda e: e.dma_start(out=cc[:].rearrange("p a b -> p (a b)"), in_=cc_d[:, :]), writes=['cc'])
            S.dma(lambda e: e.dma_start(out=bada[:], in_=bada_d.partition_broadcast(2)), writes=['bada'])
            S.op('act', lambda e: e.activation(out=ccs[:], in_=cc[:], func=AF.Silu), reads=['cc'], writes=['ccs'])
            for g in range(12):
                w = wst[g % 2]
                wk = 'wst%d' % (g % 2)
                S.dma(lambda e, w=w, g=g: e.dma_start(
                    out=w[:], in_=wada_d[:, g * 512:(g + 1) * 512].rearrange("(j p) n -> p j n", p=128)), writes=[wk])
                for j in range(8):
                    S.op('pe', lambda e, w=w, j=j: e.matmul(P[0][0:2, :], lhsT=ccs[:, j, :], rhs=w[:, j, :],
                                                           start=(j == 0), stop=(j == 7)),
                         reads=['ccs', wk], writes=['P0'])
                S.op('dve', lambda e, g=g: e.tensor_tensor(out=modrow[:, g * 512:(g + 1) * 512], in0=P[0][0:2, :],
                                                          in1=bada[:, g * 512:(g + 1) * 512], op=ALU.add),
                     reads=['P0', 'bada'], writes=['modrow'])
            for lo in (1024, 4096):
                S.op('dve', lambda e, lo=lo: e.tensor_scalar_add(out=modrow[:, lo:lo + 1024], in0=modrow[:, lo:lo + 1024],
                                                                scalar1=1.0), reads=['modrow'], writes=['modrow'])
            S.dma(lambda e: e.dma_start(out=modd[:, :], in_=modrow[:]), reads=['modrow'], writes=['modd'])
            if dbg:
                o = dout("dbg_mod", [2, 6 * D])
                S.dma(lambda e: e.dma_start(out=o[:, :], in_=modrow[:]), reads=['modrow'])
            S.barrier()
            S.flush()
        if stop_after <= 0:
            return nc, dbg_outs

        def modrow_bc(dst, row, lo):
            S.dma(lambda e: e.dma_start(out=dst[:], in_=modd[row:row + 1, lo:lo + 1024].partition_broadcast(128)),
                  reads=['modd'], writes=[dst.name if hasattr(dst, 'name') else 'x'])

        aff_all = sb(top, "aff_all", [128, 32, 16], F32)
        stAtt = contextlib.ExitStack()
        top.callback(stAtt.close)
        attT_all = sb(stAtt, "attT_all", [128, 4, TL], BF16)
        with contextlib.ExitStack() as phAB:
            qT_all = sb(phAB, "qT_all", [128, 4, TL], BF16)
            kTd = sb(phAB, "kTd", [128, 2, TT], BF16)
            Vx = sb(phAB, "Vx", [128, NT, 2, 80], BF16)
            with contextlib.ExitStack() as ph:
                winb = sb(ph, "winb", [128, 8, 2560], BF16)
                wstg = [sb(ph, "wstg%d" % i, [128, 640], F32) for i in range(2)]
                sc1p = sb(ph, "sc1p", [128, D], F32); sh1 = sb(ph, "sh1", [128, D], F32)
                csc1p = sb(ph, "csc1p", [128, D], F32); csh1 = sb(ph, "csh1", [128, D], F32)
                qg = sb(ph, "qg", [128, 512], F32); kg = sb(ph, "kg", [128, 128], F32)
                for dst, nm, row, lo in ((sh1, 'sh1', 0, 0), (sc1p, 'sc1p', 0, 1024), (csh1, 'csh1', 1, 0), (csc1p, 'csc1p', 1, 1024)):
                    S.dma(lambda e, dst=dst, row=row, lo=lo: e.dma_start(
                        out=dst[:], in_=modd[row:row + 1, lo:lo + 1024].partition_broadcast(128)), reads=['modd'], writes=[nm])
                S.dma(lambda e: e.dma_start(out=qg[:], in_=qg_d.partition_broadcast(128)), writes=['qg'])
                S.dma(lambda e: e.dma_start(out=kg[:], in_=kg_d.partition_broadcast(128)), writes=['kg'])
                for jj in range(32):
                    j = jj // 4; c0 = (jj % 4) * 640
                    w = wstg[jj % 2]; wk = 'wstg%d' % (jj % 2)
                    S.dma(lambda e, w=w, j=j, c0=c0: e.dma_start(out=w[:], in_=win_d[j * 128:(j + 1) * 128, c0:c0 + 640]), writes=[wk])
                    S.op('pool' if jj % 2 else 'dve', lambda e, w=w, j=j, c0=c0: e.tensor_copy(out=winb[:, j, c0:c0 + 640], in_=w[:]),
                         reads=[wk], writes=['winb'])
                S.op('pool', lambda e: e.memset(Vx[:], 1.0), writes=['Vx'])
                xt = [sb(ph, "xt%d" % i, [128, D], F32) for i in range(2)]
                xn = sb(ph, "xn", [128, D], F32)
                hb = sb(ph, "hb", [128, D], BF16)
                hT = sb(ph, "hT", [128, 8, 512], BF16)
                stats = sb(ph, "stats", [128, 2, 6], F32); mv = sb(ph, "mv", [128, 2], F32)
                rstd = sb(ph, "rstd", [128, 1], F32); nb = sb(ph, "nb", [128, 1], F32)
                lnt = (stats, mv, rstd, nb, 'A')
                rstg = [sb(ph, "rstg%d" % i, [128, 512], F32) for i in range(2)]
                cosT = sb(ph, "cosT", [128, 512], F32); sinS = sb(ph, "sinS", [128, 512], F32)
                sq = sb(ph, "sq", [128, 512], F32)
                ssum = sb(ph, "ssum", [128, 8], F32)
                qn = sb(ph, "qn", [128, 512], F32); qa = sb(ph, "qa", [128, 512], F32)
                qbt = sb(ph, "qbt", [128, 512], F32)
                qo = sb(ph, "qo", [128, 512], BF16)
                ko = sb(ph, "ko", [128, 2, 2, 64], BF16)
                blocks = [(0, 256)] + [(256 + 512 * i, 512) for i in range(8)]
                ti = 0
                for (u0, ntok) in blocks:
                    ntile = ntok // 128
                    is_ctx = (u0 == 0)
                    for tl in range(ntile):
                        u = u0 + tl * 128
                        gt = u // 128
                        X = xt[ti % 2]; xk = 'xt%d' % (ti % 2)
                        ti += 1
                        src = ctx_d[u:u + 128, :] if is_ctx else x_d[u - TC:u - TC + 128, :]
                        S.dma(lambda e, X=X, src=src: e.dma_start(out=X[:], in_=src), writes=[xk])
                        ln_stats(lnt, X, xk, eps5, 'eps5')
                        S.op('act', lambda e, X=X: e.activation(out=xn[:], in_=X[:], func=AF.Identity, bias=nb[:], scale=rstd[:]),
                             reads=[xk, 'Anb', 'Arstd'], writes=['xn'])
                        scp, shh, k1, k2 = (csc1p, csh1, 'csc1p', 'csh1') if is_ctx else (sc1p, sh1, 'sc1p', 'sh1')
                        S.op('pool', lambda e, scp=scp: e.tensor_tensor(out=xn[:], in0=xn[:], in1=scp[:], op=ALU.mult),
                             reads=['xn', k1], writes=['xn'])
                        S.op('dve', lambda e, shh=shh: e.tensor_tensor(out=hb[:], in0=xn[:], in1=shh[:], op=ALU.add),
                             reads=['xn', k2], writes=['hb'])
                        pTb = P[0][:].bitcast(BF16)
                        for j in range(8):
                            S.op('pe', lambda e, j=j, pTb=pTb: e.transpose(out=pTb[:, j * 128:(j + 1) * 128],
                                                                      in_=hb[:, j * 128:(j + 1) * 128], identity=identb[:]),
                                 reads=['hb', 'identb'], writes=['P0'])
                        S.op('act', lambda e, tl=tl, pTb=pTb: e.copy(out=hT[:, :, tl * 128:(tl + 1) * 128],
                                                                 in_=pTb.rearrange("p (j t) -> p j t", j=8)),
                             reads=['P0'], writes=['hT'])
                        for j in range(8):
                            S.op('pe', lambda e, j=j, tl=tl: e.matmul(P[1][:, :], lhsT=hT[:, j, tl * 128:(tl + 1) * 128],
                                                                     rhs=winb[:, j, 0:512], start=(j == 0), stop=(j == 7)),
                                 reads=['hT', 'winb'], writes=['P1'])
                        for j in range(8):
                            S.op('pe', lambda e, j=j, tl=tl: e.matmul(P[2][:, 0:256], lhsT=hT[:, j, tl * 128:(tl + 1) * 128],
                                                                     rhs=winb[:, j, 512:768], start=(j == 0), stop=(j == 7)),
                                 reads=['hT', 'winb'], writes=['P2'])
                        S.op('act', lambda e, gt=gt: e.copy(out=Vx[:, gt, :, 0:64],
                                                          in_=P[2][:, 128:256].rearrange("p (a b) -> p a b", a=2)),
                             reads=['P2'], writes=['Vx'])
                        if not is_ctx:
                            ul = u - TC
                            S.dma(lambda e, ul=ul: e.dma_start(out=cosT[:], in_=cos_d[ul:ul + 128, :]), writes=['cosT'])
                            S.dma(lambda e, ul=ul: e.dma_start(out=sinS[:], in_=sin_d[ul:ul + 128, :]), writes=['sinS'])

                        def normrope(psrc, pk, nh, gain, gk, do_rope, outfn):
                            w_ = nh * 64
                            S.op('act', lambda e: e.activation(out=sq[:, 0:w_], in_=psrc, func=AF.Square),
                                 reads=[pk], writes=['sq'])
                            S.op('dve', lambda e: e.tensor_reduce(out=ssum[:, 0:nh], in_=sq[:, 0:w_].rearrange("p (h d) -> p h d", d=64),
                                                                 axis=AX.X, op=ALU.add), reads=['sq'], writes=['ssum'])
                            S.op('act', lambda e: e.activation(out=ssum[:, 0:nh], in_=ssum[:, 0:nh], func=AF.Ln, bias=eps6[:], scale=1.0 / 64),
                                 reads=['ssum', 'eps6'], writes=['ssum'])
                            S.op('act', lambda e: e.activation(out=ssum[:, 0:nh], in_=ssum[:, 0:nh], func=AF.Exp, scale=-0.5),
                                 reads=['ssum'], writes=['ssum'])
                            S.op('dve', lambda e: e.tensor_tensor(out=qn[:, 0:w_].rearrange("p (h d) -> p h d", d=64),
                                                                 in0=psrc.rearrange("p (h d) -> p h d", d=64),
                                                                 in1=ssum[:, 0:nh].unsqueeze(2).to_broadcast([128, nh, 64]), op=ALU.mult),
                                 reads=[pk, 'ssum'], writes=['qn'])
                            if not do_rope:
                                S.op('pool', lambda e: outfn(e, qn[:, 0:w_], gain[:, 0:w_], ALU.mult), reads=['qn', gk], writes=['qko'])
                                return
                            S.op('pool', lambda e: e.tensor_tensor(out=qn[:, 0:w_], in0=qn[:, 0:w_], in1=gain[:, 0:w_], op=ALU.mult),
                                 reads=['qn', gk], writes=['qn'])
                            S.op('dve', lambda e: e.tensor_tensor(out=qa[:, 0:w_], in0=qn[:, 0:w_], in1=cosT[:, 0:w_], op=ALU.mult),
                                 reads=['qn', 'cosT'], writes=['qa'])
                            qv = qn[:, 0:w_].rearrange("p (g s q) -> p g s q", s=2, q=16)
                            bv = qbt[:, 0:w_].rearrange("p (g s q) -> p g s q", s=2, q=16)
                            sv = sinS[:, 0:w_].rearrange("p (g s q) -> p g s q", s=2, q=16)
                            for s_ in range(2):
                                S.op('pool', lambda e, s_=s_: e.tensor_tensor(out=bv[:, :, s_, :], in0=qv[:, :, 1 - s_, :],
                                                                             in1=sv[:, :, s_, :], op=ALU.mult),
                                     reads=['qn', 'sinS'], writes=['qbt'])
                            S.op('dve', lambda e: outfn(e, qa[:, 0:w_], qbt[:, 0:w_], ALU.add), reads=['qa', 'qbt'], writes=['qko'])

                        if not is_ctx:
                            normrope(P[1][:, :], 'P1', 8, qg, 'qg', True,
                                     lambda e, a, b_, op: e.tensor_tensor(out=qo[:], in0=a, in1=b_, op=op))
                            pq = P[3][:].bitcast(BF16)
                            for hp in range(4):
                                S.op('pe', lambda e, hp=hp, pq=pq: e.transpose(out=pq[:, hp * 128:(hp + 1) * 128],
                                                                          in_=qo[:, hp * 128:(hp + 1) * 128], identity=identb[:]),
                                     reads=['qko', 'identb'], writes=['P3'])
                            S.op('act', lambda e, ul=ul, pq=pq: e.copy(out=qT_all[:, :, ul:ul + 128],
                                                                   in_=pq[:, 0:512].rearrange("p (j t) -> p j t", j=4)),
                                 reads=['P3'], writes=['qT_all'])

                        def kout(e, a, b_, op):
                            return e.tensor_tensor(out=ko[:, :, 0, :], in0=a.rearrange("p (h d) -> p h d", d=64),
                                                   in1=b_.rearrange("p (h d) -> p h d", d=64), op=op)
                        normrope(P[2][:, 0:128], 'P2', 2, kg, 'kg', not is_ctx, kout)
                        S.op('pool', lambda e: e.tensor_copy(out=ko[:, :, 1, :], in_=ko[:, :, 0, :]), reads=['qko'], writes=['qko'])
                        pk_ = P[3][:].bitcast(BF16)
                        for kv in range(2):
                            S.op('pe', lambda e, kv=kv, pk_=pk_: e.transpose(
                                out=pk_[:, 512 + kv * 128:512 + (kv + 1) * 128],
                                in_=ko[:, kv, :, :].rearrange("p a d -> p (a d)"), identity=identb[:]),
                                reads=['qko', 'identb'], writes=['P3'])
                        S.op('act', lambda e, u=u, pk_=pk_: e.copy(out=kTd[:, :, u:u + 128],
                                                               in_=pk_[:, 512:768].rearrange("p (j t) -> p j t", j=2)),
                             reads=['P3'], writes=['kTd'])
                    for g in range(14):
                        pb = P[4 + g % 2]; pbk = 'P%d' % (4 + g % 2)
                        for j in range(8):
                            S.op('pe', lambda e, j=j, g=g, pb=pb, ntok=ntok: e.matmul(pb[:, 0:ntok], lhsT=winb[:, j, 768 + g * 128:768 + (g + 1) * 128],
                                                                         rhs=hT[:, j, 0:ntok], start=(j == 0), stop=(j == 7)),
                                 reads=['hT', 'winb'], writes=[pbk])
                        rs = rstg[g % 2]; rk = 'rstg%d' % (g % 2)
                        S.op('act' if g % 2 else 'dve',
                             (lambda e, rs=rs, pb=pb, ntok=ntok: e.copy(out=rs[:, 0:ntok], in_=pb[:, 0:ntok])) if g % 2 else
                             (lambda e, rs=rs, pb=pb, ntok=ntok: e.tensor_copy(out=rs[:, 0:ntok], in_=pb[:, 0:ntok])),
                             reads=[pbk], writes=[rk])
                        S.dma(lambda e, rs=rs, g=g, u0=u0, ntok=ntok: e.dma_start(out=rwT[g * 128:(g + 1) * 128, u0:u0 + ntok], in_=rs[:, 0:ntok]),
                              reads=[rk], writes=['rwT'])
                if dbg:
                    o1 = dout("dbg_qT", [128, 4 * TL], BF16)
                    S.dma(lambda e: e.dma_start(out=o1[:, :], in_=qT_all[:].rearrange("p a t -> p (a t)")), reads=['qT_all'])
                    o2 = dout("dbg_kTd", [128, 2 * TT], BF16)
                    S.dma(lambda e: e.dma_start(out=o2[:, :], in_=kTd[:].rearrange("p a t -> p (a t)")), reads=['kTd'])
                    o3 = dout("dbg_rwT", [1792, TT])
                    S.dma(lambda e: e.dma_start(out=o3[:, :], in_=rwT[:, :]), reads=['rwT'])
                S.barrier()
                S.flush()
            if stop_after <= 1:
                return nc, dbg_outs
            with contextlib.ExitStack() as ph:
                pT = [sb(ph, "pT%d" % i, [128, 512], BF16) for i in range(3)]
                rec = sb(ph, "rec", [128, 8], F32)
                atok = sb(ph, "atok", [128, 8, 64], BF16)
                pi = 0
                for g in range(int(os.environ.get('K_NG', 16))):
                    q0 = g * 256
                    for st in range(NT):
                        for hp in range(int(os.environ.get('K_NHP', 4))):
                            sb0 = 2 * (hp % 2)
                            scb = PS[:, sb0 * 512:(sb0 + 2) * 512].rearrange("p (b n) -> p b n", b=2)
                            sck = 'P%d' % sb0
                            kvh = hp // 2
                            for hh in range(int(os.environ.get('K_HH0', 0)), int(os.environ.get('K_NHH', 2))):
                                S.op('pe', lambda e, hh=hh, hp=hp, scb=scb, kvh=kvh, st=st, q0=q0: e.matmul(
                                    scb[:, hh, 0:256],
                                    lhsT=kTd[hh * 64:(hh + 1) * 64, kvh, st * 128:(st + 1) * 128],
                                    rhs=qT_all[hh * 64:(hh + 1) * 64, hp, q0:q0 + 256], start=True, stop=True),
                                    reads=['kTd', 'qT_all'], writes=[sck])
                            pt = pT[pi % 3]; ptk = 'pT%d' % (pi % 3)
                            pi += 1
                            if not os.environ.get('K_SKIP_EXP'):
                                S.op('act', lambda e, pt=pt, scb=scb: e.activation(out=pt[:].rearrange('p (b n) -> p b n', b=2), in_=scb[:, :, 0:256], func=AF.Exp, scale=0.125),
                                     reads=[sck], writes=[ptk])
                            for hh in range(2 if not os.environ.get('K_SKIP_PV') else 0):
                                head = 2 * hp + hh
                                for qt in range(2):
                                    ab = P[4 + 2 * qt + head // 4]; abk = 'P%d' % (4 + 2 * qt + head // 4)
                                    c0 = (head % 4) * 65
                                    S.op('pe', lambda e, ab=ab, c0=c0, pt=pt, hh=hh, qt=qt, st=st, kvh=kvh, head=head: e.matmul(
                                        ab[:, c0:c0 + 65], lhsT=pt[:, hh * 256 + qt * 128:hh * 256 + (qt + 1) * 128],
                                        rhs=Vx[:, st, kvh, 0:65], start=(st == 0 and head % 4 == 0), stop=(st == NT - 1 and head % 4 == 3)),
                                        reads=[ptk, 'Vx'], writes=[abk])
                    for qt in range(2 if not os.environ.get('K_SKIP_NORM') else 0):
                        for half in range(2):
                            ab = P[4 + 2 * qt + half]; abk = 'P%d' % (4 + 2 * qt + half)
                            av = ab[:, 0:260].rearrange("p (h c) -> p h c", c=65)
                            S.op('dve', lambda e, av=av, half=half: e.reciprocal(out=rec[:, half * 4:(half + 1) * 4], in_=av[:, :, 64]),
                                 reads=[abk], writes=['rec'])
                            S.op('dve', lambda e, av=av, half=half: e.tensor_tensor(
                                out=atok[:, half * 4:(half + 1) * 4, :], in0=av[:, :, 0:64],
                                in1=rec[:, half * 4:(half + 1) * 4].unsqueeze(2).to_broadcast([128, 4, 64]), op=ALU.mult),
                                reads=[abk, 'rec'], writes=['atok'])
                        pa = P[0][:].bitcast(BF16)
                        if os.environ.get('K_SKIP_TR'):
                            continue
                        for hp in range(4):
                            S.op('pe', lambda e, hp=hp, pa=pa: e.transpose(
                                out=pa[:, hp * 128:(hp + 1) * 128],
                                in_=atok[:, 2 * hp:2 * hp + 2, :].rearrange("p a d -> p (a d)"), identity=identb[:]),
                                reads=['atok', 'identb'], writes=['P0'])
                        if os.environ.get('K_SKIP_CP'):
                            continue
                        S.op('dve', lambda e, pa=pa, q0=q0, qt=qt: e.tensor_copy(
                            out=attT_all[:, :, q0 + qt * 128:q0 + (qt + 1) * 128],
                            in_=pa[:, 0:512].rearrange("p (j t) -> p j t", j=4)), reads=['P0'], writes=['attT_all'])
                if dbg:
                    o1 = dout("dbg_attT", [128, 4 * TL], BF16)
                    S.dma(lambda e: e.dma_start(out=o1[:, :], in_=attT_all[:].rearrange("p a t -> p (a t)")), reads=['attT_all'])
                S.barrier()
                S.flush()
        if stop_after <= 2:
            return nc, dbg_outs

        def TTo(eng, out, a, b_, op, R, W):
            S.op(eng, lambda e: e.tensor_tensor(out=out, in0=a, in1=b_, op=op), reads=R, writes=W)

        def CP(eng, out, in_, R, W):
            if eng == 'act':
                S.op('act', lambda e: e.copy(out=out, in_=in_), reads=R, writes=W)
            else:
                S.op(eng, lambda e: e.tensor_copy(out=out, in_=in_), reads=R, writes=W)

        def ACTF(out, in_, func, R, W, scale=1.0, bias=None):
            if bias is None:
                S.op('act', lambda e: e.activation(out=out, in_=in_, func=func, scale=scale), reads=R, writes=W)
            else:
                S.op('act', lambda e: e.activation(out=out, in_=in_, func=func, scale=scale, bias=bias), reads=R, writes=W)

        def MM(out, lhsT, rhs, R, W, start=True, stop=True):
            S.op('pe', lambda e: e.matmul(out, lhsT=lhsT, rhs=rhs, start=start, stop=stop), reads=R, writes=W)

        def TR(out, in_, idn, R, W):
            S.op('pe', lambda e: e.transpose(out=out, in_=in_, identity=idn), reads=R, writes=W)

        with contextlib.ExitStack() as ph:
            def t32(name, shape=(64, 1024)):
                return sb(ph, name, list(shape), F32)
            rp = t32("rp", (64, 8, 10)); rpd = t32("rpd", (64, 8, 8))
            lmu = t32("lmu", (128, 3)); lmd = t32("lmd", (128, 6))
            decupb = sb(ph, "decupb", [64, 2, 512], BF16); iclupb = sb(ph, "iclupb", [64, 2, 512], BF16)
            gateupb = sb(ph, "gateupb", [128, 512], BF16)
            mk4 = t32("mk4", (128, 2, 512)); mn1 = t32("mn1", (128, 2, 128)); bm = t32("bm", (128, 512))
            rst = t32("rst"); ones64 = sb(ph, "ones64", [64, 64], BF16); tiny = t32("tiny", (64, 1))
            S.dma(lambda e: e.dma_start(out=rp[:].rearrange("k h n -> k (h n)"), in_=rp_d[:, :]), writes=['rp'])
            S.dma(lambda e: e.dma_start(out=lmu[:], in_=lmu_d[:, :]), writes=['lmu'])
            S.dma(lambda e: e.dma_start(out=mk4[:].rearrange("p a n -> p (a n)"), in_=mk4_d[:, :]), writes=['mk4'])
            S.dma(lambda e: e.dma_start(out=mn1[:].rearrange("p a n -> p (a n)"), in_=mn1_d[:, :]), writes=['mn1'])
            S.dma(lambda e: e.dma_start(out=bm[:], in_=bm_d[:, :]), writes=['bm'])
            S.dma(lambda e: e.dma_start(out=rst[:], in_=rst_d[:, :]), writes=['rst'])
            S.op('pool', lambda e: e.memset(ones64[:], 1.0), writes=['ones64'])
            S.op('pool', lambda e: e.memset(tiny[:], 1e-24), writes=['tiny'])
            for i in range(3):
                S.op('dve', lambda e, i=i: e.tensor_scalar(out=rpd[:, :, 2 * i], in0=rp[:, :, i], scalar1=0.5, scalar2=None, op0=ALU.mult),
                     reads=['rp'], writes=['rpd'])
                S.op('dve', lambda e, i=i: e.tensor_scalar(out=rpd[:, :, 2 * i + 1], in0=rp[:, :, i], scalar1=-1.0, scalar2=1.0,
                                                          op0=ALU.mult, op1=ALU.add), reads=['rp'], writes=['rpd'])
                S.op('dve', lambda e, i=i: e.tensor_scalar(out=lmd[:, 2 * i:2 * i + 1], in0=lmu[:, i:i + 1], scalar1=0.5, scalar2=None, op0=ALU.mult),
                     reads=['lmu'], writes=['lmd'])
                S.op('dve', lambda e, i=i: e.tensor_scalar(out=lmd[:, 2 * i + 1:2 * i + 2], in0=lmu[:, i:i + 1], scalar1=-1.0, scalar2=1.0,
                                                          op0=ALU.mult, op1=ALU.add), reads=['lmu'], writes=['lmd'])
            S.op('dve', lambda e: e.tensor_scalar(out=rpd[:, :, 6], in0=rp[:, :, 4], scalar1=-1.0, scalar2=1.0, op0=ALU.mult, op1=ALU.add),
                 reads=['rp'], writes=['rpd'])

            def bc(t2):
                return t2.unsqueeze(2).to_broadcast([64, 8, 128])

            pin = [sb(ph, "pin%d" % i, [64, 8, 130], F32) for i in range(3)]
            plo = [sb(ph, "plo%d" % i, [128, 130], F32) for i in range(3)]
            tS = t32("tS"); xr = t32("xr"); xk = t32("xk"); xv = t32("xv")
            xlo = t32("xlo", (128, 3, 128)); twl = sb(ph, "twl", [64, 128], BF16); xalb = sb(ph, "xalb", [64, 128], BF16)
            glsb = sb(ph, "glsb", [128, 128], BF16)
            sqb = sb(ph, "sqb", [64, 1024], BF16); kk = t32("kk")
            sg = t32("sg"); ad = t32("ad"); cs = t32("cs"); ex = t32("ex"); E1 = t32("E1"); E2 = t32("E2"); E3 = t32("E3")
            bb = t32("bb"); t1 = t32("t1"); kd = bb; bt32 = t32("bt32"); kt32 = t32("kt32"); rkb = sqb; kkk = t1; rs = E1; csb = bt32; bv = kt32
            bvt = t32("bvt", (128, 512)); gtok = t32("gtok", (128, 512)); glS = t32("glS", (128, 128))
            opT = {n: sb(ph, "op_" + n, [64, 1024], BF16) for n in ("a", "r", "b", "k", "bh", "kh", "v")}
            N1Ta, N1a, IN1Ta, N2a, N2Ta, IN2Ta, N4a, N4Ta, IN4Ta, IN8Ta, AakTa, ArbTa, ArkTa = [
                sb(ph, "ba%d" % i, [128, 8, 128], BF16) for i in range(13)]
            TMa = sb(ph, "TMa", [128, 8, 5, 64], BF16)
            Xa = [sb(ph, "Xa%d" % i, [128, 8, 128], BF16) for i in range(2)]
            Bbd = sb(ph, "Bbd", [128, 512], BF16); Ubd = sb(ph, "Ubd", [128, 512], BF16); Vbd = sb(ph, "Vbd", [128, 512], BF16)
            GTs = sb(ph, "GTs", [64, 512], BF16); Es = t32("Es", (64, 512)); QTs = sb(ph, "QTs", [64, 128], BF16)
            Y0s = t32("Y0s", (128, 64)); ytmp = gtok; yc = t32("yc", (128, 64))
            Yblk = t32("Yblk", (128, 8, 64))
            H = t32("H", (64, 512)); Hb = sb(ph, "Hb", [64, 512], BF16); Ht = t32("Ht", (64, 512))

            def view_hct(t):
                return t[:, :]

            def view_out(t):
                return t[:, :]

            def g16(t):
                return t[:, :].rearrange("k (g t) -> k g t", t=16)

            def chv(t, c):
                return t[:, :].rearrange("k (h c t) -> k h c t", h=8, c=8)[:, :, c, :]

            for (dst, src, nm, rows) in ((decupb, decup_d, 'decupb', 64), (iclupb, iclup_d, 'iclupb', 64), (gateupb, gateup_d, 'gateupb', 128)):
                stg_, sk_ = (bt32, 'bt32') if rows == 64 else (bvt, 'bvt')
                S.dma(lambda e, src=src, stg_=stg_: e.dma_start(out=stg_[:, :], in_=src[:, :]), writes=[sk_])
                dv = dst[:].rearrange("p a n -> p (a n)") if rows == 64 else dst[:]
                CP('dve', dv, stg_[:, :], [sk_], [nm])
            for d in range(2):
                S.op('pool', lambda e: e.memset(H[:], 0.0), writes=['H'])
                S.op('pool', lambda e: e.memset(Hb[:], 0.0), writes=['Hb'])
                cblocks = [0, 1] if d == 0 else [1, 0]
                lblocks = list(range(2, NT)) if d == 0 else list(range(NT - 1, 1, -1))
                nblk = int(os.environ.get('K_RBLK', 99))
                for bi, blk in enumerate((cblocks + lblocks)[:nblk]):
                    u0 = blk * 128
                    is_ctx = blk < 2
                    seq_lo, seq_hi = (0, TC) if is_ctx else (TC, TT)
                    lo = max(u0 - 1, seq_lo); hi = min(u0 + 129, seq_hi)
                    c_lo = lo - (u0 - 1); c_hi = c_lo + (hi - lo)
                    for i in range(3):
                        if c_lo > 0:
                            S.op('pool', lambda e, i=i: e.memset(pin[i][:, :, 0:1], 0.0), writes=['pin%d' % i])
                        if c_hi < 130:
                            S.op('pool', lambda e, i=i: e.memset(pin[i][:, :, 129:130], 0.0), writes=['pin%d' % i])
                        S.dma(lambda e, i=i, lo=lo, hi=hi, c_lo=c_lo, c_hi=c_hi: e.dma_start(
                            out=pin[i][:, :, c_lo:c_hi],
                            in_=rwT[i * 512:(i + 1) * 512, lo:hi].rearrange("(h k) t -> k h t", k=64)), reads=['rwT'], writes=['pin%d' % i])
                    for i, (r0, nr) in enumerate(((1536, 64), (1600, 64), (1664, 128))):
                        if c_lo > 0:
                            S.op('pool', lambda e, i=i: e.memset(plo[i][:, 0:1], 0.0), writes=['plo%d' % i])
                        if c_hi < 130:
                            S.op('pool', lambda e, i=i: e.memset(plo[i][:, 129:130], 0.0), writes=['plo%d' % i])
                        S.dma(lambda e, i=i, r0=r0, nr=nr, lo=lo, hi=hi, c_lo=c_lo, c_hi=c_hi: e.dma_start(
                            out=plo[i][0:nr, c_lo:c_hi], in_=rwT[r0:r0 + nr, lo:hi]), reads=['rwT'], writes=['plo%d' % i])
                    for i, xo in enumerate((xr, xk, xv)):
                        xo3 = xo[:, :].rearrange("k (h t) -> k h t", h=8)
                        ts3 = tS[:, :].rearrange("k (h t) -> k h t", h=8)
                        TTo('pool', ts3, pin[i][:, :, 0:128], pin[i][:, :, 2:130], ALU.add, ['pin%d' % i], ['tS'])
                        TTo('pool', ts3, ts3, bc(rpd[:, :, 2 * i]), ALU.mult, ['tS', 'rpd'], ['tS'])
                        TTo('dve', xo3, pin[i][:, :, 1:129], bc(rpd[:, :, 2 * i + 1]), ALU.mult, ['pin%d' % i, 'rpd'], ['x%d' % i])
                        TTo('dve', xo3, xo3, ts3, ALU.add, ['x%d' % i, 'tS'], ['x%d' % i])
                    for i, nr in enumerate((64, 64, 128)):
                        S.op('pool', lambda e, i=i, nr=nr: e.tensor_tensor(out=tS[0:nr, 0:128] if nr == 64 else glS[:, 0:128],
                                                                         in0=plo[i][0:nr, 0:128], in1=plo[i][0:nr, 2:130], op=ALU.add),
                             reads=['plo%d' % i], writes=['tS' if nr == 64 else 'glS'])
                        S.op('dve', lambda e, i=i, nr=nr: e.tensor_scalar(out=xlo[0:nr, i, :], in0=plo[i][0:nr, 1:129],
                                                                        scalar1=lmd[0:nr, 2 * i + 1:2 * i + 2], scalar2=None, op0=ALU.mult),
                             reads=['plo%d' % i, 'lmd'], writes=['xlo'])
                        S.op('dve', lambda e, i=i, nr=nr: e.scalar_tensor_tensor(
                            out=xlo[0:nr, i, :], in0=(tS[0:nr, 0:128] if nr == 64 else glS[:, 0:128]), scalar=lmd[0:nr, 2 * i:2 * i + 1],
                            in1=xlo[0:nr, i, :], op0=ALU.mult, op1=ALU.add),
                            reads=['tS' if nr == 64 else 'glS', 'lmd', 'xlo'], writes=['xlo'])
                    ACTF(twl[:], xlo[0:64, 0, :], AF.Tanh, ['xlo'], ['twl'])
                    CP('pool', xalb[:], xlo[0:64, 1, :], ['xlo'], ['xalb'])
                    k3 = lambda t: t[:, :].rearrange("k (h t) -> k h t", h=8)
                    TTo('pool', k3(kkk), k3(xk), bc(rp[:, :, 3]), ALU.mult, ['x1', 'rp'], ['t1'])
                    ACTF(sqb[:], kkk[:], AF.Square, ['t1'], ['sqb'])
                    PP = PS[0:64, 0:1024]
                    for hf in range(2):
                        MM(PS[0:64, hf * 512:(hf + 1) * 512], ones64[:], sqb[:, hf * 512:(hf + 1) * 512], ['ones64', 'sqb'], ['P0'])
                    ACTF(rs[:], PP, AF.Ln, ['P0', 'tiny'], ['E1'], bias=tiny[:])
                    ACTF(rs[:], rs[:], AF.Exp, ['E1'], ['E1'], scale=-0.5)
                    TTo('dve', kk[:], kkk[:], rs[:], ALU.mult, ['t1', 'E1'], ['kk'])
                    for h in range(8):
                        MM(PS[0:64, h * 128:(h + 1) * 128], decupb[:, d, h * 64:(h + 1) * 64], twl[:], ['decupb', 'twl'], ['P0'])
                    TTo('dve', k3(sg), PP.rearrange("k (h t) -> k h t", h=8), bc(rp[:, :, 6 + d]), ALU.add, ['P0', 'rp'], ['sg'])
                    ACTF(sg[:], sg[:], AF.Sigmoid, ['sg'], ['sg'])
                    for h in range(8):
                        MM(PS[0:64, 1024 + h * 128:1024 + (h + 1) * 128], iclupb[:, d, h * 64:(h + 1) * 64], xalb[:], ['iclupb', 'xalb'], ['P2', 'P3'])
                    TTo('dve', k3(ad), PS[0:64, 1024:2048].rearrange("k (h t) -> k h t", h=8), bc(rp[:, :, 8 + d]), ALU.add, ['P2', 'P3', 'rp'], ['ad'])
                    ACTF(ad[:], ad[:], AF.Sigmoid, ['ad'], ['ad'])
                    S.op('dve', lambda e: e.tensor_tensor_scan(out=cs[:], data0=rst[:], data1=sg[:], initial=0.0, op0=ALU.mult, op1=ALU.add),
                         reads=['rst', 'sg'], writes=['cs'])
                    csf = cs
                    if d == 1:
                        TTo('pool', ex[:], sg[:], cs[:], ALU.subtract, ['sg', 'cs'], ['ex'])
                        csv = cs[:, :].rearrange("k (g t) -> k g t", t=16)
                        TTo('dve', csb[:, :].rearrange("k (g t) -> k g t", t=16), ex[:, :].rearrange("k (g t) -> k g t", t=16),
                           csv[:, :, 15:16].to_broadcast([64, 64, 16]), ALU.add, ['ex', 'cs'], ['bt32'])
                        csf = csb
                    ACTF(E2[:], csf[:], AF.Exp, ['cs', 'bt32'], ['E2'], scale=DEC_C)
                    TTo('pool', ex[:], csf[:], sg[:], ALU.subtract, ['cs', 'bt32', 'sg'], ['ex'])
                    ACTF(E1[:], ex[:], AF.Exp, ['ex'], ['E1'], scale=DEC_C)
                    S.op('dve', lambda e: e.reciprocal(out=E3[:], in_=E2[:]), reads=['E2'], writes=['E3'])
                    tsel = 15 if d == 0 else 0
                    def cm(t, h):
                        return t[:, :].rearrange("k (c h t) -> k c h t", c=8, h=8)[:, :, h, :]

                    def hm(t, h):
                        return t[:, :].rearrange("k (h c t) -> k h c t", h=8, c=8)[:, h, :, :]
                    TTo('pool', bb[:], kk[:], ad[:], ALU.mult, ['kk', 'ad'], ['bb'])
                    TTo('dve', bt32[:], bb[:], E3[:], ALU.mult, ['bb', 'E3'], ['bt32'])
                    TTo('pool', k3(t1), k3(ad), bc(rp[:, :, 4]), ALU.mult, ['ad', 'rp'], ['t1'])
                    TTo('dve', k3(t1), k3(t1), bc(rpd[:, :, 6]), ALU.add, ['t1', 'rpd'], ['t1'])
                    TTo('pool', kd[:], xk[:], t1[:], ALU.mult, ['x1', 't1'], ['bb'])
                    TTo('dve', kt32[:], kd[:], E3[:], ALU.mult, ['bb', 'E3'], ['kt32'])
                    for h in range(8):
                        pch = hm(E2, h)[:, :, tsel:tsel + 1].to_broadcast([64, 8, 16])
                        S.op('dve', lambda e, h=h: e.scalar_tensor_tensor(out=cm(opT['a'], h), in0=hm(kk, h), scalar=-1.0, in1=hm(E1, h),
                                                                         op0=ALU.mult, op1=ALU.mult), reads=['kk', 'E1'], writes=['op_a'])
                        TTo('pool', cm(opT['r'], h), hm(xr, h), hm(E2, h), ALU.mult, ['x0', 'E2'], ['op_r'])
                        CP('act', cm(opT['b'], h), hm(bt32, h), ['bt32'], ['op_b'])
                        TTo('pool', cm(opT['bh'], h), hm(bt32, h), pch, ALU.mult, ['bt32', 'E2'], ['op_bh'])
                        CP('act', cm(opT['k'], h), hm(kt32, h), ['kt32'], ['op_k'])
                        TTo('dve', cm(opT['kh'], h), hm(kt32, h), pch, ALU.mult, ['kt32', 'E2'], ['op_kh'])
                        CP('act', cm(opT['v'], h), hm(xv, h), ['x2'], ['op_v'])
                    if d == 0 and not is_ctx:
                        ul = u0 - TC
                        TTo('pool', k3(t1), k3(xr), bc(rp[:, :, 5]), ALU.mult, ['x0', 'rp'], ['t1'])
                        TTo('dve', rkb[:], t1[:], xk[:], ALU.mult, ['t1', 'x1'], ['sqb'])
                        for hf in range(2):
                            MM(PS[0:64, hf * 512:(hf + 1) * 512], ones64[:], rkb[:, hf * 512:(hf + 1) * 512], ['ones64', 'sqb'], ['P0'])
                        TTo('dve', bv[:], PP, xv[:], ALU.mult, ['P0', 'x2'], ['kt32'])
                        for h in range(8):
                            TR(PS[:, 1536 + h * 64:1536 + (h + 1) * 64], bv[:, h * 128:(h + 1) * 128], identf[0:64, 0:64], ['kt32', 'identf'], ['P3'])
                        CP('dve', bvt[:], PS[:, 1536:2048], ['P3'], ['bvt'])
                        S.dma(lambda e, ul=ul: e.dma_start(out=bonD[ul:ul + 128, :], in_=bvt[:]), reads=['bvt'], writes=['bonD'])
                        ACTF(glsb[:], xlo[:, 2, :], AF.Sigmoid, ['xlo'], ['glsb'])
                        MM(PS[:, 1536:2048], glsb[:], gateupb[:], ['glsb', 'gateupb'], ['P3'])
                        CP('dve', gtok[:], PS[:, 1536:2048], ['P3'], ['gtok'])
                        S.dma(lambda e, ul=ul: e.dma_start(out=gD[ul:ul + 128, :], in_=gtok[:]), reads=['gtok'], writes=['gD'])
                    PA_, PB_, PC_, PD_ = (PS[:, 0:1024], PS[:, 1024:2048], PS[:, 2048:3072], PS[:, 3072:4096])
                    kPB, kPC, kPD = ['P2', 'P3'], ['P4', 'P5'], ['P6', 'P7']
                    c8 = lambda ap: ap.rearrange("p (c n) -> p c n", c=8)
                    def bc8(m):
                        return m.unsqueeze(1).to_broadcast([128, 8, 128])
                    MSd = mk4[:, d, 0:128]; MId = mk4[:, d, 128:256]; MStd = mn1[:, d, :]
                    def ch(name, c):
                        return opT[name][:, c * 128:(c + 1) * 128]
                    for c in range(8):
                        MM(PB_[:, c * 128:(c + 1) * 128], ch('b', c), ch('a', c), ['op_b', 'op_a'], kPB)
                    for c in range(8):
                        MM(PC_[:, c * 128:(c + 1) * 128], ch('a', c), ch('b', c), ['op_a', 'op_b'], kPC)
                    for c in range(8):
                        MM(PD_[:, c * 128:(c + 1) * 128], ch('k', c), ch('a', c), ['op_k', 'op_a'], kPD)
                    TTo('dve', N1Ta[:], c8(PB_), bc8(MSd), ALU.mult, kPB + ['mk4'], ['N1Ta'])
                    TTo('dve', N1a[:], c8(PC_), bc8(MStd), ALU.mult, kPC + ['mn1'], ['N1a'])
                    TTo('dve', AakTa[:], c8(PD_), bc8(MSd), ALU.mult, kPD + ['mk4'], ['AakTa'])
                    TTo('pool', IN1Ta[:], N1Ta[:], bc8(identb[:, :]), ALU.add, ['N1Ta', 'identb'], ['IN1Ta'])
                    PBb = PB_.bitcast(BF16)
                    for c in range(8):
                        for si, nm_ in ((0, 'a'), (1, 'v'), (2, 'bh'), (3, 'kh')):
                            TR(PBb[:, c * 256 + si * 64:c * 256 + (si + 1) * 64], ch(nm_, c), identb[0:64, 0:64], ['op_' + nm_, 'identb'], kPB)
                    PBb4 = PBb.rearrange("p (c s n) -> p c s n", c=8, s=4)
                    CP('dve', TMa[:, :, 0, :], PBb4[:, :, 0, :], kPB, ['TMa'])
                    CP('dve', TMa[:, :, 2:5, :].rearrange("p c s n -> p c (s n)"), PBb.rearrange("p (c n) -> p c n", c=8)[:, :, 64:256], kPB, ['TMa'])
                    for c in range(8):
                        MM(PC_[:, c * 128:(c + 1) * 128], N1Ta[:, c, :], N1a[:, c, :], ['N1Ta', 'N1a'], kPC)
                    for c in range(8):
                        MM(PD_[:, c * 128:(c + 1) * 128], N1a[:, c, :], N1Ta[:, c, :], ['N1Ta', 'N1a'], kPD)
                    CP('act', N2a[:], c8(PC_), kPC, ['N2a'])
                    CP('dve', N2Ta[:], c8(PD_), kPD, ['N2Ta'])
                    TTo('pool', IN2Ta[:], N2Ta[:], bc8(identb[:, :]), ALU.add, ['N2Ta', 'identb'], ['IN2Ta'])
                    for c in range(8):
                        MM(PB_[:, c * 64:(c + 1) * 64], AakTa[:, c, :], TMa[:, c, 2, :], ['AakTa', 'TMa'], kPB)
                    CP('dve', TMa[:, :, 1, :], PB_[:, 0:512].rearrange("p (c n) -> p c n", c=8), kPB, ['TMa'])
                    for c in range(8):
                        MM(PC_[:, c * 128:(c + 1) * 128], N2Ta[:, c, :], N2a[:, c, :], ['N2Ta', 'N2a'], kPC)
                    for c in range(8):
                        MM(PD_[:, c * 128:(c + 1) * 128], N2a[:, c, :], N2Ta[:, c, :], ['N2Ta', 'N2a'], kPD)
                    CP('act', N4a[:], c8(PC_), kPC, ['N4a'])
                    CP('dve', N4Ta[:], c8(PD_), kPD, ['N4Ta'])
                    TTo('pool', IN4Ta[:], N4Ta[:], bc8(identb[:, :]), ALU.add, ['N4Ta', 'identb'], ['IN4Ta'])
                    for c in range(8):
                        MM(PB_[:, c * 128:(c + 1) * 128], N4a[:, c, :], N4Ta[:, c, :], ['N4a', 'N4Ta'], kPB)
                    TTo('dve', IN8Ta[:], c8(PB_), bc8(identf[:, :]), ALU.add, kPB + ['identf'], ['IN8Ta'])
                    if not is_ctx:
                        for c in range(8):
                            MM(PC_[:, c * 128:(c + 1) * 128], ch('b', c), ch('r', c), ['op_b', 'op_r'], kPC)
                        for c in range(8):
                            MM(PD_[:, c * 128:(c + 1) * 128], ch('k', c), ch('r', c), ['op_k', 'op_r'], kPD)
                        TTo('dve', ArbTa[:], c8(PC_), bc8(MId), ALU.mult, kPC + ['mk4'], ['ArbTa'])
                        TTo('dve', ArkTa[:], c8(PD_), bc8(MId), ALU.mult, kPD + ['mk4'], ['ArkTa'])
                    xsrc = None
                    for li, (INa, ik) in enumerate(((IN8Ta, 'IN8Ta'), (IN4Ta, 'IN4Ta'), (IN2Ta, 'IN2Ta'), (IN1Ta, 'IN1Ta'))):
                        Pq, kq = (PB_, kPB) if li % 2 == 0 else (PC_, kPC)
                        for c in range(8):
                            rhs_ = TMa[:, c, 0:2, :].rearrange("p a n -> p (a n)") if xsrc is None else xsrc[:, c, :]
                            MM(Pq[:, c * 128:(c + 1) * 128], INa[:, c, :], rhs_, [ik, 'TMa' if xsrc is None else xk_], kq)
                        Xn = Xa[li % 2]; xk_ = 'Xa%d' % (li % 2)
                        CP('dve' if li % 2 else 'act', Xn[:], c8(Pq), kq, [xk_])
                        xsrc = Xn
                    B3 = PS[:, 1536:2048]; B4 = PS[:, 2048:2560]; B5 = PS[:, 2560:3072]; B6 = PS[:, 3072:3584]; B7 = PS[:, 3584:4096]
                    bm3 = bm[:, :].rearrange("p (h n) -> p h n", h=8)
                    nch = int(os.environ.get('K_RCH', 8))
                    for c in (list(range(8)) if d == 0 else list(range(7, -1, -1)))[:nch]:
                        Wc = xsrc[:, c, 0:64]; U0 = xsrc[:, c, 64:128]
                        Bh_ = TMa[:, c, 3, :]; Kh_ = TMa[:, c, 4, :]; Vt_ = TMa[:, c, 2, :]
                        for dst_, src_, rk_, wk_ in ((Bbd, Bh_, 'TMa', 'Bbd'), (Ubd, U0, xk_, 'Ubd'), (Vbd, Vt_, 'TMa', 'Vbd')):
                            TTo('pool', dst_[:, :].rearrange("p (h n) -> p h n", h=8), src_.unsqueeze(1).to_broadcast([128, 8, 64]), bm3, ALU.mult,
                                [rk_, 'bm'], [wk_])
                        MM(B4[0:64, :], Wc, Bbd[:], [xk_, 'Bbd'], ['P4'])
                        CP('act', GTs[:], B4[0:64, :], ['P4'], ['GTs'])
                        MM(B5[0:64, :], Bh_, Ubd[:], ['TMa', 'Ubd'], ['P5'], start=True, stop=False)
                        MM(B5[0:64, :], Kh_, Vbd[:], ['TMa', 'Vbd'], ['P5'], start=False, stop=True)
                        CP('act', Es[:], B5[0:64, :], ['P5'], ['Es'])
                        if not is_ctx:
                            MM(B6[0:64, 0:128], Wc, ArbTa[:, c, :], [xk_, 'ArbTa'], ['P6'])
                            TTo('dve', QTs[:], B6[0:64, 0:128], ch('r', c), ALU.add, ['P6', 'op_r'], ['QTs'])
                            MM(B6[:, 128:192], ArbTa[:, c, :], U0, ['ArbTa', xk_], ['P6'], start=True, stop=False)
                            MM(B6[:, 128:192], ArkTa[:, c, :], Vt_, ['ArkTa', 'TMa'], ['P6'], start=False, stop=True)
                            CP('dve', Y0s[:], B6[:, 128:192], ['P6'], ['Y0s'])
                            MM(B7, QTs[:], Hb[:], ['QTs', 'Hb'], ['P7'])
                            TTo('dve', ytmp[:], B7, bm[:], ALU.mult, ['P7', 'bm'], ['gtok'])
                            S.op('dve', lambda e: e.tensor_reduce(out=yc[:], in_=ytmp[:, :].rearrange("p (h v) -> p v h", h=8), axis=AX.X, op=ALU.add),
                                 reads=['gtok'], writes=['yc'])
                            TTo('pool', Yblk[:, c, :], yc[:], Y0s[:], ALU.add, ['yc', 'Y0s'], ['Yblk'])
                        for h in range(8):
                            MM(B3[0:64, h * 64:(h + 1) * 64], GTs[:, h * 64:(h + 1) * 64], Hb[:, h * 64:(h + 1) * 64], ['GTs', 'Hb'], ['P3'])
                        PCc = chv(E2, c)[:, :, tsel:tsel + 1].to_broadcast([64, 8, 64])
                        TTo('pool', Ht[:, :].rearrange("k (h v) -> k h v", h=8), H[:, :].rearrange("k (h v) -> k h v", h=8), PCc, ALU.mult,
                            ['H', 'E2'], ['Ht'])
                        TTo('pool', Ht[:], Ht[:], Es[:], ALU.add, ['Ht', 'Es'], ['Ht'])
                        TTo('dve', H[:], Ht[:], B3[0:64, :], ALU.add, ['Ht', 'P3'], ['H'])
                        CP('act', Hb[:], H[:], ['H'], ['Hb'])
                    if not is_ctx:
                        ul = u0 - TC
                        for h in range(8):
                            S.dma(lambda e, h=h, ul=ul, d=d: e.dma_start(
                                out=ydir[d, ul:ul + 128, h * 64:(h + 1) * 64].rearrange("(c t) v -> t c v", t=16),
                                in_=Yblk[h * 16:(h + 1) * 16, :, :]), reads=['Yblk'], writes=['ydir'])
            if dbg:
                oy = dout("dbg_y", [2 * TL, 512]); ob = dout("dbg_bon", [TL, 512]); og = dout("dbg_g", [TL, 512])
                S.dma(lambda e: e.dma_start(out=oy[:, :], in_=ydir.rearrange("d t n -> (d t) n")), reads=['ydir'])
                S.dma(lambda e: e.dma_start(out=ob[:, :], in_=bonD[:, :]), reads=['bonD'])
                S.dma(lambda e: e.dma_start(out=og[:, :], in_=gD[:, :]), reads=['gD'])
                oH = dout("dbg_H", [64, 512])
                S.dma(lambda e: e.dma_start(out=oH[:, :], in_=H[:]), reads=['H'])
            S.barrier()
            S.flush()
        if stop_after <= 3:
            return nc, dbg_outs

        with contextlib.ExitStack() as ph:
            woutb = sb(ph, "woutb", [128, 8, D], BF16)
            cst = {}
            for nm, src in (("gt1", modd[0:1, 2048:3072]), ("sh2", modd[0:1, 3072:4096]), ("sc2p", modd[0:1, 4096:5120]),
                            ("ln1g", ln1_d[0:1, :]), ("ln1b", ln1_d[1:2, :])):
                cst[nm] = sb(ph, nm, [128, D], F32)
                S.dma(lambda e, nm=nm, src=src: e.dma_start(out=cst[nm][:], in_=src.partition_broadcast(128)), reads=['modd'], writes=[nm])
            for nm, row in (("lnxg", 0), ("lnxb", 1)):
                cst[nm] = sb(ph, nm, [128, 512], F32)
                S.dma(lambda e, nm=nm, row=row: e.dma_start(out=cst[nm][:], in_=lnx_d[row:row + 1, :].partition_broadcast(128)), writes=[nm])
            wst2 = [sb(ph, "wst2_%d" % i, [128, D], F32) for i in range(2)]
            for j in range(8):
                w = wst2[j % 2]; wk = 'wst2_%d' % (j % 2)
                S.dma(lambda e, w=w, j=j: e.dma_start(out=w[:], in_=wout_d[j * 128:(j + 1) * 128, :]), writes=[wk])
                CP('pool' if j % 2 else 'dve', woutb[:, j, :], w[:], [wk], ['woutb'])
            rwf = sb(ph, "rwf", [128, 8, 16], F32)
            S.dma(lambda e: e.dma_start(out=rwf[:], in_=rw_d.rearrange("(j p) n -> p j n", p=128)), writes=['rwf'])
            gneps = sb(ph, "gneps", [128, 1], F32)
            S.op('pool', lambda e: e.memset(gneps[:], 64e-5), writes=['gneps'])
            yf = sb(ph, "yf", [128, 512], F32); yb = sb(ph, "yb", [128, 512], F32)
            bon = sb(ph, "bon", [128, 512], F32); gg = sb(ph, "gg", [128, 512], F32)
            ysum = sb(ph, "ysum", [128, 512], F32); ysq = sb(ph, "ysq", [128, 512], F32)
            gst = sb(ph, "gst", [128, 8], F32); gvar = sb(ph, "gvar", [128, 8], F32)
            rwob = sb(ph, "rwob", [128, 512], BF16); rwoT = sb(ph, "rwoT", [128, 4, 128], BF16)
            xin_t = sb(ph, "xin_t", [128, D], F32); tres = sb(ph, "tres", [128, D], F32)
            x1t = sb(ph, "x1t", [128, D], F32); h2f = sb(ph, "h2f", [128, D], F32); h2b = sb(ph, "h2b", [128, D], BF16)
            h2T = sb(ph, "h2T", [128, 8, 128], F32)
            stats = sb(ph, "statsC", [128, 2, 6], F32); mv = sb(ph, "mvC", [128, 2], F32)
            rstd = sb(ph, "rstdC", [128, 1], F32); nb = sb(ph, "nbC", [128, 1], F32)
            lntC = (stats, mv, rstd, nb, 'C')
            lmax = sb(ph, "lmax", [128, 1], F32); lex = sb(ph, "lex", [128, 16], F32); lsum = sb(ph, "lsum", [128, 1], F32)

            def v8(t):
                return t[:, :].rearrange("p (h v) -> p h v", h=8)

            def b8(t):
                return t[:, :].unsqueeze(2).to_broadcast([128, 8, 64])
            for i in range(int(os.environ.get('K_CT', 32))):
                t0 = i * 128
                S.dma(lambda e, t0=t0: e.dma_start(out=yf[:], in_=ydir[0, t0:t0 + 128, :]), reads=['ydir'], writes=['yf'])
                S.dma(lambda e, t0=t0: e.dma_start(out=yb[:], in_=ydir[1, t0:t0 + 128, :]), reads=['ydir'], writes=['yb'])
                S.dma(lambda e, t0=t0: e.dma_start(out=bon[:], in_=bonD[t0:t0 + 128, :]), reads=['bonD'], writes=['bon'])
                S.dma(lambda e, t0=t0: e.dma_start(out=gg[:], in_=gD[t0:t0 + 128, :]), reads=['gD'], writes=['gg'])
                S.dma(lambda e, t0=t0: e.dma_start(out=xin_t[:], in_=x_d[t0:t0 + 128, :]), writes=['xin_t'])
                TTo('pool', ysum[:], yf[:], yb[:], ALU.add, ['yf', 'yb'], ['ysum'])
                S.op('dve', lambda e: e.tensor_reduce(out=gst[:], in_=v8(ysum), axis=AX.X, op=ALU.add), reads=['ysum'], writes=['gst'])
                S.op('dve', lambda e: e.tensor_scalar(out=gst[:], in0=gst[:], scalar1=-1.0 / 64, scalar2=None, op0=ALU.mult),
                     reads=['gst'], writes=['gst'])
                TTo('dve', v8(ysum), v8(ysum), b8(gst), ALU.add, ['ysum', 'gst'], ['ysum'])
                ACTF(ysq[:], ysum[:], AF.Square, ['ysum'], ['ysq'])
                S.op('dve', lambda e: e.tensor_reduce(out=gvar[:], in_=v8(ysq), axis=AX.X, op=ALU.add), reads=['ysq'], writes=['gvar'])
                ACTF(gvar[:], gvar[:], AF.Ln, ['gvar', 'gneps'], ['gvar'], scale=1.0 / 64, bias=gneps[:])
                ACTF(gvar[:], gvar[:], AF.Exp, ['gvar'], ['gvar'], scale=-0.5)
                TTo('dve', v8(ysum), v8(ysum), b8(gvar), ALU.mult, ['ysum', 'gvar'], ['ysum'])
                TTo('pool', ysum[:], ysum[:], cst['lnxg'][:], ALU.mult, ['ysum', 'lnxg'], ['ysum'])
                TTo('dve', ysum[:], ysum[:], cst['lnxb'][:], ALU.add, ['ysum', 'lnxb'], ['ysum'])
                TTo('pool', ysum[:], ysum[:], bon[:], ALU.add, ['ysum', 'bon'], ['ysum'])
                TTo('dve', rwob[:], ysum[:], gg[:], ALU.mult, ['ysum', 'gg'], ['rwob'])
                pr_ = P[0][:].bitcast(BF16)
                for j in range(4):
                    TR(pr_[:, j * 128:(j + 1) * 128], rwob[:, j * 128:(j + 1) * 128], identb[:], ['rwob', 'identb'], ['P0'])
                CP('dve', rwoT[:], pr_[:, 0:512].rearrange("p (j t) -> p j t", j=4), ['P0'], ['rwoT'])
                for half in range(2):
                    ob = PS[:, 1024 + half * 512:1024 + (half + 1) * 512]
                    for j in range(8):
                        lt = attT_all[:, j, t0:t0 + 128] if j < 4 else rwoT[:, j - 4, :]
                        MM(ob, lt, woutb[:, j, half * 512:(half + 1) * 512], ['attT_all', 'rwoT', 'woutb'], ['P2'], start=(j == 0), stop=(j == 7))
                TTo('dve', tres[:], PS[:, 1024:2048], cst['gt1'][:], ALU.mult, ['P2', 'gt1'], ['tres'])
                S.op('dve', lambda e: e.scalar_tensor_tensor(out=tres[:], in0=xin_t[:], scalar=ALPHA, in1=tres[:], op0=ALU.mult, op1=ALU.add),
                     reads=['xin_t', 'tres'], writes=['tres'])
                ln_stats(lntC, tres, 'tres', eps5, 'eps5')
                S.op('act', lambda e: e.activation(out=x1t[:], in_=tres[:], func=AF.Identity, bias=nb[:], scale=rstd[:]),
                     reads=['tres', 'Cnb', 'Crstd'], writes=['x1t'])
                TTo('pool', x1t[:], x1t[:], cst['ln1g'][:], ALU.mult, ['x1t', 'ln1g'], ['x1t'])
                TTo('dve', x1t[:], x1t[:], cst['ln1b'][:], ALU.add, ['x1t', 'ln1b'], ['x1t'])
                S.dma(lambda e, t0=t0: e.dma_start(out=x1D[t0:t0 + 128, :], in_=x1t[:]), reads=['x1t'], writes=['x1D'])
                ln_stats(lntC, x1t, 'x1t', eps5, 'eps5')
                S.op('act', lambda e: e.activation(out=h2f[:], in_=x1t[:], func=AF.Identity, bias=nb[:], scale=rstd[:]),
                     reads=['x1t', 'Cnb', 'Crstd'], writes=['h2f'])
                TTo('pool', h2f[:], h2f[:], cst['sc2p'][:], ALU.mult, ['h2f', 'sc2p'], ['h2f'])
                TTo('dve', h2f[:], h2f[:], cst['sh2'][:], ALU.add, ['h2f', 'sh2'], ['h2f'])
                CP('pool', h2b[:], h2f[:], ['h2f'], ['h2b'])
                S.dma(lambda e, t0=t0: e.dma_start(out=h2D[t0:t0 + 128, :], in_=h2b[:]), reads=['h2b'], writes=['h2D'])
                for j in range(8):
                    TR(PS[:, 2048 + j * 128:2048 + (j + 1) * 128], h2f[:, j * 128:(j + 1) * 128], identf[:], ['h2f', 'identf'], ['P4'])
                CP('dve', h2T[:].rearrange("p j t -> p (j t)"), PS[:, 2048:3072], ['P4'], ['h2T'])
                for j in range(8):
                    MM(PS[:, 3072:3088], h2T[:, j, :], rwf[:, j, :], ['h2T', 'rwf'], ['P6'], start=(j == 0), stop=(j == 7))
                S.op('dve', lambda e: e.tensor_reduce(out=lmax[:], in_=PS[:, 3072:3088], axis=AX.X, op=ALU.max), reads=['P6'], writes=['lmax'])
                S.op('dve', lambda e: e.tensor_scalar(out=lmax[:], in0=lmax[:], scalar1=-1.0, scalar2=None, op0=ALU.mult), reads=['lmax'], writes=['lmax'])
                ACTF(lex[:], PS[:, 3072:3088], AF.Exp, ['P6', 'lmax'], ['lex'], bias=lmax[:])
                S.op('dve', lambda e: e.tensor_reduce(out=lsum[:], in_=lex[:], axis=AX.X, op=ALU.add), reads=['lex'], writes=['lsum'])
                S.op('dve', lambda e: e.reciprocal(out=lsum[:], in_=lsum[:]), reads=['lsum'], writes=['lsum'])
                S.op('dve', lambda e, i=i: e.tensor_scalar(out=aff_all[:, i, :], in0=lex[:], scalar1=lsum[:], scalar2=None, op0=ALU.mult),
                     reads=['lex', 'lsum'], writes=['aff_all'])
            if dbg:
                o1 = dout("dbg_x1", [TL, D]); o2 = dout("dbg_aff", [128, 512])
                S.dma(lambda e: e.dma_start(out=o1[:, :], in_=x1D[:, :]), reads=['x1D'])
                S.dma(lambda e: e.dma_start(out=o2[:, :], in_=aff_all[:].rearrange("p a b -> p (a b)")), reads=['aff_all'])
            S.barrier()
            S.flush()
        stAtt.close()
        if stop_after <= 4:
            return nc, dbg_outs
        posm = sb(top, "posm", [128, 32, 16], F32)
        gw = sb(top, "gw", [128, 32, 16, 2], BF16)
        onesf = sb(top, "onesf", [128, 128], F32)
        iot = sb(top, "iot", [128, 516], F32)
        S.op('pool', lambda e: e.memset(onesf[:], 1.0), writes=['onesf'])
        S.dma(lambda e: e.dma_start(out=iot[:], in_=iot_d[:, :]), writes=['iot'])

        with contextlib.ExitStack() as ph:
            lo = sb(ph, "lo", [128, 16], F32); hi = sb(ph, "hi", [128, 16], F32); mid = sb(ph, "mid", [128, 16], F32)
            cmpt = sb(ph, "cmpt", [128, 32, 16], F32); cntp = sb(ph, "cntp", [128, 16], F32); ge = sb(ph, "ge", [128, 16], F32)
            dlt = sb(ph, "dlt", [128, 16], F32)
            ustr = sb(ph, "ustr", [128, 128], F32)
            mask = sb(ph, "mask", [128, 32, 16], F32); tot = sb(ph, "tot", [128, 32, 16], F32); cum = sb(ph, "cum", [128, 32, 16], F32)
            glo = sb(ph, "glo", [128, 32, 16], F32); ghi32 = sb(ph, "ghi32", [128, 32, 16], F32)
            S.dma(lambda e: e.dma_start(out=ustr[:], in_=ustr_d[:, :]), writes=['ustr'])
            S.op('pool', lambda e: e.memset(lo[:], 0.0), writes=['lo'])
            S.op('pool', lambda e: e.memset(hi[:], 1.0), writes=['hi'])
            affv = aff_all[:, :, :]
            for it in range(30):
                TTo('dve', mid[:], lo[:], hi[:], ALU.add, ['lo', 'hi'], ['mid'])
                S.op('dve', lambda e: e.tensor_scalar(out=mid[:], in0=mid[:], scalar1=0.5, scalar2=None, op0=ALU.mult), reads=['mid'], writes=['mid'])
                TTo('dve', cmpt[:], affv, mid[:, :].unsqueeze(1).to_broadcast([128, 32, 16]), ALU.is_ge, ['aff_all', 'mid'], ['cmpt'])
                S.op('dve', lambda e: e.tensor_reduce(out=cntp[:], in_=cmpt[:].rearrange("p t e -> p e t"), axis=AX.X, op=ALU.add),
                     reads=['cmpt'], writes=['cntp'])
                MM(PS[:, 0:16], onesf[:], cntp[:], ['onesf', 'cntp'], ['P0'])
                S.op('dve', lambda e: e.tensor_scalar(out=ge[:], in0=PS[:, 0:16], scalar1=511.5, scalar2=None, op0=ALU.is_ge), reads=['P0'], writes=['ge'])
                TTo('dve', dlt[:], mid[:], lo[:], ALU.subtract, ['mid', 'lo'], ['dlt'])
                TTo('dve', dlt[:], dlt[:], ge[:], ALU.mult, ['dlt', 'ge'], ['dlt'])
                TTo('dve', lo[:], lo[:], dlt[:], ALU.add, ['lo', 'dlt'], ['lo'])
                TTo('dve', dlt[:], hi[:], mid[:], ALU.subtract, ['hi', 'mid'], ['dlt'])
                TTo('dve', dlt[:], dlt[:], ge[:], ALU.mult, ['dlt', 'ge'], ['dlt'])
                TTo('dve', hi[:], mid[:], dlt[:], ALU.add, ['mid', 'dlt'], ['hi'])
            TTo('dve', mask[:], affv, lo[:, :].unsqueeze(1).to_broadcast([128, 32, 16]), ALU.is_ge, ['aff_all', 'lo'], ['mask'])
            m2 = mask[:].rearrange("p t e -> p (t e)")
            MM(PS[:, 512:1024], ustr[:], m2, ['ustr', 'mask'], ['P1'])
            MM(PS[:, 1024:1536], onesf[:], m2, ['onesf', 'mask'], ['P2'])
            CP('dve', tot[:].rearrange("p t e -> p (t e)"), PS[:, 1024:1536], ['P2'], ['tot'])
            for e_ in range(16):
                S.op('dve', lambda e, e_=e_: e.tensor_tensor_scan(out=cum[:, :, e_], data0=onesf[:, 0:32], data1=tot[:, :, e_], initial=0.0,
                                                                 op0=ALU.mult, op1=ALU.add), reads=['tot', 'onesf'], writes=['cum'])
            TTo('dve', cum[:], cum[:], tot[:], ALU.subtract, ['cum', 'tot'], ['cum'])
            TTo('dve', cum[:].rearrange("p t e -> p (t e)"), cum[:].rearrange("p t e -> p (t e)"), PS[:, 512:1024], ALU.add, ['cum', 'P1'], ['cum'])
            S.op('dve', lambda e: e.scalar_tensor_tensor(out=posm[:], in0=cum[:], scalar=1.0, in1=mask[:], op0=ALU.add, op1=ALU.mult),
                 reads=['cum', 'mask'], writes=['posm'])
            S.op('dve', lambda e: e.tensor_scalar(out=posm[:], in0=posm[:], scalar1=-1.0, scalar2=None, op0=ALU.add), reads=['posm'], writes=['posm'])
            TTo('dve', glo[:], affv, mask[:], ALU.mult, ['aff_all', 'mask'], ['glo'])
            CP('dve', gw[:, :, :, 0], glo[:], ['glo'], ['gw'])
            CP('dve', ghi32[:], gw[:, :, :, 0], ['gw'], ['ghi32'])
            TTo('dve', gw[:, :, :, 1], glo[:], ghi32[:], ALU.subtract, ['glo', 'ghi32'], ['gw'])
            if dbg:
                o1 = dout("dbg_posm", [128, 512])
                S.dma(lambda e: e.dma_start(out=o1[:, :], in_=posm[:].rearrange("p a b -> p (a b)")), reads=['posm'])
            S.barrier()
            S.flush()
        if stop_after <= 5:
            return nc, dbg_outs

        with contextlib.ExitStack() as ph:
            h2_all = sb(ph, "h2_all", [128, 32, D], BF16)
            for i in range(32):
                S.dma(lambda e, i=i: e.dma_start(out=h2_all[:, i, :], in_=h2D[i * 128:(i + 1) * 128, :]), reads=['h2D'], writes=['h2_all'])
            OH = sb(ph, "OH", [128, 32, 512], BF16)
            xinT = sb(ph, "xinT", [128, 8, 512], BF16); hidT = sb(ph, "hidT", [128, 8, 512], BF16)
            wgb = sb(ph, "wgb", [128, 8, D], BF16); wub = sb(ph, "wub", [128, 8, D], BF16); wdb = sb(ph, "wdb", [128, 8, D], BF16)
            wsg = [sb(ph, "wsg%d" % i, [128, D], F32) for i in range(3)]
            gcs = sb(ph, "gcs", [128, 4], F32); sgt = sb(ph, "sgt", [128, 512], F32)
            ywt = sb(ph, "ywt", [128, 4, D], BF16)
            wi = 0
            for ex_ in range(int(os.environ.get('K_NE', 16))):
                for (wsrc, wdst, wkey) in ((wg_d, wgb, 'wgb'), (wu_d, wub, 'wub'), (wd_d, wdb, 'wdb')):
                    for j in range(8):
                        w = wsg[wi % 3]; wk = 'wsg%d' % (wi % 3)
                        S.dma(lambda e, w=w, wsrc=wsrc, ex_=ex_, j=j: e.dma_start(out=w[:], in_=wsrc[ex_, j * 128:(j + 1) * 128, :]), writes=[wk])
                        CP(('dve', 'pool', 'act')[wi % 3], wdst[:, j, :], w[:], [wk], [wkey])
                        wi += 1
                for i in range(32):
                    S.op('dve' if i % 2 else 'pool', lambda e, i=i, ex_=ex_: e.tensor_scalar(
                        out=OH[:, i, :], in0=iot[:, 0:512], scalar1=posm[:, i, ex_:ex_ + 1], scalar2=None, op0=ALU.is_equal),
                        reads=['iot', 'posm'], writes=['OH'])
                for j in range(8):
                    pb = P[j % 2]; pk = 'P%d' % (j % 2)
                    for i in range(32):
                        MM(pb, h2_all[:, i, j * 128:(j + 1) * 128], OH[:, i, :], ['h2_all', 'OH'], [pk], start=(i == 0), stop=(i == 31))
                    CP('act' if j % 2 else 'dve', xinT[:, j, :], pb, [pk], ['xinT'])
                gcp = PS[:, 1024:1032].rearrange("p (c k) -> p c k", k=2)
                for ct in range(4):
                    for i in range(32):
                        MM(gcp[:, ct, :], OH[:, i, ct * 128:(ct + 1) * 128], gw[:, i, ex_, :], ['OH', 'gw'], ['P2'],
                           start=(i == 0 and ct == 0), stop=(i == 31 and ct == 3))
                TTo('dve', gcs[:], gcp[:, :, 0], gcp[:, :, 1], ALU.add, ['P2'], ['gcs']) if False else None
                CP('dve', sgt[:, 0:8], PS[:, 1024:1032], ['P2'], ['sgt'])
                TTo('dve', gcs[:], sgt[:, 0:8].rearrange("p (c k) -> p c k", k=2)[:, :, 0], sgt[:, 0:8].rearrange("p (c k) -> p c k", k=2)[:, :, 1],
                    ALU.add, ['sgt'], ['gcs'])
                for fc in range(8):
                    for j in range(8):
                        MM(P[4], wgb[:, j, fc * 128:(fc + 1) * 128], xinT[:, j, :], ['wgb', 'xinT'], ['P4'], start=(j == 0), stop=(j == 7))
                    for j in range(8):
                        MM(P[5], wub[:, j, fc * 128:(fc + 1) * 128], xinT[:, j, :], ['wub', 'xinT'], ['P5'], start=(j == 0), stop=(j == 7))
                    ACTF(sgt[:], P[4], AF.Silu, ['P4'], ['sgt'])
                    TTo('dve', hidT[:, fc, :], sgt[:], P[5], ALU.mult, ['sgt', 'P5'], ['hidT'])
                for ct in range(4):
                    for half in range(2):
                        pb = P[6 + half]; pk = 'P%d' % (6 + half)
                        for fc in range(8):
                            MM(pb, hidT[:, fc, ct * 128:(ct + 1) * 128], wdb[:, fc, half * 512:(half + 1) * 512], ['hidT', 'wdb'], [pk],
                               start=(fc == 0), stop=(fc == 7))
                        S.op('act', lambda e, ct=ct, half=half, pb=pb: e.activation(out=ywt[:, ct, half * 512:(half + 1) * 512], in_=pb,
                                                                                  func=AF.Copy, scale=gcs[:, ct:ct + 1]),
                             reads=[pk, 'gcs'], writes=['ywt'])
                S.dma(lambda e, ex_=ex_: e.dma_start(out=ywD[ex_].rearrange("(c p) n -> p c n", p=128), in_=ywt[:]), reads=['ywt'], writes=['ywD'])
            if dbg:
                o1 = dout("dbg_yw", [16 * 512, D], BF16)
                S.dma(lambda e: e.dma_start(out=o1[:, :], in_=ywD.rearrange("e c n -> (e c) n")), reads=['ywD'])
            S.barrier()
            S.flush()
        if stop_after <= 6:
            return nc, dbg_outs

        with contextlib.ExitStack() as ph:
            yw_all = sb(ph, "yw_all", [128, 16, 4, D], BF16)
            for ex_ in range(16):
                S.dma(lambda e, ex_=ex_: e.dma_start(out=yw_all[:, ex_, :, :], in_=ywD[ex_].rearrange("(c p) n -> p c n", p=128)),
                      reads=['ywD'], writes=['yw_all'])
            cst = {}
            for nm, src in (("gt2", modd[0:1, 5120:6144]), ("ln2g", ln2_d[0:1, :]), ("ln2b", ln2_d[1:2, :])):
                cst[nm] = sb(ph, nm, [128, D], F32)
                S.dma(lambda e, nm=nm, src=src: e.dma_start(out=cst[nm][:], in_=src.partition_broadcast(128)), reads=['modd'], writes=[nm])
            dg = sb(ph, "dg", [128, 4, 128], F32)
            OHT = sb(ph, "OHT", [128, 4, 2048], BF16)
            x1l = sb(ph, "x1l", [128, D], F32); tr2 = sb(ph, "tr2", [128, D], F32); xo = sb(ph, "xo", [128, D], F32)
            stats = sb(ph, "statsF", [128, 2, 6], F32); mv = sb(ph, "mvF", [128, 2], F32)
            rstd = sb(ph, "rstdF", [128, 1], F32); nb = sb(ph, "nbF", [128, 1], F32)
            lntF = (stats, mv, rstd, nb, 'F')
            for i in range(int(os.environ.get('K_FT', 32))):
                t0 = i * 128
                S.dma(lambda e, t0=t0: e.dma_start(out=x1l[:], in_=x1D[t0:t0 + 128, :]), reads=['x1D'], writes=['x1l'])
                for eg in range(4):
                    for k_ in range(4):
                        ex_ = eg * 4 + k_
                        S.op('dve' if k_ % 2 else 'pool', lambda e, k_=k_, ex_=ex_, i=i: e.tensor_scalar(
                            out=dg[:, k_, :], in0=identf[:], scalar1=posm[:, i, ex_:ex_ + 1], scalar2=None, op0=ALU.mult),
                            reads=['identf', 'posm'], writes=['dg'])
                    MM(PS[:, eg * 512:(eg + 1) * 512], onesf[:], dg[:].rearrange("p a t -> p (a t)"), ['onesf', 'dg'], ['P%d' % eg])
                for ct in range(4):
                    S.op('dve', lambda e, ct=ct: e.tensor_scalar(out=OHT[:, ct, :], in0=PS[:, 0:2048], scalar1=iot[:, 512 + ct:513 + ct],
                                                                scalar2=None, op0=ALU.is_equal),
                         reads=['P0', 'P1', 'P2', 'P3', 'iot'], writes=['OHT'])
                for half in range(2):
                    pb = P[4 + half]; pk = 'P%d' % (4 + half)
                    n = 0
                    for ex_ in range(16):
                        for ct in range(4):
                            MM(pb, OHT[:, ct, ex_ * 128:(ex_ + 1) * 128], yw_all[:, ex_, ct, half * 512:(half + 1) * 512], ['OHT', 'yw_all'], [pk],
                               start=(n == 0), stop=(n == 63))
                            n += 1
                TTo('dve', tr2[:], PS[:, 2048:3072], cst['gt2'][:], ALU.mult, ['P4', 'P5', 'gt2'], ['tr2'])
                S.op('dve', lambda e: e.scalar_tensor_tensor(out=tr2[:], in0=x1l[:], scalar=ALPHA, in1=tr2[:], op0=ALU.mult, op1=ALU.add),
                     reads=['x1l', 'tr2'], writes=['tr2'])
                ln_stats(lntF, tr2, 'tr2', eps5, 'eps5')
                S.op('act', lambda e: e.activation(out=xo[:], in_=tr2[:], func=AF.Identity, bias=nb[:], scale=rstd[:]),
                     reads=['tr2', 'Fnb', 'Frstd'], writes=['xo'])
                TTo('pool', xo[:], xo[:], cst['ln2g'][:], ALU.mult, ['xo', 'ln2g'], ['xo'])
                TTo('dve', xo[:], xo[:], cst['ln2b'][:], ALU.add, ['xo', 'ln2b'], ['xo'])
                S.dma(lambda e, t0=t0: e.dma_start(out=out_d[t0:t0 + 128, :], in_=xo[:]), reads=['xo'], writes=['out'])
            S.barrier()
            S.flush()
    return nc, dbg_outs


def host_inputs(inp, b):
    f = np.float32
    m = {}
    m["x"] = np.ascontiguousarray(inp["x"][b], dtype=f)
    m["ctx"] = np.ascontiguousarray(inp["ctx"][b], dtype=f)
    m["cc"] = np.ascontiguousarray(np.stack([inp["c"][b].reshape(8, 128).T, inp["c_ctx"].reshape(8, 128).T], -1).reshape(128, 16), dtype=f)
    m["w_ada"] = np.ascontiguousarray(inp["w_ada"][0], dtype=f)
    m["b_ada"] = np.ascontiguousarray(inp["b_ada"][0].reshape(1, -1), dtype=f)
    m["w_in"] = np.ascontiguousarray(inp["w_in"][0], dtype=f)
    m["qg"] = np.ascontiguousarray(np.tile(inp["q_gain"][0], 8).reshape(1, 512), dtype=f)
    m["kg"] = np.ascontiguousarray(np.tile(inp["k_gain"][0], 2).reshape(1, 128), dtype=f)
    t = np.arange(TL)
    pos = np.stack([t // 64, t % 64], -1).astype(np.float32)
    inv = (10000.0 ** (-np.arange(16, dtype=np.float32) / 16)).astype(np.float32)
    ang = pos[:, :, None] * inv[None, None, :]
    cs = np.cos(ang).astype(f); sn = np.sin(ang).astype(f)
    cos2 = np.stack([cs, cs], 2).reshape(TL, 64)
    sin2 = np.stack([-sn, sn], 2).reshape(TL, 64)
    m["cosT"] = np.ascontiguousarray(np.tile(cos2, (1, 8)), dtype=f)
    m["sinS"] = np.ascontiguousarray(np.tile(sin2, (1, 8)), dtype=f)
    m["ident"] = np.eye(128, dtype=f)
    def kh(v):
        return np.asarray(v, dtype=f).reshape(8, 64).T
    mu = inp["tshift_mu"][0]
    cols = [kh(mu[0:512]), kh(mu[512:1024]), kh(mu[1024:1536]), kh(inp["k_k"][0]), kh(inp["k_a"][0]), kh(inp["r_k"][0].reshape(-1)),
            kh(inp["decay_w0"][0, 0]), kh(inp["decay_w0"][0, 1]), kh(inp["iclr_a0"][0, 0]), kh(inp["iclr_a0"][0, 1])]
    m["rp"] = np.ascontiguousarray(np.stack(cols, -1).reshape(64, 80), dtype=f)
    lmu = np.zeros((128, 3), f)
    lmu[0:64, 0] = mu[1536:1600]; lmu[0:64, 1] = mu[1600:1664]; lmu[:, 2] = mu[1664:1792]
    m["lmu"] = lmu
    m["decup"] = np.ascontiguousarray(np.concatenate([inp["decay_up"][0, 0], inp["decay_up"][0, 1]], 1), dtype=f)
    m["iclup"] = np.ascontiguousarray(np.concatenate([inp["iclr_up"][0, 0], inp["iclr_up"][0, 1]], 1), dtype=f)
    m["gateup"] = np.ascontiguousarray(inp["gate_up"][0], dtype=f)
    hh = np.repeat(np.arange(8), 16); tt = np.tile(np.arange(16), 8)
    same = hh[:, None] == hh[None, :]
    msf = (same & (tt[:, None] < tt[None, :])).astype(f); mif = (same & (tt[:, None] <= tt[None, :])).astype(f)
    msb = msf.T.copy(); mib = mif.T.copy()
    m["mk4"] = np.ascontiguousarray(np.concatenate([msf, mif, msf, mif, msb, mib, msb, mib], 1), dtype=f)
    m["mn1"] = np.ascontiguousarray(np.concatenate([msb, msf], 1), dtype=f)
    m["bm"] = np.ascontiguousarray((hh[:, None] == np.repeat(np.arange(8), 64)[None, :]).astype(f))
    rst = np.ones((64, 1024), f); rst[:, ::16] = 0.0
    m["rst"] = rst
    m["w_out"] = np.ascontiguousarray(inp["w_out"][0], dtype=f)
    m["lnx"] = np.ascontiguousarray(np.stack([inp["lnx_g"][0], inp["lnx_b"][0]]), dtype=f)
    m["ln1"] = np.ascontiguousarray(np.stack([inp["ln1_g"][0], inp["ln1_b"][0]]), dtype=f)
    m["ln2"] = np.ascontiguousarray(np.stack([inp["ln2_g"][0], inp["ln2_b"][0]]), dtype=f)
    m["router_w"] = np.ascontiguousarray(inp["router_w"][0], dtype=f)
    m["exp_w_gate"] = np.ascontiguousarray(inp["exp_w_gate"][0], dtype=f)
    m["exp_w_up"] = np.ascontiguousarray(inp["exp_w_up"][0], dtype=f)
    m["exp_w_down"] = np.ascontiguousarray(inp["exp_w_down"][0], dtype=f)
    iot = np.zeros((128, 516), f)
    iot[:, 0:512] = np.arange(512, dtype=f)[None, :]
    iot[:, 512:516] = np.arange(128, dtype=f)[:, None] + 128.0 * np.arange(4, dtype=f)[None, :]
    m["iot"] = iot
    m["ustr"] = np.triu(np.ones((128, 128), f), 1)
    return m


_NC_CACHE = {}


def kernel(**inputs):
    inp = {k: np.asarray(v) for k, v in inputs.items()}
    if "full" not in _NC_CACHE:
        _NC_CACHE["full"] = build_nc()[0]
    nc = _NC_CACHE["full"]
    in_maps = [host_inputs(inp, c // 2) for c in range(8)]
    res = run_bass_kernel_spmd(nc, in_maps, core_ids=list(range(8)))
    out = np.stack([res.results[2 * b]["out"] for b in range(4)], 0).astype(np.float32)
    return out
```

```python
import contextlib
import os
import numpy as np
import concourse.bass as bass
import concourse.mybir as mybir
from concourse.bass_utils import run_bass_kernel_spmd

F32 = mybir.dt.float32
BF16 = mybir.dt.bfloat16
AF = mybir.ActivationFunctionType
ALU = mybir.AluOpType
AX = mybir.AxisListType

D = 1024
TL = 4096
TC = 256
TT = TL + TC
NT = TT // 128
ALPHA = 2.0 ** 0.25
DEC_C = -float(np.exp(-0.5))


class Sched:
    CE = ('pe', 'act', 'dve', 'pool')

    def __init__(self, nc, stack, ndma=32):
        self.nc = nc
        self.ops = {e: [] for e in ('pe', 'act', 'dve', 'pool', 'sp')}
        self.cnt = {e: 0 for e in self.CE}
        self.last_w = {}
        self.readers = {}
        self.waited = {e: {} for e in self.ops}
        self.ndma = ndma
        self.dma_val = [0] * ndma
        self.dma_i = 0
        names = list(self.CE) + ['d%d' % i for i in range(ndma)]
        self.sems = {n: stack.enter_context(nc.semaphore('s_' + n)) for n in names}

    def _deps(self, eng, reads, writes):
        deps = {}

        def add(tok):
            if tok is None:
                return
            s, v = tok
            if deps.get(s, 0) < v:
                deps[s] = v
        for r in reads:
            add(self.last_w.get(r))
        for w in writes:
            add(self.last_w.get(w))
            for t in self.readers.get(w, ()):
                add(t)
        waits = []
        for s, v in deps.items():
            if s == eng and (eng == 'pe' or os.environ.get('K_NOSELF')):
                continue
            if self.waited[eng].get(s, 0) >= v:
                continue
            self.waited[eng][s] = v
            waits.append((s, v))
        return waits

    def _commit(self, tok, reads, writes):
        for r in reads:
            self.readers.setdefault(r, []).append(tok)
        for w in writes:
            self.last_w[w] = tok
            self.readers[w] = []

    capture = None
    pend = None

    def drain(self, lst, n):
        for _ in range(min(n, len(lst))):
            kind, a = lst.pop(0)
            (self.op if kind == 'op' else self.dma)(*a)

    def op(self, eng, fn, reads=(), writes=()):
        if self.capture is not None:
            self.capture.append(('op', (eng, fn, tuple(reads), tuple(writes))))
            return
        waits = self._deps(eng, reads, writes)
        self.cnt[eng] += 1
        tok = (eng, self.cnt[eng])
        self.ops[eng].append((waits, fn, (eng, 1)))
        self._commit(tok, reads, writes)

    def dma(self, fn, reads=(), writes=(), q='sp'):
        if self.capture is not None:
            self.capture.append(('dma', (fn, tuple(reads), tuple(writes), q)))
            return
        slot = self.dma_i % self.ndma
        self.dma_i += 1
        s = 'd%d' % slot
        waits = self._deps(q, reads, writes)
        pv = self.dma_val[slot]
        if pv > 0 and self.waited[q].get(s, 0) < pv:
            self.waited[q][s] = pv
            waits.append((s, pv))
        self.dma_val[slot] = pv + 16
        tok = (s, pv + 16)
        self.ops[q].append((waits, fn, (s, 16)))
        self._commit(tok, reads, writes)

    def barrier(self):
        allw = [(e, c) for e, c in self.cnt.items() if c > 0]
        allw += [('d%d' % i, v) for i, v in enumerate(self.dma_val) if v > 0]
        for eng in self.ops:
            waits = []
            for s, v in allw:
                if self.waited[eng].get(s, 0) >= v:
                    continue
                self.waited[eng][s] = v
                waits.append((s, v))
            if waits:
                self.ops[eng].append((waits, None, None))
        self.last_w = {}
        self.readers = {}

    def flush(self):
        nc = self.nc
        sems = self.sems
        ops = self.ops
        self.ops = {e: [] for e in ops}
        if os.environ.get('K_STATS'):
            print("FLUSH", {e: (len(v), sum(len(w[0]) for w in v)) for e, v in ops.items()})
        with nc.Block() as block:
            def run(engname, engobj):
                for waits, fn, inc in ops[engname]:
                    for ws, wv in waits:
                        engobj.wait_ge(sems[ws], wv)
                    if fn is not None:
                        ins = fn(engobj)
                        ins.then_inc(sems[inc[0]], inc[1])

            @block.sync
            def _(e):
                run('sp', e)

            @block.tensor
            def _(e):
                run('pe', e)

            @block.scalar
            def _(e):
                run('act', e)

            @block.vector
            def _(e):
                run('dve', e)

            @block.gpsimd
            def _(e):
                run('pool', e)


def build_nc(stop_after=99, dbg=False):
    nc = bass.Bass("TRN2", target_bir_lowering=False)

    def din(name, shape, dt=F32):
        return nc.dram_tensor(name, list(shape), dt, kind="ExternalInput").ap()

    def dscr(name, shape, dt=F32):
        return nc.dram_tensor(name, list(shape), dt, kind="Internal").ap()

    x_d = din("x", [TL, D]); ctx_d = din("ctx", [TC, D])
    cc_d = din("cc", [128, 16])
    wada_d = din("w_ada", [D, 6 * D]); bada_d = din("b_ada", [1, 6 * D])
    win_d = din("w_in", [D, 2560])
    qg_d = din("qg", [1, 512]); kg_d = din("kg", [1, 128])
    cos_d = din("cosT", [TL, 512]); sin_d = din("sinS", [TL, 512])
    ident_d = din("ident", [128, 128])
    rp_d = din("rp", [64, 8 * 10])
    lmu_d = din("lmu", [128, 3])
    decup_d = din("decup", [64, 2 * 512]); iclup_d = din("iclup", [64, 2 * 512]); gateup_d = din("gateup", [128, 512])
    mk4_d = din("mk4", [128, 2 * 512]); mn1_d = din("mn1", [128, 2 * 128]); bm_d = din("bm", [128, 512])
    rst_d = din("rst", [64, 1024])
    wout_d = din("w_out", [D, D]); lnx_d = din("lnx", [2, 512])
    ln1_d = din("ln1", [2, D]); ln2_d = din("ln2", [2, D])
    rw_d = din("router_w", [D, 16])
    x1D = dscr("x1D", [TL, D]); h2D = dscr("h2D", [TL, D], BF16)
    attD = dscr("attD", [128, 4 * TL], BF16)
    wg_d = din("exp_w_gate", [16, D, D]); wu_d = din("exp_w_up", [16, D, D]); wd_d = din("exp_w_down", [16, D, D])
    iot_d = din("iot", [128, 512 + 4]); ustr_d = din("ustr", [128, 128])
    ywD = dscr("ywD", [16, 512, D], BF16)
    ydir = dscr("ydir", [2, TL, 512]); bonD = dscr("bonD", [TL, 512]); gD = dscr("gD", [TL, 512])
    out_d = nc.dram_tensor("out", [TL, D], F32, kind="ExternalOutput").ap()
    modd = dscr("modd", [2, 6 * D])
    rwT = dscr("rwT", [1792, TT])
    dbg_outs = {}

    def dout(name, shape, dt=F32):
        ap = nc.dram_tensor(name, list(shape), dt, kind="ExternalOutput").ap()
        dbg_outs[name] = ap
        return ap

    with contextlib.ExitStack() as top:
        S = Sched(nc, top)

        def sb(stack, name, shape, dt):
            return stack.enter_context(nc.sbuf_tensor("sb_" + name, list(shape), dt))

        PS = top.enter_context(nc.psum_tensor("PS", [128, 8 * 512], F32))
        P = [PS[:, i * 512:(i + 1) * 512] for i in range(8)]
        PK = ['P%d' % i for i in range(8)]
        identf = sb(top, "identf", [128, 128], F32)
        identb = sb(top, "identb", [128, 128], BF16)
        eps5 = sb(top, "eps5", [128, 1], F32)
        eps6 = sb(top, "eps6", [128, 1], F32)
        S.dma(lambda e: e.dma_start(out=identf[:], in_=ident_d[:, :]), writes=['identf'])
        S.op('dve', lambda e: e.tensor_copy(out=identb[:], in_=identf[:]), reads=['identf'], writes=['identb'])
        S.op('pool', lambda e: e.memset(eps5[:], 1e-5), writes=['eps5'])
        S.op('pool', lambda e: e.memset(eps6[:], 1e-6), writes=['eps6'])

        def ln_stats(stack_tiles, src, key_src, eps_t, eps_key):
            stats, mv, rstd, nb, kq = stack_tiles
            for cch in range(2):
                S.op('dve', lambda e, cch=cch: e.bn_stats(out=stats[:, cch, :], in_=src[:, cch * 512:(cch + 1) * 512]),
                     reads=[key_src], writes=[kq + 'stats'])
            S.op('dve', lambda e: e.bn_aggr(out=mv[:], in_=stats[:]), reads=[kq + 'stats'], writes=[kq + 'mv'])
            S.op('act', lambda e: e.activation(out=rstd[:], in_=mv[:, 1:2], func=AF.Ln, bias=eps_t[:], scale=1.0),
                 reads=[kq + 'mv', eps_key], writes=[kq + 'rstd'])
            S.op('act', lambda e: e.activation(out=rstd[:], in_=rstd[:], func=AF.Exp, scale=-0.5),
                 reads=[kq + 'rstd'], writes=[kq + 'rstd'])
            S.op('dve', lambda e: e.scalar_tensor_tensor(out=nb[:], in0=mv[:, 0:1], scalar=-1.0, in1=rstd[:],
                                                        op0=ALU.mult, op1=ALU.mult),
                 reads=[kq + 'mv', kq + 'rstd'], writes=[kq + 'nb'])

        with contextlib.ExitStack() as ph:
            cc = sb(ph, "cc", [128, 8, 2], F32)
            ccs = sb(ph, "ccs", [128, 8, 2], F32)
            wst = [sb(ph, "wst%d" % i, [128, 8, 512], F32) for i in range(2)]
            bada = sb(ph, "bada", [2, 6 * D], F32)
            modrow = sb(ph, "modrow", [2, 6 * D], F32)
            S.dma(lambda e: e.dma_start(out=cc[:].rearrange("p a b -> p (a b)"), in_=cc_d[:, :]), writes=['cc'])
            S.dma(lambda e: e.dma_start(out=bada[:], in_=bada_d.partition_broadcast(2)), writes=['bada'])
            S.op('act', lambda e: e.activation(out=ccs[:], in_=cc[:], func=AF.Silu), reads=['cc'], writes=['ccs'])
            for g in range(12):
                w = wst[g % 2]
                wk = 'wst%d' % (g % 2)
                S.dma(lambda e, w=w, g=g: e.dma_start(
                    out=w[:], in_=wada_d[:, g * 512:(g + 1) * 512].rearrange("(j p) n -> p j n", p=128)), writes=[wk])
                for j in range(8):
                    S.op('pe', lambda e, w=w, j=j: e.matmul(P[0][0:2, :], lhsT=ccs[:, j, :], rhs=w[:, j, :],
                                                           start=(j == 0), stop=(j == 7)),
                         reads=['ccs', wk], writes=['P0'])
                S.op('dve', lambda e, g=g: e.tensor_tensor(out=modrow[:, g * 512:(g + 1) * 512], in0=P[0][0:2, :],
                                                          in1=bada[:, g * 512:(g + 1) * 512], op=ALU.add),
                     reads=['P0', 'bada'], writes=['modrow'])
            for lo in (1024, 4096):
                S.op('dve', lambda e, lo=lo: e.tensor_scalar_add(out=modrow[:, lo:lo + 1024], in0=modrow[:, lo:lo + 1024],
                                                                scalar1=1.0), reads=['modrow'], writes=['modrow'])
            S.dma(lambda e: e.dma_start(out=modd[:, :], in_=modrow[:]), reads=['modrow'], writes=['modd'])
            if dbg:
                o = dout("dbg_mod", [2, 6 * D])
                S.dma(lambda e: e.dma_start(out=o[:, :], in_=modrow[:]), reads=['modrow'])
            S.barrier()
            S.flush()
        if stop_after <= 0:
            return nc, dbg_outs

        def modrow_bc(dst, row, lo):
            S.dma(lambda e: e.dma_start(out=dst[:], in_=modd[row:row + 1, lo:lo + 1024].partition_broadcast(128)),
                  reads=['modd'], writes=[dst.name if hasattr(dst, 'name') else 'x'])

        aff_all = sb(top, "aff_all", [128, 32, 16], F32)
        with contextlib.ExitStack() as phAB:
            qT_all = sb(phAB, "qT_all", [128, 4, TL], BF16)
            kTd = sb(phAB, "kTd", [128, 2, TT], BF16)
            Vx = sb(phAB, "Vx", [128, NT, 2, 80], BF16)
            attT_all = sb(phAB, "attT_all", [128, 4, TL], BF16)
            with contextlib.ExitStack() as ph:
                winb = sb(ph, "winb", [128, 8, 2560], BF16)
                wstg = [sb(ph, "wstg%d" % i, [128, 640], F32) for i in range(2)]
                sc1p = sb(ph, "sc1p", [128, D], F32); sh1 = sb(ph, "sh1", [128, D], F32)
                csc1p = sb(ph, "csc1p", [128, D], F32); csh1 = sb(ph, "csh1", [128, D], F32)
                qg = sb(ph, "qg", [128, 512], F32); kg = sb(ph, "kg", [128, 128], F32)
                for dst, nm, row, lo in ((sh1, 'sh1', 0, 0), (sc1p, 'sc1p', 0, 1024), (csh1, 'csh1', 1, 0), (csc1p, 'csc1p', 1, 1024)):
                    S.dma(lambda e, dst=dst, row=row, lo=lo: e.dma_start(
                        out=dst[:], in_=modd[row:row + 1, lo:lo + 1024].partition_broadcast(128)), reads=['modd'], writes=[nm])
                S.dma(lambda e: e.dma_start(out=qg[:], in_=qg_d.partition_broadcast(128)), writes=['qg'])
                S.dma(lambda e: e.dma_start(out=kg[:], in_=kg_d.partition_broadcast(128)), writes=['kg'])
                for jj in range(32):
                    j = jj // 4; c0 = (jj % 4) * 640
                    w = wstg[jj % 2]; wk = 'wstg%d' % (jj % 2)
                    S.dma(lambda e, w=w, j=j, c0=c0: e.dma_start(out=w[:], in_=win_d[j * 128:(j + 1) * 128, c0:c0 + 640]), writes=[wk])
                    S.op('pool' if jj % 2 else 'dve', lambda e, w=w, j=j, c0=c0: e.tensor_copy(out=winb[:, j, c0:c0 + 640], in_=w[:]),
                         reads=[wk], writes=['winb'])
                S.op('pool', lambda e: e.memset(Vx[:], 1.0), writes=['Vx'])
                xt = [sb(ph, "xt%d" % i, [128, D], F32) for i in range(2)]
                xn = sb(ph, "xn", [128, D], F32)
                hb = sb(ph, "hb", [128, D], BF16)
                hT = sb(ph, "hT", [128, 8, 512], BF16)
                stats = sb(ph, "stats", [128, 2, 6], F32); mv = sb(ph, "mv", [128, 2], F32)
                rstd = sb(ph, "rstd", [128, 1], F32); nb = sb(ph, "nb", [128, 1], F32)
                lnt = (stats, mv, rstd, nb, 'A')
                rstg = [sb(ph, "rstg%d" % i, [128, 512], F32) for i in range(2)]
                cosT = sb(ph, "cosT", [128, 512], F32); sinS = sb(ph, "sinS", [128, 512], F32)
                sq = sb(ph, "sq", [128, 512], F32)
                ssum = sb(ph, "ssum", [128, 8], F32)
                qn = sb(ph, "qn", [128, 512], F32); qa = sb(ph, "qa", [128, 512], F32)
                qbt = sb(ph, "qbt", [128, 512], F32)
                qo = sb(ph, "qo", [128, 512], BF16)
                ko = sb(ph, "ko", [128, 2, 2, 64], BF16)
                blocks = [(0, 256)] + [(256 + 512 * i, 512) for i in range(8)]
                ti = 0
                for (u0, ntok) in blocks:
                    ntile = ntok // 128
                    is_ctx = (u0 == 0)
                    for tl in range(ntile):
                        u = u0 + tl * 128
                        gt = u // 128
                        X = xt[ti % 2]; xk = 'xt%d' % (ti % 2)
                        ti += 1
                        src = ctx_d[u:u + 128, :] if is_ctx else x_d[u - TC:u - TC + 128, :]
                        S.dma(lambda e, X=X, src=src: e.dma_start(out=X[:], in_=src), writes=[xk])
                        ln_stats(lnt, X, xk, eps5, 'eps5')
                        S.op('act', lambda e, X=X: e.activation(out=xn[:], in_=X[:], func=AF.Identity, bias=nb[:], scale=rstd[:]),
                             reads=[xk, 'Anb', 'Arstd'], writes=['xn'])
                        scp, shh, k1, k2 = (csc1p, csh1, 'csc1p', 'csh1') if is_ctx else (sc1p, sh1, 'sc1p', 'sh1')
                        S.op('pool', lambda e, scp=scp: e.tensor_tensor(out=xn[:], in0=xn[:], in1=scp[:], op=ALU.mult),
                             reads=['xn', k1], writes=['xn'])
                        S.op('dve', lambda e, shh=shh: e.tensor_tensor(out=hb[:], in0=xn[:], in1=shh[:], op=ALU.add),
                             reads=['xn', k2], writes=['hb'])
                        pTb = P[0][:].bitcast(BF16)
                        for j in range(8):
                            S.op('pe', lambda e, j=j, pTb=pTb: e.transpose(out=pTb[:, j * 128:(j + 1) * 128],
                                                                      in_=hb[:, j * 128:(j + 1) * 128], identity=identb[:]),
                                 reads=['hb', 'identb'], writes=['P0'])
                        S.op('act', lambda e, tl=tl, pTb=pTb: e.copy(out=hT[:, :, tl * 128:(tl + 1) * 128],
                                                                 in_=pTb.rearrange("p (j t) -> p j t", j=8)),
                             reads=['P0'], writes=['hT'])
                        for j in range(8):
                            S.op('pe', lambda e, j=j, tl=tl: e.matmul(P[1][:, :], lhsT=hT[:, j, tl * 128:(tl + 1) * 128],
                                                                     rhs=winb[:, j, 0:512], start=(j == 0), stop=(j == 7)),
                                 reads=['hT', 'winb'], writes=['P1'])
                        for j in range(8):
                            S.op('pe', lambda e, j=j, tl=tl: e.matmul(P[2][:, 0:256], lhsT=hT[:, j, tl * 128:(tl + 1) * 128],
                                                                     rhs=winb[:, j, 512:768], start=(j == 0), stop=(j == 7)),
                                 reads=['hT', 'winb'], writes=['P2'])
                        S.op('act', lambda e, gt=gt: e.copy(out=Vx[:, gt, :, 0:64],
                                                          in_=P[2][:, 128:256].rearrange("p (a b) -> p a b", a=2)),
                             reads=['P2'], writes=['Vx'])
                        if not is_ctx:
                            ul = u - TC
                            S.dma(lambda e, ul=ul: e.dma_start(out=cosT[:], in_=cos_d[ul:ul + 128, :]), writes=['cosT'])
                            S.dma(lambda e, ul=ul: e.dma_start(out=sinS[:], in_=sin_d[ul:ul + 128, :]), writes=['sinS'])

                        def normrope(psrc, pk, nh, gain, gk, do_rope, outfn):
                            w_ = nh * 64
                            S.op('act', lambda e: e.activation(out=sq[:, 0:w_], in_=psrc, func=AF.Square),
                                 reads=[pk], writes=['sq'])
                            S.op('dve', lambda e: e.tensor_reduce(out=ssum[:, 0:nh], in_=sq[:, 0:w_].rearrange("p (h d) -> p h d", d=64),
                                                                 axis=AX.X, op=ALU.add), reads=['sq'], writes=['ssum'])
                            S.op('act', lambda e: e.activation(out=ssum[:, 0:nh], in_=ssum[:, 0:nh], func=AF.Ln, bias=eps6[:], scale=1.0 / 64),
                                 reads=['ssum', 'eps6'], writes=['ssum'])
                            S.op('act', lambda e: e.activation(out=ssum[:, 0:nh], in_=ssum[:, 0:nh], func=AF.Exp, scale=-0.5),
                                 reads=['ssum'], writes=['ssum'])
                            S.op('dve', lambda e: e.tensor_tensor(out=qn[:, 0:w_].rearrange("p (h d) -> p h d", d=64),
                                                                 in0=psrc.rearrange("p (h d) -> p h d", d=64),
                                                                 in1=ssum[:, 0:nh].unsqueeze(2).to_broadcast([128, nh, 64]), op=ALU.mult),
                                 reads=[pk, 'ssum'], writes=['qn'])
                            if not do_rope:
                                S.op('pool', lambda e: outfn(e, qn[:, 0:w_], gain[:, 0:w_], ALU.mult), reads=['qn', gk], writes=['qko'])
                                return
                            S.op('pool', lambda e: e.tensor_tensor(out=qn[:, 0:w_], in0=qn[:, 0:w_], in1=gain[:, 0:w_], op=ALU.mult),
                                 reads=['qn', gk], writes=['qn'])
                            S.op('dve', lambda e: e.tensor_tensor(out=qa[:, 0:w_], in0=qn[:, 0:w_], in1=cosT[:, 0:w_], op=ALU.mult),
                                 reads=['qn', 'cosT'], writes=['qa'])
                            qv = qn[:, 0:w_].rearrange("p (g s q) -> p g s q", s=2, q=16)
                            bv = qbt[:, 0:w_].rearrange("p (g s q) -> p g s q", s=2, q=16)
                            sv = sinS[:, 0:w_].rearrange("p (g s q) -> p g s q", s=2, q=16)
                            for s_ in range(2):
                                S.op('pool', lambda e, s_=s_: e.tensor_tensor(out=bv[:, :, s_, :], in0=qv[:, :, 1 - s_, :],
                                                                             in1=sv[:, :, s_, :], op=ALU.mult),
                                     reads=['qn', 'sinS'], writes=['qbt'])
                            S.op('dve', lambda e: outfn(e, qa[:, 0:w_], qbt[:, 0:w_], ALU.add), reads=['qa', 'qbt'], writes=['qko'])

                        if not is_ctx:
                            normrope(P[1][:, :], 'P1', 8, qg, 'qg', True,
                                     lambda e, a, b_, op: e.tensor_tensor(out=qo[:], in0=a, in1=b_, op=op))
                            pq = P[3][:].bitcast(BF16)
                            for hp in range(4):
                                S.op('pe', lambda e, hp=hp, pq=pq: e.transpose(out=pq[:, hp * 128:(hp + 1) * 128],
                                                                          in_=qo[:, hp * 128:(hp + 1) * 128], identity=identb[:]),
                                     reads=['qko', 'identb'], writes=['P3'])
                            S.op('act', lambda e, ul=ul, pq=pq: e.copy(out=qT_all[:, :, ul:ul + 128],
                                                                   in_=pq[:, 0:512].rearrange("p (j t) -> p j t", j=4)),
                                 reads=['P3'], writes=['qT_all'])

                        def kout(e, a, b_, op):
                            return e.tensor_tensor(out=ko[:, :, 0, :], in0=a.rearrange("p (h d) -> p h d", d=64),
                                                   in1=b_.rearrange("p (h d) -> p h d", d=64), op=op)
                        normrope(P[2][:, 0:128], 'P2', 2, kg, 'kg', not is_ctx, kout)
                        S.op('pool', lambda e: e.tensor_copy(out=ko[:, :, 1, :], in_=ko[:, :, 0, :]), reads=['qko'], writes=['qko'])
                        pk_ = P[3][:].bitcast(BF16)
                        for kv in range(2):
                            S.op('pe', lambda e, kv=kv, pk_=pk_: e.transpose(
                                out=pk_[:, 512 + kv * 128:512 + (kv + 1) * 128],
                                in_=ko[:, kv, :, :].rearrange("p a d -> p (a d)"), identity=identb[:]),
                                reads=['qko', 'identb'], writes=['P3'])
                        S.op('act', lambda e, u=u, pk_=pk_: e.copy(out=kTd[:, :, u:u + 128],
                                                               in_=pk_[:, 512:768].rearrange("p (j t) -> p j t", j=2)),
                             reads=['P3'], writes=['kTd'])
                    for g in range(14):
                        pb = P[4 + g % 2]; pbk = 'P%d' % (4 + g % 2)
                        for j in range(8):
                            S.op('pe', lambda e, j=j, g=g, pb=pb, ntok=ntok: e.matmul(pb[:, 0:ntok], lhsT=winb[:, j, 768 + g * 128:768 + (g + 1) * 128],
                                                                         rhs=hT[:, j, 0:ntok], start=(j == 0), stop=(j == 7)),
                                 reads=['hT', 'winb'], writes=[pbk])
                        rs = rstg[g % 2]; rk = 'rstg%d' % (g % 2)
                        S.op('act' if g % 2 else 'dve',
                             (lambda e, rs=rs, pb=pb, ntok=ntok: e.copy(out=rs[:, 0:ntok], in_=pb[:, 0:ntok])) if g % 2 else
                             (lambda e, rs=rs, pb=pb, ntok=ntok: e.tensor_copy(out=rs[:, 0:ntok], in_=pb[:, 0:ntok])),
                             reads=[pbk], writes=[rk])
                        S.dma(lambda e, rs=rs, g=g, u0=u0, ntok=ntok: e.dma_start(out=rwT[g * 128:(g + 1) * 128, u0:u0 + ntok], in_=rs[:, 0:ntok]),
                              reads=[rk], writes=['rwT'])
                if dbg:
                    o1 = dout("dbg_qT", [128, 4 * TL], BF16)
                    S.dma(lambda e: e.dma_start(out=o1[:, :], in_=qT_all[:].rearrange("p a t -> p (a t)")), reads=['qT_all'])
                    o2 = dout("dbg_kTd", [128, 2 * TT], BF16)
                    S.dma(lambda e: e.dma_start(out=o2[:, :], in_=kTd[:].rearrange("p a t -> p (a t)")), reads=['kTd'])
                    o3 = dout("dbg_rwT", [1792, TT])
                    S.dma(lambda e: e.dma_start(out=o3[:, :], in_=rwT[:, :]), reads=['rwT'])
                S.barrier()
                S.flush()
            if stop_after <= 1:
                return nc, dbg_outs
            with contextlib.ExitStack() as ph:
                pT = [sb(ph, "pT%d" % i, [128, 512], BF16) for i in range(3)]
                rec = sb(ph, "rec", [128, 8], F32)
                atok = sb(ph, "atok", [128, 8, 64], BF16)
                pi = 0
                for g in range(int(os.environ.get('K_NG', 16))):
                    q0 = g * 256
                    for st in range(NT):
                        for hp in range(int(os.environ.get('K_NHP', 4))):
                            sb0 = 2 * (hp % 2)
                            scb = PS[:, sb0 * 512:(sb0 + 2) * 512].rearrange("p (b n) -> p b n", b=2)
                            sck = 'P%d' % sb0
                            kvh = hp // 2
                            for hh in range(int(os.environ.get('K_HH0', 0)), int(os.environ.get('K_NHH', 2))):
                                S.op('pe', lambda e, hh=hh, hp=hp, scb=scb, kvh=kvh, st=st, q0=q0: e.matmul(
                                    scb[:, hh, 0:256],
                                    lhsT=kTd[hh * 64:(hh + 1) * 64, kvh, st * 128:(st + 1) * 128],
                                    rhs=qT_all[hh * 64:(hh + 1) * 64, hp, q0:q0 + 256], start=True, stop=True),
                                    reads=['kTd', 'qT_all'], writes=[sck])
                            pt = pT[pi % 3]; ptk = 'pT%d' % (pi % 3)
                            pi += 1
                            if not os.environ.get('K_SKIP_EXP'):
                                S.op('act', lambda e, pt=pt, scb=scb: e.activation(out=pt[:].rearrange('p (b n) -> p b n', b=2), in_=scb[:, :, 0:256], func=AF.Exp, scale=0.125),
                                     reads=[sck], writes=[ptk])
                            for hh in range(2 if not os.environ.get('K_SKIP_PV') else 0):
                                head = 2 * hp + hh
                                for qt in range(2):
                                    ab = P[4 + 2 * qt + head // 4]; abk = 'P%d' % (4 + 2 * qt + head // 4)
                                    c0 = (head % 4) * 65
                                    S.op('pe', lambda e, ab=ab, c0=c0, pt=pt, hh=hh, qt=qt, st=st, kvh=kvh, head=head: e.matmul(
                                        ab[:, c0:c0 + 65], lhsT=pt[:, hh * 256 + qt * 128:hh * 256 + (qt + 1) * 128],
                                        rhs=Vx[:, st, kvh, 0:65], start=(st == 0 and head % 4 == 0), stop=(st == NT - 1 and head % 4 == 3)),
                                        reads=[ptk, 'Vx'], writes=[abk])
                    for qt in range(2 if not os.environ.get('K_SKIP_NORM') else 0):
                        for half in range(2):
                            ab = P[4 + 2 * qt + half]; abk = 'P%d' % (4 + 2 * qt + half)
                            av = ab[:, 0:260].rearrange("p (h c) -> p h c", c=65)
                            S.op('dve', lambda e, av=av, half=half: e.reciprocal(out=rec[:, half * 4:(half + 1) * 4], in_=av[:, :, 64]),
                                 reads=[abk], writes=['rec'])
                            S.op('dve', lambda e, av=av, half=half: e.tensor_tensor(
                                out=atok[:, half * 4:(half + 1) * 4, :], in0=av[:, :, 0:64],
                                in1=rec[:, half * 4:(half + 1) * 4].unsqueeze(2).to_broadcast([128, 4, 64]), op=ALU.mult),
                                reads=[abk, 'rec'], writes=['atok'])
                        pa = P[0][:].bitcast(BF16)
                        if os.environ.get('K_SKIP_TR'):
                            continue
                        for hp in range(4):
                            S.op('pe', lambda e, hp=hp, pa=pa: e.transpose(
                                out=pa[:, hp * 128:(hp + 1) * 128],
                                in_=atok[:, 2 * hp:2 * hp + 2, :].rearrange("p a d -> p (a d)"), identity=identb[:]),
                                reads=['atok', 'identb'], writes=['P0'])
                        if os.environ.get('K_SKIP_CP'):
                            continue
                        S.op('dve', lambda e, pa=pa, q0=q0, qt=qt: e.tensor_copy(
                            out=attT_all[:, :, q0 + qt * 128:q0 + (qt + 1) * 128],
                            in_=pa[:, 0:512].rearrange("p (j t) -> p j t", j=4)), reads=['P0'], writes=['attT_all'])
                S.dma(lambda e: e.dma_start(out=attD[:, :], in_=attT_all[:].rearrange("p a t -> p (a t)")), reads=['attT_all'], writes=['attD'])
                if dbg:
                    o1 = dout("dbg_attT", [128, 4 * TL], BF16)
                    S.dma(lambda e: e.dma_start(out=o1[:, :], in_=attT_all[:].rearrange("p a t -> p (a t)")), reads=['attT_all'])
                S.barrier()
                S.flush()
        if stop_after <= 2:
            return nc, dbg_outs

        def TTo(eng, out, a, b_, op, R, W):
            S.op(eng, lambda e: e.tensor_tensor(out=out, in0=a, in1=b_, op=op), reads=R, writes=W)

        def CP(eng, out, in_, R, W):
            if eng == 'act':
                S.op('act', lambda e: e.copy(out=out, in_=in_), reads=R, writes=W)
            else:
                S.op(eng, lambda e: e.tensor_copy(out=out, in_=in_), reads=R, writes=W)

        def ACTF(out, in_, func, R, W, scale=1.0, bias=None):
            if bias is None:
                S.op('act', lambda e: e.activation(out=out, in_=in_, func=func, scale=scale), reads=R, writes=W)
            else:
                S.op('act', lambda e: e.activation(out=out, in_=in_, func=func, scale=scale, bias=bias), reads=R, writes=W)

        def MM(out, lhsT, rhs, R, W, start=True, stop=True):
            S.op('pe', lambda e: e.matmul(out, lhsT=lhsT, rhs=rhs, start=start, stop=stop), reads=R, writes=W)

        def TR(out, in_, idn, R, W):
            S.op('pe', lambda e: e.transpose(out=out, in_=in_, identity=idn), reads=R, writes=W)

        with contextlib.ExitStack() as ph:
            def t32(name, shape=(64, 1024)):
                return sb(ph, name, list(shape), F32)
            rp = t32("rp", (64, 8, 10)); rpd = t32("rpd", (64, 8, 8))
            lmu = t32("lmu", (128, 3)); lmd = t32("lmd", (128, 6))
            decupb = sb(ph, "decupb", [64, 2, 512], BF16); iclupb = sb(ph, "iclupb", [64, 2, 512], BF16)
            gateupb = sb(ph, "gateupb", [128, 512], BF16)
            mk4 = t32("mk4", (128, 2, 512)); mn1 = t32("mn1", (128, 2, 128)); bm = t32("bm", (128, 512))
            rst = t32("rst"); ones64 = sb(ph, "ones64", [64, 64], BF16); tiny = t32("tiny", (64, 1))
            S.dma(lambda e: e.dma_start(out=rp[:].rearrange("k h n -> k (h n)"), in_=rp_d[:, :]), writes=['rp'])
            S.dma(lambda e: e.dma_start(out=lmu[:], in_=lmu_d[:, :]), writes=['lmu'])
            S.dma(lambda e: e.dma_start(out=mk4[:].rearrange("p a n -> p (a n)"), in_=mk4_d[:, :]), writes=['mk4'])
            S.dma(lambda e: e.dma_start(out=mn1[:].rearrange("p a n -> p (a n)"), in_=mn1_d[:, :]), writes=['mn1'])
            S.dma(lambda e: e.dma_start(out=bm[:], in_=bm_d[:, :]), writes=['bm'])
            S.dma(lambda e: e.dma_start(out=rst[:], in_=rst_d[:, :]), writes=['rst'])
            S.op('pool', lambda e: e.memset(ones64[:], 1.0), writes=['ones64'])
            S.op('pool', lambda e: e.memset(tiny[:], 1e-24), writes=['tiny'])
            for i in range(3):
                S.op('dve', lambda e, i=i: e.tensor_scalar(out=rpd[:, :, 2 * i], in0=rp[:, :, i], scalar1=0.5, scalar2=None, op0=ALU.mult),
                     reads=['rp'], writes=['rpd'])
                S.op('dve', lambda e, i=i: e.tensor_scalar(out=rpd[:, :, 2 * i + 1], in0=rp[:, :, i], scalar1=-1.0, scalar2=1.0,
                                                          op0=ALU.mult, op1=ALU.add), reads=['rp'], writes=['rpd'])
                S.op('dve', lambda e, i=i: e.tensor_scalar(out=lmd[:, 2 * i:2 * i + 1], in0=lmu[:, i:i + 1], scalar1=0.5, scalar2=None, op0=ALU.mult),
                     reads=['lmu'], writes=['lmd'])
                S.op('dve', lambda e, i=i: e.tensor_scalar(out=lmd[:, 2 * i + 1:2 * i + 2], in0=lmu[:, i:i + 1], scalar1=-1.0, scalar2=1.0,
                                                          op0=ALU.mult, op1=ALU.add), reads=['lmu'], writes=['lmd'])
            S.op('dve', lambda e: e.tensor_scalar(out=rpd[:, :, 6], in0=rp[:, :, 4], scalar1=-1.0, scalar2=1.0, op0=ALU.mult, op1=ALU.add),
                 reads=['rp'], writes=['rpd'])

            def bc(t2):
                return t2.unsqueeze(2).to_broadcast([64, 8, 128])

            pin = [sb(ph, "pin%d" % i, [64, 8, 130], F32) for i in range(3)]
            plo = [sb(ph, "plo%d" % i, [128, 130], F32) for i in range(3)]
            tS = t32("tS"); xr = t32("xr"); xk = t32("xk"); xv = t32("xv")
            xlo = t32("xlo", (128, 3, 128)); twl = sb(ph, "twl", [64, 128], BF16); xalb = sb(ph, "xalb", [64, 128], BF16)
            glsb = sb(ph, "glsb", [128, 128], BF16)
            sqb = sb(ph, "sqb", [64, 1024], BF16); kk = t32("kk")
            sg = t32("sg"); ad = t32("ad"); cs = t32("cs"); ex = t32("ex"); E1 = t32("E1"); E2s = [t32("E2_0"), t32("E2_1")]; E3 = t32("E3")
            bb = t32("bb"); t1 = t32("t1"); kd = bb; bt32 = t32("bt32"); kt32 = t32("kt32"); rkb = sqb; kkk = t1; rs = E1; csb = bt32; bv = kt32
            bvt = t32("bvt", (128, 512)); gtok = t32("gtok", (128, 512)); glS = t32("glS", (128, 128))
            opTs = [{n: sb(ph, "op%d_" % q_ + n, [64, 1024], BF16) for n in ("a", "r", "b", "k", "bh", "kh", "v")} for q_ in range(2)]
            N1Ta, N1a, IN1Ta, N2a, N2Ta, IN2Ta, N4a, N4Ta, IN4Ta, IN8Ta, AakTa, ArbTa, ArkTa = [
                sb(ph, "ba%d" % i, [128, 8, 128], BF16) for i in range(13)]
            TMa = sb(ph, "TMa", [128, 8, 5, 64], BF16)
            Xa = [sb(ph, "Xa%d" % i, [128, 8, 128], BF16) for i in range(2)]
            Bbd = sb(ph, "Bbd", [128, 512], BF16); Ubd = sb(ph, "Ubd", [128, 512], BF16); Vbd = sb(ph, "Vbd", [128, 512], BF16)
            GTs = sb(ph, "GTs", [64, 512], BF16); Es = t32("Es", (64, 512)); QTs = sb(ph, "QTs", [64, 128], BF16)
            Y0s = t32("Y0s", (128, 64)); ytmp = gtok; yc = t32("yc", (128, 64))
            Yblk = t32("Yblk", (128, 8, 64))
            H = t32("H", (64, 512)); Hb = sb(ph, "Hb", [64, 512], BF16); Ht = t32("Ht", (64, 512))

            def view_hct(t):
                return t[:, :]

            def view_out(t):
                return t[:, :]

            def g16(t):
                return t[:, :].rearrange("k (g t) -> k g t", t=16)

            def chv(t, c):
                return t[:, :].rearrange("k (h c t) -> k h c t", h=8, c=8)[:, :, c, :]

            for (dst, src, nm, rows) in ((decupb, decup_d, 'decupb', 64), (iclupb, iclup_d, 'iclupb', 64), (gateupb, gateup_d, 'gateupb', 128)):
                stg_, sk_ = (bt32, 'bt32') if rows == 64 else (bvt, 'bvt')
                S.dma(lambda e, src=src, stg_=stg_: e.dma_start(out=stg_[:, :], in_=src[:, :]), writes=[sk_])
                dv = dst[:].rearrange("p a n -> p (a n)") if rows == 64 else dst[:]
                CP('dve', dv, stg_[:, :], [sk_], [nm])
            blocks = []
            nblk = int(os.environ.get('K_RBLK', 99))
            for d_ in range(2):
                cb_ = [0, 1] if d_ == 0 else [1, 0]
                lb_ = list(range(2, NT)) if d_ == 0 else list(range(NT - 1, 1, -1))
                blocks += [(d_, b_, j_ == 0) for j_, b_ in enumerate((cb_ + lb_)[:nblk])]

            def emit_prep(idx):
                d, blk, first = blocks[idx]
                opT = opTs[idx % 2]; E2 = E2s[idx % 2]; e2k = 'E2_%d' % (idx % 2); opk = 'o%d_' % (idx % 2)

                u0 = blk * 128
                is_ctx = blk < 2
                seq_lo, seq_hi = (0, TC) if is_ctx else (TC, TT)
                lo = max(u0 - 1, seq_lo); hi = min(u0 + 129, seq_hi)
                c_lo = lo - (u0 - 1); c_hi = c_lo + (hi - lo)
                for i in range(3):
                    if c_lo > 0:
                        S.op('pool', lambda e, i=i: e.memset(pin[i][:, :, 0:1], 0.0), writes=['pin%d' % i])
                    if c_hi < 130:
                        S.op('pool', lambda e, i=i: e.memset(pin[i][:, :, 129:130], 0.0), writes=['pin%d' % i])
                    S.dma(lambda e, i=i, lo=lo, hi=hi, c_lo=c_lo, c_hi=c_hi: e.dma_start(
                        out=pin[i][:, :, c_lo:c_hi],
                        in_=rwT[i * 512:(i + 1) * 512, lo:hi].rearrange("(h k) t -> k h t", k=64)), reads=['rwT'], writes=['pin%d' % i])
                for i, (r0, nr) in enumerate(((1536, 64), (1600, 64), (1664, 128))):
                    if c_lo > 0:
                        S.op('pool', lambda e, i=i: e.memset(plo[i][:, 0:1], 0.0), writes=['plo%d' % i])
                    if c_hi < 130:
                        S.op('pool', lambda e, i=i: e.memset(plo[i][:, 129:130], 0.0), writes=['plo%d' % i])
                    S.dma(lambda e, i=i, r0=r0, nr=nr, lo=lo, hi=hi, c_lo=c_lo, c_hi=c_hi: e.dma_start(
                        out=plo[i][0:nr, c_lo:c_hi], in_=rwT[r0:r0 + nr, lo:hi]), reads=['rwT'], writes=['plo%d' % i])
                for i, xo in enumerate((xr, xk, xv)):
                    xo3 = xo[:, :].rearrange("k (h t) -> k h t", h=8)
                    ts3 = tS[:, :].rearrange("k (h t) -> k h t", h=8)
                    TTo('pool', ts3, pin[i][:, :, 0:128], pin[i][:, :, 2:130], ALU.add, ['pin%d' % i], ['tS'])
                    TTo('pool', ts3, ts3, bc(rpd[:, :, 2 * i]), ALU.mult, ['tS', 'rpd'], ['tS'])
                    TTo('dve', xo3, pin[i][:, :, 1:129], bc(rpd[:, :, 2 * i + 1]), ALU.mult, ['pin%d' % i, 'rpd'], ['x%d' % i])
                    TTo('dve', xo3, xo3, ts3, ALU.add, ['x%d' % i, 'tS'], ['x%d' % i])
                for i, nr in enumerate((64, 64, 128)):
                    S.op('pool', lambda e, i=i, nr=nr: e.tensor_tensor(out=tS[0:nr, 0:128] if nr == 64 else glS[:, 0:128],
                                                                     in0=plo[i][0:nr, 0:128], in1=plo[i][0:nr, 2:130], op=ALU.add),
                         reads=['plo%d' % i], writes=['tS' if nr == 64 else 'glS'])
                    S.op('dve', lambda e, i=i, nr=nr: e.tensor_scalar(out=xlo[0:nr, i, :], in0=plo[i][0:nr, 1:129],
                                                                    scalar1=lmd[0:nr, 2 * i + 1:2 * i + 2], scalar2=None, op0=ALU.mult),
                         reads=['plo%d' % i, 'lmd'], writes=['xlo'])
                    S.op('dve', lambda e, i=i, nr=nr: e.scalar_tensor_tensor(
                        out=xlo[0:nr, i, :], in0=(tS[0:nr, 0:128] if nr == 64 else glS[:, 0:128]), scalar=lmd[0:nr, 2 * i:2 * i + 1],
                        in1=xlo[0:nr, i, :], op0=ALU.mult, op1=ALU.add),
                        reads=['tS' if nr == 64 else 'glS', 'lmd', 'xlo'], writes=['xlo'])
                ACTF(twl[:], xlo[0:64, 0, :], AF.Tanh, ['xlo'], ['twl'])
                CP('pool', xalb[:], xlo[0:64, 1, :], ['xlo'], ['xalb'])
                k3 = lambda t: t[:, :].rearrange("k (h t) -> k h t", h=8)
                TTo('pool', k3(kkk), k3(xk), bc(rp[:, :, 3]), ALU.mult, ['x1', 'rp'], ['t1'])
                ACTF(sqb[:], kkk[:], AF.Square, ['t1'], ['sqb'])
                PP = PS[0:64, 0:1024]
                for hf in range(2):
                    MM(PS[0:64, hf * 512:(hf + 1) * 512], ones64[:], sqb[:, hf * 512:(hf + 1) * 512], ['ones64', 'sqb'], ['P0'])
                ACTF(rs[:], PP, AF.Ln, ['P0', 'tiny'], ['E1'], bias=tiny[:])
                ACTF(rs[:], rs[:], AF.Exp, ['E1'], ['E1'], scale=-0.5)
                TTo('dve', kk[:], kkk[:], rs[:], ALU.mult, ['t1', 'E1'], ['kk'])
                for h in range(8):
                    MM(PS[0:64, h * 128:(h + 1) * 128], decupb[:, d, h * 64:(h + 1) * 64], twl[:], ['decupb', 'twl'], ['P0'])
                TTo('dve', k3(sg), PP.rearrange("k (h t) -> k h t", h=8), bc(rp[:, :, 6 + d]), ALU.add, ['P0', 'rp'], ['sg'])
                ACTF(sg[:], sg[:], AF.Sigmoid, ['sg'], ['sg'])
                for h in range(8):
                    MM(PS[0:64, h * 128:(h + 1) * 128], iclupb[:, d, h * 64:(h + 1) * 64], xalb[:], ['iclupb', 'xalb'], ['P0'])
                TTo('dve', k3(ad), PP.rearrange("k (h t) -> k h t", h=8), bc(rp[:, :, 8 + d]), ALU.add, ['P0', 'rp'], ['ad'])
                ACTF(ad[:], ad[:], AF.Sigmoid, ['ad'], ['ad'])
                S.op('dve', lambda e: e.tensor_tensor_scan(out=cs[:], data0=rst[:], data1=sg[:], initial=0.0, op0=ALU.mult, op1=ALU.add),
                     reads=['rst', 'sg'], writes=['cs'])
                csf = cs
                if d == 1:
                    TTo('pool', ex[:], sg[:], cs[:], ALU.subtract, ['sg', 'cs'], ['ex'])
                    csv = cs[:, :].rearrange("k (g t) -> k g t", t=16)
                    TTo('dve', csb[:, :].rearrange("k (g t) -> k g t", t=16), ex[:, :].rearrange("k (g t) -> k g t", t=16),
                       csv[:, :, 15:16].to_broadcast([64, 64, 16]), ALU.add, ['ex', 'cs'], ['bt32'])
                    csf = csb
                ACTF(E2[:], csf[:], AF.Exp, ['cs', 'bt32'], [e2k], scale=DEC_C)
                TTo('pool', ex[:], csf[:], sg[:], ALU.subtract, ['cs', 'bt32', 'sg'], ['ex'])
                ACTF(E1[:], ex[:], AF.Exp, ['ex'], ['E1'], scale=DEC_C)
                S.op('dve', lambda e: e.reciprocal(out=E3[:], in_=E2[:]), reads=[e2k], writes=['E3'])
                tsel = 15 if d == 0 else 0
                def cm(t, h):
                    return t[:, :].rearrange("k (c h t) -> k c h t", c=8, h=8)[:, :, h, :]

                def hm(t, h):
                    return t[:, :].rearrange("k (h c t) -> k h c t", h=8, c=8)[:, h, :, :]
                TTo('pool', bb[:], kk[:], ad[:], ALU.mult, ['kk', 'ad'], ['bb'])
                TTo('dve', bt32[:], bb[:], E3[:], ALU.mult, ['bb', 'E3'], ['bt32'])
                TTo('pool', k3(t1), k3(ad), bc(rp[:, :, 4]), ALU.mult, ['ad', 'rp'], ['t1'])
                TTo('dve', k3(t1), k3(t1), bc(rpd[:, :, 6]), ALU.add, ['t1', 'rpd'], ['t1'])
                TTo('pool', kd[:], xk[:], t1[:], ALU.mult, ['x1', 't1'], ['bb'])
                TTo('dve', kt32[:], kd[:], E3[:], ALU.mult, ['bb', 'E3'], ['kt32'])
                for h in range(8):
                    pch = hm(E2, h)[:, :, tsel:tsel + 1].to_broadcast([64, 8, 16])
                    S.op('dve', lambda e, h=h: e.scalar_tensor_tensor(out=cm(opT['a'], h), in0=hm(kk, h), scalar=-1.0, in1=hm(E1, h),
                                                                     op0=ALU.mult, op1=ALU.mult), reads=['kk', 'E1'], writes=[opk + 'a'])
                    TTo('pool', cm(opT['r'], h), hm(xr, h), hm(E2, h), ALU.mult, ['x0', e2k], [opk + 'r'])
                    CP('act', cm(opT['b'], h), hm(bt32, h), ['bt32'], [opk + 'b'])
                    TTo('pool', cm(opT['bh'], h), hm(bt32, h), pch, ALU.mult, ['bt32', e2k], [opk + 'bh'])
                    CP('act', cm(opT['k'], h), hm(kt32, h), ['kt32'], [opk + 'k'])
                    TTo('dve', cm(opT['kh'], h), hm(kt32, h), pch, ALU.mult, ['kt32', e2k], [opk + 'kh'])
                    CP('act', cm(opT['v'], h), hm(xv, h), ['x2'], [opk + 'v'])
                if d == 0 and not is_ctx:
                    ul = u0 - TC
                    TTo('pool', k3(t1), k3(xr), bc(rp[:, :, 5]), ALU.mult, ['x0', 'rp'], ['t1'])
                    TTo('dve', rkb[:], t1[:], xk[:], ALU.mult, ['t1', 'x1'], ['sqb'])
                    for hf in range(2):
                        MM(PS[0:64, hf * 512:(hf + 1) * 512], ones64[:], rkb[:, hf * 512:(hf + 1) * 512], ['ones64', 'sqb'], ['P0'])
                    TTo('dve', bv[:], PP, xv[:], ALU.mult, ['P0', 'x2'], ['kt32'])
                    for h in range(8):
                        TR(PS[:, 512 + h * 64:512 + (h + 1) * 64], bv[:, h * 128:(h + 1) * 128], identf[0:64, 0:64], ['kt32', 'identf'], ['P0'])
                    CP('dve', bvt[:], PS[:, 512:1024], ['P0'], ['bvt'])
                    S.dma(lambda e, ul=ul: e.dma_start(out=bonD[ul:ul + 128, :], in_=bvt[:]), reads=['bvt'], writes=['bonD'])
                    ACTF(glsb[:], xlo[:, 2, :], AF.Sigmoid, ['xlo'], ['glsb'])
                    MM(PS[:, 512:1024], glsb[:], gateupb[:], ['glsb', 'gateupb'], ['P0'])
                    CP('dve', gtok[:], PS[:, 512:1024], ['P0'], ['gtok'])
                    S.dma(lambda e, ul=ul: e.dma_start(out=gD[ul:ul + 128, :], in_=gtok[:]), reads=['gtok'], writes=['gD'])

            def emit_chunks(idx):
                d, blk, first = blocks[idx]
                opT = opTs[idx % 2]; E2 = E2s[idx % 2]; e2k = 'E2_%d' % (idx % 2); opk = 'o%d_' % (idx % 2)
                u0 = blk * 128
                is_ctx = blk < 2
                tsel = 15 if d == 0 else 0
                if first:
                    S.op('pool', lambda e: e.memset(H[:], 0.0), writes=['H'])
                    S.op('pool', lambda e: e.memset(Hb[:], 0.0), writes=['Hb'])

                PA_, PB_, PC_, PD_ = (PS[:, 0:1024], PS[:, 1024:2048], PS[:, 2048:3072], PS[:, 3072:4096])
                kPB, kPC, kPD = ['P2', 'P3'], ['P4', 'P5'], ['P6', 'P7']
                c8 = lambda ap: ap.rearrange("p (c n) -> p c n", c=8)
                def bc8(m):
                    return m.unsqueeze(1).to_broadcast([128, 8, 128])
                MSd = mk4[:, d, 0:128]; MId = mk4[:, d, 128:256]; MStd = mn1[:, d, :]
                def ch(name, c):
                    return opT[name][:, c * 128:(c + 1) * 128]
                for c in range(8):
                    MM(PB_[:, c * 128:(c + 1) * 128], ch('b', c), ch('a', c), [opk + 'b', opk + 'a'], kPB)
                for c in range(8):
                    MM(PC_[:, c * 128:(c + 1) * 128], ch('a', c), ch('b', c), [opk + 'a', opk + 'b'], kPC)
                for c in range(8):
                    MM(PD_[:, c * 128:(c + 1) * 128], ch('k', c), ch('a', c), [opk + 'k', opk + 'a'], kPD)
                TTo('dve', N1Ta[:], c8(PB_), bc8(MSd), ALU.mult, kPB + ['mk4'], ['N1Ta'])
                TTo('dve', N1a[:], c8(PC_), bc8(MStd), ALU.mult, kPC + ['mn1'], ['N1a'])
                TTo('dve', AakTa[:], c8(PD_), bc8(MSd), ALU.mult, kPD + ['mk4'], ['AakTa'])
                TTo('pool', IN1Ta[:], N1Ta[:], bc8(identb[:, :]), ALU.add, ['N1Ta', 'identb'], ['IN1Ta'])
                PBb = PB_.bitcast(BF16)
                for c in range(8):
                    for si, nm_ in ((0, 'a'), (1, 'v'), (2, 'bh'), (3, 'kh')):
                        TR(PBb[:, c * 256 + si * 64:c * 256 + (si + 1) * 64], ch(nm_, c), identb[0:64, 0:64], [opk + nm_, 'identb'], kPB)
                PBb4 = PBb.rearrange("p (c s n) -> p c s n", c=8, s=4)
                CP('dve', TMa[:, :, 0, :], PBb4[:, :, 0, :], kPB, ['TMa'])
                CP('dve', TMa[:, :, 2:5, :].rearrange("p c s n -> p c (s n)"), PBb.rearrange("p (c n) -> p c n", c=8)[:, :, 64:256], kPB, ['TMa'])
                for c in range(8):
                    MM(PC_[:, c * 128:(c + 1) * 128], N1Ta[:, c, :], N1a[:, c, :], ['N1Ta', 'N1a'], kPC)
                for c in range(8):
                    MM(PD_[:, c * 128:(c + 1) * 128], N1a[:, c, :], N1Ta[:, c, :], ['N1Ta', 'N1a'], kPD)
                CP('act', N2a[:], c8(PC_), kPC, ['N2a'])
                CP('dve', N2Ta[:], c8(PD_), kPD, ['N2Ta'])
                TTo('pool', IN2Ta[:], N2Ta[:], bc8(identb[:, :]), ALU.add, ['N2Ta', 'identb'], ['IN2Ta'])
                for c in range(8):
                    MM(PB_[:, c * 64:(c + 1) * 64], AakTa[:, c, :], TMa[:, c, 2, :], ['AakTa', 'TMa'], kPB)
                CP('dve', TMa[:, :, 1, :], PB_[:, 0:512].rearrange("p (c n) -> p c n", c=8), kPB, ['TMa'])
                for c in range(8):
                    MM(PC_[:, c * 128:(c + 1) * 128], N2Ta[:, c, :], N2a[:, c, :], ['N2Ta', 'N2a'], kPC)
                for c in range(8):
                    MM(PD_[:, c * 128:(c + 1) * 128], N2a[:, c, :], N2Ta[:, c, :], ['N2Ta', 'N2a'], kPD)
                CP('act', N4a[:], c8(PC_), kPC, ['N4a'])
                CP('dve', N4Ta[:], c8(PD_), kPD, ['N4Ta'])
                TTo('pool', IN4Ta[:], N4Ta[:], bc8(identb[:, :]), ALU.add, ['N4Ta', 'identb'], ['IN4Ta'])
                for c in range(8):
                    MM(PB_[:, c * 128:(c + 1) * 128], N4a[:, c, :], N4Ta[:, c, :], ['N4a', 'N4Ta'], kPB)
                TTo('dve', IN8Ta[:], c8(PB_), bc8(identf[:, :]), ALU.add, kPB + ['identf'], ['IN8Ta'])
                if not is_ctx:
                    for c in range(8):
                        MM(PC_[:, c * 128:(c + 1) * 128], ch('b', c), ch('r', c), [opk + 'b', opk + 'r'], kPC)
                    for c in range(8):
                        MM(PD_[:, c * 128:(c + 1) * 128], ch('k', c), ch('r', c), [opk + 'k', opk + 'r'], kPD)
                    TTo('dve', ArbTa[:], c8(PC_), bc8(MId), ALU.mult, kPC + ['mk4'], ['ArbTa'])
                    TTo('dve', ArkTa[:], c8(PD_), bc8(MId), ALU.mult, kPD + ['mk4'], ['ArkTa'])
                xsrc = None
                for li, (INa, ik) in enumerate(((IN8Ta, 'IN8Ta'), (IN4Ta, 'IN4Ta'), (IN2Ta, 'IN2Ta'), (IN1Ta, 'IN1Ta'))):
                    Pq, kq = (PB_, kPB) if li % 2 == 0 else (PC_, kPC)
                    for c in range(8):
                        rhs_ = TMa[:, c, 0:2, :].rearrange("p a n -> p (a n)") if xsrc is None else xsrc[:, c, :]
                        MM(Pq[:, c * 128:(c + 1) * 128], INa[:, c, :], rhs_, [ik, 'TMa' if xsrc is None else xk_], kq)
                    Xn = Xa[li % 2]; xk_ = 'Xa%d' % (li % 2)
                    CP('dve' if li % 2 else 'act', Xn[:], c8(Pq), kq, [xk_])
                    xsrc = Xn
                B3 = PS[:, 1536:2048]; B4 = PS[:, 2048:2560]; B5 = PS[:, 2560:3072]; B6 = PS[:, 3072:3584]; B7 = PS[:, 3584:4096]
                bm3 = bm[:, :].rearrange("p (h n) -> p h n", h=8)
                nch = int(os.environ.get('K_RCH', 8))
                for c in (list(range(8)) if d == 0 else list(range(7, -1, -1)))[:nch]:
                    Wc = xsrc[:, c, 0:64]; U0 = xsrc[:, c, 64:128]
                    Bh_ = TMa[:, c, 3, :]; Kh_ = TMa[:, c, 4, :]; Vt_ = TMa[:, c, 2, :]
                    for dst_, src_, rk_, wk_ in ((Bbd, Bh_, 'TMa', 'Bbd'), (Ubd, U0, xk_, 'Ubd'), (Vbd, Vt_, 'TMa', 'Vbd')):
                        TTo('pool', dst_[:, :].rearrange("p (h n) -> p h n", h=8), src_.unsqueeze(1).to_broadcast([128, 8, 64]), bm3, ALU.mult,
                            [rk_, 'bm'], [wk_])
                    MM(B4[0:64, :], Wc, Bbd[:], [xk_, 'Bbd'], ['P4'])
                    CP('act', GTs[:], B4[0:64, :], ['P4'], ['GTs'])
                    MM(B5[0:64, :], Bh_, Ubd[:], ['TMa', 'Ubd'], ['P5'], start=True, stop=False)
                    MM(B5[0:64, :], Kh_, Vbd[:], ['TMa', 'Vbd'], ['P5'], start=False, stop=True)
                    CP('act', Es[:], B5[0:64, :], ['P5'], ['Es'])
                    if not is_ctx:
                        MM(B6[0:64, 0:128], Wc, ArbTa[:, c, :], [xk_, 'ArbTa'], ['P6'])
                        TTo('dve', QTs[:], B6[0:64, 0:128], ch('r', c), ALU.add, ['P6', opk + 'r'], ['QTs'])
                        MM(B6[:, 128:192], ArbTa[:, c, :], U0, ['ArbTa', xk_], ['P6'], start=True, stop=False)
                        MM(B6[:, 128:192], ArkTa[:, c, :], Vt_, ['ArkTa', 'TMa'], ['P6'], start=False, stop=True)
                        CP('dve', Y0s[:], B6[:, 128:192], ['P6'], ['Y0s'])
                        MM(B7, QTs[:], Hb[:], ['QTs', 'Hb'], ['P7'])
                        TTo('dve', ytmp[:], B7, bm[:], ALU.mult, ['P7', 'bm'], ['gtok'])
                        S.op('dve', lambda e: e.tensor_reduce(out=yc[:], in_=ytmp[:, :].rearrange("p (h v) -> p v h", h=8), axis=AX.X, op=ALU.add),
                             reads=['gtok'], writes=['yc'])
                        TTo('pool', Yblk[:, c, :], yc[:], Y0s[:], ALU.add, ['yc', 'Y0s'], ['Yblk'])
                    for h in range(8):
                        MM(B3[0:64, h * 64:(h + 1) * 64], GTs[:, h * 64:(h + 1) * 64], Hb[:, h * 64:(h + 1) * 64], ['GTs', 'Hb'], ['P3'])
                    PCc = chv(E2, c)[:, :, tsel:tsel + 1].to_broadcast([64, 8, 64])
                    TTo('pool', Ht[:, :].rearrange("k (h v) -> k h v", h=8), H[:, :].rearrange("k (h v) -> k h v", h=8), PCc, ALU.mult,
                        ['H', e2k], ['Ht'])
                    TTo('pool', Ht[:], Ht[:], Es[:], ALU.add, ['Ht', 'Es'], ['Ht'])
                    TTo('dve', H[:], Ht[:], B3[0:64, :], ALU.add, ['Ht', 'P3'], ['H'])
                    CP('act', Hb[:], H[:], ['H'], ['Hb'])
                    S.drain(S.pend, (len(S.pend) + 7) // 8)
                if not is_ctx:
                    ul = u0 - TC
                    for h in range(8):
                        S.dma(lambda e, h=h, ul=ul, d=d: e.dma_start(
                            out=ydir[d, ul:ul + 128, h * 64:(h + 1) * 64].rearrange("(c t) v -> t c v", t=16),
                            in_=Yblk[h * 16:(h + 1) * 16, :, :]), reads=['Yblk'], writes=['ydir'])

            S.capture = []
            emit_prep(0)
            pend = S.capture; S.capture = None
            S.drain(pend, len(pend))
            for idx in range(len(blocks)):
                pend = []
                if idx + 1 < len(blocks):
                    S.capture = []
                    emit_prep(idx + 1)
                    pend = S.capture; S.capture = None
                S.pend = pend
                emit_chunks(idx)
                S.drain(pend, len(pend))

            if dbg:
                oy = dout("dbg_y", [2 * TL, 512]); ob = dout("dbg_bon", [TL, 512]); og = dout("dbg_g", [TL, 512])
                S.dma(lambda e: e.dma_start(out=oy[:, :], in_=ydir.rearrange("d t n -> (d t) n")), reads=['ydir'])
                S.dma(lambda e: e.dma_start(out=ob[:, :], in_=bonD[:, :]), reads=['bonD'])
                S.dma(lambda e: e.dma_start(out=og[:, :], in_=gD[:, :]), reads=['gD'])
                oH = dout("dbg_H", [64, 512])
                S.dma(lambda e: e.dma_start(out=oH[:, :], in_=H[:]), reads=['H'])
            S.barrier()
            S.flush()
        if stop_after <= 3:
            return nc, dbg_outs

        with contextlib.ExitStack() as ph:
            woutb = sb(ph, "woutb", [128, 8, D], BF16)
            attT_all = sb(ph, "attT_c", [128, 4, TL], BF16)
            S.dma(lambda e: e.dma_start(out=attT_all[:].rearrange("p a t -> p (a t)"), in_=attD[:, :]), reads=['attD'], writes=['attT_all'])
            cst = {}
            for nm, src in (("gt1", modd[0:1, 2048:3072]), ("sh2", modd[0:1, 3072:4096]), ("sc2p", modd[0:1, 4096:5120]),
                            ("ln1g", ln1_d[0:1, :]), ("ln1b", ln1_d[1:2, :])):
                cst[nm] = sb(ph, nm, [128, D], F32)
                S.dma(lambda e, nm=nm, src=src: e.dma_start(out=cst[nm][:], in_=src.partition_broadcast(128)), reads=['modd'], writes=[nm])
            for nm, row in (("lnxg", 0), ("lnxb", 1)):
                cst[nm] = sb(ph, nm, [128, 512], F32)
                S.dma(lambda e, nm=nm, row=row: e.dma_start(out=cst[nm][:], in_=lnx_d[row:row + 1, :].partition_broadcast(128)), writes=[nm])
            wst2 = [sb(ph, "wst2_%d" % i, [128, D], F32) for i in range(2)]
            for j in range(8):
                w = wst2[j % 2]; wk = 'wst2_%d' % (j % 2)
                S.dma(lambda e, w=w, j=j: e.dma_start(out=w[:], in_=wout_d[j * 128:(j + 1) * 128, :]), writes=[wk])
                CP('pool' if j % 2 else 'dve', woutb[:, j, :], w[:], [wk], ['woutb'])
            rwf = sb(ph, "rwf", [128, 8, 16], F32)
            S.dma(lambda e: e.dma_start(out=rwf[:], in_=rw_d.rearrange("(j p) n -> p j n", p=128)), writes=['rwf'])
            gneps = sb(ph, "gneps", [128, 1], F32)
            S.op('pool', lambda e: e.memset(gneps[:], 64e-5), writes=['gneps'])
            yf = sb(ph, "yf", [128, 512], F32); yb = sb(ph, "yb", [128, 512], F32)
            bon = sb(ph, "bon", [128, 512], F32); gg = sb(ph, "gg", [128, 512], F32)
            ysum = sb(ph, "ysum", [128, 512], F32); ysq = sb(ph, "ysq", [128, 512], F32)
            gst = sb(ph, "gst", [128, 8], F32); gvar = sb(ph, "gvar", [128, 8], F32)
            rwob = sb(ph, "rwob", [128, 512], BF16); rwoT = sb(ph, "rwoT", [128, 4, 128], BF16)
            xin_t = sb(ph, "xin_t", [128, D], F32); tres = sb(ph, "tres", [128, D], F32)
            x1t = sb(ph, "x1t", [128, D], F32); h2f = sb(ph, "h2f", [128, D], F32); h2b = sb(ph, "h2b", [128, D], BF16)
            h2T = sb(ph, "h2T", [128, 8, 128], F32)
            stats = sb(ph, "statsC", [128, 2, 6], F32); mv = sb(ph, "mvC", [128, 2], F32)
            rstd = sb(ph, "rstdC", [128, 1], F32); nb = sb(ph, "nbC", [128, 1], F32)
            lntC = (stats, mv, rstd, nb, 'C')
            lmax = sb(ph, "lmax", [128, 1], F32); lex = sb(ph, "lex", [128, 16], F32); lsum = sb(ph, "lsum", [128, 1], F32)

            def v8(t):
                return t[:, :].rearrange("p (h v) -> p h v", h=8)

            def b8(t):
                return t[:, :].unsqueeze(2).to_broadcast([128, 8, 64])
            for i in range(int(os.environ.get('K_CT', 32))):
                t0 = i * 128
                S.dma(lambda e, t0=t0: e.dma_start(out=yf[:], in_=ydir[0, t0:t0 + 128, :]), reads=['ydir'], writes=['yf'])
                S.dma(lambda e, t0=t0: e.dma_start(out=yb[:], in_=ydir[1, t0:t0 + 128, :]), reads=['ydir'], writes=['yb'])
                S.dma(lambda e, t0=t0: e.dma_start(out=bon[:], in_=bonD[t0:t0 + 128, :]), reads=['bonD'], writes=['bon'])
                S.dma(lambda e, t0=t0: e.dma_start(out=gg[:], in_=gD[t0:t0 + 128, :]), reads=['gD'], writes=['gg'])
                S.dma(lambda e, t0=t0: e.dma_start(out=xin_t[:], in_=x_d[t0:t0 + 128, :]), writes=['xin_t'])
                TTo('pool', ysum[:], yf[:], yb[:], ALU.add, ['yf', 'yb'], ['ysum'])
                S.op('dve', lambda e: e.tensor_reduce(out=gst[:], in_=v8(ysum), axis=AX.X, op=ALU.add), reads=['ysum'], writes=['gst'])
                S.op('dve', lambda e: e.tensor_scalar(out=gst[:], in0=gst[:], scalar1=-1.0 / 64, scalar2=None, op0=ALU.mult),
                     reads=['gst'], writes=['gst'])
                TTo('dve', v8(ysum), v8(ysum), b8(gst), ALU.add, ['ysum', 'gst'], ['ysum'])
                ACTF(ysq[:], ysum[:], AF.Square, ['ysum'], ['ysq'])
                S.op('dve', lambda e: e.tensor_reduce(out=gvar[:], in_=v8(ysq), axis=AX.X, op=ALU.add), reads=['ysq'], writes=['gvar'])
                ACTF(gvar[:], gvar[:], AF.Ln, ['gvar', 'gneps'], ['gvar'], scale=1.0 / 64, bias=gneps[:])
                ACTF(gvar[:], gvar[:], AF.Exp, ['gvar'], ['gvar'], scale=-0.5)
                TTo('dve', v8(ysum), v8(ysum), b8(gvar), ALU.mult, ['ysum', 'gvar'], ['ysum'])
                TTo('pool', ysum[:], ysum[:], cst['lnxg'][:], ALU.mult, ['ysum', 'lnxg'], ['ysum'])
                TTo('dve', ysum[:], ysum[:], cst['lnxb'][:], ALU.add, ['ysum', 'lnxb'], ['ysum'])
                TTo('pool', ysum[:], ysum[:], bon[:], ALU.add, ['ysum', 'bon'], ['ysum'])
                TTo('dve', rwob[:], ysum[:], gg[:], ALU.mult, ['ysum', 'gg'], ['rwob'])
                pr_ = P[0][:].bitcast(BF16)
                for j in range(4):
                    TR(pr_[:, j * 128:(j + 1) * 128], rwob[:, j * 128:(j + 1) * 128], identb[:], ['rwob', 'identb'], ['P0'])
                CP('dve', rwoT[:], pr_[:, 0:512].rearrange("p (j t) -> p j t", j=4), ['P0'], ['rwoT'])
                for half in range(2):
                    ob = PS[:, 1024 + half * 512:1024 + (half + 1) * 512]
                    for j in range(8):
                        lt = attT_all[:, j, t0:t0 + 128] if j < 4 else rwoT[:, j - 4, :]
                        MM(ob, lt, woutb[:, j, half * 512:(half + 1) * 512], ['attT_all', 'rwoT', 'woutb'], ['P2'], start=(j == 0), stop=(j == 7))
                TTo('dve', tres[:], PS[:, 1024:2048], cst['gt1'][:], ALU.mult, ['P2', 'gt1'], ['tres'])
                S.op('dve', lambda e: e.scalar_tensor_tensor(out=tres[:], in0=xin_t[:], scalar=ALPHA, in1=tres[:], op0=ALU.mult, op1=ALU.add),
                     reads=['xin_t', 'tres'], writes=['tres'])
                ln_stats(lntC, tres, 'tres', eps5, 'eps5')
                S.op('act', lambda e: e.activation(out=x1t[:], in_=tres[:], func=AF.Identity, bias=nb[:], scale=rstd[:]),
                     reads=['tres', 'Cnb', 'Crstd'], writes=['x1t'])
                TTo('pool', x1t[:], x1t[:], cst['ln1g'][:], ALU.mult, ['x1t', 'ln1g'], ['x1t'])
                TTo('dve', x1t[:], x1t[:], cst['ln1b'][:], ALU.add, ['x1t', 'ln1b'], ['x1t'])
                S.dma(lambda e, t0=t0: e.dma_start(out=x1D[t0:t0 + 128, :], in_=x1t[:]), reads=['x1t'], writes=['x1D'])
                ln_stats(lntC, x1t, 'x1t', eps5, 'eps5')
                S.op('act', lambda e: e.activation(out=h2f[:], in_=x1t[:], func=AF.Identity, bias=nb[:], scale=rstd[:]),
                     reads=['x1t', 'Cnb', 'Crstd'], writes=['h2f'])
                TTo('pool', h2f[:], h2f[:], cst['sc2p'][:], ALU.mult, ['h2f', 'sc2p'], ['h2f'])
                TTo('dve', h2f[:], h2f[:], cst['sh2'][:], ALU.add, ['h2f', 'sh2'], ['h2f'])
                CP('pool', h2b[:], h2f[:], ['h2f'], ['h2b'])
                S.dma(lambda e, t0=t0: e.dma_start(out=h2D[t0:t0 + 128, :], in_=h2b[:]), reads=['h2b'], writes=['h2D'])
                for j in range(8):
                    TR(PS[:, 2048 + j * 128:2048 + (j + 1) * 128], h2f[:, j * 128:(j + 1) * 128], identf[:], ['h2f', 'identf'], ['P4'])
                CP('dve', h2T[:].rearrange("p j t -> p (j t)"), PS[:, 2048:3072], ['P4'], ['h2T'])
                for j in range(8):
                    MM(PS[:, 3072:3088], h2T[:, j, :], rwf[:, j, :], ['h2T', 'rwf'], ['P6'], start=(j == 0), stop=(j == 7))
                S.op('dve', lambda e: e.tensor_reduce(out=lmax[:], in_=PS[:, 3072:3088], axis=AX.X, op=ALU.max), reads=['P6'], writes=['lmax'])
                S.op('dve', lambda e: e.tensor_scalar(out=lmax[:], in0=lmax[:], scalar1=-1.0, scalar2=None, op0=ALU.mult), reads=['lmax'], writes=['lmax'])
                ACTF(lex[:], PS[:, 3072:3088], AF.Exp, ['P6', 'lmax'], ['lex'], bias=lmax[:])
                S.op('dve', lambda e: e.tensor_reduce(out=lsum[:], in_=lex[:], axis=AX.X, op=ALU.add), reads=['lex'], writes=['lsum'])
                S.op('dve', lambda e: e.reciprocal(out=lsum[:], in_=lsum[:]), reads=['lsum'], writes=['lsum'])
                S.op('dve', lambda e, i=i: e.tensor_scalar(out=aff_all[:, i, :], in0=lex[:], scalar1=lsum[:], scalar2=None, op0=ALU.mult),
                     reads=['lex', 'lsum'], writes=['aff_all'])
            if dbg:
                o1 = dout("dbg_x1", [TL, D]); o2 = dout("dbg_aff", [128, 512])
                S.dma(lambda e: e.dma_start(out=o1[:, :], in_=x1D[:, :]), reads=['x1D'])
                S.dma(lambda e: e.dma_start(out=o2[:, :], in_=aff_all[:].rearrange("p a b -> p (a b)")), reads=['aff_all'])
            S.barrier()
            S.flush()
        if stop_after <= 4:
            return nc, dbg_outs
        posm = sb(top, "posm", [128, 32, 16], F32)
        gw = sb(top, "gw", [128, 32, 16, 2], BF16)
        onesf = sb(top, "onesf", [128, 128], F32)
        iot = sb(top, "iot", [128, 516], F32)
        S.op('pool', lambda e: e.memset(onesf[:], 1.0), writes=['onesf'])
        S.dma(lambda e: e.dma_start(out=iot[:], in_=iot_d[:, :]), writes=['iot'])

        with contextlib.ExitStack() as ph:
            lo = sb(ph, "lo", [128, 16], F32); hi = sb(ph, "hi", [128, 16], F32); mid = sb(ph, "mid", [128, 16], F32)
            cmpt = sb(ph, "cmpt", [128, 32, 16], F32); cntp = sb(ph, "cntp", [128, 16], F32); ge = sb(ph, "ge", [128, 16], F32)
            dlt = sb(ph, "dlt", [128, 16], F32)
            ustr = sb(ph, "ustr", [128, 128], F32)
            mask = sb(ph, "mask", [128, 32, 16], F32); tot = sb(ph, "tot", [128, 32, 16], F32); cum = sb(ph, "cum", [128, 32, 16], F32)
            glo = sb(ph, "glo", [128, 32, 16], F32); ghi32 = sb(ph, "ghi32", [128, 32, 16], F32)
            S.dma(lambda e: e.dma_start(out=ustr[:], in_=ustr_d[:, :]), writes=['ustr'])
            S.op('pool', lambda e: e.memset(lo[:], 0.0), writes=['lo'])
            S.op('pool', lambda e: e.memset(hi[:], 1.0), writes=['hi'])
            affv = aff_all[:, :, :]
            for it in range(30):
                TTo('dve', mid[:], lo[:], hi[:], ALU.add, ['lo', 'hi'], ['mid'])
                S.op('dve', lambda e: e.tensor_scalar(out=mid[:], in0=mid[:], scalar1=0.5, scalar2=None, op0=ALU.mult), reads=['mid'], writes=['mid'])
                TTo('dve', cmpt[:], affv, mid[:, :].unsqueeze(1).to_broadcast([128, 32, 16]), ALU.is_ge, ['aff_all', 'mid'], ['cmpt'])
                S.op('dve', lambda e: e.tensor_reduce(out=cntp[:], in_=cmpt[:].rearrange("p t e -> p e t"), axis=AX.X, op=ALU.add),
                     reads=['cmpt'], writes=['cntp'])
                MM(PS[:, 0:16], onesf[:], cntp[:], ['onesf', 'cntp'], ['P0'])
                S.op('dve', lambda e: e.tensor_scalar(out=ge[:], in0=PS[:, 0:16], scalar1=511.5, scalar2=None, op0=ALU.is_ge), reads=['P0'], writes=['ge'])
                TTo('dve', dlt[:], mid[:], lo[:], ALU.subtract, ['mid', 'lo'], ['dlt'])
                TTo('dve', dlt[:], dlt[:], ge[:], ALU.mult, ['dlt', 'ge'], ['dlt'])
                TTo('dve', lo[:], lo[:], dlt[:], ALU.add, ['lo', 'dlt'], ['lo'])
                TTo('dve', dlt[:], hi[:], mid[:], ALU.subtract, ['hi', 'mid'], ['dlt'])
                TTo('dve', dlt[:], dlt[:], ge[:], ALU.mult, ['dlt', 'ge'], ['dlt'])
                TTo('dve', hi[:], mid[:], dlt[:], ALU.add, ['mid', 'dlt'], ['hi'])
            TTo('dve', mask[:], affv, lo[:, :].unsqueeze(1).to_broadcast([128, 32, 16]), ALU.is_ge, ['aff_all', 'lo'], ['mask'])
            m2 = mask[:].rearrange("p t e -> p (t e)")
            MM(PS[:, 512:1024], ustr[:], m2, ['ustr', 'mask'], ['P1'])
            MM(PS[:, 1024:1536], onesf[:], m2, ['onesf', 'mask'], ['P2'])
            CP('dve', tot[:].rearrange("p t e -> p (t e)"), PS[:, 1024:1536], ['P2'], ['tot'])
            for e_ in range(16):
                S.op('dve', lambda e, e_=e_: e.tensor_tensor_scan(out=cum[:, :, e_], data0=onesf[:, 0:32], data1=tot[:, :, e_], initial=0.0,
                                                                 op0=ALU.mult, op1=ALU.add), reads=['tot', 'onesf'], writes=['cum'])
            TTo('dve', cum[:], cum[:], tot[:], ALU.subtract, ['cum', 'tot'], ['cum'])
            TTo('dve', cum[:].rearrange("p t e -> p (t e)"), cum[:].rearrange("p t e -> p (t e)"), PS[:, 512:1024], ALU.add, ['cum', 'P1'], ['cum'])
            S.op('dve', lambda e: e.scalar_tensor_tensor(out=posm[:], in0=cum[:], scalar=1.0, in1=mask[:], op0=ALU.add, op1=ALU.mult),
                 reads=['cum', 'mask'], writes=['posm'])
            S.op('dve', lambda e: e.tensor_scalar(out=posm[:], in0=posm[:], scalar1=-1.0, scalar2=None, op0=ALU.add), reads=['posm'], writes=['posm'])
            TTo('dve', glo[:], affv, mask[:], ALU.mult, ['aff_all', 'mask'], ['glo'])
            CP('dve', gw[:, :, :, 0], glo[:], ['glo'], ['gw'])
            CP('dve', ghi32[:], gw[:, :, :, 0], ['gw'], ['ghi32'])
            TTo('dve', gw[:, :, :, 1], glo[:], ghi32[:], ALU.subtract, ['glo', 'ghi32'], ['gw'])
            if dbg:
                o1 = dout("dbg_posm", [128, 512])
                S.dma(lambda e: e.dma_start(out=o1[:, :], in_=posm[:].rearrange("p a b -> p (a b)")), reads=['posm'])
            S.barrier()
            S.flush()
        if stop_after <= 5:
            return nc, dbg_outs

        with contextlib.ExitStack() as ph:
            h2_all = sb(ph, "h2_all", [128, 32, D], BF16)
            for i in range(32):
                S.dma(lambda e, i=i: e.dma_start(out=h2_all[:, i, :], in_=h2D[i * 128:(i + 1) * 128, :]), reads=['h2D'], writes=['h2_all'])
            OH = sb(ph, "OH", [128, 32, 512], BF16)
            xinT = sb(ph, "xinT", [128, 8, 512], BF16); hidT = sb(ph, "hidT", [128, 8, 512], BF16)
            wgb = sb(ph, "wgb", [128, 8, D], BF16); wub = sb(ph, "wub", [128, 8, D], BF16); wdb = sb(ph, "wdb", [128, 8, D], BF16)
            wsg = [sb(ph, "wsg%d" % i, [128, D], F32) for i in range(3)]
            gcs = sb(ph, "gcs", [128, 4], F32); sgt = sb(ph, "sgt", [128, 512], F32)
            ywt = sb(ph, "ywt", [128, 4, D], BF16)
            wi = 0
            for ex_ in range(int(os.environ.get('K_NE', 16))):
                for (wsrc, wdst, wkey) in ((wg_d, wgb, 'wgb'), (wu_d, wub, 'wub'), (wd_d, wdb, 'wdb')):
                    for j in range(8):
                        w = wsg[wi % 3]; wk = 'wsg%d' % (wi % 3)
                        S.dma(lambda e, w=w, wsrc=wsrc, ex_=ex_, j=j: e.dma_start(out=w[:], in_=wsrc[ex_, j * 128:(j + 1) * 128, :]), writes=[wk])
                        CP(('dve', 'pool', 'act')[wi % 3], wdst[:, j, :], w[:], [wk], [wkey])
                        wi += 1
                for i in range(32):
                    S.op('dve' if i % 2 else 'pool', lambda e, i=i, ex_=ex_: e.tensor_scalar(
                        out=OH[:, i, :], in0=iot[:, 0:512], scalar1=posm[:, i, ex_:ex_ + 1], scalar2=None, op0=ALU.is_equal),
                        reads=['iot', 'posm'], writes=['OH'])
                for j in range(8):
                    pb = P[j % 2]; pk = 'P%d' % (j % 2)
                    for i in range(32):
                        MM(pb, h2_all[:, i, j * 128:(j + 1) * 128], OH[:, i, :], ['h2_all', 'OH'], [pk], start=(i == 0), stop=(i == 31))
                    CP('act' if j % 2 else 'dve', xinT[:, j, :], pb, [pk], ['xinT'])
                gcp = PS[:, 1024:1032].rearrange("p (c k) -> p c k", k=2)
                for ct in range(4):
                    for i in range(32):
                        MM(gcp[:, ct, :], OH[:, i, ct * 128:(ct + 1) * 128], gw[:, i, ex_, :], ['OH', 'gw'], ['P2'],
                           start=(i == 0 and ct == 0), stop=(i == 31 and ct == 3))
                TTo('dve', gcs[:], gcp[:, :, 0], gcp[:, :, 1], ALU.add, ['P2'], ['gcs']) if False else None
                CP('dve', sgt[:, 0:8], PS[:, 1024:1032], ['P2'], ['sgt'])
                TTo('dve', gcs[:], sgt[:, 0:8].rearrange("p (c k) -> p c k", k=2)[:, :, 0], sgt[:, 0:8].rearrange("p (c k) -> p c k", k=2)[:, :, 1],
                    ALU.add, ['sgt'], ['gcs'])
                for fc in range(8):
                    for j in range(8):
                        MM(P[4], wgb[:, j, fc * 128:(fc + 1) * 128], xinT[:, j, :], ['wgb', 'xinT'], ['P4'], start=(j == 0), stop=(j == 7))
                    for j in range(8):
                        MM(P[5], wub[:, j, fc * 128:(fc + 1) * 128], xinT[:, j, :], ['wub', 'xinT'], ['P5'], start=(j == 0), stop=(j == 7))
                    ACTF(sgt[:], P[4], AF.Silu, ['P4'], ['sgt'])
                    TTo('dve', hidT[:, fc, :], sgt[:], P[5], ALU.mult, ['sgt', 'P5'], ['hidT'])
                for ct in range(4):
                    for half in range(2):
                        pb = P[6 + half]; pk = 'P%d' % (6 + half)
                        for fc in range(8):
                            MM(pb, hidT[:, fc, ct * 128:(ct + 1) * 128], wdb[:, fc, half * 512:(half + 1) * 512], ['hidT', 'wdb'], [pk],
                               start=(fc == 0), stop=(fc == 7))
                        S.op('act', lambda e, ct=ct, half=half, pb=pb: e.activation(out=ywt[:, ct, half * 512:(half + 1) * 512], in_=pb,
                                                                                  func=AF.Copy, scale=gcs[:, ct:ct + 1]),
                             reads=[pk, 'gcs'], writes=['ywt'])
                S.dma(lambda e, ex_=ex_: e.dma_start(out=ywD[ex_].rearrange("(c p) n -> p c n", p=128), in_=ywt[:]), reads=['ywt'], writes=['ywD'])
            if dbg:
                o1 = dout("dbg_yw", [16 * 512, D], BF16)
                S.dma(lambda e: e.dma_start(out=o1[:, :], in_=ywD.rearrange("e c n -> (e c) n")), reads=['ywD'])
            S.barrier()
            S.flush()
        if stop_after <= 6:
            return nc, dbg_outs

        with contextlib.ExitStack() as ph:
            yw_all = sb(ph, "yw_all", [128, 16, 4, D], BF16)
            for ex_ in range(16):
                S.dma(lambda e, ex_=ex_: e.dma_start(out=yw_all[:, ex_, :, :], in_=ywD[ex_].rearrange("(c p) n -> p c n", p=128)),
                      reads=['ywD'], writes=['yw_all'])
            cst = {}
            for nm, src in (("gt2", modd[0:1, 5120:6144]), ("ln2g", ln2_d[0:1, :]), ("ln2b", ln2_d[1:2, :])):
                cst[nm] = sb(ph, nm, [128, D], F32)
                S.dma(lambda e, nm=nm, src=src: e.dma_start(out=cst[nm][:], in_=src.partition_broadcast(128)), reads=['modd'], writes=[nm])
            dg = sb(ph, "dg", [128, 4, 128], F32)
            OHT = sb(ph, "OHT", [128, 4, 2048], BF16)
            x1l = sb(ph, "x1l", [128, D], F32); tr2 = sb(ph, "tr2", [128, D], F32); xo = sb(ph, "xo", [128, D], F32)
            stats = sb(ph, "statsF", [128, 2, 6], F32); mv = sb(ph, "mvF", [128, 2], F32)
            rstd = sb(ph, "rstdF", [128, 1], F32); nb = sb(ph, "nbF", [128, 1], F32)
            lntF = (stats, mv, rstd, nb, 'F')
            for i in range(int(os.environ.get('K_FT', 32))):
                t0 = i * 128
                S.dma(lambda e, t0=t0: e.dma_start(out=x1l[:], in_=x1D[t0:t0 + 128, :]), reads=['x1D'], writes=['x1l'])
                for eg in range(4):
                    for k_ in range(4):
                        ex_ = eg * 4 + k_
                        S.op('dve' if k_ % 2 else 'pool', lambda e, k_=k_, ex_=ex_, i=i: e.tensor_scalar(
                            out=dg[:, k_, :], in0=identf[:], scalar1=posm[:, i, ex_:ex_ + 1], scalar2=None, op0=ALU.mult),
                            reads=['identf', 'posm'], writes=['dg'])
                    MM(PS[:, eg * 512:(eg + 1) * 512], onesf[:], dg[:].rearrange("p a t -> p (a t)"), ['onesf', 'dg'], ['P%d' % eg])
                for ct in range(4):
                    S.op('dve', lambda e, ct=ct: e.tensor_scalar(out=OHT[:, ct, :], in0=PS[:, 0:2048], scalar1=iot[:, 512 + ct:513 + ct],
                                                                scalar2=None, op0=ALU.is_equal),
                         reads=['P0', 'P1', 'P2', 'P3', 'iot'], writes=['OHT'])
                for half in range(2):
                    pb = P[4 + half]; pk = 'P%d' % (4 + half)
                    n = 0
                    for ex_ in range(16):
                        for ct in range(4):
                            MM(pb, OHT[:, ct, ex_ * 128:(ex_ + 1) * 128], yw_all[:, ex_, ct, half * 512:(half + 1) * 512], ['OHT', 'yw_all'], [pk],
                               start=(n == 0), stop=(n == 63))
                            n += 1
                TTo('dve', tr2[:], PS[:, 2048:3072], cst['gt2'][:], ALU.mult, ['P4', 'P5', 'gt2'], ['tr2'])
                S.op('dve', lambda e: e.scalar_tensor_tensor(out=tr2[:], in0=x1l[:], scalar=ALPHA, in1=tr2[:], op0=ALU.mult, op1=ALU.add),
                     reads=['x1l', 'tr2'], writes=['tr2'])
                ln_stats(lntF, tr2, 'tr2', eps5, 'eps5')
                S.op('act', lambda e: e.activation(out=xo[:], in_=tr2[:], func=AF.Identity, bias=nb[:], scale=rstd[:]),
                     reads=['tr2', 'Fnb', 'Frstd'], writes=['xo'])
                TTo('pool', xo[:], xo[:], cst['ln2g'][:], ALU.mult, ['xo', 'ln2g'], ['xo'])
                TTo('dve', xo[:], xo[:], cst['ln2b'][:], ALU.add, ['xo', 'ln2b'], ['xo'])
                S.dma(lambda e, t0=t0: e.dma_start(out=out_d[t0:t0 + 128, :], in_=xo[:]), reads=['xo'], writes=['out'])
            S.barrier()
            S.flush()
    return nc, dbg_outs


def host_inputs(inp, b):
    f = np.float32
    m = {}
    m["x"] = np.ascontiguousarray(inp["x"][b], dtype=f)
    m["ctx"] = np.ascontiguousarray(inp["ctx"][b], dtype=f)
    m["cc"] = np.ascontiguousarray(np.stack([inp["c"][b].reshape(8, 128).T, inp["c_ctx"].reshape(8, 128).T], -1).reshape(128, 16), dtype=f)
    m["w_ada"] = np.ascontiguousarray(inp["w_ada"][0], dtype=f)
    m["b_ada"] = np.ascontiguousarray(inp["b_ada"][0].reshape(1, -1), dtype=f)
    m["w_in"] = np.ascontiguousarray(inp["w_in"][0], dtype=f)
    m["qg"] = np.ascontiguousarray(np.tile(inp["q_gain"][0], 8).reshape(1, 512), dtype=f)
    m["kg"] = np.ascontiguousarray(np.tile(inp["k_gain"][0], 2).reshape(1, 128), dtype=f)
    t = np.arange(TL)
    pos = np.stack([t // 64, t % 64], -1).astype(np.float32)
    inv = (10000.0 ** (-np.arange(16, dtype=np.float32) / 16)).astype(np.float32)
    ang = pos[:, :, None] * inv[None, None, :]
    cs = np.cos(ang).astype(f); sn = np.sin(ang).astype(f)
    cos2 = np.stack([cs, cs], 2).reshape(TL, 64)
    sin2 = np.stack([-sn, sn], 2).reshape(TL, 64)
    m["cosT"] = np.ascontiguousarray(np.tile(cos2, (1, 8)), dtype=f)
    m["sinS"] = np.ascontiguousarray(np.tile(sin2, (1, 8)), dtype=f)
    m["ident"] = np.eye(128, dtype=f)
    def kh(v):
        return np.asarray(v, dtype=f).reshape(8, 64).T
    mu = inp["tshift_mu"][0]
    cols = [kh(mu[0:512]), kh(mu[512:1024]), kh(mu[1024:1536]), kh(inp["k_k"][0]), kh(inp["k_a"][0]), kh(inp["r_k"][0].reshape(-1)),
            kh(inp["decay_w0"][0, 0]), kh(inp["decay_w0"][0, 1]), kh(inp["iclr_a0"][0, 0]), kh(inp["iclr_a0"][0, 1])]
    m["rp"] = np.ascontiguousarray(np.stack(cols, -1).reshape(64, 80), dtype=f)
    lmu = np.zeros((128, 3), f)
    lmu[0:64, 0] = mu[1536:1600]; lmu[0:64, 1] = mu[1600:1664]; lmu[:, 2] = mu[1664:1792]
    m["lmu"] = lmu
    m["decup"] = np.ascontiguousarray(np.concatenate([inp["decay_up"][0, 0], inp["decay_up"][0, 1]], 1), dtype=f)
    m["iclup"] = np.ascontiguousarray(np.concatenate([inp["iclr_up"][0, 0], inp["iclr_up"][0, 1]], 1), dtype=f)
    m["gateup"] = np.ascontiguousarray(inp["gate_up"][0], dtype=f)
    hh = np.repeat(np.arange(8), 16); tt = np.tile(np.arange(16), 8)
    same = hh[:, None] == hh[None, :]
    msf = (same & (tt[:, None] < tt[None, :])).astype(f); mif = (same & (tt[:, None] <= tt[None, :])).astype(f)
    msb = msf.T.copy(); mib = mif.T.copy()
    m["mk4"] = np.ascontiguousarray(np.concatenate([msf, mif, msf, mif, msb, mib, msb, mib], 1), dtype=f)
    m["mn1"] = np.ascontiguousarray(np.concatenate([msb, msf], 1), dtype=f)
    m["bm"] = np.ascontiguousarray((hh[:, None] == np.repeat(np.arange(8), 64)[None, :]).astype(f))
    rst = np.ones((64, 1024), f); rst[:, ::16] = 0.0
    m["rst"] = rst
    m["w_out"] = np.ascontiguousarray(inp["w_out"][0], dtype=f)
    m["lnx"] = np.ascontiguousarray(np.stack([inp["lnx_g"][0], inp["lnx_b"][0]]), dtype=f)
    m["ln1"] = np.ascontiguousarray(np.stack([inp["ln1_g"][0], inp["ln1_b"][0]]), dtype=f)
    m["ln2"] = np.ascontiguousarray(np.stack([inp["ln2_g"][0], inp["ln2_b"][0]]), dtype=f)
    m["router_w"] = np.ascontiguousarray(inp["router_w"][0], dtype=f)
    m["exp_w_gate"] = np.ascontiguousarray(inp["exp_w_gate"][0], dtype=f)
    m["exp_w_up"] = np.ascontiguousarray(inp["exp_w_up"][0], dtype=f)
    m["exp_w_down"] = np.ascontiguousarray(inp["exp_w_down"][0], dtype=f)
    iot = np.zeros((128, 516), f)
    iot[:, 0:512] = np.arange(512, dtype=f)[None, :]
    iot[:, 512:516] = np.arange(128, dtype=f)[:, None] + 128.0 * np.arange(4, dtype=f)[None, :]
    m["iot"] = iot
    m["ustr"] = np.triu(np.ones((128, 128), f), 1)
    return m


_NC_CACHE = {}


def kernel(**inputs):
    inp = {k: np.asarray(v) for k, v in inputs.items()}
    if "full" not in _NC_CACHE:
        _NC_CACHE["full"] = build_nc()[0]
    nc = _NC_CACHE["full"]
    in_maps = [host_inputs(inp, c // 2) for c in range(8)]
    res = run_bass_kernel_spmd(nc, in_maps, core_ids=list(range(8)))
    out = np.stack([res.results[2 * b]["out"] for b in range(4)], 0).astype(np.float32)
    return out
```

```python
import contextlib
import os
import numpy as np
import concourse.bass as bass
import concourse.mybir as mybir
from concourse.bass_utils import run_bass_kernel_spmd

F32 = mybir.dt.float32
BF16 = mybir.dt.bfloat16
AF = mybir.ActivationFunctionType
ALU = mybir.AluOpType
AX = mybir.AxisListType

D = 1024
TL = 4096
TC = 256
TT = TL + TC
NT = TT // 128
ALPHA = 2.0 ** 0.25
DEC_C = -float(np.exp(-0.5))


class Sched:
    CE = ('pe', 'act', 'dve', 'pool')

    def __init__(self, nc, stack, ndma=32):
        self.nc = nc
        self.ops = {e: [] for e in ('pe', 'act', 'dve', 'pool', 'sp')}
        self.cnt = {e: 0 for e in self.CE}
        self.last_w = {}
        self.readers = {}
        self.waited = {e: {} for e in self.ops}
        self.ndma = ndma
        self.dma_val = [0] * ndma
        self.dma_i = 0
        names = list(self.CE) + ['d%d' % i for i in range(ndma)]
        self.sems = {n: stack.enter_context(nc.semaphore('s_' + n)) for n in names}

    def _deps(self, eng, reads, writes):
        deps = {}

        def add(tok):
            if tok is None:
                return
            s, v = tok
            if deps.get(s, 0) < v:
                deps[s] = v
        for r in reads:
            add(self.last_w.get(r))
        for w in writes:
            add(self.last_w.get(w))
            for t in self.readers.get(w, ()):
                add(t)
        waits = []
        for s, v in deps.items():
            if s == eng and (eng == 'pe' or os.environ.get('K_NOSELF')):
                continue
            if self.waited[eng].get(s, 0) >= v:
                continue
            self.waited[eng][s] = v
            waits.append((s, v))
        return waits

    def _commit(self, tok, reads, writes):
        for r in reads:
            self.readers.setdefault(r, []).append(tok)
        for w in writes:
            self.last_w[w] = tok
            self.readers[w] = []

    capture = None
    pend = None

    def drain(self, lst, n):
        for _ in range(min(n, len(lst))):
            kind, a = lst.pop(0)
            (self.op if kind == 'op' else self.dma)(*a)

    def op(self, eng, fn, reads=(), writes=()):
        if self.capture is not None:
            self.capture.append(('op', (eng, fn, tuple(reads), tuple(writes))))
            return
        waits = self._deps(eng, reads, writes)
        self.cnt[eng] += 1
        tok = (eng, self.cnt[eng])
        self.ops[eng].append((waits, fn, (eng, 1)))
        self._commit(tok, reads, writes)

    def dma(self, fn, reads=(), writes=(), q='sp'):
        if self.capture is not None:
            self.capture.append(('dma', (fn, tuple(reads), tuple(writes), q)))
            return
        slot = self.dma_i % self.ndma
        self.dma_i += 1
        s = 'd%d' % slot
        waits = self._deps(q, reads, writes)
        pv = self.dma_val[slot]
        if pv > 0 and self.waited[q].get(s, 0) < pv:
            self.waited[q][s] = pv
            waits.append((s, pv))
        self.dma_val[slot] = pv + 16
        tok = (s, pv + 16)
        self.ops[q].append((waits, fn, (s, 16)))
        self._commit(tok, reads, writes)

    def barrier(self):
        allw = [(e, c) for e, c in self.cnt.items() if c > 0]
        allw += [('d%d' % i, v) for i, v in enumerate(self.dma_val) if v > 0]
        for eng in self.ops:
            waits = []
            for s, v in allw:
                if self.waited[eng].get(s, 0) >= v:
                    continue
                self.waited[eng][s] = v
                waits.append((s, v))
            if waits:
                self.ops[eng].append((waits, None, None))
        self.last_w = {}
        self.readers = {}

    def flush(self):
        nc = self.nc
        sems = self.sems
        ops = self.ops
        self.ops = {e: [] for e in ops}
        if os.environ.get('K_STATS'):
            print("FLUSH", {e: (len(v), sum(len(w[0]) for w in v)) for e, v in ops.items()})
        with nc.Block() as block:
            def run(engname, engobj):
                for waits, fn, inc in ops[engname]:
                    for ws, wv in waits:
                        engobj.wait_ge(sems[ws], wv)
                    if fn is not None:
                        ins = fn(engobj)
                        ins.then_inc(sems[inc[0]], inc[1])

            @block.sync
            def _(e):
                run('sp', e)

            @block.tensor
            def _(e):
                run('pe', e)

            @block.scalar
            def _(e):
                run('act', e)

            @block.vector
            def _(e):
                run('dve', e)

            @block.gpsimd
            def _(e):
                run('pool', e)


def build_nc(stop_after=99, dbg=False):
    nc = bass.Bass("TRN2", target_bir_lowering=False)

    def din(name, shape, dt=F32):
        return nc.dram_tensor(name, list(shape), dt, kind="ExternalInput").ap()

    def dscr(name, shape, dt=F32):
        return nc.dram_tensor(name, list(shape), dt, kind="Internal").ap()

    x_d = din("x", [TL, D]); ctx_d = din("ctx", [TC, D])
    cc_d = din("cc", [128, 16])
    wada_d = din("w_ada", [D, 6 * D]); bada_d = din("b_ada", [1, 6 * D])
    win_d = din("w_in", [D, 2560])
    qg_d = din("qg", [1, 512]); kg_d = din("kg", [1, 128])
    cos_d = din("cosT", [TL, 512]); sin_d = din("sinS", [TL, 512])
    ident_d = din("ident", [128, 128])
    rp_d = din("rp", [64, 8 * 10])
    lmu_d = din("lmu", [128, 3])
    decup_d = din("decup", [64, 2 * 512]); iclup_d = din("iclup", [64, 2 * 512]); gateup_d = din("gateup", [128, 512])
    mk4_d = din("mk4", [128, 2 * 512]); mn1_d = din("mn1", [128, 2 * 128]); bm_d = din("bm", [128, 512])
    rst_d = din("rst", [64, 1024])
    wout_d = din("w_out", [D, D]); lnx_d = din("lnx", [2, 512])
    ln1_d = din("ln1", [2, D]); ln2_d = din("ln2", [2, D])
    rw_d = din("router_w", [D, 16])
    x1D = dscr("x1D", [TL, D]); h2D = dscr("h2D", [TL, D], BF16)
    attD = dscr("attD", [128, 4 * TL], BF16)
    wg_d = din("exp_w_gate", [16, D, D]); wu_d = din("exp_w_up", [16, D, D]); wd_d = din("exp_w_down", [16, D, D])
    iot_d = din("iot", [128, 512 + 4]); ustr_d = din("ustr", [128, 128])
    ywD = dscr("ywD", [16, 512, D], BF16)
    ydir = dscr("ydir", [2, TL, 512]); bonD = dscr("bonD", [TL, 512]); gD = dscr("gD", [TL, 512])
    out_d = nc.dram_tensor("out", [TL, D], F32, kind="ExternalOutput").ap()
    modd = dscr("modd", [2, 6 * D])
    rwT = dscr("rwT", [1792, TT])
    dbg_outs = {}

    def dout(name, shape, dt=F32):
        ap = nc.dram_tensor(name, list(shape), dt, kind="ExternalOutput").ap()
        dbg_outs[name] = ap
        return ap

    with contextlib.ExitStack() as top:
        S = Sched(nc, top)

        def sb(stack, name, shape, dt):
            return stack.enter_context(nc.sbuf_tensor("sb_" + name, list(shape), dt))

        PS = top.enter_context(nc.psum_tensor("PS", [128, 8 * 512], F32))
        P = [PS[:, i * 512:(i + 1) * 512] for i in range(8)]
        PK = ['P%d' % i for i in range(8)]
        identf = sb(top, "identf", [128, 128], F32)
        identb = sb(top, "identb", [128, 128], BF16)
        eps5 = sb(top, "eps5", [128, 1], F32)
        eps6 = sb(top, "eps6", [128, 1], F32)
        S.dma(lambda e: e.dma_start(out=identf[:], in_=ident_d[:, :]), writes=['identf'])
        S.op('dve', lambda e: e.tensor_copy(out=identb[:], in_=identf[:]), reads=['identf'], writes=['identb'])
        S.op('pool', lambda e: e.memset(eps5[:], 1e-5), writes=['eps5'])
        S.op('pool', lambda e: e.memset(eps6[:], 1e-6), writes=['eps6'])

        def ln_stats(stack_tiles, src, key_src, eps_t, eps_key):
            stats, mv, rstd, nb, kq = stack_tiles
            for cch in range(2):
                S.op('dve', lambda e, cch=cch: e.bn_stats(out=stats[:, cch, :], in_=src[:, cch * 512:(cch + 1) * 512]),
                     reads=[key_src], writes=[kq + 'stats'])
            S.op('dve', lambda e: e.bn_aggr(out=mv[:], in_=stats[:]), reads=[kq + 'stats'], writes=[kq + 'mv'])
            S.op('act', lambda e: e.activation(out=rstd[:], in_=mv[:, 1:2], func=AF.Ln, bias=eps_t[:], scale=1.0),
                 reads=[kq + 'mv', eps_key], writes=[kq + 'rstd'])
            S.op('act', lambda e: e.activation(out=rstd[:], in_=rstd[:], func=AF.Exp, scale=-0.5),
                 reads=[kq + 'rstd'], writes=[kq + 'rstd'])
            S.op('dve', lambda e: e.scalar_tensor_tensor(out=nb[:], in0=mv[:, 0:1], scalar=-1.0, in1=rstd[:],
                                                        op0=ALU.mult, op1=ALU.mult),
                 reads=[kq + 'mv', kq + 'rstd'], writes=[kq + 'nb'])

        with contextlib.ExitStack() as ph:
            cc = sb(ph, "cc", [128, 8, 2], F32)
            ccs = sb(ph, "ccs", [128, 8, 2], F32)
            wst = [sb(ph, "wst%d" % i, [128, 8, 512], F32) for i in range(2)]
            bada = sb(ph, "bada", [2, 6 * D], F32)
            modrow = sb(ph, "modrow", [2, 6 * D], F32)
            S.dma(lambda e: e.dma_start(out=cc[:].rearrange("p a b -> p (a b)"), in_=cc_d[:, :]), writes=['cc'])
            S.dma(lambda e: e.dma_start(out=bada[:], in_=bada_d.partition_broadcast(2)), writes=['bada'])
            S.op('act', lambda e: e.activation(out=ccs[:], in_=cc[:], func=AF.Silu), reads=['cc'], writes=['ccs'])
            for g in range(12):
                w = wst[g % 2]
                wk = 'wst%d' % (g % 2)
                S.dma(lambda e, w=w, g=g: e.dma_start(
                    out=w[:], in_=wada_d[:, g * 512:(g + 1) * 512].rearrange("(j p) n -> p j n", p=128)), writes=[wk])
                for j in range(8):
                    S.op('pe', lambda e, w=w, j=j: e.matmul(P[0][0:2, :], lhsT=ccs[:, j, :], rhs=w[:, j, :],
                                                           start=(j == 0), stop=(j == 7)),
                         reads=['ccs', wk], writes=['P0'])
                S.op('dve', lambda e, g=g: e.tensor_tensor(out=modrow[:, g * 512:(g + 1) * 512], in0=P[0][0:2, :],
                                                          in1=bada[:, g * 512:(g + 1) * 512], op=ALU.add),
                     reads=['P0', 'bada'], writes=['modrow'])
            for lo in (1024, 4096):
                S.op('dve', lambda e, lo=lo: e.tensor_scalar_add(out=modrow[:, lo:lo + 1024], in0=modrow[:, lo:lo + 1024],
                                                                scalar1=1.0), reads=['modrow'], writes=['modrow'])
            S.dma(lambda e: e.dma_start(out=modd[:, :], in_=modrow[:]), reads=['modrow'], writes=['modd'])
            if dbg:
                o = dout("dbg_mod", [2, 6 * D])
                S.dma(lambda e: e.dma_start(out=o[:, :], in_=modrow[:]), reads=['modrow'])
            S.barrier()
            S.flush()
        if stop_after <= 0:
            return nc, dbg_outs

        def modrow_bc(dst, row, lo):
            S.dma(lambda e: e.dma_start(out=dst[:], in_=modd[row:row + 1, lo:lo + 1024].partition_broadcast(128)),
                  reads=['modd'], writes=[dst.name if hasattr(dst, 'name') else 'x'])

        aff_all = sb(top, "aff_all", [128, 32, 16], F32)
        with contextlib.ExitStack() as phAB:
            qT_all = sb(phAB, "qT_all", [128, 4, TL], BF16)
            kTd = sb(phAB, "kTd", [128, 2, TT], BF16)
            Vx = sb(phAB, "Vx", [128, NT, 2, 80], BF16)
            attT_all = sb(phAB, "attT_all", [128, 4, TL], BF16)
            with contextlib.ExitStack() as ph:
                winb = sb(ph, "winb", [128, 8, 2560], BF16)
                wstg = [sb(ph, "wstg%d" % i, [128, 640], F32) for i in range(2)]
                sc1p = sb(ph, "sc1p", [128, D], F32); sh1 = sb(ph, "sh1", [128, D], F32)
                csc1p = sb(ph, "csc1p", [128, D], F32); csh1 = sb(ph, "csh1", [128, D], F32)
                qg = sb(ph, "qg", [128, 512], F32); kg = sb(ph, "kg", [128, 128], F32)
                for dst, nm, row, lo in ((sh1, 'sh1', 0, 0), (sc1p, 'sc1p', 0, 1024), (csh1, 'csh1', 1, 0), (csc1p, 'csc1p', 1, 1024)):
                    S.dma(lambda e, dst=dst, row=row, lo=lo: e.dma_start(
                        out=dst[:], in_=modd[row:row + 1, lo:lo + 1024].partition_broadcast(128)), reads=['modd'], writes=[nm])
                S.dma(lambda e: e.dma_start(out=qg[:], in_=qg_d.partition_broadcast(128)), writes=['qg'])
                S.dma(lambda e: e.dma_start(out=kg[:], in_=kg_d.partition_broadcast(128)), writes=['kg'])
                for jj in range(32):
                    j = jj // 4; c0 = (jj % 4) * 640
                    w = wstg[jj % 2]; wk = 'wstg%d' % (jj % 2)
                    S.dma(lambda e, w=w, j=j, c0=c0: e.dma_start(out=w[:], in_=win_d[j * 128:(j + 1) * 128, c0:c0 + 640]), writes=[wk])
                    S.op('pool' if jj % 2 else 'dve', lambda e, w=w, j=j, c0=c0: e.tensor_copy(out=winb[:, j, c0:c0 + 640], in_=w[:]),
                         reads=[wk], writes=['winb'])
                S.op('pool', lambda e: e.memset(Vx[:], 1.0), writes=['Vx'])
                xt = [sb(ph, "xt%d" % i, [128, D], F32) for i in range(2)]
                xn = sb(ph, "xn", [128, D], F32)
                hb = sb(ph, "hb", [128, D], BF16)
                hT = sb(ph, "hT", [128, 8, 512], BF16)
                stats = sb(ph, "stats", [128, 2, 6], F32); mv = sb(ph, "mv", [128, 2], F32)
                rstd = sb(ph, "rstd", [128, 1], F32); nb = sb(ph, "nb", [128, 1], F32)
                lnt = (stats, mv, rstd, nb, 'A')
                rstg = [sb(ph, "rstg%d" % i, [128, 512], F32) for i in range(2)]
                cosT = sb(ph, "cosT", [128, 512], F32); sinS = sb(ph, "sinS", [128, 512], F32)
                sq = sb(ph, "sq", [128, 512], F32)
                ssum = sb(ph, "ssum", [128, 8], F32)
                qn = sb(ph, "qn", [128, 512], F32); qa = sb(ph, "qa", [128, 512], F32)
                qbt = sb(ph, "qbt", [128, 512], F32)
                qo = sb(ph, "qo", [128, 512], BF16)
                ko = sb(ph, "ko", [128, 2, 2, 64], BF16)
                blocks = [(0, 256)] + [(256 + 512 * i, 512) for i in range(8)]
                ti = 0
                for (u0, ntok) in blocks:
                    ntile = ntok // 128
                    is_ctx = (u0 == 0)
                    for tl in range(ntile):
                        u = u0 + tl * 128
                        gt = u // 128
                        X = xt[ti % 2]; xk = 'xt%d' % (ti % 2)
                        ti += 1
                        src = ctx_d[u:u + 128, :] if is_ctx else x_d[u - TC:u - TC + 128, :]
                        S.dma(lambda e, X=X, src=src: e.dma_start(out=X[:], in_=src), writes=[xk])
                        ln_stats(lnt, X, xk, eps5, 'eps5')
                        S.op('act', lambda e, X=X: e.activation(out=xn[:], in_=X[:], func=AF.Identity, bias=nb[:], scale=rstd[:]),
                             reads=[xk, 'Anb', 'Arstd'], writes=['xn'])
                        scp, shh, k1, k2 = (csc1p, csh1, 'csc1p', 'csh1') if is_ctx else (sc1p, sh1, 'sc1p', 'sh1')
                        S.op('pool', lambda e, scp=scp: e.tensor_tensor(out=xn[:], in0=xn[:], in1=scp[:], op=ALU.mult),
                             reads=['xn', k1], writes=['xn'])
                        S.op('dve', lambda e, shh=shh: e.tensor_tensor(out=hb[:], in0=xn[:], in1=shh[:], op=ALU.add),
                             reads=['xn', k2], writes=['hb'])
                        pTb = P[0][:].bitcast(BF16)
                        for j in range(8):
                            S.op('pe', lambda e, j=j, pTb=pTb: e.transpose(out=pTb[:, j * 128:(j + 1) * 128],
                                                                      in_=hb[:, j * 128:(j + 1) * 128], identity=identb[:]),
                                 reads=['hb', 'identb'], writes=['P0'])
                        S.op('act', lambda e, tl=tl, pTb=pTb: e.copy(out=hT[:, :, tl * 128:(tl + 1) * 128],
                                                                 in_=pTb.rearrange("p (j t) -> p j t", j=8)),
                             reads=['P0'], writes=['hT'])
                        for j in range(8):
                            S.op('pe', lambda e, j=j, tl=tl: e.matmul(P[1][:, :], lhsT=hT[:, j, tl * 128:(tl + 1) * 128],
                                                                     rhs=winb[:, j, 0:512], start=(j == 0), stop=(j == 7)),
                                 reads=['hT', 'winb'], writes=['P1'])
                        for j in range(8):
                            S.op('pe', lambda e, j=j, tl=tl: e.matmul(P[2][:, 0:256], lhsT=hT[:, j, tl * 128:(tl + 1) * 128],
                                                                     rhs=winb[:, j, 512:768], start=(j == 0), stop=(j == 7)),
                                 reads=['hT', 'winb'], writes=['P2'])
                        S.op('act', lambda e, gt=gt: e.copy(out=Vx[:, gt, :, 0:64],
                                                          in_=P[2][:, 128:256].rearrange("p (a b) -> p a b", a=2)),
                             reads=['P2'], writes=['Vx'])
                        if not is_ctx:
                            ul = u - TC
                            S.dma(lambda e, ul=ul: e.dma_start(out=cosT[:], in_=cos_d[ul:ul + 128, :]), writes=['cosT'])
                            S.dma(lambda e, ul=ul: e.dma_start(out=sinS[:], in_=sin_d[ul:ul + 128, :]), writes=['sinS'])

                        def normrope(psrc, pk, nh, gain, gk, do_rope, outfn):
                            w_ = nh * 64
                            S.op('act', lambda e: e.activation(out=sq[:, 0:w_], in_=psrc, func=AF.Square),
                                 reads=[pk], writes=['sq'])
                            S.op('dve', lambda e: e.tensor_reduce(out=ssum[:, 0:nh], in_=sq[:, 0:w_].rearrange("p (h d) -> p h d", d=64),
                                                                 axis=AX.X, op=ALU.add), reads=['sq'], writes=['ssum'])
                            S.op('act', lambda e: e.activation(out=ssum[:, 0:nh], in_=ssum[:, 0:nh], func=AF.Ln, bias=eps6[:], scale=1.0 / 64),
                                 reads=['ssum', 'eps6'], writes=['ssum'])
                            S.op('act', lambda e: e.activation(out=ssum[:, 0:nh], in_=ssum[:, 0:nh], func=AF.Exp, scale=-0.5),
                                 reads=['ssum'], writes=['ssum'])
                            S.op('dve', lambda e: e.tensor_tensor(out=qn[:, 0:w_].rearrange("p (h d) -> p h d", d=64),
                                                                 in0=psrc.rearrange("p (h d) -> p h d", d=64),
                                                                 in1=ssum[:, 0:nh].unsqueeze(2).to_broadcast([128, nh, 64]), op=ALU.mult),
                                 reads=[pk, 'ssum'], writes=['qn'])
                            if not do_rope:
                                S.op('pool', lambda e: outfn(e, qn[:, 0:w_], gain[:, 0:w_], ALU.mult), reads=['qn', gk], writes=['qko'])
                                return
                            S.op('pool', lambda e: e.tensor_tensor(out=qn[:, 0:w_], in0=qn[:, 0:w_], in1=gain[:, 0:w_], op=ALU.mult),
                                 reads=['qn', gk], writes=['qn'])
                            S.op('dve', lambda e: e.tensor_tensor(out=qa[:, 0:w_], in0=qn[:, 0:w_], in1=cosT[:, 0:w_], op=ALU.mult),
                                 reads=['qn', 'cosT'], writes=['qa'])
                            qv = qn[:, 0:w_].rearrange("p (g s q) -> p g s q", s=2, q=16)
                            bv = qbt[:, 0:w_].rearrange("p (g s q) -> p g s q", s=2, q=16)
                            sv = sinS[:, 0:w_].rearrange("p (g s q) -> p g s q", s=2, q=16)
                            for s_ in range(2):
                                S.op('pool', lambda e, s_=s_: e.tensor_tensor(out=bv[:, :, s_, :], in0=qv[:, :, 1 - s_, :],
                                                                             in1=sv[:, :, s_, :], op=ALU.mult),
                                     reads=['qn', 'sinS'], writes=['qbt'])
                            S.op('dve', lambda e: outfn(e, qa[:, 0:w_], qbt[:, 0:w_], ALU.add), reads=['qa', 'qbt'], writes=['qko'])

                        if not is_ctx:
                            normrope(P[1][:, :], 'P1', 8, qg, 'qg', True,
                                     lambda e, a, b_, op: e.tensor_tensor(out=qo[:], in0=a, in1=b_, op=op))
                            pq = P[3][:].bitcast(BF16)
                            for hp in range(4):
                                S.op('pe', lambda e, hp=hp, pq=pq: e.transpose(out=pq[:, hp * 128:(hp + 1) * 128],
                                                                          in_=qo[:, hp * 128:(hp + 1) * 128], identity=identb[:]),
                                     reads=['qko', 'identb'], writes=['P3'])
                            S.op('act', lambda e, ul=ul, pq=pq: e.copy(out=qT_all[:, :, ul:ul + 128],
                                                                   in_=pq[:, 0:512].rearrange("p (j t) -> p j t", j=4)),
                                 reads=['P3'], writes=['qT_all'])

                        def kout(e, a, b_, op):
                            return e.tensor_tensor(out=ko[:, :, 0, :], in0=a.rearrange("p (h d) -> p h d", d=64),
                                                   in1=b_.rearrange("p (h d) -> p h d", d=64), op=op)
                        normrope(P[2][:, 0:128], 'P2', 2, kg, 'kg', not is_ctx, kout)
                        S.op('pool', lambda e: e.tensor_copy(out=ko[:, :, 1, :], in_=ko[:, :, 0, :]), reads=['qko'], writes=['qko'])
                        pk_ = P[3][:].bitcast(BF16)
                        for kv in range(2):
                            S.op('pe', lambda e, kv=kv, pk_=pk_: e.transpose(
                                out=pk_[:, 512 + kv * 128:512 + (kv + 1) * 128],
                                in_=ko[:, kv, :, :].rearrange("p a d -> p (a d)"), identity=identb[:]),
                                reads=['qko', 'identb'], writes=['P3'])
                        S.op('act', lambda e, u=u, pk_=pk_: e.copy(out=kTd[:, :, u:u + 128],
                                                               in_=pk_[:, 512:768].rearrange("p (j t) -> p j t", j=2)),
                             reads=['P3'], writes=['kTd'])
                    for g in range(14):
                        pb = P[4 + g % 2]; pbk = 'P%d' % (4 + g % 2)
                        for j in range(8):
                            S.op('pe', lambda e, j=j, g=g, pb=pb, ntok=ntok: e.matmul(pb[:, 0:ntok], lhsT=winb[:, j, 768 + g * 128:768 + (g + 1) * 128],
                                                                         rhs=hT[:, j, 0:ntok], start=(j == 0), stop=(j == 7)),
                                 reads=['hT', 'winb'], writes=[pbk])
                        rs = rstg[g % 2]; rk = 'rstg%d' % (g % 2)
                        S.op('act' if g % 2 else 'dve',
                             (lambda e, rs=rs, pb=pb, ntok=ntok: e.copy(out=rs[:, 0:ntok], in_=pb[:, 0:ntok])) if g % 2 else
                             (lambda e, rs=rs, pb=pb, ntok=ntok: e.tensor_copy(out=rs[:, 0:ntok], in_=pb[:, 0:ntok])),
                             reads=[pbk], writes=[rk])
                        S.dma(lambda e, rs=rs, g=g, u0=u0, ntok=ntok: e.dma_start(out=rwT[g * 128:(g + 1) * 128, u0:u0 + ntok], in_=rs[:, 0:ntok]),
                              reads=[rk], writes=['rwT'])
                if dbg:
                    o1 = dout("dbg_qT", [128, 4 * TL], BF16)
                    S.dma(lambda e: e.dma_start(out=o1[:, :], in_=qT_all[:].rearrange("p a t -> p (a t)")), reads=['qT_all'])
                    o2 = dout("dbg_kTd", [128, 2 * TT], BF16)
                    S.dma(lambda e: e.dma_start(out=o2[:, :], in_=kTd[:].rearrange("p a t -> p (a t)")), reads=['kTd'])
                    o3 = dout("dbg_rwT", [1792, TT])
                    S.dma(lambda e: e.dma_start(out=o3[:, :], in_=rwT[:, :]), reads=['rwT'])
                S.barrier()
                S.flush()
            if stop_after <= 1:
                return nc, dbg_outs
            with contextlib.ExitStack() as ph:
                pT = [sb(ph, "pT%d" % i, [128, 512], BF16) for i in range(3)]
                rec = sb(ph, "rec", [128, 8], F32)
                atok = sb(ph, "atok", [128, 8, 64], BF16)
                pi = 0
                for g in range(int(os.environ.get('K_NG', 16))):
                    q0 = g * 256
                    for st in range(NT):
                        for hp in range(int(os.environ.get('K_NHP', 4))):
                            sb0 = 2 * (hp % 2)
                            scb = PS[:, sb0 * 512:(sb0 + 2) * 512].rearrange("p (b n) -> p b n", b=2)
                            sck = 'P%d' % sb0
                            kvh = hp // 2
                            for hh in range(int(os.environ.get('K_HH0', 0)), int(os.environ.get('K_NHH', 2))):
                                S.op('pe', lambda e, hh=hh, hp=hp, scb=scb, kvh=kvh, st=st, q0=q0: e.matmul(
                                    scb[:, hh, 0:256],
                                    lhsT=kTd[hh * 64:(hh + 1) * 64, kvh, st * 128:(st + 1) * 128],
                                    rhs=qT_all[hh * 64:(hh + 1) * 64, hp, q0:q0 + 256], start=True, stop=True),
                                    reads=['kTd', 'qT_all'], writes=[sck])
                            pt = pT[pi % 3]; ptk = 'pT%d' % (pi % 3)
                            pi += 1
                            if not os.environ.get('K_SKIP_EXP'):
                                S.op('act', lambda e, pt=pt, scb=scb: e.activation(out=pt[:].rearrange('p (b n) -> p b n', b=2), in_=scb[:, :, 0:256], func=AF.Exp, scale=0.125),
                                     reads=[sck], writes=[ptk])
                            for hh in range(2 if not os.environ.get('K_SKIP_PV') else 0):
                                head = 2 * hp + hh
                                for qt in range(2):
                                    ab = P[4 + 2 * qt + head // 4]; abk = 'P%d' % (4 + 2 * qt + head // 4)
                                    c0 = (head % 4) * 65
                                    S.op('pe', lambda e, ab=ab, c0=c0, pt=pt, hh=hh, qt=qt, st=st, kvh=kvh, head=head: e.matmul(
                                        ab[:, c0:c0 + 65], lhsT=pt[:, hh * 256 + qt * 128:hh * 256 + (qt + 1) * 128],
                                        rhs=Vx[:, st, kvh, 0:65], start=(st == 0 and head % 4 == 0), stop=(st == NT - 1 and head % 4 == 3)),
                                        reads=[ptk, 'Vx'], writes=[abk])
                    for qt in range(2 if not os.environ.get('K_SKIP_NORM') else 0):
                        for half in range(2):
                            ab = P[4 + 2 * qt + half]; abk = 'P%d' % (4 + 2 * qt + half)
                            av = ab[:, 0:260].rearrange("p (h c) -> p h c", c=65)
                            S.op('dve', lambda e, av=av, half=half: e.reciprocal(out=rec[:, half * 4:(half + 1) * 4], in_=av[:, :, 64]),
                                 reads=[abk], writes=['rec'])
                            S.op('dve', lambda e, av=av, half=half: e.tensor_tensor(
                                out=atok[:, half * 4:(half + 1) * 4, :], in0=av[:, :, 0:64],
                                in1=rec[:, half * 4:(half + 1) * 4].unsqueeze(2).to_broadcast([128, 4, 64]), op=ALU.mult),
                                reads=[abk, 'rec'], writes=['atok'])
                        pa = P[0][:].bitcast(BF16)
                        if os.environ.get('K_SKIP_TR'):
                            continue
                        for hp in range(4):
                            S.op('pe', lambda e, hp=hp, pa=pa: e.transpose(
                                out=pa[:, hp * 128:(hp + 1) * 128],
                                in_=atok[:, 2 * hp:2 * hp + 2, :].rearrange("p a d -> p (a d)"), identity=identb[:]),
                                reads=['atok', 'identb'], writes=['P0'])
                        if os.environ.get('K_SKIP_CP'):
                            continue
                        S.op('dve', lambda e, pa=pa, q0=q0, qt=qt: e.tensor_copy(
                            out=attT_all[:, :, q0 + qt * 128:q0 + (qt + 1) * 128],
                            in_=pa[:, 0:512].rearrange("p (j t) -> p j t", j=4)), reads=['P0'], writes=['attT_all'])
                S.dma(lambda e: e.dma_start(out=attD[:, :], in_=attT_all[:].rearrange("p a t -> p (a t)")), reads=['attT_all'], writes=['attD'])
                if dbg:
                    o1 = dout("dbg_attT", [128, 4 * TL], BF16)
                    S.dma(lambda e: e.dma_start(out=o1[:, :], in_=attT_all[:].rearrange("p a t -> p (a t)")), reads=['attT_all'])
                S.barrier()
                S.flush()
        if stop_after <= 2:
            return nc, dbg_outs

        def TTo(eng, out, a, b_, op, R, W):
            S.op(eng, lambda e: e.tensor_tensor(out=out, in0=a, in1=b_, op=op), reads=R, writes=W)

        def CP(eng, out, in_, R, W):
            if eng == 'act':
                S.op('act', lambda e: e.copy(out=out, in_=in_), reads=R, writes=W)
            else:
                S.op(eng, lambda e: e.tensor_copy(out=out, in_=in_), reads=R, writes=W)

        def ACTF(out, in_, func, R, W, scale=1.0, bias=None):
            if bias is None:
                S.op('act', lambda e: e.activation(out=out, in_=in_, func=func, scale=scale), reads=R, writes=W)
            else:
                S.op('act', lambda e: e.activation(out=out, in_=in_, func=func, scale=scale, bias=bias), reads=R, writes=W)

        def MM(out, lhsT, rhs, R, W, start=True, stop=True):
            S.op('pe', lambda e: e.matmul(out, lhsT=lhsT, rhs=rhs, start=start, stop=stop), reads=R, writes=W)

        def TR(out, in_, idn, R, W):
            S.op('pe', lambda e: e.transpose(out=out, in_=in_, identity=idn), reads=R, writes=W)

        with contextlib.ExitStack() as ph:
            def t32(name, shape=(64, 1024)):
                return sb(ph, name, list(shape), F32)
            rp = t32("rp", (64, 8, 10)); rpd = t32("rpd", (64, 8, 8))
            lmu = t32("lmu", (128, 3)); lmd = t32("lmd", (128, 6))
            decupb = sb(ph, "decupb", [64, 2, 512], BF16); iclupb = sb(ph, "iclupb", [64, 2, 512], BF16)
            gateupb = sb(ph, "gateupb", [128, 512], BF16)
            mk4 = t32("mk4", (128, 2, 512)); mn1 = t32("mn1", (128, 2, 128)); bm = t32("bm", (128, 512))
            rst = t32("rst"); ones64 = sb(ph, "ones64", [64, 64], BF16); tiny = t32("tiny", (64, 1))
            S.dma(lambda e: e.dma_start(out=rp[:].rearrange("k h n -> k (h n)"), in_=rp_d[:, :]), writes=['rp'])
            S.dma(lambda e: e.dma_start(out=lmu[:], in_=lmu_d[:, :]), writes=['lmu'])
            S.dma(lambda e: e.dma_start(out=mk4[:].rearrange("p a n -> p (a n)"), in_=mk4_d[:, :]), writes=['mk4'])
            S.dma(lambda e: e.dma_start(out=mn1[:].rearrange("p a n -> p (a n)"), in_=mn1_d[:, :]), writes=['mn1'])
            S.dma(lambda e: e.dma_start(out=bm[:], in_=bm_d[:, :]), writes=['bm'])
            S.dma(lambda e: e.dma_start(out=rst[:], in_=rst_d[:, :]), writes=['rst'])
            S.op('pool', lambda e: e.memset(ones64[:], 1.0), writes=['ones64'])
            S.op('pool', lambda e: e.memset(tiny[:], 1e-24), writes=['tiny'])
            for i in range(3):
                S.op('dve', lambda e, i=i: e.tensor_scalar(out=rpd[:, :, 2 * i], in0=rp[:, :, i], scalar1=0.5, scalar2=None, op0=ALU.mult),
                     reads=['rp'], writes=['rpd'])
                S.op('dve', lambda e, i=i: e.tensor_scalar(out=rpd[:, :, 2 * i + 1], in0=rp[:, :, i], scalar1=-1.0, scalar2=1.0,
                                                          op0=ALU.mult, op1=ALU.add), reads=['rp'], writes=['rpd'])
                S.op('dve', lambda e, i=i: e.tensor_scalar(out=lmd[:, 2 * i:2 * i + 1], in0=lmu[:, i:i + 1], scalar1=0.5, scalar2=None, op0=ALU.mult),
                     reads=['lmu'], writes=['lmd'])
                S.op('dve', lambda e, i=i: e.tensor_scalar(out=lmd[:, 2 * i + 1:2 * i + 2], in0=lmu[:, i:i + 1], scalar1=-1.0, scalar2=1.0,
                                                          op0=ALU.mult, op1=ALU.add), reads=['lmu'], writes=['lmd'])
            S.op('dve', lambda e: e.tensor_scalar(out=rpd[:, :, 6], in0=rp[:, :, 4], scalar1=-1.0, scalar2=1.0, op0=ALU.mult, op1=ALU.add),
                 reads=['rp'], writes=['rpd'])

            def bc(t2):
                return t2.unsqueeze(2).to_broadcast([64, 8, 128])

            pin = [sb(ph, "pin%d" % i, [64, 8, 130], F32) for i in range(3)]
            plo = [sb(ph, "plo%d" % i, [128, 130], F32) for i in range(3)]
            tS = t32("tS"); xr = t32("xr"); xk = t32("xk"); xv = t32("xv")
            xlo = t32("xlo", (128, 3, 128)); twl = sb(ph, "twl", [64, 128], BF16); xalb = sb(ph, "xalb", [64, 128], BF16)
            glsb = sb(ph, "glsb", [128, 128], BF16)
            sqb = sb(ph, "sqb", [64, 1024], BF16); kk = t32("kk")
            sg = t32("sg"); ad = t32("ad"); cs = t32("cs"); ex = t32("ex"); E1 = t32("E1"); E2s = [t32("E2_0"), t32("E2_1")]; E3 = t32("E3")
            bb = t32("bb"); t1 = t32("t1"); kd = bb; bt32 = t32("bt32"); kt32 = t32("kt32"); rkb = sqb; kkk = t1; rs = E1; csb = bt32; bv = kt32
            bvt = t32("bvt", (128, 512)); gtok = t32("gtok", (128, 512)); glS = t32("glS", (128, 128))
            opTs = [{n: sb(ph, "op%d_" % q_ + n, [64, 1024], BF16) for n in ("a", "r", "b", "k", "bh", "kh", "v")} for q_ in range(2)]
            N1Ta, N1a, IN1Ta, N2a, N2Ta, IN2Ta, N4a, N4Ta, IN4Ta, IN8Ta, AakTa, ArbTa, ArkTa = [
                sb(ph, "ba%d" % i, [128, 8, 128], BF16) for i in range(13)]
            TMa = sb(ph, "TMa", [128, 8, 5, 64], BF16)
            Xa = [sb(ph, "Xa%d" % i, [128, 8, 128], BF16) for i in range(2)]
            Bbd = sb(ph, "Bbd", [128, 512], BF16); Ubd = sb(ph, "Ubd", [128, 512], BF16); Vbd = sb(ph, "Vbd", [128, 512], BF16)
            GTs = sb(ph, "GTs", [64, 512], BF16); Es = t32("Es", (64, 512)); QTs = sb(ph, "QTs", [64, 128], BF16)
            Y0s = t32("Y0s", (128, 64)); ytmp = gtok; yc = t32("yc", (128, 64))
            Yblk = t32("Yblk", (128, 8, 64))
            H = t32("H", (64, 512)); Hb = sb(ph, "Hb", [64, 512], BF16); Ht = t32("Ht", (64, 512))

            def view_hct(t):
                return t[:, :]

            def view_out(t):
                return t[:, :]

            def g16(t):
                return t[:, :].rearrange("k (g t) -> k g t", t=16)

            def chv(t, c):
                return t[:, :].rearrange("k (h c t) -> k h c t", h=8, c=8)[:, :, c, :]

            for (dst, src, nm, rows) in ((decupb, decup_d, 'decupb', 64), (iclupb, iclup_d, 'iclupb', 64), (gateupb, gateup_d, 'gateupb', 128)):
                stg_, sk_ = (bt32, 'bt32') if rows == 64 else (bvt, 'bvt')
                S.dma(lambda e, src=src, stg_=stg_: e.dma_start(out=stg_[:, :], in_=src[:, :]), writes=[sk_])
                dv = dst[:].rearrange("p a n -> p (a n)") if rows == 64 else dst[:]
                CP('dve', dv, stg_[:, :], [sk_], [nm])
            blocks = []
            nblk = int(os.environ.get('K_RBLK', 99))
            for d_ in range(2):
                cb_ = [0, 1] if d_ == 0 else [1, 0]
                lb_ = list(range(2, NT)) if d_ == 0 else list(range(NT - 1, 1, -1))
                blocks += [(d_, b_, j_ == 0) for j_, b_ in enumerate((cb_ + lb_)[:nblk])]

            def emit_prep(idx):
                d, blk, first = blocks[idx]
                opT = opTs[idx % 2]; E2 = E2s[idx % 2]; e2k = 'E2_%d' % (idx % 2); opk = 'o%d_' % (idx % 2)

                u0 = blk * 128
                is_ctx = blk < 2
                seq_lo, seq_hi = (0, TC) if is_ctx else (TC, TT)
                lo = max(u0 - 1, seq_lo); hi = min(u0 + 129, seq_hi)
                c_lo = lo - (u0 - 1); c_hi = c_lo + (hi - lo)
                for i in range(3):
                    if c_lo > 0:
                        S.op('pool', lambda e, i=i: e.memset(pin[i][:, :, 0:1], 0.0), writes=['pin%d' % i])
                    if c_hi < 130:
                        S.op('pool', lambda e, i=i: e.memset(pin[i][:, :, 129:130], 0.0), writes=['pin%d' % i])
                    S.dma(lambda e, i=i, lo=lo, hi=hi, c_lo=c_lo, c_hi=c_hi: e.dma_start(
                        out=pin[i][:, :, c_lo:c_hi],
                        in_=rwT[i * 512:(i + 1) * 512, lo:hi].rearrange("(h k) t -> k h t", k=64)), reads=['rwT'], writes=['pin%d' % i])
                for i, (r0, nr) in enumerate(((1536, 64), (1600, 64), (1664, 128))):
                    if c_lo > 0:
                        S.op('pool', lambda e, i=i: e.memset(plo[i][:, 0:1], 0.0), writes=['plo%d' % i])
                    if c_hi < 130:
                        S.op('pool', lambda e, i=i: e.memset(plo[i][:, 129:130], 0.0), writes=['plo%d' % i])
                    S.dma(lambda e, i=i, r0=r0, nr=nr, lo=lo, hi=hi, c_lo=c_lo, c_hi=c_hi: e.dma_start(
                        out=plo[i][0:nr, c_lo:c_hi], in_=rwT[r0:r0 + nr, lo:hi]), reads=['rwT'], writes=['plo%d' % i])
                for i, xo in enumerate((xr, xk, xv)):
                    xo3 = xo[:, :].rearrange("k (h t) -> k h t", h=8)
                    ts3 = tS[:, :].rearrange("k (h t) -> k h t", h=8)
                    TTo('pool', ts3, pin[i][:, :, 0:128], pin[i][:, :, 2:130], ALU.add, ['pin%d' % i], ['tS'])
                    TTo('pool', ts3, ts3, bc(rpd[:, :, 2 * i]), ALU.mult, ['tS', 'rpd'], ['tS'])
                    TTo('dve', xo3, pin[i][:, :, 1:129], bc(rpd[:, :, 2 * i + 1]), ALU.mult, ['pin%d' % i, 'rpd'], ['x%d' % i])
                    TTo('dve', xo3, xo3, ts3, ALU.add, ['x%d' % i, 'tS'], ['x%d' % i])
                for i, nr in enumerate((64, 64, 128)):
                    S.op('pool', lambda e, i=i, nr=nr: e.tensor_tensor(out=tS[0:nr, 0:128] if nr == 64 else glS[:, 0:128],
                                                                     in0=plo[i][0:nr, 0:128], in1=plo[i][0:nr, 2:130], op=ALU.add),
                         reads=['plo%d' % i], writes=['tS' if nr == 64 else 'glS'])
                    S.op('dve', lambda e, i=i, nr=nr: e.tensor_scalar(out=xlo[0:nr, i, :], in0=plo[i][0:nr, 1:129],
                                                                    scalar1=lmd[0:nr, 2 * i + 1:2 * i + 2], scalar2=None, op0=ALU.mult),
                         reads=['plo%d' % i, 'lmd'], writes=['xlo'])
                    S.op('dve', lambda e, i=i, nr=nr: e.scalar_tensor_tensor(
                        out=xlo[0:nr, i, :], in0=(tS[0:nr, 0:128] if nr == 64 else glS[:, 0:128]), scalar=lmd[0:nr, 2 * i:2 * i + 1],
                        in1=xlo[0:nr, i, :], op0=ALU.mult, op1=ALU.add),
                        reads=['tS' if nr == 64 else 'glS', 'lmd', 'xlo'], writes=['xlo'])
                ACTF(twl[:], xlo[0:64, 0, :], AF.Tanh, ['xlo'], ['twl'])
                CP('pool', xalb[:], xlo[0:64, 1, :], ['xlo'], ['xalb'])
                k3 = lambda t: t[:, :].rearrange("k (h t) -> k h t", h=8)
                TTo('pool', k3(kkk), k3(xk), bc(rp[:, :, 3]), ALU.mult, ['x1', 'rp'], ['t1'])
                ACTF(sqb[:], kkk[:], AF.Square, ['t1'], ['sqb'])
                PP = PS[0:64, 0:1024]
                for hf in range(2):
                    MM(PS[0:64, hf * 512:(hf + 1) * 512], ones64[:], sqb[:, hf * 512:(hf + 1) * 512], ['ones64', 'sqb'], ['P0'])
                ACTF(rs[:], PP, AF.Ln, ['P0', 'tiny'], ['E1'], bias=tiny[:])
                ACTF(rs[:], rs[:], AF.Exp, ['E1'], ['E1'], scale=-0.5)
                TTo('dve', kk[:], kkk[:], rs[:], ALU.mult, ['t1', 'E1'], ['kk'])
                for h in range(8):
                    MM(PS[0:64, h * 128:(h + 1) * 128], decupb[:, d, h * 64:(h + 1) * 64], twl[:], ['decupb', 'twl'], ['P0'])
                TTo('dve', k3(sg), PP.rearrange("k (h t) -> k h t", h=8), bc(rp[:, :, 6 + d]), ALU.add, ['P0', 'rp'], ['sg'])
                ACTF(sg[:], sg[:], AF.Sigmoid, ['sg'], ['sg'])
                for h in range(8):
                    MM(PS[0:64, h * 128:(h + 1) * 128], iclupb[:, d, h * 64:(h + 1) * 64], xalb[:], ['iclupb', 'xalb'], ['P0'])
                TTo('dve', k3(ad), PP.rearrange("k (h t) -> k h t", h=8), bc(rp[:, :, 8 + d]), ALU.add, ['P0', 'rp'], ['ad'])
                ACTF(ad[:], ad[:], AF.Sigmoid, ['ad'], ['ad'])
                S.op('dve', lambda e: e.tensor_tensor_scan(out=cs[:], data0=rst[:], data1=sg[:], initial=0.0, op0=ALU.mult, op1=ALU.add),
                     reads=['rst', 'sg'], writes=['cs'])
                csf = cs
                if d == 1:
                    TTo('pool', ex[:], sg[:], cs[:], ALU.subtract, ['sg', 'cs'], ['ex'])
                    csv = cs[:, :].rearrange("k (g t) -> k g t", t=16)
                    TTo('dve', csb[:, :].rearrange("k (g t) -> k g t", t=16), ex[:, :].rearrange("k (g t) -> k g t", t=16),
                       csv[:, :, 15:16].to_broadcast([64, 64, 16]), ALU.add, ['ex', 'cs'], ['bt32'])
                    csf = csb
                ACTF(E2[:], csf[:], AF.Exp, ['cs', 'bt32'], [e2k], scale=DEC_C)
                TTo('pool', ex[:], csf[:], sg[:], ALU.subtract, ['cs', 'bt32', 'sg'], ['ex'])
                ACTF(E1[:], ex[:], AF.Exp, ['ex'], ['E1'], scale=DEC_C)
                S.op('dve', lambda e: e.reciprocal(out=E3[:], in_=E2[:]), reads=[e2k], writes=['E3'])
                tsel = 15 if d == 0 else 0
                def cm(t, h):
                    return t[:, :].rearrange("k (c h t) -> k c h t", c=8, h=8)[:, :, h, :]

                def hm(t, h):
                    return t[:, :].rearrange("k (h c t) -> k h c t", h=8, c=8)[:, h, :, :]
                TTo('pool', bb[:], kk[:], ad[:], ALU.mult, ['kk', 'ad'], ['bb'])
                TTo('dve', bt32[:], bb[:], E3[:], ALU.mult, ['bb', 'E3'], ['bt32'])
                TTo('pool', k3(t1), k3(ad), bc(rp[:, :, 4]), ALU.mult, ['ad', 'rp'], ['t1'])
                TTo('dve', k3(t1), k3(t1), bc(rpd[:, :, 6]), ALU.add, ['t1', 'rpd'], ['t1'])
                TTo('pool', kd[:], xk[:], t1[:], ALU.mult, ['x1', 't1'], ['bb'])
                TTo('dve', kt32[:], kd[:], E3[:], ALU.mult, ['bb', 'E3'], ['kt32'])
                for h in range(8):
                    pch = hm(E2, h)[:, :, tsel:tsel + 1].to_broadcast([64, 8, 16])
                    S.op('dve', lambda e, h=h: e.scalar_tensor_tensor(out=cm(opT['a'], h), in0=hm(kk, h), scalar=-1.0, in1=hm(E1, h),
                                                                     op0=ALU.mult, op1=ALU.mult), reads=['kk', 'E1'], writes=[opk + 'a'])
                    TTo('pool', cm(opT['r'], h), hm(xr, h), hm(E2, h), ALU.mult, ['x0', e2k], [opk + 'r'])
                    CP('act', cm(opT['b'], h), hm(bt32, h), ['bt32'], [opk + 'b'])
                    TTo('pool', cm(opT['bh'], h), hm(bt32, h), pch, ALU.mult, ['bt32', e2k], [opk + 'bh'])
                    CP('act', cm(opT['k'], h), hm(kt32, h), ['kt32'], [opk + 'k'])
                    TTo('dve', cm(opT['kh'], h), hm(kt32, h), pch, ALU.mult, ['kt32', e2k], [opk + 'kh'])
                    CP('act', cm(opT['v'], h), hm(xv, h), ['x2'], [opk + 'v'])
                if d == 0 and not is_ctx:
                    ul = u0 - TC
                    TTo('pool', k3(t1), k3(xr), bc(rp[:, :, 5]), ALU.mult, ['x0', 'rp'], ['t1'])
                    TTo('dve', rkb[:], t1[:], xk[:], ALU.mult, ['t1', 'x1'], ['sqb'])
                    for hf in range(2):
                        MM(PS[0:64, hf * 512:(hf + 1) * 512], ones64[:], rkb[:, hf * 512:(hf + 1) * 512], ['ones64', 'sqb'], ['P0'])
                    TTo('dve', bv[:], PP, xv[:], ALU.mult, ['P0', 'x2'], ['kt32'])
                    for h in range(8):
                        TR(PS[:, 512 + h * 64:512 + (h + 1) * 64], bv[:, h * 128:(h + 1) * 128], identf[0:64, 0:64], ['kt32', 'identf'], ['P0'])
                    CP('dve', bvt[:], PS[:, 512:1024], ['P0'], ['bvt'])
                    S.dma(lambda e, ul=ul: e.dma_start(out=bonD[ul:ul + 128, :], in_=bvt[:]), reads=['bvt'], writes=['bonD'])
                    ACTF(glsb[:], xlo[:, 2, :], AF.Sigmoid, ['xlo'], ['glsb'])
                    MM(PS[:, 512:1024], glsb[:], gateupb[:], ['glsb', 'gateupb'], ['P0'])
                    CP('dve', gtok[:], PS[:, 512:1024], ['P0'], ['gtok'])
                    S.dma(lambda e, ul=ul: e.dma_start(out=gD[ul:ul + 128, :], in_=gtok[:]), reads=['gtok'], writes=['gD'])

            def emit_chunks(idx):
                d, blk, first = blocks[idx]
                opT = opTs[idx % 2]; E2 = E2s[idx % 2]; e2k = 'E2_%d' % (idx % 2); opk = 'o%d_' % (idx % 2)
                u0 = blk * 128
                is_ctx = blk < 2
                tsel = 15 if d == 0 else 0
                if first:
                    S.op('pool', lambda e: e.memset(H[:], 0.0), writes=['H'])
                    S.op('pool', lambda e: e.memset(Hb[:], 0.0), writes=['Hb'])

                PA_, PB_, PC_, PD_ = (PS[:, 0:1024], PS[:, 1024:2048], PS[:, 2048:3072], PS[:, 3072:4096])
                kPB, kPC, kPD = ['P2', 'P3'], ['P4', 'P5'], ['P6', 'P7']
                c8 = lambda ap: ap.rearrange("p (c n) -> p c n", c=8)
                def bc8(m):
                    return m.unsqueeze(1).to_broadcast([128, 8, 128])
                MSd = mk4[:, d, 0:128]; MId = mk4[:, d, 128:256]; MStd = mn1[:, d, :]
                def ch(name, c):
                    return opT[name][:, c * 128:(c + 1) * 128]
                for c in range(8):
                    MM(PB_[:, c * 128:(c + 1) * 128], ch('b', c), ch('a', c), [opk + 'b', opk + 'a'], kPB)
                for c in range(8):
                    MM(PC_[:, c * 128:(c + 1) * 128], ch('a', c), ch('b', c), [opk + 'a', opk + 'b'], kPC)
                for c in range(8):
                    MM(PD_[:, c * 128:(c + 1) * 128], ch('k', c), ch('a', c), [opk + 'k', opk + 'a'], kPD)
                TTo('dve', N1Ta[:], c8(PB_), bc8(MSd), ALU.mult, kPB + ['mk4'], ['N1Ta'])
                TTo('dve', N1a[:], c8(PC_), bc8(MStd), ALU.mult, kPC + ['mn1'], ['N1a'])
                TTo('dve', AakTa[:], c8(PD_), bc8(MSd), ALU.mult, kPD + ['mk4'], ['AakTa'])
                TTo('pool', IN1Ta[:], N1Ta[:], bc8(identb[:, :]), ALU.add, ['N1Ta', 'identb'], ['IN1Ta'])
                PBb = PB_.bitcast(BF16)
                for c in range(8):
                    for si, nm_ in ((0, 'a'), (1, 'v'), (2, 'bh'), (3, 'kh')):
                        TR(PBb[:, c * 256 + si * 64:c * 256 + (si + 1) * 64], ch(nm_, c), identb[0:64, 0:64], [opk + nm_, 'identb'], kPB)
                PBb4 = PBb.rearrange("p (c s n) -> p c s n", c=8, s=4)
                CP('dve', TMa[:, :, 0, :], PBb4[:, :, 0, :], kPB, ['TMa'])
                CP('dve', TMa[:, :, 2:5, :].rearrange("p c s n -> p c (s n)"), PBb.rearrange("p (c n) -> p c n", c=8)[:, :, 64:256], kPB, ['TMa'])
                for c in range(8):
                    MM(PC_[:, c * 128:(c + 1) * 128], N1Ta[:, c, :], N1a[:, c, :], ['N1Ta', 'N1a'], kPC)
                for c in range(8):
                    MM(PD_[:, c * 128:(c + 1) * 128], N1a[:, c, :], N1Ta[:, c, :], ['N1Ta', 'N1a'], kPD)
                CP('act', N2a[:], c8(PC_), kPC, ['N2a'])
                CP('dve', N2Ta[:], c8(PD_), kPD, ['N2Ta'])
                TTo('pool', IN2Ta[:], N2Ta[:], bc8(identb[:, :]), ALU.add, ['N2Ta', 'identb'], ['IN2Ta'])
                for c in range(8):
                    MM(PB_[:, c * 64:(c + 1) * 64], AakTa[:, c, :], TMa[:, c, 2, :], ['AakTa', 'TMa'], kPB)
                CP('dve', TMa[:, :, 1, :], PB_[:, 0:512].rearrange("p (c n) -> p c n", c=8), kPB, ['TMa'])
                for c in range(8):
                    MM(PC_[:, c * 128:(c + 1) * 128], N2Ta[:, c, :], N2a[:, c, :], ['N2Ta', 'N2a'], kPC)
                for c in range(8):
                    MM(PD_[:, c * 128:(c + 1) * 128], N2a[:, c, :], N2Ta[:, c, :], ['N2Ta', 'N2a'], kPD)
                CP('act', N4a[:], c8(PC_), kPC, ['N4a'])
                CP('dve', N4Ta[:], c8(PD_), kPD, ['N4Ta'])
                TTo('pool', IN4Ta[:], N4Ta[:], bc8(identb[:, :]), ALU.add, ['N4Ta', 'identb'], ['IN4Ta'])
                for c in range(8):
                    MM(PB_[:, c * 128:(c + 1) * 128], N4a[:, c, :], N4Ta[:, c, :], ['N4a', 'N4Ta'], kPB)
                TTo('dve', IN8Ta[:], c8(PB_), bc8(identf[:, :]), ALU.add, kPB + ['identf'], ['IN8Ta'])
                if not is_ctx:
                    for c in range(8):
                        MM(PC_[:, c * 128:(c + 1) * 128], ch('b', c), ch('r', c), [opk + 'b', opk + 'r'], kPC)
                    for c in range(8):
                        MM(PD_[:, c * 128:(c + 1) * 128], ch('k', c), ch('r', c), [opk + 'k', opk + 'r'], kPD)
                    TTo('dve', ArbTa[:], c8(PC_), bc8(MId), ALU.mult, kPC + ['mk4'], ['ArbTa'])
                    TTo('dve', ArkTa[:], c8(PD_), bc8(MId), ALU.mult, kPD + ['mk4'], ['ArkTa'])
                xsrc = None
                for li, (INa, ik) in enumerate(((IN8Ta, 'IN8Ta'), (IN4Ta, 'IN4Ta'), (IN2Ta, 'IN2Ta'), (IN1Ta, 'IN1Ta'))):
                    Pq, kq = (PB_, kPB) if li % 2 == 0 else (PC_, kPC)
                    for c in range(8):
                        rhs_ = TMa[:, c, 0:2, :].rearrange("p a n -> p (a n)") if xsrc is None else xsrc[:, c, :]
                        MM(Pq[:, c * 128:(c + 1) * 128], INa[:, c, :], rhs_, [ik, 'TMa' if xsrc is None else xk_], kq)
                    Xn = Xa[li % 2]; xk_ = 'Xa%d' % (li % 2)
                    CP('dve' if li % 2 else 'act', Xn[:], c8(Pq), kq, [xk_])
                    xsrc = Xn
                B3 = PS[:, 1536:2048]; B4 = PS[:, 2048:2560]; B5 = PS[:, 2560:3072]; B6 = PS[:, 3072:3584]; B7 = PS[:, 3584:4096]
                bm3 = bm[:, :].rearrange("p (h n) -> p h n", h=8)
                nch = int(os.environ.get('K_RCH', 8))
                for c in (list(range(8)) if d == 0 else list(range(7, -1, -1)))[:nch]:
                    Wc = xsrc[:, c, 0:64]; U0 = xsrc[:, c, 64:128]
                    Bh_ = TMa[:, c, 3, :]; Kh_ = TMa[:, c, 4, :]; Vt_ = TMa[:, c, 2, :]
                    for dst_, src_, rk_, wk_ in ((Bbd, Bh_, 'TMa', 'Bbd'), (Ubd, U0, xk_, 'Ubd'), (Vbd, Vt_, 'TMa', 'Vbd')):
                        TTo('pool', dst_[:, :].rearrange("p (h n) -> p h n", h=8), src_.unsqueeze(1).to_broadcast([128, 8, 64]), bm3, ALU.mult,
                            [rk_, 'bm'], [wk_])
                    MM(B4[0:64, :], Wc, Bbd[:], [xk_, 'Bbd'], ['P4'])
                    CP('act', GTs[:], B4[0:64, :], ['P4'], ['GTs'])
                    MM(B5[0:64, :], Bh_, Ubd[:], ['TMa', 'Ubd'], ['P5'], start=True, stop=False)
                    MM(B5[0:64, :], Kh_, Vbd[:], ['TMa', 'Vbd'], ['P5'], start=False, stop=True)
                    CP('act', Es[:], B5[0:64, :], ['P5'], ['Es'])
                    if not is_ctx:
                        MM(B6[0:64, 0:128], Wc, ArbTa[:, c, :], [xk_, 'ArbTa'], ['P6'])
                        TTo('dve', QTs[:], B6[0:64, 0:128], ch('r', c), ALU.add, ['P6', opk + 'r'], ['QTs'])
                        MM(B6[:, 128:192], ArbTa[:, c, :], U0, ['ArbTa', xk_], ['P6'], start=True, stop=False)
                        MM(B6[:, 128:192], ArkTa[:, c, :], Vt_, ['ArkTa', 'TMa'], ['P6'], start=False, stop=True)
                        CP('dve', Y0s[:], B6[:, 128:192], ['P6'], ['Y0s'])
                        MM(B7, QTs[:], Hb[:], ['QTs', 'Hb'], ['P7'])
                        TTo('dve', ytmp[:], B7, bm[:], ALU.mult, ['P7', 'bm'], ['gtok'])
                        S.op('dve', lambda e: e.tensor_reduce(out=yc[:], in_=ytmp[:, :].rearrange("p (h v) -> p v h", h=8), axis=AX.X, op=ALU.add),
                             reads=['gtok'], writes=['yc'])
                        TTo('pool', Yblk[:, c, :], yc[:], Y0s[:], ALU.add, ['yc', 'Y0s'], ['Yblk'])
                    for h in range(8):
                        MM(B3[0:64, h * 64:(h + 1) * 64], GTs[:, h * 64:(h + 1) * 64], Hb[:, h * 64:(h + 1) * 64], ['GTs', 'Hb'], ['P3'])
                    PCc = chv(E2, c)[:, :, tsel:tsel + 1].to_broadcast([64, 8, 64])
                    TTo('pool', Ht[:, :].rearrange("k (h v) -> k h v", h=8), H[:, :].rearrange("k (h v) -> k h v", h=8), PCc, ALU.mult,
                        ['H', e2k], ['Ht'])
                    TTo('pool', Ht[:], Ht[:], Es[:], ALU.add, ['Ht', 'Es'], ['Ht'])
                    TTo('dve', H[:], Ht[:], B3[0:64, :], ALU.add, ['Ht', 'P3'], ['H'])
                    CP('act', Hb[:], H[:], ['H'], ['Hb'])
                    S.drain(S.pend, (len(S.pend) + 7) // 8)
                if not is_ctx:
                    ul = u0 - TC
                    for h in range(8):
                        S.dma(lambda e, h=h, ul=ul, d=d: e.dma_start(
                            out=ydir[d, ul:ul + 128, h * 64:(h + 1) * 64].rearrange("(c t) v -> t c v", t=16),
                            in_=Yblk[h * 16:(h + 1) * 16, :, :]), reads=['Yblk'], writes=['ydir'])

            S.capture = []
            emit_prep(0)
            pend = S.capture; S.capture = None
            S.drain(pend, len(pend))
            for idx in range(len(blocks)):
                pend = []
                if idx + 1 < len(blocks):
                    S.capture = []
                    emit_prep(idx + 1)
                    pend = S.capture; S.capture = None
                S.pend = pend
                emit_chunks(idx)
                S.drain(pend, len(pend))

            if dbg:
                oy = dout("dbg_y", [2 * TL, 512]); ob = dout("dbg_bon", [TL, 512]); og = dout("dbg_g", [TL, 512])
                S.dma(lambda e: e.dma_start(out=oy[:, :], in_=ydir.rearrange("d t n -> (d t) n")), reads=['ydir'])
                S.dma(lambda e: e.dma_start(out=ob[:, :], in_=bonD[:, :]), reads=['bonD'])
                S.dma(lambda e: e.dma_start(out=og[:, :], in_=gD[:, :]), reads=['gD'])
                oH = dout("dbg_H", [64, 512])
                S.dma(lambda e: e.dma_start(out=oH[:, :], in_=H[:]), reads=['H'])
            S.barrier()
            S.flush()
        if stop_after <= 3:
            return nc, dbg_outs

        with contextlib.ExitStack() as ph:
            woutb = sb(ph, "woutb", [128, 8, D], BF16)
            attT_all = sb(ph, "attT_c", [128, 4, TL], BF16)
            S.dma(lambda e: e.dma_start(out=attT_all[:].rearrange("p a t -> p (a t)"), in_=attD[:, :]), reads=['attD'], writes=['attT_all'])
            cst = {}
            for nm, src in (("gt1", modd[0:1, 2048:3072]), ("sh2", modd[0:1, 3072:4096]), ("sc2p", modd[0:1, 4096:5120]),
                            ("ln1g", ln1_d[0:1, :]), ("ln1b", ln1_d[1:2, :])):
                cst[nm] = sb(ph, nm, [128, D], F32)
                S.dma(lambda e, nm=nm, src=src: e.dma_start(out=cst[nm][:], in_=src.partition_broadcast(128)), reads=['modd'], writes=[nm])
            for nm, row in (("lnxg", 0), ("lnxb", 1)):
                cst[nm] = sb(ph, nm, [128, 512], F32)
                S.dma(lambda e, nm=nm, row=row: e.dma_start(out=cst[nm][:], in_=lnx_d[row:row + 1, :].partition_broadcast(128)), writes=[nm])
            wst2 = [sb(ph, "wst2_%d" % i, [128, D], F32) for i in range(2)]
            for j in range(8):
                w = wst2[j % 2]; wk = 'wst2_%d' % (j % 2)
                S.dma(lambda e, w=w, j=j: e.dma_start(out=w[:], in_=wout_d[j * 128:(j + 1) * 128, :]), writes=[wk])
                CP('pool' if j % 2 else 'dve', woutb[:, j, :], w[:], [wk], ['woutb'])
            rwf = sb(ph, "rwf", [128, 8, 16], F32)
            S.dma(lambda e: e.dma_start(out=rwf[:], in_=rw_d.rearrange("(j p) n -> p j n", p=128)), writes=['rwf'])
            gneps = sb(ph, "gneps", [128, 1], F32)
            S.op('pool', lambda e: e.memset(gneps[:], 64e-5), writes=['gneps'])
            yf = sb(ph, "yf", [128, 512], F32); yb = sb(ph, "yb", [128, 512], F32)
            bon = sb(ph, "bon", [128, 512], F32); gg = sb(ph, "gg", [128, 512], F32)
            ysum = sb(ph, "ysum", [128, 512], F32); ysq = sb(ph, "ysq", [128, 512], F32)
            gst = sb(ph, "gst", [128, 8], F32); gvar = sb(ph, "gvar", [128, 8], F32)
            rwob = sb(ph, "rwob", [128, 512], BF16); rwoT = sb(ph, "rwoT", [128, 4, 128], BF16)
            xin_t = sb(ph, "xin_t", [128, D], F32); tres = sb(ph, "tres", [128, D], F32)
            x1t = sb(ph, "x1t", [128, D], F32); h2f = sb(ph, "h2f", [128, D], F32); h2b = sb(ph, "h2b", [128, D], BF16)
            h2T = sb(ph, "h2T", [128, 8, 128], F32)
            stats = sb(ph, "statsC", [128, 2, 6], F32); mv = sb(ph, "mvC", [128, 2], F32)
            rstd = sb(ph, "rstdC", [128, 1], F32); nb = sb(ph, "nbC", [128, 1], F32)
            lntC = (stats, mv, rstd, nb, 'C')
            lmax = sb(ph, "lmax", [128, 1], F32); lex = sb(ph, "lex", [128, 16], F32); lsum = sb(ph, "lsum", [128, 1], F32)

            def v8(t):
                return t[:, :].rearrange("p (h v) -> p h v", h=8)

            def b8(t):
                return t[:, :].unsqueeze(2).to_broadcast([128, 8, 64])
            for i in range(int(os.environ.get('K_CT', 32))):
                t0 = i * 128
                S.dma(lambda e, t0=t0: e.dma_start(out=yf[:], in_=ydir[0, t0:t0 + 128, :]), reads=['ydir'], writes=['yf'])
                S.dma(lambda e, t0=t0: e.dma_start(out=yb[:], in_=ydir[1, t0:t0 + 128, :]), reads=['ydir'], writes=['yb'])
                S.dma(lambda e, t0=t0: e.dma_start(out=bon[:], in_=bonD[t0:t0 + 128, :]), reads=['bonD'], writes=['bon'])
                S.dma(lambda e, t0=t0: e.dma_start(out=gg[:], in_=gD[t0:t0 + 128, :]), reads=['gD'], writes=['gg'])
                S.dma(lambda e, t0=t0: e.dma_start(out=xin_t[:], in_=x_d[t0:t0 + 128, :]), writes=['xin_t'])
                TTo('pool', ysum[:], yf[:], yb[:], ALU.add, ['yf', 'yb'], ['ysum'])
                S.op('dve', lambda e: e.tensor_reduce(out=gst[:], in_=v8(ysum), axis=AX.X, op=ALU.add), reads=['ysum'], writes=['gst'])
                S.op('dve', lambda e: e.tensor_scalar(out=gst[:], in0=gst[:], scalar1=-1.0 / 64, scalar2=None, op0=ALU.mult),
                     reads=['gst'], writes=['gst'])
                TTo('dve', v8(ysum), v8(ysum), b8(gst), ALU.add, ['ysum', 'gst'], ['ysum'])
                ACTF(ysq[:], ysum[:], AF.Square, ['ysum'], ['ysq'])
                S.op('dve', lambda e: e.tensor_reduce(out=gvar[:], in_=v8(ysq), axis=AX.X, op=ALU.add), reads=['ysq'], writes=['gvar'])
                ACTF(gvar[:], gvar[:], AF.Ln, ['gvar', 'gneps'], ['gvar'], scale=1.0 / 64, bias=gneps[:])
                ACTF(gvar[:], gvar[:], AF.Exp, ['gvar'], ['gvar'], scale=-0.5)
                TTo('dve', v8(ysum), v8(ysum), b8(gvar), ALU.mult, ['ysum', 'gvar'], ['ysum'])
                TTo('pool', ysum[:], ysum[:], cst['lnxg'][:], ALU.mult, ['ysum', 'lnxg'], ['ysum'])
                TTo('dve', ysum[:], ysum[:], cst['lnxb'][:], ALU.add, ['ysum', 'lnxb'], ['ysum'])
                TTo('pool', ysum[:], ysum[:], bon[:], ALU.add, ['ysum', 'bon'], ['ysum'])
                TTo('dve', rwob[:], ysum[:], gg[:], ALU.mult, ['ysum', 'gg'], ['rwob'])
                pr_ = P[0][:].bitcast(BF16)
                for j in range(4):
                    TR(pr_[:, j * 128:(j + 1) * 128], rwob[:, j * 128:(j + 1) * 128], identb[:], ['rwob', 'identb'], ['P0'])
                CP('dve', rwoT[:], pr_[:, 0:512].rearrange("p (j t) -> p j t", j=4), ['P0'], ['rwoT'])
                for half in range(2):
                    ob = PS[:, 1024 + half * 512:1024 + (half + 1) * 512]
                    for j in range(8):
                        lt = attT_all[:, j, t0:t0 + 128] if j < 4 else rwoT[:, j - 4, :]
                        MM(ob, lt, woutb[:, j, half * 512:(half + 1) * 512], ['attT_all', 'rwoT', 'woutb'], ['P2'], start=(j == 0), stop=(j == 7))
                TTo('dve', tres[:], PS[:, 1024:2048], cst['gt1'][:], ALU.mult, ['P2', 'gt1'], ['tres'])
                S.op('dve', lambda e: e.scalar_tensor_tensor(out=tres[:], in0=xin_t[:], scalar=ALPHA, in1=tres[:], op0=ALU.mult, op1=ALU.add),
                     reads=['xin_t', 'tres'], writes=['tres'])
                ln_stats(lntC, tres, 'tres', eps5, 'eps5')
                S.op('act', lambda e: e.activation(out=x1t[:], in_=tres[:], func=AF.Identity, bias=nb[:], scale=rstd[:]),
                     reads=['tres', 'Cnb', 'Crstd'], writes=['x1t'])
                TTo('pool', x1t[:], x1t[:], cst['ln1g'][:], ALU.mult, ['x1t', 'ln1g'], ['x1t'])
                TTo('dve', x1t[:], x1t[:], cst['ln1b'][:], ALU.add, ['x1t', 'ln1b'], ['x1t'])
                S.dma(lambda e, t0=t0: e.dma_start(out=x1D[t0:t0 + 128, :], in_=x1t[:]), reads=['x1t'], writes=['x1D'])
                ln_stats(lntC, x1t, 'x1t', eps5, 'eps5')
                S.op('act', lambda e: e.activation(out=h2f[:], in_=x1t[:], func=AF.Identity, bias=nb[:], scale=rstd[:]),
                     reads=['x1t', 'Cnb', 'Crstd'], writes=['h2f'])
                TTo('pool', h2f[:], h2f[:], cst['sc2p'][:], ALU.mult, ['h2f', 'sc2p'], ['h2f'])
                TTo('dve', h2f[:], h2f[:], cst['sh2'][:], ALU.add, ['h2f', 'sh2'], ['h2f'])
                CP('pool', h2b[:], h2f[:], ['h2f'], ['h2b'])
                S.dma(lambda e, t0=t0: e.dma_start(out=h2D[t0:t0 + 128, :], in_=h2b[:]), reads=['h2b'], writes=['h2D'])
                for j in range(8):
                    TR(PS[:, 2048 + j * 128:2048 + (j + 1) * 128], h2f[:, j * 128:(j + 1) * 128], identf[:], ['h2f', 'identf'], ['P4'])
                CP('dve', h2T[:].rearrange("p j t -> p (j t)"), PS[:, 2048:3072], ['P4'], ['h2T'])
                for j in range(8):
                    MM(PS[:, 3072:3088], h2T[:, j, :], rwf[:, j, :], ['h2T', 'rwf'], ['P6'], start=(j == 0), stop=(j == 7))
                S.op('dve', lambda e: e.tensor_reduce(out=lmax[:], in_=PS[:, 3072:3088], axis=AX.X, op=ALU.max), reads=['P6'], writes=['lmax'])
                S.op('dve', lambda e: e.tensor_scalar(out=lmax[:], in0=lmax[:], scalar1=-1.0, scalar2=None, op0=ALU.mult), reads=['lmax'], writes=['lmax'])
                ACTF(lex[:], PS[:, 3072:3088], AF.Exp, ['P6', 'lmax'], ['lex'], bias=lmax[:])
                S.op('dve', lambda e: e.tensor_reduce(out=lsum[:], in_=lex[:], axis=AX.X, op=ALU.add), reads=['lex'], writes=['lsum'])
                S.op('dve', lambda e: e.reciprocal(out=lsum[:], in_=lsum[:]), reads=['lsum'], writes=['lsum'])
                S.op('dve', lambda e, i=i: e.tensor_scalar(out=aff_all[:, i, :], in0=lex[:], scalar1=lsum[:], scalar2=None, op0=ALU.mult),
                     reads=['lex', 'lsum'], writes=['aff_all'])
            if dbg:
                o1 = dout("dbg_x1", [TL, D]); o2 = dout("dbg_aff", [128, 512])
                S.dma(lambda e: e.dma_start(out=o1[:, :], in_=x1D[:, :]), reads=['x1D'])
                S.dma(lambda e: e.dma_start(out=o2[:, :], in_=aff_all[:].rearrange("p a b -> p (a b)")), reads=['aff_all'])
            S.barrier()
            S.flush()
        if stop_after <= 4:
            return nc, dbg_outs
        posm = sb(top, "posm", [128, 32, 16], F32)
        gw = sb(top, "gw", [128, 32, 16, 2], BF16)
        onesf = sb(top, "onesf", [128, 128], F32)
        iot = sb(top, "iot", [128, 516], F32)
        S.op('pool', lambda e: e.memset(onesf[:], 1.0), writes=['onesf'])
        S.dma(lambda e: e.dma_start(out=iot[:], in_=iot_d[:, :]), writes=['iot'])

        with contextlib.ExitStack() as ph:
            lo = sb(ph, "lo", [128, 16], F32); hi = sb(ph, "hi", [128, 16], F32); mid = sb(ph, "mid", [128, 16], F32)
            cmpt = sb(ph, "cmpt", [128, 32, 16], F32); cntp = sb(ph, "cntp", [128, 16], F32); ge = sb(ph, "ge", [128, 16], F32)
            dlt = sb(ph, "dlt", [128, 16], F32)
            ustr = sb(ph, "ustr", [128, 128], F32)
            mask = sb(ph, "mask", [128, 32, 16], F32); tot = sb(ph, "tot", [128, 32, 16], F32); cum = sb(ph, "cum", [128, 32, 16], F32)
            glo = sb(ph, "glo", [128, 32, 16], F32); ghi32 = sb(ph, "ghi32", [128, 32, 16], F32)
            S.dma(lambda e: e.dma_start(out=ustr[:], in_=ustr_d[:, :]), writes=['ustr'])
            S.op('pool', lambda e: e.memset(lo[:], 0.0), writes=['lo'])
            S.op('pool', lambda e: e.memset(hi[:], 1.0), writes=['hi'])
            affv = aff_all[:, :, :]
            for it in range(30):
                TTo('dve', mid[:], lo[:], hi[:], ALU.add, ['lo', 'hi'], ['mid'])
                S.op('dve', lambda e: e.tensor_scalar(out=mid[:], in0=mid[:], scalar1=0.5, scalar2=None, op0=ALU.mult), reads=['mid'], writes=['mid'])
                TTo('dve', cmpt[:], affv, mid[:, :].unsqueeze(1).to_broadcast([128, 32, 16]), ALU.is_ge, ['aff_all', 'mid'], ['cmpt'])
                S.op('dve', lambda e: e.tensor_reduce(out=cntp[:], in_=cmpt[:].rearrange("p t e -> p e t"), axis=AX.X, op=ALU.add),
                     reads=['cmpt'], writes=['cntp'])
                MM(PS[:, 0:16], onesf[:], cntp[:], ['onesf', 'cntp'], ['P0'])
                S.op('dve', lambda e: e.tensor_scalar(out=ge[:], in0=PS[:, 0:16], scalar1=511.5, scalar2=None, op0=ALU.is_ge), reads=['P0'], writes=['ge'])
                TTo('dve', dlt[:], mid[:], lo[:], ALU.subtract, ['mid', 'lo'], ['dlt'])
                TTo('dve', dlt[:], dlt[:], ge[:], ALU.mult, ['dlt', 'ge'], ['dlt'])
                TTo('dve', lo[:], lo[:], dlt[:], ALU.add, ['lo', 'dlt'], ['lo'])
                TTo('dve', dlt[:], hi[:], mid[:], ALU.subtract, ['hi', 'mid'], ['dlt'])
                TTo('dve', dlt[:], dlt[:], ge[:], ALU.mult, ['dlt', 'ge'], ['dlt'])
                TTo('dve', hi[:], mid[:], dlt[:], ALU.add, ['mid', 'dlt'], ['hi'])
            TTo('dve', mask[:], affv, lo[:, :].unsqueeze(1).to_broadcast([128, 32, 16]), ALU.is_ge, ['aff_all', 'lo'], ['mask'])
            m2 = mask[:].rearrange("p t e -> p (t e)")
            MM(PS[:, 512:1024], ustr[:], m2, ['ustr', 'mask'], ['P1'])
            MM(PS[:, 1024:1536], onesf[:], m2, ['onesf', 'mask'], ['P2'])
            CP('dve', tot[:].rearrange("p t e -> p (t e)"), PS[:, 1024:1536], ['P2'], ['tot'])
            for e_ in range(16):
                S.op('dve', lambda e, e_=e_: e.tensor_tensor_scan(out=cum[:, :, e_], data0=onesf[:, 0:32], data1=tot[:, :, e_], initial=0.0,
                                                                 op0=ALU.mult, op1=ALU.add), reads=['tot', 'onesf'], writes=['cum'])
            TTo('dve', cum[:], cum[:], tot[:], ALU.subtract, ['cum', 'tot'], ['cum'])
            TTo('dve', cum[:].rearrange("p t e -> p (t e)"), cum[:].rearrange("p t e -> p (t e)"), PS[:, 512:1024], ALU.add, ['cum', 'P1'], ['cum'])
            S.op('dve', lambda e: e.scalar_tensor_tensor(out=posm[:], in0=cum[:], scalar=1.0, in1=mask[:], op0=ALU.add, op1=ALU.mult),
                 reads=['cum', 'mask'], writes=['posm'])
            S.op('dve', lambda e: e.tensor_scalar(out=posm[:], in0=posm[:], scalar1=-1.0, scalar2=None, op0=ALU.add), reads=['posm'], writes=['posm'])
            TTo('dve', glo[:], affv, mask[:], ALU.mult, ['aff_all', 'mask'], ['glo'])
            CP('dve', gw[:, :, :, 0], glo[:], ['glo'], ['gw'])
            CP('dve', ghi32[:], gw[:, :, :, 0], ['gw'], ['ghi32'])
            TTo('dve', gw[:, :, :, 1], glo[:], ghi32[:], ALU.subtract, ['glo', 'ghi32'], ['gw'])
            if dbg:
                o1 = dout("dbg_posm", [128, 512])
                S.dma(lambda e: e.dma_start(out=o1[:, :], in_=posm[:].rearrange("p a b -> p (a b)")), reads=['posm'])
            S.barrier()
            S.flush()
        if stop_after <= 5:
            return nc, dbg_outs

        with contextlib.ExitStack() as ph:
            h2_all = sb(ph, "h2_all", [128, 32, D], BF16)
            for i in range(32):
                S.dma(lambda e, i=i: e.dma_start(out=h2_all[:, i, :], in_=h2D[i * 128:(i + 1) * 128, :]), reads=['h2D'], writes=['h2_all'])
            OH = sb(ph, "OH", [128, 32, 512], BF16)
            xinT = sb(ph, "xinT", [128, 8, 512], BF16); hidT = sb(ph, "hidT", [128, 8, 512], BF16)
            wgb = sb(ph, "wgb", [128, 8, D], BF16); wub = sb(ph, "wub", [128, 8, D], BF16); wdb = sb(ph, "wdb", [128, 8, D], BF16)
            wsg = [sb(ph, "wsg%d" % i, [128, D], F32) for i in range(6)]
            gcs = sb(ph, "gcs", [128, 4], F32); sgt = sb(ph, "sgt", [128, 512], F32)
            ywt = sb(ph, "ywt", [128, 4, D], BF16)
            wi = 0
            for ex_ in range(int(os.environ.get('K_NE', 16))):
                for i in range(32):
                    S.op('dve' if i % 2 else 'pool', lambda e, i=i, ex_=ex_: e.tensor_scalar(
                        out=OH[:, i, :], in0=iot[:, 0:512], scalar1=posm[:, i, ex_:ex_ + 1], scalar2=None, op0=ALU.is_equal),
                        reads=['iot', 'posm'], writes=['OH'])
                for j in range(8):
                    pb = P[j % 2]; pk = 'P%d' % (j % 2)
                    for i in range(32):
                        MM(pb, h2_all[:, i, j * 128:(j + 1) * 128], OH[:, i, :], ['h2_all', 'OH'], [pk], start=(i == 0), stop=(i == 31))
                    CP('act' if j % 2 else 'dve', xinT[:, j, :], pb, [pk], ['xinT'])
                for (wsrc, wdst, wkey) in ((wg_d, wgb, 'wgb'), (wu_d, wub, 'wub'), (wd_d, wdb, 'wdb')):
                    for j in range(8):
                        w = wsg[wi % 6]; wk = 'wsg%d' % (wi % 6)
                        S.dma(lambda e, w=w, wsrc=wsrc, ex_=ex_, j=j: e.dma_start(out=w[:], in_=wsrc[ex_, j * 128:(j + 1) * 128, :]), writes=[wk])
                        CP(('dve', 'pool', 'act')[wi % 3], wdst[:, j, :], w[:], [wk], [wkey])
                        wi += 1
                gcp = PS[:, 1024:1032].rearrange("p (c k) -> p c k", k=2)
                for ct in range(4):
                    for i in range(32):
                        MM(gcp[:, ct, :], OH[:, i, ct * 128:(ct + 1) * 128], gw[:, i, ex_, :], ['OH', 'gw'], ['P2'],
                           start=(i == 0 and ct == 0), stop=(i == 31 and ct == 3))
                TTo('dve', gcs[:], gcp[:, :, 0], gcp[:, :, 1], ALU.add, ['P2'], ['gcs']) if False else None
                CP('dve', sgt[:, 0:8], PS[:, 1024:1032], ['P2'], ['sgt'])
                TTo('dve', gcs[:], sgt[:, 0:8].rearrange("p (c k) -> p c k", k=2)[:, :, 0], sgt[:, 0:8].rearrange("p (c k) -> p c k", k=2)[:, :, 1],
                    ALU.add, ['sgt'], ['gcs'])
                for fc in range(8):
                    for j in range(8):
                        MM(P[4], wgb[:, j, fc * 128:(fc + 1) * 128], xinT[:, j, :], ['wgb', 'xinT'], ['P4'], start=(j == 0), stop=(j == 7))
                    for j in range(8):
                        MM(P[5], wub[:, j, fc * 128:(fc + 1) * 128], xinT[:, j, :], ['wub', 'xinT'], ['P5'], start=(j == 0), stop=(j == 7))
                    ACTF(sgt[:], P[4], AF.Silu, ['P4'], ['sgt'])
                    TTo('dve', hidT[:, fc, :], sgt[:], P[5], ALU.mult, ['sgt', 'P5'], ['hidT'])
                for ct in range(4):
                    for half in range(2):
                        pb = P[6 + half]; pk = 'P%d' % (6 + half)
                        for fc in range(8):
                            MM(pb, hidT[:, fc, ct * 128:(ct + 1) * 128], wdb[:, fc, half * 512:(half + 1) * 512], ['hidT', 'wdb'], [pk],
                               start=(fc == 0), stop=(fc == 7))
                        S.op('act', lambda e, ct=ct, half=half, pb=pb: e.activation(out=ywt[:, ct, half * 512:(half + 1) * 512], in_=pb,
                                                                                  func=AF.Copy, scale=gcs[:, ct:ct + 1]),
                             reads=[pk, 'gcs'], writes=['ywt'])
                S.dma(lambda e, ex_=ex_: e.dma_start(out=ywD[ex_].rearrange("(c p) n -> p c n", p=128), in_=ywt[:]), reads=['ywt'], writes=['ywD'])
            if dbg:
                o1 = dout("dbg_yw", [16 * 512, D], BF16)
                S.dma(lambda e: e.dma_start(out=o1[:, :], in_=ywD.rearrange("e c n -> (e c) n")), reads=['ywD'])
            S.barrier()
            S.flush()
        if stop_after <= 6:
            return nc, dbg_outs

        with contextlib.ExitStack() as ph:
            yw_all = sb(ph, "yw_all", [128, 16, 4, D], BF16)
            for ex_ in range(16):
                S.dma(lambda e, ex_=ex_: e.dma_start(out=yw_all[:, ex_, :, :], in_=ywD[ex_].rearrange("(c p) n -> p c n", p=128)),
                      reads=['ywD'], writes=['yw_all'])
            cst = {}
            for nm, src in (("gt2", modd[0:1, 5120:6144]), ("ln2g", ln2_d[0:1, :]), ("ln2b", ln2_d[1:2, :])):
                cst[nm] = sb(ph, nm, [128, D], F32)
                S.dma(lambda e, nm=nm, src=src: e.dma_start(out=cst[nm][:], in_=src.partition_broadcast(128)), reads=['modd'], writes=[nm])
            dg = sb(ph, "dg", [128, 4, 128], F32)
            OHTs = [sb(ph, "OHT%d" % q_, [128, 4, 2048], BF16) for q_ in range(2)]
            x1ls = [sb(ph, "x1l%d" % q_, [128, D], F32) for q_ in range(2)]
            tr2s = [sb(ph, "tr2%d" % q_, [128, D], F32) for q_ in range(2)]
            xos = [sb(ph, "xo%d" % q_, [128, D], F32) for q_ in range(2)]
            stats = sb(ph, "statsF", [128, 2, 6], F32); mv = sb(ph, "mvF", [128, 2], F32)
            rstd = sb(ph, "rstdF", [128, 1], F32); nb = sb(ph, "nbF", [128, 1], F32)
            lntF = (stats, mv, rstd, nb, 'F')
            nFT = int(os.environ.get('K_FT', 32))

            def f_front(i):
                q_ = i % 2
                OHT = OHTs[q_]; kOHT = 'OHT%d' % q_
                for eg in range(4):
                    for k_ in range(4):
                        ex_ = eg * 4 + k_
                        S.op('dve' if k_ % 2 else 'pool', lambda e, k_=k_, ex_=ex_, i=i: e.tensor_scalar(
                            out=dg[:, k_, :], in0=identf[:], scalar1=posm[:, i, ex_:ex_ + 1], scalar2=None, op0=ALU.mult),
                            reads=['identf', 'posm'], writes=['dg'])
                    MM(PS[:, eg * 512:(eg + 1) * 512], onesf[:], dg[:].rearrange("p a t -> p (a t)"), ['onesf', 'dg'], ['P%d' % eg])

            def f_cmp(i):
                q_ = i % 2
                OHT = OHTs[q_]; kOHT = 'OHT%d' % q_
                for ct in range(4):
                    S.op('dve', lambda e, ct=ct, OHT=OHT: e.tensor_scalar(out=OHT[:, ct, :], in0=PS[:, 0:2048], scalar1=iot[:, 512 + ct:513 + ct],
                                                                         scalar2=None, op0=ALU.is_equal),
                         reads=['P0', 'P1', 'P2', 'P3', 'iot'], writes=[kOHT])

            def f_back(i):
                t0 = i * 128
                q_ = i % 2
                OHT = OHTs[q_]; x1l = x1ls[q_]; tr2 = tr2s[q_]; xo = xos[q_]
                kOHT = 'OHT%d' % q_; kx1l = 'x1l%d' % q_; ktr2 = 'tr2%d' % q_; kxo = 'xo%d' % q_
                S.dma(lambda e, t0=t0, x1l=x1l: e.dma_start(out=x1l[:], in_=x1D[t0:t0 + 128, :]), reads=['x1D'], writes=[kx1l])
                for half in range(2):
                    pb = P[4 + half]; pk = 'P%d' % (4 + half)
                    n = 0
                    for ex_ in range(16):
                        for ct in range(4):
                            MM(pb, OHT[:, ct, ex_ * 128:(ex_ + 1) * 128], yw_all[:, ex_, ct, half * 512:(half + 1) * 512], [kOHT, 'yw_all'], [pk],
                               start=(n == 0), stop=(n == 63))
                            n += 1
                if i + 1 < nFT:
                    f_cmp(i + 1)
                TTo('dve', tr2[:], PS[:, 2048:3072], cst['gt2'][:], ALU.mult, ['P4', 'P5', 'gt2'], [ktr2])
                S.op('dve', lambda e, tr2=tr2, x1l=x1l: e.scalar_tensor_tensor(out=tr2[:], in0=x1l[:], scalar=ALPHA, in1=tr2[:], op0=ALU.mult, op1=ALU.add),
                     reads=[kx1l, ktr2], writes=[ktr2])
                ln_stats(lntF, tr2, ktr2, eps5, 'eps5')
                S.op('act', lambda e, xo=xo, tr2=tr2: e.activation(out=xo[:], in_=tr2[:], func=AF.Identity, bias=nb[:], scale=rstd[:]),
                     reads=[ktr2, 'Fnb', 'Frstd'], writes=[kxo])
                TTo('pool', xo[:], xo[:], cst['ln2g'][:], ALU.mult, [kxo, 'ln2g'], [kxo])
                TTo('dve', xo[:], xo[:], cst['ln2b'][:], ALU.add, [kxo, 'ln2b'], [kxo])
                S.dma(lambda e, t0=t0, xo=xo: e.dma_start(out=out_d[t0:t0 + 128, :], in_=xo[:]), reads=[kxo], writes=['out'])

            f_front(0)
            f_cmp(0)
            for i in range(nFT):
                if i + 1 < nFT:
                    f_front(i + 1)
                f_back(i)
            S.barrier()
            S.flush()
    return nc, dbg_outs


def host_inputs(inp, b):
    f = np.float32
    m = {}
    m["x"] = np.ascontiguousarray(inp["x"][b], dtype=f)
    m["ctx"] = np.ascontiguousarray(inp["ctx"][b], dtype=f)
    m["cc"] = np.ascontiguousarray(np.stack([inp["c"][b].reshape(8, 128).T, inp["c_ctx"].reshape(8, 128).T], -1).reshape(128, 16), dtype=f)
    m["w_ada"] = np.ascontiguousarray(inp["w_ada"][0], dtype=f)
    m["b_ada"] = np.ascontiguousarray(inp["b_ada"][0].reshape(1, -1), dtype=f)
    m["w_in"] = np.ascontiguousarray(inp["w_in"][0], dtype=f)
    m["qg"] = np.ascontiguousarray(np.tile(inp["q_gain"][0], 8).reshape(1, 512), dtype=f)
    m["kg"] = np.ascontiguousarray(np.tile(inp["k_gain"][0], 2).reshape(1, 128), dtype=f)
    t = np.arange(TL)
    pos = np.stack([t // 64, t % 64], -1).astype(np.float32)
    inv = (10000.0 ** (-np.arange(16, dtype=np.float32) / 16)).astype(np.float32)
    ang = pos[:, :, None] * inv[None, None, :]
    cs = np.cos(ang).astype(f); sn = np.sin(ang).astype(f)
    cos2 = np.stack([cs, cs], 2).reshape(TL, 64)
    sin2 = np.stack([-sn, sn], 2).reshape(TL, 64)
    m["cosT"] = np.ascontiguousarray(np.tile(cos2, (1, 8)), dtype=f)
    m["sinS"] = np.ascontiguousarray(np.tile(sin2, (1, 8)), dtype=f)
    m["ident"] = np.eye(128, dtype=f)
    def kh(v):
        return np.asarray(v, dtype=f).reshape(8, 64).T
    mu = inp["tshift_mu"][0]
    cols = [kh(mu[0:512]), kh(mu[512:1024]), kh(mu[1024:1536]), kh(inp["k_k"][0]), kh(inp["k_a"][0]), kh(inp["r_k"][0].reshape(-1)),
            kh(inp["decay_w0"][0, 0]), kh(inp["decay_w0"][0, 1]), kh(inp["iclr_a0"][0, 0]), kh(inp["iclr_a0"][0, 1])]
    m["rp"] = np.ascontiguousarray(np.stack(cols, -1).reshape(64, 80), dtype=f)
    lmu = np.zeros((128, 3), f)
    lmu[0:64, 0] = mu[1536:1600]; lmu[0:64, 1] = mu[1600:1664]; lmu[:, 2] = mu[1664:1792]
    m["lmu"] = lmu
    m["decup"] = np.ascontiguousarray(np.concatenate([inp["decay_up"][0, 0], inp["decay_up"][0, 1]], 1), dtype=f)
    m["iclup"] = np.ascontiguousarray(np.concatenate([inp["iclr_up"][0, 0], inp["iclr_up"][0, 1]], 1), dtype=f)
    m["gateup"] = np.ascontiguousarray(inp["gate_up"][0], dtype=f)
    hh = np.repeat(np.arange(8), 16); tt = np.tile(np.arange(16), 8)
    same = hh[:, None] == hh[None, :]
    msf = (same & (tt[:, None] < tt[None, :])).astype(f); mif = (same & (tt[:, None] <= tt[None, :])).astype(f)
    msb = msf.T.copy(); mib = mif.T.copy()
    m["mk4"] = np.ascontiguousarray(np.concatenate([msf, mif, msf, mif, msb, mib, msb, mib], 1), dtype=f)
    m["mn1"] = np.ascontiguousarray(np.concatenate([msb, msf], 1), dtype=f)
    m["bm"] = np.ascontiguousarray((hh[:, None] == np.repeat(np.arange(8), 64)[None, :]).astype(f))
    rst = np.ones((64, 1024), f); rst[:, ::16] = 0.0
    m["rst"] = rst
    m["w_out"] = np.ascontiguousarray(inp["w_out"][0], dtype=f)
    m["lnx"] = np.ascontiguousarray(np.stack([inp["lnx_g"][0], inp["lnx_b"][0]]), dtype=f)
    m["ln1"] = np.ascontiguousarray(np.stack([inp["ln1_g"][0], inp["ln1_b"][0]]), dtype=f)
    m["ln2"] = np.ascontiguousarray(np.stack([inp["ln2_g"][0], inp["ln2_b"][0]]), dtype=f)
    m["router_w"] = np.ascontiguousarray(inp["router_w"][0], dtype=f)
    m["exp_w_gate"] = np.ascontiguousarray(inp["exp_w_gate"][0], dtype=f)
    m["exp_w_up"] = np.ascontiguousarray(inp["exp_w_up"][0], dtype=f)
    m["exp_w_down"] = np.ascontiguousarray(inp["exp_w_down"][0], dtype=f)
    iot = np.zeros((128, 516), f)
    iot[:, 0:512] = np.arange(512, dtype=f)[None, :]
    iot[:, 512:516] = np.arange(128, dtype=f)[:, None] + 128.0 * np.arange(4, dtype=f)[None, :]
    m["iot"] = iot
    m["ustr"] = np.triu(np.ones((128, 128), f), 1)
    return m


_NC_CACHE = {}


def kernel(**inputs):
    inp = {k: np.asarray(v) for k, v in inputs.items()}
    if "full" not in _NC_CACHE:
        _NC_CACHE["full"] = build_nc()[0]
    nc = _NC_CACHE["full"]
    in_maps = [host_inputs(inp, c // 2) for c in range(8)]
    res = run_bass_kernel_spmd(nc, in_maps, core_ids=list(range(8)))
    out = np.stack([res.results[2 * b]["out"] for b in range(4)], 0).astype(np.float32)
    return out
```

```python
import contextlib
import os
import numpy as np
import concourse.bass as bass
import concourse.mybir as mybir
from concourse.bass_utils import run_bass_kernel_spmd

F32 = mybir.dt.float32
BF16 = mybir.dt.bfloat16
AF = mybir.ActivationFunctionType
ALU = mybir.AluOpType
AX = mybir.AxisListType

D = 1024
TL = 4096
TC = 256
TT = TL + TC
NT = TT // 128
ALPHA = 2.0 ** 0.25
DEC_C = -float(np.exp(-0.5))


class Sched:
    CE = ('pe', 'act', 'dve', 'pool')

    def __init__(self, nc, stack, ndma=32):
        self.nc = nc
        self.ops = {e: [] for e in ('pe', 'act', 'dve', 'pool', 'sp')}
        self.cnt = {e: 0 for e in self.CE}
        self.last_w = {}
        self.readers = {}
        self.waited = {e: {} for e in self.ops}
        self.ndma = ndma
        self.dma_val = [0] * ndma
        self.dma_i = 0
        names = list(self.CE) + ['d%d' % i for i in range(ndma)]
        self.sems = {n: stack.enter_context(nc.semaphore('s_' + n)) for n in names}

    def _deps(self, eng, reads, writes):
        deps = {}

        def add(tok):
            if tok is None:
                return
            s, v = tok
            if deps.get(s, 0) < v:
                deps[s] = v
        for r in reads:
            add(self.last_w.get(r))
        for w in writes:
            add(self.last_w.get(w))
            for t in self.readers.get(w, ()):
                add(t)
        waits = []
        for s, v in deps.items():
            if s == eng and (eng == 'pe' or os.environ.get('K_NOSELF')):
                continue
            if self.waited[eng].get(s, 0) >= v:
                continue
            self.waited[eng][s] = v
            waits.append((s, v))
        return waits

    def _commit(self, tok, reads, writes):
        for r in reads:
            self.readers.setdefault(r, []).append(tok)
        for w in writes:
            self.last_w[w] = tok
            self.readers[w] = []

    capture = None
    pend = None

    def drain(self, lst, n):
        for _ in range(min(n, len(lst))):
            kind, a = lst.pop(0)
            (self.op if kind == 'op' else self.dma)(*a)

    def op(self, eng, fn, reads=(), writes=()):
        if self.capture is not None:
            self.capture.append(('op', (eng, fn, tuple(reads), tuple(writes))))
            return
        waits = self._deps(eng, reads, writes)
        self.cnt[eng] += 1
        tok = (eng, self.cnt[eng])
        self.ops[eng].append((waits, fn, (eng, 1)))
        self._commit(tok, reads, writes)

    def dma(self, fn, reads=(), writes=(), q='sp'):
        if self.capture is not None:
            self.capture.append(('dma', (fn, tuple(reads), tuple(writes), q)))
            return
        slot = self.dma_i % self.ndma
        self.dma_i += 1
        s = 'd%d' % slot
        waits = self._deps(q, reads, writes)
        pv = self.dma_val[slot]
        if pv > 0 and self.waited[q].get(s, 0) < pv:
            self.waited[q][s] = pv
            waits.append((s, pv))
        self.dma_val[slot] = pv + 16
        tok = (s, pv + 16)
        self.ops[q].append((waits, fn, (s, 16)))
        self._commit(tok, reads, writes)

    def barrier(self):
        allw = [(e, c) for e, c in self.cnt.items() if c > 0]
        allw += [('d%d' % i, v) for i, v in enumerate(self.dma_val) if v > 0]
        for eng in self.ops:
            waits = []
            for s, v in allw:
                if self.waited[eng].get(s, 0) >= v:
                    continue
                self.waited[eng][s] = v
                waits.append((s, v))
            if waits:
                self.ops[eng].append((waits, None, None))
        self.last_w = {}
        self.readers = {}

    def flush(self):
        nc = self.nc
        sems = self.sems
        ops = self.ops
        self.ops = {e: [] for e in ops}
        if os.environ.get('K_STATS'):
            print("FLUSH", {e: (len(v), sum(len(w[0]) for w in v)) for e, v in ops.items()})
        with nc.Block() as block:
            def run(engname, engobj):
                for waits, fn, inc in ops[engname]:
                    for ws, wv in waits:
                        engobj.wait_ge(sems[ws], wv)
                    if fn is not None:
                        ins = fn(engobj)
                        ins.then_inc(sems[inc[0]], inc[1])

            @block.sync
            def _(e):
                run('sp', e)

            @block.tensor
            def _(e):
                run('pe', e)

            @block.scalar
            def _(e):
                run('act', e)

            @block.vector
            def _(e):
                run('dve', e)

            @block.gpsimd
            def _(e):
                run('pool', e)


def build_nc(stop_after=99, dbg=False):
    nc = bass.Bass("TRN2", target_bir_lowering=False)

    def din(name, shape, dt=F32):
        return nc.dram_tensor(name, list(shape), dt, kind="ExternalInput").ap()

    def dscr(name, shape, dt=F32):
        return nc.dram_tensor(name, list(shape), dt, kind="Internal").ap()

    x_d = din("x", [TL, D]); ctx_d = din("ctx", [TC, D])
    cc_d = din("cc", [128, 16])
    wada_d = din("w_ada", [D, 6 * D]); bada_d = din("b_ada", [1, 6 * D])
    win_d = din("w_in", [D, 2560])
    qg_d = din("qg", [1, 512]); kg_d = din("kg", [1, 128])
    cos_d = din("cosT", [TL, 512]); sin_d = din("sinS", [TL, 512])
    ident_d = din("ident", [128, 128])
    rp_d = din("rp", [64, 8 * 10])
    lmu_d = din("lmu", [128, 3])
    decup_d = din("decup", [64, 2 * 512]); iclup_d = din("iclup", [64, 2 * 512]); gateup_d = din("gateup", [128, 512])
    mk4_d = din("mk4", [128, 2 * 512]); mn1_d = din("mn1", [128, 2 * 128]); bm_d = din("bm", [128, 512])
    rst_d = din("rst", [64, 1024])
    wout_d = din("w_out", [D, D]); lnx_d = din("lnx", [2, 512])
    ln1_d = din("ln1", [2, D]); ln2_d = din("ln2", [2, D])
    rw_d = din("router_w", [D, 16])
    x1D = dscr("x1D", [TL, D]); h2D = dscr("h2D", [TL, D], BF16)
    attD = dscr("attD", [128, 4 * TL], BF16)
    wg_d = din("exp_w_gate", [16, D, D]); wu_d = din("exp_w_up", [16, D, D]); wd_d = din("exp_w_down", [16, D, D])
    iot_d = din("iot", [128, 512 + 4]); ustr_d = din("ustr", [128, 128])
    ywD = dscr("ywD", [16, 512, D], BF16)
    ydir = dscr("ydir", [2, TL, 512]); bonD = dscr("bonD", [TL, 512]); gD = dscr("gD", [TL, 512])
    out_d = nc.dram_tensor("out", [TL, D], F32, kind="ExternalOutput").ap()
    modd = dscr("modd", [2, 6 * D])
    rwT = dscr("rwT", [1792, TT])
    dbg_outs = {}

    def dout(name, shape, dt=F32):
        ap = nc.dram_tensor(name, list(shape), dt, kind="ExternalOutput").ap()
        dbg_outs[name] = ap
        return ap

    with contextlib.ExitStack() as top:
        S = Sched(nc, top)

        def sb(stack, name, shape, dt):
            return stack.enter_context(nc.sbuf_tensor("sb_" + name, list(shape), dt))

        PS = top.enter_context(nc.psum_tensor("PS", [128, 8 * 512], F32))
        P = [PS[:, i * 512:(i + 1) * 512] for i in range(8)]
        PK = ['P%d' % i for i in range(8)]
        identf = sb(top, "identf", [128, 128], F32)
        identb = sb(top, "identb", [128, 128], BF16)
        eps5 = sb(top, "eps5", [128, 1], F32)
        eps6 = sb(top, "eps6", [128, 1], F32)
        S.dma(lambda e: e.dma_start(out=identf[:], in_=ident_d[:, :]), writes=['identf'])
        S.op('dve', lambda e: e.tensor_copy(out=identb[:], in_=identf[:]), reads=['identf'], writes=['identb'])
        S.op('pool', lambda e: e.memset(eps5[:], 1e-5), writes=['eps5'])
        S.op('pool', lambda e: e.memset(eps6[:], 1e-6), writes=['eps6'])

        def ln_stats(stack_tiles, src, key_src, eps_t, eps_key):
            stats, mv, rstd, nb, kq = stack_tiles
            for cch in range(2):
                S.op('dve', lambda e, cch=cch: e.bn_stats(out=stats[:, cch, :], in_=src[:, cch * 512:(cch + 1) * 512]),
                     reads=[key_src], writes=[kq + 'stats'])
            S.op('dve', lambda e: e.bn_aggr(out=mv[:], in_=stats[:]), reads=[kq + 'stats'], writes=[kq + 'mv'])
            S.op('act', lambda e: e.activation(out=rstd[:], in_=mv[:, 1:2], func=AF.Ln, bias=eps_t[:], scale=1.0),
                 reads=[kq + 'mv', eps_key], writes=[kq + 'rstd'])
            S.op('act', lambda e: e.activation(out=rstd[:], in_=rstd[:], func=AF.Exp, scale=-0.5),
                 reads=[kq + 'rstd'], writes=[kq + 'rstd'])
            S.op('dve', lambda e: e.scalar_tensor_tensor(out=nb[:], in0=mv[:, 0:1], scalar=-1.0, in1=rstd[:],
                                                        op0=ALU.mult, op1=ALU.mult),
                 reads=[kq + 'mv', kq + 'rstd'], writes=[kq + 'nb'])

        with contextlib.ExitStack() as ph:
            cc = sb(ph, "cc", [128, 8, 2], F32)
            ccs = sb(ph, "ccs", [128, 8, 2], F32)
            wst = [sb(ph, "wst%d" % i, [128, 8, 512], F32) for i in range(2)]
            bada = sb(ph, "bada", [2, 6 * D], F32)
            modrow = sb(ph, "modrow", [2, 6 * D], F32)
            S.dma(lambda e: e.dma_start(out=cc[:].rearrange("p a b -> p (a b)"), in_=cc_d[:, :]), writes=['cc'])
            S.dma(lambda e: e.dma_start(out=bada[:], in_=bada_d.partition_broadcast(2)), writes=['bada'])
            S.op('act', lambda e: e.activation(out=ccs[:], in_=cc[:], func=AF.Silu), reads=['cc'], writes=['ccs'])
            for g in range(12):
                w = wst[g % 2]
                wk = 'wst%d' % (g % 2)
                S.dma(lambda e, w=w, g=g: e.dma_start(
                    out=w[:], in_=wada_d[:, g * 512:(g + 1) * 512].rearrange("(j p) n -> p j n", p=128)), writes=[wk])
                for j in range(8):
                    S.op('pe', lambda e, w=w, j=j: e.matmul(P[0][0:2, :], lhsT=ccs[:, j, :], rhs=w[:, j, :],
                                                           start=(j == 0), stop=(j == 7)),
                         reads=['ccs', wk], writes=['P0'])
                S.op('dve', lambda e, g=g: e.tensor_tensor(out=modrow[:, g * 512:(g + 1) * 512], in0=P[0][0:2, :],
                                                          in1=bada[:, g * 512:(g + 1) * 512], op=ALU.add),
                     reads=['P0', 'bada'], writes=['modrow'])
            for lo in (1024, 4096):
                S.op('dve', lambda e, lo=lo: e.tensor_scalar_add(out=modrow[:, lo:lo + 1024], in0=modrow[:, lo:lo + 1024],
                                                                scalar1=1.0), reads=['modrow'], writes=['modrow'])
            S.dma(lambda e: e.dma_start(out=modd[:, :], in_=modrow[:]), reads=['modrow'], writes=['modd'])
            if dbg:
                o = dout("dbg_mod", [2, 6 * D])
                S.dma(lambda e: e.dma_start(out=o[:, :], in_=modrow[:]), reads=['modrow'])
            S.barrier()
            S.flush()
        if stop_after <= 0:
            return nc, dbg_outs

        def modrow_bc(dst, row, lo):
            S.dma(lambda e: e.dma_start(out=dst[:], in_=modd[row:row + 1, lo:lo + 1024].partition_broadcast(128)),
                  reads=['modd'], writes=[dst.name if hasattr(dst, 'name') else 'x'])

        aff_all = sb(top, "aff_all", [128, 32, 16], F32)
        with contextlib.ExitStack() as phAB:
            qT_all = sb(phAB, "qT_all", [128, 4, TL], BF16)
            kTd = sb(phAB, "kTd", [128, 2, TT], BF16)
            Vx = sb(phAB, "Vx", [128, NT, 2, 80], BF16)
            attT_all = sb(phAB, "attT_all", [128, 4, TL], BF16)
            with contextlib.ExitStack() as ph:
                winb = sb(ph, "winb", [128, 8, 2560], BF16)
                wstg = [sb(ph, "wstg%d" % i, [128, 640], F32) for i in range(2)]
                sc1p = sb(ph, "sc1p", [128, D], F32); sh1 = sb(ph, "sh1", [128, D], F32)
                csc1p = sb(ph, "csc1p", [128, D], F32); csh1 = sb(ph, "csh1", [128, D], F32)
                qg = sb(ph, "qg", [128, 512], F32); kg = sb(ph, "kg", [128, 128], F32)
                for dst, nm, row, lo in ((sh1, 'sh1', 0, 0), (sc1p, 'sc1p', 0, 1024), (csh1, 'csh1', 1, 0), (csc1p, 'csc1p', 1, 1024)):
                    S.dma(lambda e, dst=dst, row=row, lo=lo: e.dma_start(
                        out=dst[:], in_=modd[row:row + 1, lo:lo + 1024].partition_broadcast(128)), reads=['modd'], writes=[nm])
                S.dma(lambda e: e.dma_start(out=qg[:], in_=qg_d.partition_broadcast(128)), writes=['qg'])
                S.dma(lambda e: e.dma_start(out=kg[:], in_=kg_d.partition_broadcast(128)), writes=['kg'])
                for jj in range(32):
                    j = jj // 4; c0 = (jj % 4) * 640
                    w = wstg[jj % 2]; wk = 'wstg%d' % (jj % 2)
                    S.dma(lambda e, w=w, j=j, c0=c0: e.dma_start(out=w[:], in_=win_d[j * 128:(j + 1) * 128, c0:c0 + 640]), writes=[wk])
                    S.op('pool' if jj % 2 else 'dve', lambda e, w=w, j=j, c0=c0: e.tensor_copy(out=winb[:, j, c0:c0 + 640], in_=w[:]),
                         reads=[wk], writes=['winb'])
                S.op('pool', lambda e: e.memset(Vx[:], 1.0), writes=['Vx'])
                xt = [sb(ph, "xt%d" % i, [128, D], F32) for i in range(2)]
                xn = sb(ph, "xn", [128, D], F32)
                hb = sb(ph, "hb", [128, D], BF16)
                hT = sb(ph, "hT", [128, 8, 512], BF16)
                stats = sb(ph, "stats", [128, 2, 6], F32); mv = sb(ph, "mv", [128, 2], F32)
                rstd = sb(ph, "rstd", [128, 1], F32); nb = sb(ph, "nb", [128, 1], F32)
                lnt = (stats, mv, rstd, nb, 'A')
                rstg = [sb(ph, "rstg%d" % i, [128, 512], F32) for i in range(2)]
                cosT = sb(ph, "cosT", [128, 512], F32); sinS = sb(ph, "sinS", [128, 512], F32)
                sq = sb(ph, "sq", [128, 512], F32)
                ssum = sb(ph, "ssum", [128, 8], F32)
                qn = sb(ph, "qn", [128, 512], F32); qa = sb(ph, "qa", [128, 512], F32)
                qbt = sb(ph, "qbt", [128, 512], F32)
                qo = sb(ph, "qo", [128, 512], BF16)
                ko = sb(ph, "ko", [128, 2, 2, 64], BF16)
                blocks = [(0, 256)] + [(256 + 512 * i, 512) for i in range(8)]
                ti = 0
                for (u0, ntok) in blocks:
                    ntile = ntok // 128
                    is_ctx = (u0 == 0)
                    for tl in range(ntile):
                        u = u0 + tl * 128
                        gt = u // 128
                        X = xt[ti % 2]; xk = 'xt%d' % (ti % 2)
                        ti += 1
                        src = ctx_d[u:u + 128, :] if is_ctx else x_d[u - TC:u - TC + 128, :]
                        S.dma(lambda e, X=X, src=src: e.dma_start(out=X[:], in_=src), writes=[xk])
                        ln_stats(lnt, X, xk, eps5, 'eps5')
                        S.op('act', lambda e, X=X: e.activation(out=xn[:], in_=X[:], func=AF.Identity, bias=nb[:], scale=rstd[:]),
                             reads=[xk, 'Anb', 'Arstd'], writes=['xn'])
                        scp, shh, k1, k2 = (csc1p, csh1, 'csc1p', 'csh1') if is_ctx else (sc1p, sh1, 'sc1p', 'sh1')
                        S.op('pool', lambda e, scp=scp: e.tensor_tensor(out=xn[:], in0=xn[:], in1=scp[:], op=ALU.mult),
                             reads=['xn', k1], writes=['xn'])
                        S.op('dve', lambda e, shh=shh: e.tensor_tensor(out=hb[:], in0=xn[:], in1=shh[:], op=ALU.add),
                             reads=['xn', k2], writes=['hb'])
                        pTb = P[0][:].bitcast(BF16)
                        for j in range(8):
                            S.op('pe', lambda e, j=j, pTb=pTb: e.transpose(out=pTb[:, j * 128:(j + 1) * 128],
                                                                      in_=hb[:, j * 128:(j + 1) * 128], identity=identb[:]),
                                 reads=['hb', 'identb'], writes=['P0'])
                        S.op('act', lambda e, tl=tl, pTb=pTb: e.copy(out=hT[:, :, tl * 128:(tl + 1) * 128],
                                                                 in_=pTb.rearrange("p (j t) -> p j t", j=8)),
                             reads=['P0'], writes=['hT'])
                        for j in range(8):
                            S.op('pe', lambda e, j=j, tl=tl: e.matmul(P[1][:, :], lhsT=hT[:, j, tl * 128:(tl + 1) * 128],
                                                                     rhs=winb[:, j, 0:512], start=(j == 0), stop=(j == 7)),
                                 reads=['hT', 'winb'], writes=['P1'])
                        for j in range(8):
                            S.op('pe', lambda e, j=j, tl=tl: e.matmul(P[2][:, 0:256], lhsT=hT[:, j, tl * 128:(tl + 1) * 128],
                                                                     rhs=winb[:, j, 512:768], start=(j == 0), stop=(j == 7)),
                                 reads=['hT', 'winb'], writes=['P2'])
                        S.op('act', lambda e, gt=gt: e.copy(out=Vx[:, gt, :, 0:64],
                                                          in_=P[2][:, 128:256].rearrange("p (a b) -> p a b", a=2)),
                             reads=['P2'], writes=['Vx'])
                        if not is_ctx:
                            ul = u - TC
                            S.dma(lambda e, ul=ul: e.dma_start(out=cosT[:], in_=cos_d[ul:ul + 128, :]), writes=['cosT'])
                            S.dma(lambda e, ul=ul: e.dma_start(out=sinS[:], in_=sin_d[ul:ul + 128, :]), writes=['sinS'])

                        def normrope(psrc, pk, nh, gain, gk, do_rope, outfn):
                            w_ = nh * 64
                            S.op('act', lambda e: e.activation(out=sq[:, 0:w_], in_=psrc, func=AF.Square),
                                 reads=[pk], writes=['sq'])
                            S.op('dve', lambda e: e.tensor_reduce(out=ssum[:, 0:nh], in_=sq[:, 0:w_].rearrange("p (h d) -> p h d", d=64),
                                                                 axis=AX.X, op=ALU.add), reads=['sq'], writes=['ssum'])
                            S.op('act', lambda e: e.activation(out=ssum[:, 0:nh], in_=ssum[:, 0:nh], func=AF.Ln, bias=eps6[:], scale=1.0 / 64),
                                 reads=['ssum', 'eps6'], writes=['ssum'])
                            S.op('act', lambda e: e.activation(out=ssum[:, 0:nh], in_=ssum[:, 0:nh], func=AF.Exp, scale=-0.5),
                                 reads=['ssum'], writes=['ssum'])
                            S.op('dve', lambda e: e.tensor_tensor(out=qn[:, 0:w_].rearrange("p (h d) -> p h d", d=64),
                                                                 in0=psrc.rearrange("p (h d) -> p h d", d=64),
                                                                 in1=ssum[:, 0:nh].unsqueeze(2).to_broadcast([128, nh, 64]), op=ALU.mult),
                                 reads=[pk, 'ssum'], writes=['qn'])
                            if not do_rope:
                                S.op('pool', lambda e: outfn(e, qn[:, 0:w_], gain[:, 0:w_], ALU.mult), reads=['qn', gk], writes=['qko'])
                                return
                            S.op('pool', lambda e: e.tensor_tensor(out=qn[:, 0:w_], in0=qn[:, 0:w_], in1=gain[:, 0:w_], op=ALU.mult),
                                 reads=['qn', gk], writes=['qn'])
                            S.op('dve', lambda e: e.tensor_tensor(out=qa[:, 0:w_], in0=qn[:, 0:w_], in1=cosT[:, 0:w_], op=ALU.mult),
                                 reads=['qn', 'cosT'], writes=['qa'])
                            qv = qn[:, 0:w_].rearrange("p (g s q) -> p g s q", s=2, q=16)
                            bv = qbt[:, 0:w_].rearrange("p (g s q) -> p g s q", s=2, q=16)
                            sv = sinS[:, 0:w_].rearrange("p (g s q) -> p g s q", s=2, q=16)
                            for s_ in range(2):
                                S.op('pool', lambda e, s_=s_: e.tensor_tensor(out=bv[:, :, s_, :], in0=qv[:, :, 1 - s_, :],
                                                                             in1=sv[:, :, s_, :], op=ALU.mult),
                                     reads=['qn', 'sinS'], writes=['qbt'])
                            S.op('dve', lambda e: outfn(e, qa[:, 0:w_], qbt[:, 0:w_], ALU.add), reads=['qa', 'qbt'], writes=['qko'])

                        if not is_ctx:
                            normrope(P[1][:, :], 'P1', 8, qg, 'qg', True,
                                     lambda e, a, b_, op: e.tensor_tensor(out=qo[:], in0=a, in1=b_, op=op))
                            pq = P[3][:].bitcast(BF16)
                            for hp in range(4):
                                S.op('pe', lambda e, hp=hp, pq=pq: e.transpose(out=pq[:, hp * 128:(hp + 1) * 128],
                                                                          in_=qo[:, hp * 128:(hp + 1) * 128], identity=identb[:]),
                                     reads=['qko', 'identb'], writes=['P3'])
                            S.op('act', lambda e, ul=ul, pq=pq: e.copy(out=qT_all[:, :, ul:ul + 128],
                                                                   in_=pq[:, 0:512].rearrange("p (j t) -> p j t", j=4)),
                                 reads=['P3'], writes=['qT_all'])

                        def kout(e, a, b_, op):
                            return e.tensor_tensor(out=ko[:, :, 0, :], in0=a.rearrange("p (h d) -> p h d", d=64),
                                                   in1=b_.rearrange("p (h d) -> p h d", d=64), op=op)
                        normrope(P[2][:, 0:128], 'P2', 2, kg, 'kg', not is_ctx, kout)
                        S.op('pool', lambda e: e.tensor_copy(out=ko[:, :, 1, :], in_=ko[:, :, 0, :]), reads=['qko'], writes=['qko'])
                        pk_ = P[3][:].bitcast(BF16)
                        for kv in range(2):
                            S.op('pe', lambda e, kv=kv, pk_=pk_: e.transpose(
                                out=pk_[:, 512 + kv * 128:512 + (kv + 1) * 128],
                                in_=ko[:, kv, :, :].rearrange("p a d -> p (a d)"), identity=identb[:]),
                                reads=['qko', 'identb'], writes=['P3'])
                        S.op('act', lambda e, u=u, pk_=pk_: e.copy(out=kTd[:, :, u:u + 128],
                                                               in_=pk_[:, 512:768].rearrange("p (j t) -> p j t", j=2)),
                             reads=['P3'], writes=['kTd'])
                    for g in range(14):
                        pb = P[4 + g % 2]; pbk = 'P%d' % (4 + g % 2)
                        for j in range(8):
                            S.op('pe', lambda e, j=j, g=g, pb=pb, ntok=ntok: e.matmul(pb[:, 0:ntok], lhsT=winb[:, j, 768 + g * 128:768 + (g + 1) * 128],
                                                                         rhs=hT[:, j, 0:ntok], start=(j == 0), stop=(j == 7)),
                                 reads=['hT', 'winb'], writes=[pbk])
                        rs = rstg[g % 2]; rk = 'rstg%d' % (g % 2)
                        S.op('act' if g % 2 else 'dve',
                             (lambda e, rs=rs, pb=pb, ntok=ntok: e.copy(out=rs[:, 0:ntok], in_=pb[:, 0:ntok])) if g % 2 else
                             (lambda e, rs=rs, pb=pb, ntok=ntok: e.tensor_copy(out=rs[:, 0:ntok], in_=pb[:, 0:ntok])),
                             reads=[pbk], writes=[rk])
                        S.dma(lambda e, rs=rs, g=g, u0=u0, ntok=ntok: e.dma_start(out=rwT[g * 128:(g + 1) * 128, u0:u0 + ntok], in_=rs[:, 0:ntok]),
                              reads=[rk], writes=['rwT'])
                if dbg:
                    o1 = dout("dbg_qT", [128, 4 * TL], BF16)
                    S.dma(lambda e: e.dma_start(out=o1[:, :], in_=qT_all[:].rearrange("p a t -> p (a t)")), reads=['qT_all'])
                    o2 = dout("dbg_kTd", [128, 2 * TT], BF16)
                    S.dma(lambda e: e.dma_start(out=o2[:, :], in_=kTd[:].rearrange("p a t -> p (a t)")), reads=['kTd'])
                    o3 = dout("dbg_rwT", [1792, TT])
                    S.dma(lambda e: e.dma_start(out=o3[:, :], in_=rwT[:, :]), reads=['rwT'])
                S.barrier()
                S.flush()
            if stop_after <= 1:
                return nc, dbg_outs
            with contextlib.ExitStack() as ph:
                pT = [sb(ph, "pT%d" % i, [128, 512], BF16) for i in range(3)]
                rec = sb(ph, "rec", [128, 8], F32)
                atok = sb(ph, "atok", [128, 8, 64], BF16)
                pi = 0
                for g in range(int(os.environ.get('K_NG', 16))):
                    q0 = g * 256
                    for st in range(NT):
                        for hp in range(int(os.environ.get('K_NHP', 4))):
                            sb0 = 2 * (hp % 2)
                            scb = PS[:, sb0 * 512:(sb0 + 2) * 512].rearrange("p (b n) -> p b n", b=2)
                            sck = 'P%d' % sb0
                            kvh = hp // 2
                            for hh in range(int(os.environ.get('K_HH0', 0)), int(os.environ.get('K_NHH', 2))):
                                S.op('pe', lambda e, hh=hh, hp=hp, scb=scb, kvh=kvh, st=st, q0=q0: e.matmul(
                                    scb[:, hh, 0:256],
                                    lhsT=kTd[hh * 64:(hh + 1) * 64, kvh, st * 128:(st + 1) * 128],
                                    rhs=qT_all[hh * 64:(hh + 1) * 64, hp, q0:q0 + 256], start=True, stop=True),
                                    reads=['kTd', 'qT_all'], writes=[sck])
                            pt = pT[pi % 3]; ptk = 'pT%d' % (pi % 3)
                            pi += 1
                            if not os.environ.get('K_SKIP_EXP'):
                                S.op('act', lambda e, pt=pt, scb=scb: e.activation(out=pt[:].rearrange('p (b n) -> p b n', b=2), in_=scb[:, :, 0:256], func=AF.Exp, scale=0.125),
                                     reads=[sck], writes=[ptk])
                            for hh in range(2 if not os.environ.get('K_SKIP_PV') else 0):
                                head = 2 * hp + hh
                                for qt in range(2):
                                    ab = P[4 + 2 * qt + head // 4]; abk = 'P%d' % (4 + 2 * qt + head // 4)
                                    c0 = (head % 4) * 65
                                    S.op('pe', lambda e, ab=ab, c0=c0, pt=pt, hh=hh, qt=qt, st=st, kvh=kvh, head=head: e.matmul(
                                        ab[:, c0:c0 + 65], lhsT=pt[:, hh * 256 + qt * 128:hh * 256 + (qt + 1) * 128],
                                        rhs=Vx[:, st, kvh, 0:65], start=(st == 0 and head % 4 == 0), stop=(st == NT - 1 and head % 4 == 3)),
                                        reads=[ptk, 'Vx'], writes=[abk])
                    for qt in range(2 if not os.environ.get('K_SKIP_NORM') else 0):
                        for half in range(2):
                            ab = P[4 + 2 * qt + half]; abk = 'P%d' % (4 + 2 * qt + half)
                            av = ab[:, 0:260].rearrange("p (h c) -> p h c", c=65)
                            S.op('dve', lambda e, av=av, half=half: e.reciprocal(out=rec[:, half * 4:(half + 1) * 4], in_=av[:, :, 64]),
                                 reads=[abk], writes=['rec'])
                            S.op('dve', lambda e, av=av, half=half: e.tensor_tensor(
                                out=atok[:, half * 4:(half + 1) * 4, :], in0=av[:, :, 0:64],
                                in1=rec[:, half * 4:(half + 1) * 4].unsqueeze(2).to_broadcast([128, 4, 64]), op=ALU.mult),
                                reads=[abk, 'rec'], writes=['atok'])
                        pa = P[0][:].bitcast(BF16)
                        if os.environ.get('K_SKIP_TR'):
                            continue
                        for hp in range(4):
                            S.op('pe', lambda e, hp=hp, pa=pa: e.transpose(
                                out=pa[:, hp * 128:(hp + 1) * 128],
                                in_=atok[:, 2 * hp:2 * hp + 2, :].rearrange("p a d -> p (a d)"), identity=identb[:]),
                                reads=['atok', 'identb'], writes=['P0'])
                        if os.environ.get('K_SKIP_CP'):
                            continue
                        S.op('dve', lambda e, pa=pa, q0=q0, qt=qt: e.tensor_copy(
                            out=attT_all[:, :, q0 + qt * 128:q0 + (qt + 1) * 128],
                            in_=pa[:, 0:512].rearrange("p (j t) -> p j t", j=4)), reads=['P0'], writes=['attT_all'])
                S.dma(lambda e: e.dma_start(out=attD[:, :], in_=attT_all[:].rearrange("p a t -> p (a t)")), reads=['attT_all'], writes=['attD'])
                if dbg:
                    o1 = dout("dbg_attT", [128, 4 * TL], BF16)
                    S.dma(lambda e: e.dma_start(out=o1[:, :], in_=attT_all[:].rearrange("p a t -> p (a t)")), reads=['attT_all'])
                S.barrier()
                S.flush()
        if stop_after <= 2:
            return nc, dbg_outs

        def TTo(eng, out, a, b_, op, R, W):
            S.op(eng, lambda e: e.tensor_tensor(out=out, in0=a, in1=b_, op=op), reads=R, writes=W)

        def CP(eng, out, in_, R, W):
            if eng == 'act':
                S.op('act', lambda e: e.copy(out=out, in_=in_), reads=R, writes=W)
            else:
                S.op(eng, lambda e: e.tensor_copy(out=out, in_=in_), reads=R, writes=W)

        def ACTF(out, in_, func, R, W, scale=1.0, bias=None):
            if bias is None:
                S.op('act', lambda e: e.activation(out=out, in_=in_, func=func, scale=scale), reads=R, writes=W)
            else:
                S.op('act', lambda e: e.activation(out=out, in_=in_, func=func, scale=scale, bias=bias), reads=R, writes=W)

        def MM(out, lhsT, rhs, R, W, start=True, stop=True):
            S.op('pe', lambda e: e.matmul(out, lhsT=lhsT, rhs=rhs, start=start, stop=stop), reads=R, writes=W)

        def TR(out, in_, idn, R, W):
            S.op('pe', lambda e: e.transpose(out=out, in_=in_, identity=idn), reads=R, writes=W)

        with contextlib.ExitStack() as ph:
            def t32(name, shape=(64, 1024)):
                return sb(ph, name, list(shape), F32)
            rp = t32("rp", (64, 8, 10)); rpd = t32("rpd", (64, 8, 8))
            lmu = t32("lmu", (128, 3)); lmd = t32("lmd", (128, 6))
            decupb = sb(ph, "decupb", [64, 2, 512], BF16); iclupb = sb(ph, "iclupb", [64, 2, 512], BF16)
            gateupb = sb(ph, "gateupb", [128, 512], BF16)
            mk4 = t32("mk4", (128, 2, 512)); mn1 = t32("mn1", (128, 2, 128)); bm = t32("bm", (128, 512))
            rst = t32("rst"); ones64 = sb(ph, "ones64", [64, 64], BF16); tiny = t32("tiny", (64, 1))
            S.dma(lambda e: e.dma_start(out=rp[:].rearrange("k h n -> k (h n)"), in_=rp_d[:, :]), writes=['rp'])
            S.dma(lambda e: e.dma_start(out=lmu[:], in_=lmu_d[:, :]), writes=['lmu'])
            S.dma(lambda e: e.dma_start(out=mk4[:].rearrange("p a n -> p (a n)"), in_=mk4_d[:, :]), writes=['mk4'])
            S.dma(lambda e: e.dma_start(out=mn1[:].rearrange("p a n -> p (a n)"), in_=mn1_d[:, :]), writes=['mn1'])
            S.dma(lambda e: e.dma_start(out=bm[:], in_=bm_d[:, :]), writes=['bm'])
            S.dma(lambda e: e.dma_start(out=rst[:], in_=rst_d[:, :]), writes=['rst'])
            S.op('pool', lambda e: e.memset(ones64[:], 1.0), writes=['ones64'])
            S.op('pool', lambda e: e.memset(tiny[:], 1e-24), writes=['tiny'])
            for i in range(3):
                S.op('dve', lambda e, i=i: e.tensor_scalar(out=rpd[:, :, 2 * i], in0=rp[:, :, i], scalar1=0.5, scalar2=None, op0=ALU.mult),
                     reads=['rp'], writes=['rpd'])
                S.op('dve', lambda e, i=i: e.tensor_scalar(out=rpd[:, :, 2 * i + 1], in0=rp[:, :, i], scalar1=-1.0, scalar2=1.0,
                                                          op0=ALU.mult, op1=ALU.add), reads=['rp'], writes=['rpd'])
                S.op('dve', lambda e, i=i: e.tensor_scalar(out=lmd[:, 2 * i:2 * i + 1], in0=lmu[:, i:i + 1], scalar1=0.5, scalar2=None, op0=ALU.mult),
                     reads=['lmu'], writes=['lmd'])
                S.op('dve', lambda e, i=i: e.tensor_scalar(out=lmd[:, 2 * i + 1:2 * i + 2], in0=lmu[:, i:i + 1], scalar1=-1.0, scalar2=1.0,
                                                          op0=ALU.mult, op1=ALU.add), reads=['lmu'], writes=['lmd'])
            S.op('dve', lambda e: e.tensor_scalar(out=rpd[:, :, 6], in0=rp[:, :, 4], scalar1=-1.0, scalar2=1.0, op0=ALU.mult, op1=ALU.add),
                 reads=['rp'], writes=['rpd'])

            def bc(t2):
                return t2.unsqueeze(2).to_broadcast([64, 8, 128])

            pin = [sb(ph, "pin%d" % i, [64, 8, 130], F32) for i in range(3)]
            plo = [sb(ph, "plo%d" % i, [128, 130], F32) for i in range(3)]
            tS = t32("tS"); xr = t32("xr"); xk = t32("xk"); xv = t32("xv")
            xlo = t32("xlo", (128, 3, 128)); twl = sb(ph, "twl", [64, 128], BF16); xalb = sb(ph, "xalb", [64, 128], BF16)
            glsb = sb(ph, "glsb", [128, 128], BF16)
            sqb = sb(ph, "sqb", [64, 1024], BF16); kk = t32("kk")
            sg = t32("sg"); ad = t32("ad"); cs = t32("cs"); ex = t32("ex"); E1 = t32("E1"); E2s = [t32("E2_0"), t32("E2_1")]; E3 = t32("E3")
            bb = t32("bb"); t1 = t32("t1"); kd = bb; bt32 = t32("bt32"); kt32 = t32("kt32"); rkb = sqb; kkk = t1; rs = E1; csb = bt32; bv = kt32
            bvt = t32("bvt", (128, 512)); gtok = t32("gtok", (128, 512)); glS = t32("glS", (128, 128))
            opTs = [{n: sb(ph, "op%d_" % q_ + n, [64, 1024], BF16) for n in ("a", "r", "b", "k", "bh", "kh", "v")} for q_ in range(2)]
            N1Ta, N1a, IN1Ta, N2a, N2Ta, IN2Ta, N4a, N4Ta, IN4Ta, IN8Ta, AakTa, ArbTa, ArkTa = [
                sb(ph, "ba%d" % i, [128, 8, 128], BF16) for i in range(13)]
            TMa = sb(ph, "TMa", [128, 8, 5, 64], BF16)
            Xa = [sb(ph, "Xa%d" % i, [128, 8, 128], BF16) for i in range(2)]
            Bbd = sb(ph, "Bbd", [128, 512], BF16); Ubd = sb(ph, "Ubd", [128, 512], BF16); Vbd = sb(ph, "Vbd", [128, 512], BF16)
            GTs = sb(ph, "GTs", [64, 512], BF16); Es = t32("Es", (64, 512)); QTs = sb(ph, "QTs", [64, 128], BF16)
            Y0s = t32("Y0s", (128, 64)); ytmp = gtok; yc = t32("yc", (128, 64))
            Yblk = t32("Yblk", (128, 8, 64))
            H = t32("H", (64, 512)); Hb = sb(ph, "Hb", [64, 512], BF16); Ht = t32("Ht", (64, 512))

            def view_hct(t):
                return t[:, :]

            def view_out(t):
                return t[:, :]

            def g16(t):
                return t[:, :].rearrange("k (g t) -> k g t", t=16)

            def chv(t, c):
                return t[:, :].rearrange("k (h c t) -> k h c t", h=8, c=8)[:, :, c, :]

            for (dst, src, nm, rows) in ((decupb, decup_d, 'decupb', 64), (iclupb, iclup_d, 'iclupb', 64), (gateupb, gateup_d, 'gateupb', 128)):
                stg_, sk_ = (bt32, 'bt32') if rows == 64 else (bvt, 'bvt')
                S.dma(lambda e, src=src, stg_=stg_: e.dma_start(out=stg_[:, :], in_=src[:, :]), writes=[sk_])
                dv = dst[:].rearrange("p a n -> p (a n)") if rows == 64 else dst[:]
                CP('dve', dv, stg_[:, :], [sk_], [nm])
            blocks = []
            nblk = int(os.environ.get('K_RBLK', 99))
            for d_ in range(2):
                cb_ = [0, 1] if d_ == 0 else [1, 0]
                lb_ = list(range(2, NT)) if d_ == 0 else list(range(NT - 1, 1, -1))
                blocks += [(d_, b_, j_ == 0) for j_, b_ in enumerate((cb_ + lb_)[:nblk])]

            def emit_prep(idx):
                d, blk, first = blocks[idx]
                opT = opTs[idx % 2]; E2 = E2s[idx % 2]; e2k = 'E2_%d' % (idx % 2); opk = 'o%d_' % (idx % 2)

                u0 = blk * 128
                is_ctx = blk < 2
                seq_lo, seq_hi = (0, TC) if is_ctx else (TC, TT)
                lo = max(u0 - 1, seq_lo); hi = min(u0 + 129, seq_hi)
                c_lo = lo - (u0 - 1); c_hi = c_lo + (hi - lo)
                for i in range(3):
                    if c_lo > 0:
                        S.op('pool', lambda e, i=i: e.memset(pin[i][:, :, 0:1], 0.0), writes=['pin%d' % i])
                    if c_hi < 130:
                        S.op('pool', lambda e, i=i: e.memset(pin[i][:, :, 129:130], 0.0), writes=['pin%d' % i])
                    S.dma(lambda e, i=i, lo=lo, hi=hi, c_lo=c_lo, c_hi=c_hi: e.dma_start(
                        out=pin[i][:, :, c_lo:c_hi],
                        in_=rwT[i * 512:(i + 1) * 512, lo:hi].rearrange("(h k) t -> k h t", k=64)), reads=['rwT'], writes=['pin%d' % i])
                for i, (r0, nr) in enumerate(((1536, 64), (1600, 64), (1664, 128))):
                    if c_lo > 0:
                        S.op('pool', lambda e, i=i: e.memset(plo[i][:, 0:1], 0.0), writes=['plo%d' % i])
                    if c_hi < 130:
                        S.op('pool', lambda e, i=i: e.memset(plo[i][:, 129:130], 0.0), writes=['plo%d' % i])
                    S.dma(lambda e, i=i, r0=r0, nr=nr, lo=lo, hi=hi, c_lo=c_lo, c_hi=c_hi: e.dma_start(
                        out=plo[i][0:nr, c_lo:c_hi], in_=rwT[r0:r0 + nr, lo:hi]), reads=['rwT'], writes=['plo%d' % i])
                for i, xo in enumerate((xr, xk, xv)):
                    xo3 = xo[:, :].rearrange("k (h t) -> k h t", h=8)
                    ts3 = tS[:, :].rearrange("k (h t) -> k h t", h=8)
                    TTo('pool', ts3, pin[i][:, :, 0:128], pin[i][:, :, 2:130], ALU.add, ['pin%d' % i], ['tS'])
                    TTo('pool', ts3, ts3, bc(rpd[:, :, 2 * i]), ALU.mult, ['tS', 'rpd'], ['tS'])
                    TTo('dve', xo3, pin[i][:, :, 1:129], bc(rpd[:, :, 2 * i + 1]), ALU.mult, ['pin%d' % i, 'rpd'], ['x%d' % i])
                    TTo('dve', xo3, xo3, ts3, ALU.add, ['x%d' % i, 'tS'], ['x%d' % i])
                for i, nr in enumerate((64, 64, 128)):
                    S.op('pool', lambda e, i=i, nr=nr: e.tensor_tensor(out=tS[0:nr, 0:128] if nr == 64 else glS[:, 0:128],
                                                                     in0=plo[i][0:nr, 0:128], in1=plo[i][0:nr, 2:130], op=ALU.add),
                         reads=['plo%d' % i], writes=['tS' if nr == 64 else 'glS'])
                    S.op('dve', lambda e, i=i, nr=nr: e.tensor_scalar(out=xlo[0:nr, i, :], in0=plo[i][0:nr, 1:129],
                                                                    scalar1=lmd[0:nr, 2 * i + 1:2 * i + 2], scalar2=None, op0=ALU.mult),
                         reads=['plo%d' % i, 'lmd'], writes=['xlo'])
                    S.op('dve', lambda e, i=i, nr=nr: e.scalar_tensor_tensor(
                        out=xlo[0:nr, i, :], in0=(tS[0:nr, 0:128] if nr == 64 else glS[:, 0:128]), scalar=lmd[0:nr, 2 * i:2 * i + 1],
                        in1=xlo[0:nr, i, :], op0=ALU.mult, op1=ALU.add),
                        reads=['tS' if nr == 64 else 'glS', 'lmd', 'xlo'], writes=['xlo'])
                ACTF(twl[:], xlo[0:64, 0, :], AF.Tanh, ['xlo'], ['twl'])
                CP('pool', xalb[:], xlo[0:64, 1, :], ['xlo'], ['xalb'])
                k3 = lambda t: t[:, :].rearrange("k (h t) -> k h t", h=8)
                TTo('pool', k3(kkk), k3(xk), bc(rp[:, :, 3]), ALU.mult, ['x1', 'rp'], ['t1'])
                ACTF(sqb[:], kkk[:], AF.Square, ['t1'], ['sqb'])
                PP = PS[0:64, 0:1024]
                for hf in range(2):
                    MM(PS[0:64, hf * 512:(hf + 1) * 512], ones64[:], sqb[:, hf * 512:(hf + 1) * 512], ['ones64', 'sqb'], ['P0'])
                ACTF(rs[:], PP, AF.Ln, ['P0', 'tiny'], ['E1'], bias=tiny[:])
                ACTF(rs[:], rs[:], AF.Exp, ['E1'], ['E1'], scale=-0.5)
                TTo('dve', kk[:], kkk[:], rs[:], ALU.mult, ['t1', 'E1'], ['kk'])
                for h in range(8):
                    MM(PS[0:64, h * 128:(h + 1) * 128], decupb[:, d, h * 64:(h + 1) * 64], twl[:], ['decupb', 'twl'], ['P0'])
                TTo('dve', k3(sg), PP.rearrange("k (h t) -> k h t", h=8), bc(rp[:, :, 6 + d]), ALU.add, ['P0', 'rp'], ['sg'])
                ACTF(sg[:], sg[:], AF.Sigmoid, ['sg'], ['sg'])
                for h in range(8):
                    MM(PS[0:64, h * 128:(h + 1) * 128], iclupb[:, d, h * 64:(h + 1) * 64], xalb[:], ['iclupb', 'xalb'], ['P0'])
                TTo('dve', k3(ad), PP.rearrange("k (h t) -> k h t", h=8), bc(rp[:, :, 8 + d]), ALU.add, ['P0', 'rp'], ['ad'])
                ACTF(ad[:], ad[:], AF.Sigmoid, ['ad'], ['ad'])
                S.op('dve', lambda e: e.tensor_tensor_scan(out=cs[:], data0=rst[:], data1=sg[:], initial=0.0, op0=ALU.mult, op1=ALU.add),
                     reads=['rst', 'sg'], writes=['cs'])
                csf = cs
                if d == 1:
                    TTo('pool', ex[:], sg[:], cs[:], ALU.subtract, ['sg', 'cs'], ['ex'])
                    csv = cs[:, :].rearrange("k (g t) -> k g t", t=16)
                    TTo('dve', csb[:, :].rearrange("k (g t) -> k g t", t=16), ex[:, :].rearrange("k (g t) -> k g t", t=16),
                       csv[:, :, 15:16].to_broadcast([64, 64, 16]), ALU.add, ['ex', 'cs'], ['bt32'])
                    csf = csb
                ACTF(E2[:], csf[:], AF.Exp, ['cs', 'bt32'], [e2k], scale=DEC_C)
                TTo('pool', ex[:], csf[:], sg[:], ALU.subtract, ['cs', 'bt32', 'sg'], ['ex'])
                ACTF(E1[:], ex[:], AF.Exp, ['ex'], ['E1'], scale=DEC_C)
                S.op('dve', lambda e: e.reciprocal(out=E3[:], in_=E2[:]), reads=[e2k], writes=['E3'])
                tsel = 15 if d == 0 else 0
                def cm(t, h):
                    return t[:, :].rearrange("k (c h t) -> k c h t", c=8, h=8)[:, :, h, :]

                def hm(t, h):
                    return t[:, :].rearrange("k (h c t) -> k h c t", h=8, c=8)[:, h, :, :]
                TTo('pool', bb[:], kk[:], ad[:], ALU.mult, ['kk', 'ad'], ['bb'])
                TTo('dve', bt32[:], bb[:], E3[:], ALU.mult, ['bb', 'E3'], ['bt32'])
                TTo('pool', k3(t1), k3(ad), bc(rp[:, :, 4]), ALU.mult, ['ad', 'rp'], ['t1'])
                TTo('dve', k3(t1), k3(t1), bc(rpd[:, :, 6]), ALU.add, ['t1', 'rpd'], ['t1'])
                TTo('pool', kd[:], xk[:], t1[:], ALU.mult, ['x1', 't1'], ['bb'])
                TTo('dve', kt32[:], kd[:], E3[:], ALU.mult, ['bb', 'E3'], ['kt32'])
                for h in range(8):
                    pch = hm(E2, h)[:, :, tsel:tsel + 1].to_broadcast([64, 8, 16])
                    S.op('dve', lambda e, h=h: e.scalar_tensor_tensor(out=cm(opT['a'], h), in0=hm(kk, h), scalar=-1.0, in1=hm(E1, h),
                                                                     op0=ALU.mult, op1=ALU.mult), reads=['kk', 'E1'], writes=[opk + 'a'])
                    TTo('pool', cm(opT['r'], h), hm(xr, h), hm(E2, h), ALU.mult, ['x0', e2k], [opk + 'r'])
                    CP('act', cm(opT['b'], h), hm(bt32, h), ['bt32'], [opk + 'b'])
                    TTo('pool', cm(opT['bh'], h), hm(bt32, h), pch, ALU.mult, ['bt32', e2k], [opk + 'bh'])
                    CP('act', cm(opT['k'], h), hm(kt32, h), ['kt32'], [opk + 'k'])
                    TTo('dve', cm(opT['kh'], h), hm(kt32, h), pch, ALU.mult, ['kt32', e2k], [opk + 'kh'])
                    CP('act', cm(opT['v'], h), hm(xv, h), ['x2'], [opk + 'v'])
                if d == 0 and not is_ctx:
                    ul = u0 - TC
                    TTo('pool', k3(t1), k3(xr), bc(rp[:, :, 5]), ALU.mult, ['x0', 'rp'], ['t1'])
                    TTo('dve', rkb[:], t1[:], xk[:], ALU.mult, ['t1', 'x1'], ['sqb'])
                    for hf in range(2):
                        MM(PS[0:64, hf * 512:(hf + 1) * 512], ones64[:], rkb[:, hf * 512:(hf + 1) * 512], ['ones64', 'sqb'], ['P0'])
                    TTo('dve', bv[:], PP, xv[:], ALU.mult, ['P0', 'x2'], ['kt32'])
                    for h in range(8):
                        TR(PS[:, 512 + h * 64:512 + (h + 1) * 64], bv[:, h * 128:(h + 1) * 128], identf[0:64, 0:64], ['kt32', 'identf'], ['P0'])
                    CP('dve', bvt[:], PS[:, 512:1024], ['P0'], ['bvt'])
                    S.dma(lambda e, ul=ul: e.dma_start(out=bonD[ul:ul + 128, :], in_=bvt[:]), reads=['bvt'], writes=['bonD'])
                    ACTF(glsb[:], xlo[:, 2, :], AF.Sigmoid, ['xlo'], ['glsb'])
                    MM(PS[:, 512:1024], glsb[:], gateupb[:], ['glsb', 'gateupb'], ['P0'])
                    CP('dve', gtok[:], PS[:, 512:1024], ['P0'], ['gtok'])
                    S.dma(lambda e, ul=ul: e.dma_start(out=gD[ul:ul + 128, :], in_=gtok[:]), reads=['gtok'], writes=['gD'])

            def emit_chunks(idx):
                d, blk, first = blocks[idx]
                opT = opTs[idx % 2]; E2 = E2s[idx % 2]; e2k = 'E2_%d' % (idx % 2); opk = 'o%d_' % (idx % 2)
                u0 = blk * 128
                is_ctx = blk < 2
                tsel = 15 if d == 0 else 0
                if first:
                    S.op('pool', lambda e: e.memset(H[:], 0.0), writes=['H'])
                    S.op('pool', lambda e: e.memset(Hb[:], 0.0), writes=['Hb'])

                PA_, PB_, PC_, PD_ = (PS[:, 0:1024], PS[:, 1024:2048], PS[:, 2048:3072], PS[:, 3072:4096])
                kPB, kPC, kPD = ['P2', 'P3'], ['P4', 'P5'], ['P6', 'P7']
                c8 = lambda ap: ap.rearrange("p (c n) -> p c n", c=8)
                def bc8(m):
                    return m.unsqueeze(1).to_broadcast([128, 8, 128])
                MSd = mk4[:, d, 0:128]; MId = mk4[:, d, 128:256]; MStd = mn1[:, d, :]
                def ch(name, c):
                    return opT[name][:, c * 128:(c + 1) * 128]
                for c in range(8):
                    MM(PB_[:, c * 128:(c + 1) * 128], ch('b', c), ch('a', c), [opk + 'b', opk + 'a'], kPB)
                for c in range(8):
                    MM(PC_[:, c * 128:(c + 1) * 128], ch('a', c), ch('b', c), [opk + 'a', opk + 'b'], kPC)
                for c in range(8):
                    MM(PD_[:, c * 128:(c + 1) * 128], ch('k', c), ch('a', c), [opk + 'k', opk + 'a'], kPD)
                TTo('dve', N1Ta[:], c8(PB_), bc8(MSd), ALU.mult, kPB + ['mk4'], ['N1Ta'])
                TTo('dve', N1a[:], c8(PC_), bc8(MStd), ALU.mult, kPC + ['mn1'], ['N1a'])
                TTo('dve', AakTa[:], c8(PD_), bc8(MSd), ALU.mult, kPD + ['mk4'], ['AakTa'])
                TTo('pool', IN1Ta[:], N1Ta[:], bc8(identb[:, :]), ALU.add, ['N1Ta', 'identb'], ['IN1Ta'])
                PBb = PB_.bitcast(BF16)
                for c in range(8):
                    for si, nm_ in ((0, 'a'), (1, 'v'), (2, 'bh'), (3, 'kh')):
                        TR(PBb[:, c * 256 + si * 64:c * 256 + (si + 1) * 64], ch(nm_, c), identb[0:64, 0:64], [opk + nm_, 'identb'], kPB)
                PBb4 = PBb.rearrange("p (c s n) -> p c s n", c=8, s=4)
                CP('dve', TMa[:, :, 0, :], PBb4[:, :, 0, :], kPB, ['TMa'])
                CP('dve', TMa[:, :, 2:5, :].rearrange("p c s n -> p c (s n)"), PBb.rearrange("p (c n) -> p c n", c=8)[:, :, 64:256], kPB, ['TMa'])
                for c in range(8):
                    MM(PC_[:, c * 128:(c + 1) * 128], N1Ta[:, c, :], N1a[:, c, :], ['N1Ta', 'N1a'], kPC)
                for c in range(8):
                    MM(PD_[:, c * 128:(c + 1) * 128], N1a[:, c, :], N1Ta[:, c, :], ['N1Ta', 'N1a'], kPD)
                CP('act', N2a[:], c8(PC_), kPC, ['N2a'])
                CP('dve', N2Ta[:], c8(PD_), kPD, ['N2Ta'])
                TTo('pool', IN2Ta[:], N2Ta[:], bc8(identb[:, :]), ALU.add, ['N2Ta', 'identb'], ['IN2Ta'])
                for c in range(8):
                    MM(PB_[:, c * 64:(c + 1) * 64], AakTa[:, c, :], TMa[:, c, 2, :], ['AakTa', 'TMa'], kPB)
                CP('dve', TMa[:, :, 1, :], PB_[:, 0:512].rearrange("p (c n) -> p c n", c=8), kPB, ['TMa'])
                for c in range(8):
                    MM(PC_[:, c * 128:(c + 1) * 128], N2Ta[:, c, :], N2a[:, c, :], ['N2Ta', 'N2a'], kPC)
                for c in range(8):
                    MM(PD_[:, c * 128:(c + 1) * 128], N2a[:, c, :], N2Ta[:, c, :], ['N2Ta', 'N2a'], kPD)
                CP('act', N4a[:], c8(PC_), kPC, ['N4a'])
                CP('dve', N4Ta[:], c8(PD_), kPD, ['N4Ta'])
                TTo('pool', IN4Ta[:], N4Ta[:], bc8(identb[:, :]), ALU.add, ['N4Ta', 'identb'], ['IN4Ta'])
                for c in range(8):
                    MM(PB_[:, c * 128:(c + 1) * 128], N4a[:, c, :], N4Ta[:, c, :], ['N4a', 'N4Ta'], kPB)
                TTo('dve', IN8Ta[:], c8(PB_), bc8(identf[:, :]), ALU.add, kPB + ['identf'], ['IN8Ta'])
                if not is_ctx:
                    for c in range(8):
                        MM(PC_[:, c * 128:(c + 1) * 128], ch('b', c), ch('r', c), [opk + 'b', opk + 'r'], kPC)
                    for c in range(8):
                        MM(PD_[:, c * 128:(c + 1) * 128], ch('k', c), ch('r', c), [opk + 'k', opk + 'r'], kPD)
                    TTo('dve', ArbTa[:], c8(PC_), bc8(MId), ALU.mult, kPC + ['mk4'], ['ArbTa'])
                    TTo('dve', ArkTa[:], c8(PD_), bc8(MId), ALU.mult, kPD + ['mk4'], ['ArkTa'])
                xsrc = None
                for li, (INa, ik) in enumerate(((IN8Ta, 'IN8Ta'), (IN4Ta, 'IN4Ta'), (IN2Ta, 'IN2Ta'), (IN1Ta, 'IN1Ta'))):
                    Pq, kq = (PB_, kPB) if li % 2 == 0 else (PC_, kPC)
                    for c in range(8):
                        rhs_ = TMa[:, c, 0:2, :].rearrange("p a n -> p (a n)") if xsrc is None else xsrc[:, c, :]
                        MM(Pq[:, c * 128:(c + 1) * 128], INa[:, c, :], rhs_, [ik, 'TMa' if xsrc is None else xk_], kq)
                    Xn = Xa[li % 2]; xk_ = 'Xa%d' % (li % 2)
                    CP('dve' if li % 2 else 'act', Xn[:], c8(Pq), kq, [xk_])
                    xsrc = Xn
                B3 = PS[:, 1536:2048]; B4 = PS[:, 2048:2560]; B5 = PS[:, 2560:3072]; B6 = PS[:, 3072:3584]; B7 = PS[:, 3584:4096]
                bm3 = bm[:, :].rearrange("p (h n) -> p h n", h=8)
                nch = int(os.environ.get('K_RCH', 8))
                for c in (list(range(8)) if d == 0 else list(range(7, -1, -1)))[:nch]:
                    Wc = xsrc[:, c, 0:64]; U0 = xsrc[:, c, 64:128]
                    Bh_ = TMa[:, c, 3, :]; Kh_ = TMa[:, c, 4, :]; Vt_ = TMa[:, c, 2, :]
                    for dst_, src_, rk_, wk_ in ((Bbd, Bh_, 'TMa', 'Bbd'), (Ubd, U0, xk_, 'Ubd'), (Vbd, Vt_, 'TMa', 'Vbd')):
                        TTo('pool', dst_[:, :].rearrange("p (h n) -> p h n", h=8), src_.unsqueeze(1).to_broadcast([128, 8, 64]), bm3, ALU.mult,
                            [rk_, 'bm'], [wk_])
                    MM(B4[0:64, :], Wc, Bbd[:], [xk_, 'Bbd'], ['P4'])
                    CP('act', GTs[:], B4[0:64, :], ['P4'], ['GTs'])
                    if not is_ctx:
                        MM(B6[0:64, 0:128], Wc, ArbTa[:, c, :], [xk_, 'ArbTa'], ['P6'])
                        TTo('dve', QTs[:], B6[0:64, 0:128], ch('r', c), ALU.add, ['P6', opk + 'r'], ['QTs'])
                        MM(B6[:, 128:192], ArbTa[:, c, :], U0, ['ArbTa', xk_], ['P6'], start=True, stop=False)
                        MM(B6[:, 128:192], ArkTa[:, c, :], Vt_, ['ArkTa', 'TMa'], ['P6'], start=False, stop=True)
                        CP('dve', Y0s[:], B6[:, 128:192], ['P6'], ['Y0s'])
                        MM(B7, QTs[:], Hb[:], ['QTs', 'Hb'], ['P7'])
                        TTo('dve', ytmp[:], B7, bm[:], ALU.mult, ['P7', 'bm'], ['gtok'])
                        S.op('dve', lambda e: e.tensor_reduce(out=yc[:], in_=ytmp[:, :].rearrange("p (h v) -> p v h", h=8), axis=AX.X, op=ALU.add),
                             reads=['gtok'], writes=['yc'])
                        TTo('pool', Yblk[:, c, :], yc[:], Y0s[:], ALU.add, ['yc', 'Y0s'], ['Yblk'])
                    MM(B3[0:64, :], Bh_, Ubd[:], ['TMa', 'Ubd'], ['P3'], start=True, stop=False)
                    MM(B3[0:64, :], Kh_, Vbd[:], ['TMa', 'Vbd'], ['P3'], start=False, stop=False)
                    for h in range(8):
                        MM(B3[0:64, h * 64:(h + 1) * 64], GTs[:, h * 64:(h + 1) * 64], Hb[:, h * 64:(h + 1) * 64], ['GTs', 'Hb'], ['P3'],
                           start=False, stop=(h == 7))
                    PCc = chv(E2, c)[:, :, tsel:tsel + 1].to_broadcast([64, 8, 64])
                    TTo('pool', Ht[:, :].rearrange("k (h v) -> k h v", h=8), H[:, :].rearrange("k (h v) -> k h v", h=8), PCc, ALU.mult,
                        ['H', e2k], ['Ht'])
                    TTo('dve', Hb[:], Ht[:], B3[0:64, :], ALU.add, ['Ht', 'P3'], ['Hb'])
                    TTo('dve', H[:], Ht[:], B3[0:64, :], ALU.add, ['Ht', 'P3'], ['H'])
                    S.drain(S.pend, (len(S.pend) + 7) // 8)
                if not is_ctx:
                    ul = u0 - TC
                    for h in range(8):
                        S.dma(lambda e, h=h, ul=ul, d=d: e.dma_start(
                            out=ydir[d, ul:ul + 128, h * 64:(h + 1) * 64].rearrange("(c t) v -> t c v", t=16),
                            in_=Yblk[h * 16:(h + 1) * 16, :, :]), reads=['Yblk'], writes=['ydir'])

            S.capture = []
            emit_prep(0)
            pend = S.capture; S.capture = None
            S.drain(pend, len(pend))
            for idx in range(len(blocks)):
                pend = []
                if idx + 1 < len(blocks):
                    S.capture = []
                    emit_prep(idx + 1)
                    pend = S.capture; S.capture = None
                S.pend = pend
                emit_chunks(idx)
                S.drain(pend, len(pend))

            if dbg:
                oy = dout("dbg_y", [2 * TL, 512]); ob = dout("dbg_bon", [TL, 512]); og = dout("dbg_g", [TL, 512])
                S.dma(lambda e: e.dma_start(out=oy[:, :], in_=ydir.rearrange("d t n -> (d t) n")), reads=['ydir'])
                S.dma(lambda e: e.dma_start(out=ob[:, :], in_=bonD[:, :]), reads=['bonD'])
                S.dma(lambda e: e.dma_start(out=og[:, :], in_=gD[:, :]), reads=['gD'])
                oH = dout("dbg_H", [64, 512])
                S.dma(lambda e: e.dma_start(out=oH[:, :], in_=H[:]), reads=['H'])
            S.barrier()
            S.flush()
        if stop_after <= 3:
            return nc, dbg_outs

        with contextlib.ExitStack() as ph:
            woutb = sb(ph, "woutb", [128, 8, D], BF16)
            attT_all = sb(ph, "attT_c", [128, 4, TL], BF16)
            S.dma(lambda e: e.dma_start(out=attT_all[:].rearrange("p a t -> p (a t)"), in_=attD[:, :]), reads=['attD'], writes=['attT_all'])
            cst = {}
            for nm, src in (("gt1", modd[0:1, 2048:3072]), ("sh2", modd[0:1, 3072:4096]), ("sc2p", modd[0:1, 4096:5120]),
                            ("ln1g", ln1_d[0:1, :]), ("ln1b", ln1_d[1:2, :])):
                cst[nm] = sb(ph, nm, [128, D], F32)
                S.dma(lambda e, nm=nm, src=src: e.dma_start(out=cst[nm][:], in_=src.partition_broadcast(128)), reads=['modd'], writes=[nm])
            for nm, row in (("lnxg", 0), ("lnxb", 1)):
                cst[nm] = sb(ph, nm, [128, 512], F32)
                S.dma(lambda e, nm=nm, row=row: e.dma_start(out=cst[nm][:], in_=lnx_d[row:row + 1, :].partition_broadcast(128)), writes=[nm])
            wst2 = [sb(ph, "wst2_%d" % i, [128, D], F32) for i in range(2)]
            for j in range(8):
                w = wst2[j % 2]; wk = 'wst2_%d' % (j % 2)
                S.dma(lambda e, w=w, j=j: e.dma_start(out=w[:], in_=wout_d[j * 128:(j + 1) * 128, :]), writes=[wk])
                CP('pool' if j % 2 else 'dve', woutb[:, j, :], w[:], [wk], ['woutb'])
            rwf = sb(ph, "rwf", [128, 8, 16], F32)
            S.dma(lambda e: e.dma_start(out=rwf[:], in_=rw_d.rearrange("(j p) n -> p j n", p=128)), writes=['rwf'])
            gneps = sb(ph, "gneps", [128, 1], F32)
            S.op('pool', lambda e: e.memset(gneps[:], 64e-5), writes=['gneps'])
            yf = sb(ph, "yf", [128, 512], F32); yb = sb(ph, "yb", [128, 512], F32)
            bon = sb(ph, "bon", [128, 512], F32); gg = sb(ph, "gg", [128, 512], F32)
            ysum = sb(ph, "ysum", [128, 512], F32); ysq = sb(ph, "ysq", [128, 512], F32)
            gst = sb(ph, "gst", [128, 8], F32); gvar = sb(ph, "gvar", [128, 8], F32)
            rwob = sb(ph, "rwob", [128, 512], BF16); rwoT = sb(ph, "rwoT", [128, 4, 128], BF16)
            xin_t = sb(ph, "xin_t", [128, D], F32); tres = sb(ph, "tres", [128, D], F32)
            x1t = sb(ph, "x1t", [128, D], F32); h2f = sb(ph, "h2f", [128, D], F32); h2b = sb(ph, "h2b", [128, D], BF16)
            h2T = sb(ph, "h2T", [128, 8, 128], F32)
            stats = sb(ph, "statsC", [128, 2, 6], F32); mv = sb(ph, "mvC", [128, 2], F32)
            rstd = sb(ph, "rstdC", [128, 1], F32); nb = sb(ph, "nbC", [128, 1], F32)
            lntC = (stats, mv, rstd, nb, 'C')
            lmax = sb(ph, "lmax", [128, 1], F32); lex = sb(ph, "lex", [128, 16], F32); lsum = sb(ph, "lsum", [128, 1], F32)

            def v8(t):
                return t[:, :].rearrange("p (h v) -> p h v", h=8)

            def b8(t):
                return t[:, :].unsqueeze(2).to_broadcast([128, 8, 64])
            for i in range(int(os.environ.get('K_CT', 32))):
                t0 = i * 128
                S.dma(lambda e, t0=t0: e.dma_start(out=yf[:], in_=ydir[0, t0:t0 + 128, :]), reads=['ydir'], writes=['yf'])
                S.dma(lambda e, t0=t0: e.dma_start(out=yb[:], in_=ydir[1, t0:t0 + 128, :]), reads=['ydir'], writes=['yb'])
                S.dma(lambda e, t0=t0: e.dma_start(out=bon[:], in_=bonD[t0:t0 + 128, :]), reads=['bonD'], writes=['bon'])
                S.dma(lambda e, t0=t0: e.dma_start(out=gg[:], in_=gD[t0:t0 + 128, :]), reads=['gD'], writes=['gg'])
                S.dma(lambda e, t0=t0: e.dma_start(out=xin_t[:], in_=x_d[t0:t0 + 128, :]), writes=['xin_t'])
                TTo('pool', ysum[:], yf[:], yb[:], ALU.add, ['yf', 'yb'], ['ysum'])
                S.op('dve', lambda e: e.tensor_reduce(out=gst[:], in_=v8(ysum), axis=AX.X, op=ALU.add), reads=['ysum'], writes=['gst'])
                S.op('dve', lambda e: e.tensor_scalar(out=gst[:], in0=gst[:], scalar1=-1.0 / 64, scalar2=None, op0=ALU.mult),
                     reads=['gst'], writes=['gst'])
                TTo('dve', v8(ysum), v8(ysum), b8(gst), ALU.add, ['ysum', 'gst'], ['ysum'])
                ACTF(ysq[:], ysum[:], AF.Square, ['ysum'], ['ysq'])
                S.op('dve', lambda e: e.tensor_reduce(out=gvar[:], in_=v8(ysq), axis=AX.X, op=ALU.add), reads=['ysq'], writes=['gvar'])
                ACTF(gvar[:], gvar[:], AF.Ln, ['gvar', 'gneps'], ['gvar'], scale=1.0 / 64, bias=gneps[:])
                ACTF(gvar[:], gvar[:], AF.Exp, ['gvar'], ['gvar'], scale=-0.5)
                TTo('dve', v8(ysum), v8(ysum), b8(gvar), ALU.mult, ['ysum', 'gvar'], ['ysum'])
                TTo('pool', ysum[:], ysum[:], cst['lnxg'][:], ALU.mult, ['ysum', 'lnxg'], ['ysum'])
                TTo('dve', ysum[:], ysum[:], cst['lnxb'][:], ALU.add, ['ysum', 'lnxb'], ['ysum'])
                TTo('pool', ysum[:], ysum[:], bon[:], ALU.add, ['ysum', 'bon'], ['ysum'])
                TTo('dve', rwob[:], ysum[:], gg[:], ALU.mult, ['ysum', 'gg'], ['rwob'])
                pr_ = P[0][:].bitcast(BF16)
                for j in range(4):
                    TR(pr_[:, j * 128:(j + 1) * 128], rwob[:, j * 128:(j + 1) * 128], identb[:], ['rwob', 'identb'], ['P0'])
                CP('dve', rwoT[:], pr_[:, 0:512].rearrange("p (j t) -> p j t", j=4), ['P0'], ['rwoT'])
                for half in range(2):
                    ob = PS[:, 1024 + half * 512:1024 + (half + 1) * 512]
                    for j in range(8):
                        lt = attT_all[:, j, t0:t0 + 128] if j < 4 else rwoT[:, j - 4, :]
                        MM(ob, lt, woutb[:, j, half * 512:(half + 1) * 512], ['attT_all', 'rwoT', 'woutb'], ['P2'], start=(j == 0), stop=(j == 7))
                TTo('dve', tres[:], PS[:, 1024:2048], cst['gt1'][:], ALU.mult, ['P2', 'gt1'], ['tres'])
                S.op('dve', lambda e: e.scalar_tensor_tensor(out=tres[:], in0=xin_t[:], scalar=ALPHA, in1=tres[:], op0=ALU.mult, op1=ALU.add),
                     reads=['xin_t', 'tres'], writes=['tres'])
                ln_stats(lntC, tres, 'tres', eps5, 'eps5')
                S.op('act', lambda e: e.activation(out=x1t[:], in_=tres[:], func=AF.Identity, bias=nb[:], scale=rstd[:]),
                     reads=['tres', 'Cnb', 'Crstd'], writes=['x1t'])
                TTo('pool', x1t[:], x1t[:], cst['ln1g'][:], ALU.mult, ['x1t', 'ln1g'], ['x1t'])
                TTo('dve', x1t[:], x1t[:], cst['ln1b'][:], ALU.add, ['x1t', 'ln1b'], ['x1t'])
                S.dma(lambda e, t0=t0: e.dma_start(out=x1D[t0:t0 + 128, :], in_=x1t[:]), reads=['x1t'], writes=['x1D'])
                ln_stats(lntC, x1t, 'x1t', eps5, 'eps5')
                S.op('act', lambda e: e.activation(out=h2f[:], in_=x1t[:], func=AF.Identity, bias=nb[:], scale=rstd[:]),
                     reads=['x1t', 'Cnb', 'Crstd'], writes=['h2f'])
                TTo('pool', h2f[:], h2f[:], cst['sc2p'][:], ALU.mult, ['h2f', 'sc2p'], ['h2f'])
                TTo('dve', h2f[:], h2f[:], cst['sh2'][:], ALU.add, ['h2f', 'sh2'], ['h2f'])
                CP('pool', h2b[:], h2f[:], ['h2f'], ['h2b'])
                S.dma(lambda e, t0=t0: e.dma_start(out=h2D[t0:t0 + 128, :], in_=h2b[:]), reads=['h2b'], writes=['h2D'])
                for j in range(8):
                    TR(PS[:, 2048 + j * 128:2048 + (j + 1) * 128], h2f[:, j * 128:(j + 1) * 128], identf[:], ['h2f', 'identf'], ['P4'])
                CP('dve', h2T[:].rearrange("p j t -> p (j t)"), PS[:, 2048:3072], ['P4'], ['h2T'])
                for j in range(8):
                    MM(PS[:, 3072:3088], h2T[:, j, :], rwf[:, j, :], ['h2T', 'rwf'], ['P6'], start=(j == 0), stop=(j == 7))
                S.op('dve', lambda e: e.tensor_reduce(out=lmax[:], in_=PS[:, 3072:3088], axis=AX.X, op=ALU.max), reads=['P6'], writes=['lmax'])
                S.op('dve', lambda e: e.tensor_scalar(out=lmax[:], in0=lmax[:], scalar1=-1.0, scalar2=None, op0=ALU.mult), reads=['lmax'], writes=['lmax'])
                ACTF(lex[:], PS[:, 3072:3088], AF.Exp, ['P6', 'lmax'], ['lex'], bias=lmax[:])
                S.op('dve', lambda e: e.tensor_reduce(out=lsum[:], in_=lex[:], axis=AX.X, op=ALU.add), reads=['lex'], writes=['lsum'])
                S.op('dve', lambda e: e.reciprocal(out=lsum[:], in_=lsum[:]), reads=['lsum'], writes=['lsum'])
                S.op('dve', lambda e, i=i: e.tensor_scalar(out=aff_all[:, i, :], in0=lex[:], scalar1=lsum[:], scalar2=None, op0=ALU.mult),
                     reads=['lex', 'lsum'], writes=['aff_all'])
            if dbg:
                o1 = dout("dbg_x1", [TL, D]); o2 = dout("dbg_aff", [128, 512])
                S.dma(lambda e: e.dma_start(out=o1[:, :], in_=x1D[:, :]), reads=['x1D'])
                S.dma(lambda e: e.dma_start(out=o2[:, :], in_=aff_all[:].rearrange("p a b -> p (a b)")), reads=['aff_all'])
            S.barrier()
            S.flush()
        if stop_after <= 4:
            return nc, dbg_outs
        posm = sb(top, "posm", [128, 32, 16], F32)
        gw = sb(top, "gw", [128, 32, 16, 2], BF16)
        onesf = sb(top, "onesf", [128, 128], F32)
        iot = sb(top, "iot", [128, 516], F32)
        S.op('pool', lambda e: e.memset(onesf[:], 1.0), writes=['onesf'])
        S.dma(lambda e: e.dma_start(out=iot[:], in_=iot_d[:, :]), writes=['iot'])

        with contextlib.ExitStack() as ph:
            lo = sb(ph, "lo", [128, 16], F32); hi = sb(ph, "hi", [128, 16], F32); mid = sb(ph, "mid", [128, 16], F32)
            cmpt = sb(ph, "cmpt", [128, 32, 16], F32); cntp = sb(ph, "cntp", [128, 16], F32); ge = sb(ph, "ge", [128, 16], F32)
            dlt = sb(ph, "dlt", [128, 16], F32)
            ustr = sb(ph, "ustr", [128, 128], F32)
            mask = sb(ph, "mask", [128, 32, 16], F32); tot = sb(ph, "tot", [128, 32, 16], F32); cum = sb(ph, "cum", [128, 32, 16], F32)
            glo = sb(ph, "glo", [128, 32, 16], F32); ghi32 = sb(ph, "ghi32", [128, 32, 16], F32)
            S.dma(lambda e: e.dma_start(out=ustr[:], in_=ustr_d[:, :]), writes=['ustr'])
            S.op('pool', lambda e: e.memset(lo[:], 0.0), writes=['lo'])
            S.op('pool', lambda e: e.memset(hi[:], 1.0), writes=['hi'])
            affv = aff_all[:, :, :]
            for it in range(30):
                TTo('dve', mid[:], lo[:], hi[:], ALU.add, ['lo', 'hi'], ['mid'])
                S.op('dve', lambda e: e.tensor_scalar(out=mid[:], in0=mid[:], scalar1=0.5, scalar2=None, op0=ALU.mult), reads=['mid'], writes=['mid'])
                TTo('dve', cmpt[:], affv, mid[:, :].unsqueeze(1).to_broadcast([128, 32, 16]), ALU.is_ge, ['aff_all', 'mid'], ['cmpt'])
                S.op('dve', lambda e: e.tensor_reduce(out=cntp[:], in_=cmpt[:].rearrange("p t e -> p e t"), axis=AX.X, op=ALU.add),
                     reads=['cmpt'], writes=['cntp'])
                MM(PS[:, 0:16], onesf[:], cntp[:], ['onesf', 'cntp'], ['P0'])
                S.op('dve', lambda e: e.tensor_scalar(out=ge[:], in0=PS[:, 0:16], scalar1=511.5, scalar2=None, op0=ALU.is_ge), reads=['P0'], writes=['ge'])
                TTo('dve', dlt[:], mid[:], lo[:], ALU.subtract, ['mid', 'lo'], ['dlt'])
                TTo('dve', dlt[:], dlt[:], ge[:], ALU.mult, ['dlt', 'ge'], ['dlt'])
                TTo('dve', lo[:], lo[:], dlt[:], ALU.add, ['lo', 'dlt'], ['lo'])
                TTo('dve', dlt[:], hi[:], mid[:], ALU.subtract, ['hi', 'mid'], ['dlt'])
                TTo('dve', dlt[:], dlt[:], ge[:], ALU.mult, ['dlt', 'ge'], ['dlt'])
                TTo('dve', hi[:], mid[:], dlt[:], ALU.add, ['mid', 'dlt'], ['hi'])
            TTo('dve', mask[:], affv, lo[:, :].unsqueeze(1).to_broadcast([128, 32, 16]), ALU.is_ge, ['aff_all', 'lo'], ['mask'])
            m2 = mask[:].rearrange("p t e -> p (t e)")
            MM(PS[:, 512:1024], ustr[:], m2, ['ustr', 'mask'], ['P1'])
            MM(PS[:, 1024:1536], onesf[:], m2, ['onesf', 'mask'], ['P2'])
            CP('dve', tot[:].rearrange("p t e -> p (t e)"), PS[:, 1024:1536], ['P2'], ['tot'])
            for e_ in range(16):
                S.op('dve', lambda e, e_=e_: e.tensor_tensor_scan(out=cum[:, :, e_], data0=onesf[:, 0:32], data1=tot[:, :, e_], initial=0.0,
                                                                 op0=ALU.mult, op1=ALU.add), reads=['tot', 'onesf'], writes=['cum'])
            TTo('dve', cum[:], cum[:], tot[:], ALU.subtract, ['cum', 'tot'], ['cum'])
            TTo('dve', cum[:].rearrange("p t e -> p (t e)"), cum[:].rearrange("p t e -> p (t e)"), PS[:, 512:1024], ALU.add, ['cum', 'P1'], ['cum'])
            S.op('dve', lambda e: e.scalar_tensor_tensor(out=posm[:], in0=cum[:], scalar=1.0, in1=mask[:], op0=ALU.add, op1=ALU.mult),
                 reads=['cum', 'mask'], writes=['posm'])
            S.op('dve', lambda e: e.tensor_scalar(out=posm[:], in0=posm[:], scalar1=-1.0, scalar2=None, op0=ALU.add), reads=['posm'], writes=['posm'])
            TTo('dve', glo[:], affv, mask[:], ALU.mult, ['aff_all', 'mask'], ['glo'])
            CP('dve', gw[:, :, :, 0], glo[:], ['glo'], ['gw'])
            CP('dve', ghi32[:], gw[:, :, :, 0], ['gw'], ['ghi32'])
            TTo('dve', gw[:, :, :, 1], glo[:], ghi32[:], ALU.subtract, ['glo', 'ghi32'], ['gw'])
            if dbg:
                o1 = dout("dbg_posm", [128, 512])
                S.dma(lambda e: e.dma_start(out=o1[:, :], in_=posm[:].rearrange("p a b -> p (a b)")), reads=['posm'])
            S.barrier()
            S.flush()
        if stop_after <= 5:
            return nc, dbg_outs

        with contextlib.ExitStack() as ph:
            h2_all = sb(ph, "h2_all", [128, 32, D], BF16)
            for i in range(32):
                S.dma(lambda e, i=i: e.dma_start(out=h2_all[:, i, :], in_=h2D[i * 128:(i + 1) * 128, :]), reads=['h2D'], writes=['h2_all'])
            OH = sb(ph, "OH", [128, 32, 512], BF16)
            xinT = sb(ph, "xinT", [128, 8, 512], BF16); hidT = sb(ph, "hidT", [128, 8, 512], BF16)
            wgb = sb(ph, "wgb", [128, 8, D], BF16); wub = sb(ph, "wub", [128, 8, D], BF16); wdb = sb(ph, "wdb", [128, 8, D], BF16)
            wsg = [sb(ph, "wsg%d" % i, [128, D], F32) for i in range(6)]
            gcs = sb(ph, "gcs", [128, 4], F32); sgt = sb(ph, "sgt", [128, 512], F32)
            ywt = sb(ph, "ywt", [128, 4, D], BF16)
            wi = 0
            for ex_ in range(int(os.environ.get('K_NE', 16))):
                for i in range(32):
                    S.op('dve' if i % 2 else 'pool', lambda e, i=i, ex_=ex_: e.tensor_scalar(
                        out=OH[:, i, :], in0=iot[:, 0:512], scalar1=posm[:, i, ex_:ex_ + 1], scalar2=None, op0=ALU.is_equal),
                        reads=['iot', 'posm'], writes=['OH'])
                for j in range(8):
                    pb = P[j % 2]; pk = 'P%d' % (j % 2)
                    for i in range(32):
                        MM(pb, h2_all[:, i, j * 128:(j + 1) * 128], OH[:, i, :], ['h2_all', 'OH'], [pk], start=(i == 0), stop=(i == 31))
                    CP('act' if j % 2 else 'dve', xinT[:, j, :], pb, [pk], ['xinT'])
                for (wsrc, wdst, wkey) in ((wg_d, wgb, 'wgb'), (wu_d, wub, 'wub'), (wd_d, wdb, 'wdb')):
                    for j in range(8):
                        w = wsg[wi % 6]; wk = 'wsg%d' % (wi % 6)
                        S.dma(lambda e, w=w, wsrc=wsrc, ex_=ex_, j=j: e.dma_start(out=w[:], in_=wsrc[ex_, j * 128:(j + 1) * 128, :]), writes=[wk])
                        CP(('dve', 'pool', 'act')[wi % 3], wdst[:, j, :], w[:], [wk], [wkey])
                        wi += 1
                gcp = PS[:, 1024:1032].rearrange("p (c k) -> p c k", k=2)
                for ct in range(4):
                    for i in range(32):
                        MM(gcp[:, ct, :], OH[:, i, ct * 128:(ct + 1) * 128], gw[:, i, ex_, :], ['OH', 'gw'], ['P2'],
                           start=(i == 0 and ct == 0), stop=(i == 31 and ct == 3))
                TTo('dve', gcs[:], gcp[:, :, 0], gcp[:, :, 1], ALU.add, ['P2'], ['gcs']) if False else None
                CP('dve', sgt[:, 0:8], PS[:, 1024:1032], ['P2'], ['sgt'])
                TTo('dve', gcs[:], sgt[:, 0:8].rearrange("p (c k) -> p c k", k=2)[:, :, 0], sgt[:, 0:8].rearrange("p (c k) -> p c k", k=2)[:, :, 1],
                    ALU.add, ['sgt'], ['gcs'])
                for fc in range(8):
                    for j in range(8):
                        MM(P[4], wgb[:, j, fc * 128:(fc + 1) * 128], xinT[:, j, :], ['wgb', 'xinT'], ['P4'], start=(j == 0), stop=(j == 7))
                    for j in range(8):
                        MM(P[5], wub[:, j, fc * 128:(fc + 1) * 128], xinT[:, j, :], ['wub', 'xinT'], ['P5'], start=(j == 0), stop=(j == 7))
                    ACTF(sgt[:], P[4], AF.Silu, ['P4'], ['sgt'])
                    TTo('dve', hidT[:, fc, :], sgt[:], P[5], ALU.mult, ['sgt', 'P5'], ['hidT'])
                for ct in range(4):
                    for half in range(2):
                        pb = P[6 + half]; pk = 'P%d' % (6 + half)
                        for fc in range(8):
                            MM(pb, hidT[:, fc, ct * 128:(ct + 1) * 128], wdb[:, fc, half * 512:(half + 1) * 512], ['hidT', 'wdb'], [pk],
                               start=(fc == 0), stop=(fc == 7))
                        S.op('act', lambda e, ct=ct, half=half, pb=pb: e.activation(out=ywt[:, ct, half * 512:(half + 1) * 512], in_=pb,
                                                                                  func=AF.Copy, scale=gcs[:, ct:ct + 1]),
                             reads=[pk, 'gcs'], writes=['ywt'])
                S.dma(lambda e, ex_=ex_: e.dma_start(out=ywD[ex_].rearrange("(c p) n -> p c n", p=128), in_=ywt[:]), reads=['ywt'], writes=['ywD'])
            if dbg:
                o1 = dout("dbg_yw", [16 * 512, D], BF16)
                S.dma(lambda e: e.dma_start(out=o1[:, :], in_=ywD.rearrange("e c n -> (e c) n")), reads=['ywD'])
            S.barrier()
            S.flush()
        if stop_after <= 6:
            return nc, dbg_outs

        with contextlib.ExitStack() as ph:
            yw_all = sb(ph, "yw_all", [128, 16, 4, D], BF16)
            for ex_ in range(16):
                S.dma(lambda e, ex_=ex_: e.dma_start(out=yw_all[:, ex_, :, :], in_=ywD[ex_].rearrange("(c p) n -> p c n", p=128)),
                      reads=['ywD'], writes=['yw_all'])
            cst = {}
            for nm, src in (("gt2", modd[0:1, 5120:6144]), ("ln2g", ln2_d[0:1, :]), ("ln2b", ln2_d[1:2, :])):
                cst[nm] = sb(ph, nm, [128, D], F32)
                S.dma(lambda e, nm=nm, src=src: e.dma_start(out=cst[nm][:], in_=src.partition_broadcast(128)), reads=['modd'], writes=[nm])
            dg = sb(ph, "dg", [128, 4, 128], F32)
            OHTs = [sb(ph, "OHT%d" % q_, [128, 4, 2048], BF16) for q_ in range(2)]
            x1ls = [sb(ph, "x1l%d" % q_, [128, D], F32) for q_ in range(2)]
            tr2s = [sb(ph, "tr2%d" % q_, [128, D], F32) for q_ in range(2)]
            xos = [sb(ph, "xo%d" % q_, [128, D], F32) for q_ in range(2)]
            stats = sb(ph, "statsF", [128, 2, 6], F32); mv = sb(ph, "mvF", [128, 2], F32)
            rstd = sb(ph, "rstdF", [128, 1], F32); nb = sb(ph, "nbF", [128, 1], F32)
            lntF = (stats, mv, rstd, nb, 'F')
            nFT = int(os.environ.get('K_FT', 32))

            def f_front(i):
                q_ = i % 2
                OHT = OHTs[q_]; kOHT = 'OHT%d' % q_
                for eg in range(4):
                    for k_ in range(4):
                        ex_ = eg * 4 + k_
                        S.op('dve' if k_ % 2 else 'pool', lambda e, k_=k_, ex_=ex_, i=i: e.tensor_scalar(
                            out=dg[:, k_, :], in0=identf[:], scalar1=posm[:, i, ex_:ex_ + 1], scalar2=None, op0=ALU.mult),
                            reads=['identf', 'posm'], writes=['dg'])
                    MM(PS[:, eg * 512:(eg + 1) * 512], onesf[:], dg[:].rearrange("p a t -> p (a t)"), ['onesf', 'dg'], ['P%d' % eg])

            def f_cmp(i):
                q_ = i % 2
                OHT = OHTs[q_]; kOHT = 'OHT%d' % q_
                for ct in range(4):
                    S.op('dve', lambda e, ct=ct, OHT=OHT: e.tensor_scalar(out=OHT[:, ct, :], in0=PS[:, 0:2048], scalar1=iot[:, 512 + ct:513 + ct],
                                                                         scalar2=None, op0=ALU.is_equal),
                         reads=['P0', 'P1', 'P2', 'P3', 'iot'], writes=[kOHT])

            def f_back(i):
                t0 = i * 128
                q_ = i % 2
                OHT = OHTs[q_]; x1l = x1ls[q_]; tr2 = tr2s[q_]; xo = xos[q_]
                kOHT = 'OHT%d' % q_; kx1l = 'x1l%d' % q_; ktr2 = 'tr2%d' % q_; kxo = 'xo%d' % q_
                S.dma(lambda e, t0=t0, x1l=x1l: e.dma_start(out=x1l[:], in_=x1D[t0:t0 + 128, :]), reads=['x1D'], writes=[kx1l])
                for half in range(2):
                    pb = P[4 + half]; pk = 'P%d' % (4 + half)
                    n = 0
                    for ex_ in range(16):
                        for ct in range(4):
                            MM(pb, OHT[:, ct, ex_ * 128:(ex_ + 1) * 128], yw_all[:, ex_, ct, half * 512:(half + 1) * 512], [kOHT, 'yw_all'], [pk],
                               start=(n == 0), stop=(n == 63))
                            n += 1
                if i + 1 < nFT:
                    f_cmp(i + 1)
                TTo('dve', tr2[:], PS[:, 2048:3072], cst['gt2'][:], ALU.mult, ['P4', 'P5', 'gt2'], [ktr2])
                S.op('dve', lambda e, tr2=tr2, x1l=x1l: e.scalar_tensor_tensor(out=tr2[:], in0=x1l[:], scalar=ALPHA, in1=tr2[:], op0=ALU.mult, op1=ALU.add),
                     reads=[kx1l, ktr2], writes=[ktr2])
                ln_stats(lntF, tr2, ktr2, eps5, 'eps5')
                S.op('act', lambda e, xo=xo, tr2=tr2: e.activation(out=xo[:], in_=tr2[:], func=AF.Identity, bias=nb[:], scale=rstd[:]),
                     reads=[ktr2, 'Fnb', 'Frstd'], writes=[kxo])
                TTo('pool', xo[:], xo[:], cst['ln2g'][:], ALU.mult, [kxo, 'ln2g'], [kxo])
                TTo('dve', xo[:], xo[:], cst['ln2b'][:], ALU.add, [kxo, 'ln2b'], [kxo])
                S.dma(lambda e, t0=t0, xo=xo: e.dma_start(out=out_d[t0:t0 + 128, :], in_=xo[:]), reads=[kxo], writes=['out'])

            f_front(0)
            f_cmp(0)
            for i in range(nFT):
                if i + 1 < nFT:
                    f_front(i + 1)
                f_back(i)
            S.barrier()
            S.flush()
    return nc, dbg_outs


def host_inputs(inp, b):
    f = np.float32
    m = {}
    m["x"] = np.ascontiguousarray(inp["x"][b], dtype=f)
    m["ctx"] = np.ascontiguousarray(inp["ctx"][b], dtype=f)
    m["cc"] = np.ascontiguousarray(np.stack([inp["c"][b].reshape(8, 128).T, inp["c_ctx"].reshape(8, 128).T], -1).reshape(128, 16), dtype=f)
    m["w_ada"] = np.ascontiguousarray(inp["w_ada"][0], dtype=f)
    m["b_ada"] = np.ascontiguousarray(inp["b_ada"][0].reshape(1, -1), dtype=f)
    m["w_in"] = np.ascontiguousarray(inp["w_in"][0], dtype=f)
    m["qg"] = np.ascontiguousarray(np.tile(inp["q_gain"][0], 8).reshape(1, 512), dtype=f)
    m["kg"] = np.ascontiguousarray(np.tile(inp["k_gain"][0], 2).reshape(1, 128), dtype=f)
    t = np.arange(TL)
    pos = np.stack([t // 64, t % 64], -1).astype(np.float32)
    inv = (10000.0 ** (-np.arange(16, dtype=np.float32) / 16)).astype(np.float32)
    ang = pos[:, :, None] * inv[None, None, :]
    cs = np.cos(ang).astype(f); sn = np.sin(ang).astype(f)
    cos2 = np.stack([cs, cs], 2).reshape(TL, 64)
    sin2 = np.stack([-sn, sn], 2).reshape(TL, 64)
    m["cosT"] = np.ascontiguousarray(np.tile(cos2, (1, 8)), dtype=f)
    m["sinS"] = np.ascontiguousarray(np.tile(sin2, (1, 8)), dtype=f)
    m["ident"] = np.eye(128, dtype=f)
    def kh(v):
        return np.asarray(v, dtype=f).reshape(8, 64).T
    mu = inp["tshift_mu"][0]
    cols = [kh(mu[0:512]), kh(mu[512:1024]), kh(mu[1024:1536]), kh(inp["k_k"][0]), kh(inp["k_a"][0]), kh(inp["r_k"][0].reshape(-1)),
            kh(inp["decay_w0"][0, 0]), kh(inp["decay_w0"][0, 1]), kh(inp["iclr_a0"][0, 0]), kh(inp["iclr_a0"][0, 1])]
    m["rp"] = np.ascontiguousarray(np.stack(cols, -1).reshape(64, 80), dtype=f)
    lmu = np.zeros((128, 3), f)
    lmu[0:64, 0] = mu[1536:1600]; lmu[0:64, 1] = mu[1600:1664]; lmu[:, 2] = mu[1664:1792]
    m["lmu"] = lmu
    m["decup"] = np.ascontiguousarray(np.concatenate([inp["decay_up"][0, 0], inp["decay_up"][0, 1]], 1), dtype=f)
    m["iclup"] = np.ascontiguousarray(np.concatenate([inp["iclr_up"][0, 0], inp["iclr_up"][0, 1]], 1), dtype=f)
    m["gateup"] = np.ascontiguousarray(inp["gate_up"][0], dtype=f)
    hh = np.repeat(np.arange(8), 16); tt = np.tile(np.arange(16), 8)
    same = hh[:, None] == hh[None, :]
    msf = (same & (tt[:, None] < tt[None, :])).astype(f); mif = (same & (tt[:, None] <= tt[None, :])).astype(f)
    msb = msf.T.copy(); mib = mif.T.copy()
    m["mk4"] = np.ascontiguousarray(np.concatenate([msf, mif, msf, mif, msb, mib, msb, mib], 1), dtype=f)
    m["mn1"] = np.ascontiguousarray(np.concatenate([msb, msf], 1), dtype=f)
    m["bm"] = np.ascontiguousarray((hh[:, None] == np.repeat(np.arange(8), 64)[None, :]).astype(f))
    rst = np.ones((64, 1024), f); rst[:, ::16] = 0.0
    m["rst"] = rst
    m["w_out"] = np.ascontiguousarray(inp["w_out"][0], dtype=f)
    m["lnx"] = np.ascontiguousarray(np.stack([inp["lnx_g"][0], inp["lnx_b"][0]]), dtype=f)
    m["ln1"] = np.ascontiguousarray(np.stack([inp["ln1_g"][0], inp["ln1_b"][0]]), dtype=f)
    m["ln2"] = np.ascontiguousarray(np.stack([inp["ln2_g"][0], inp["ln2_b"][0]]), dtype=f)
    m["router_w"] = np.ascontiguousarray(inp["router_w"][0], dtype=f)
    m["exp_w_gate"] = np.ascontiguousarray(inp["exp_w_gate"][0], dtype=f)
    m["exp_w_up"] = np.ascontiguousarray(inp["exp_w_up"][0], dtype=f)
    m["exp_w_down"] = np.ascontiguousarray(inp["exp_w_down"][0], dtype=f)
    iot = np.zeros((128, 516), f)
    iot[:, 0:512] = np.arange(512, dtype=f)[None, :]
    iot[:, 512:516] = np.arange(128, dtype=f)[:, None] + 128.0 * np.arange(4, dtype=f)[None, :]
    m["iot"] = iot
    m["ustr"] = np.triu(np.ones((128, 128), f), 1)
    return m


_NC_CACHE = {}


def kernel(**inputs):
    inp = {k: np.asarray(v) for k, v in inputs.items()}
    if "full" not in _NC_CACHE:
        _NC_CACHE["full"] = build_nc()[0]
    nc = _NC_CACHE["full"]
    in_maps = [host_inputs(inp, c // 2) for c in range(8)]
    res = run_bass_kernel_spmd(nc, in_maps, core_ids=list(range(8)))
    out = np.stack([res.results[2 * b]["out"] for b in range(4)], 0).astype(np.float32)
    return out
```

```python
import contextlib
import os
import numpy as np
import concourse.bass as bass
import concourse.mybir as mybir
from concourse.bass_utils import run_bass_kernel_spmd

F32 = mybir.dt.float32
BF16 = mybir.dt.bfloat16
AF = mybir.ActivationFunctionType
ALU = mybir.AluOpType
AX = mybir.AxisListType

D = 1024
TL = 4096
TC = 256
TT = TL + TC
NT = TT // 128
ALPHA = 2.0 ** 0.25
DEC_C = -float(np.exp(-0.5))


class Sched:
    CE = ('pe', 'act', 'dve', 'pool')

    def __init__(self, nc, stack, ndma=32):
        self.nc = nc
        self.ops = {e: [] for e in ('pe', 'act', 'dve', 'pool', 'sp')}
        self.cnt = {e: 0 for e in self.CE}
        self.last_w = {}
        self.readers = {}
        self.waited = {e: {} for e in self.ops}
        self.ndma = ndma
        self.dma_val = [0] * ndma
        self.dma_i = 0
        names = list(self.CE) + ['d%d' % i for i in range(ndma)]
        self.sems = {n: stack.enter_context(nc.semaphore('s_' + n)) for n in names}

    def _deps(self, eng, reads, writes):
        deps = {}

        def add(tok):
            if tok is None:
                return
            s, v = tok
            if deps.get(s, 0) < v:
                deps[s] = v
        for r in reads:
            add(self.last_w.get(r))
        for w in writes:
            add(self.last_w.get(w))
            for t in self.readers.get(w, ()):
                add(t)
        waits = []
        for s, v in deps.items():
            if s == eng and (eng == 'pe' or os.environ.get('K_NOSELF')):
                continue
            if self.waited[eng].get(s, 0) >= v:
                continue
            self.waited[eng][s] = v
            waits.append((s, v))
        return waits

    def _commit(self, tok, reads, writes):
        for r in reads:
            self.readers.setdefault(r, []).append(tok)
        for w in writes:
            self.last_w[w] = tok
            self.readers[w] = []

    capture = None
    pend = None

    def drain(self, lst, n):
        for _ in range(min(n, len(lst))):
            kind, a = lst.pop(0)
            (self.op if kind == 'op' else self.dma)(*a)

    def op(self, eng, fn, reads=(), writes=()):
        if self.capture is not None:
            self.capture.append(('op', (eng, fn, tuple(reads), tuple(writes))))
            return
        waits = self._deps(eng, reads, writes)
        self.cnt[eng] += 1
        tok = (eng, self.cnt[eng])
        self.ops[eng].append((waits, fn, (eng, 1)))
        self._commit(tok, reads, writes)

    def dma(self, fn, reads=(), writes=(), q='sp'):
        if self.capture is not None:
            self.capture.append(('dma', (fn, tuple(reads), tuple(writes), q)))
            return
        slot = self.dma_i % self.ndma
        self.dma_i += 1
        s = 'd%d' % slot
        waits = self._deps(q, reads, writes)
        pv = self.dma_val[slot]
        if pv > 0 and self.waited[q].get(s, 0) < pv:
            self.waited[q][s] = pv
            waits.append((s, pv))
        self.dma_val[slot] = pv + 16
        tok = (s, pv + 16)
        self.ops[q].append((waits, fn, (s, 16)))
        self._commit(tok, reads, writes)

    def barrier(self):
        allw = [(e, c) for e, c in self.cnt.items() if c > 0]
        allw += [('d%d' % i, v) for i, v in enumerate(self.dma_val) if v > 0]
        for eng in self.ops:
            waits = []
            for s, v in allw:
                if self.waited[eng].get(s, 0) >= v:
                    continue
                self.waited[eng][s] = v
                waits.append((s, v))
            if waits:
                self.ops[eng].append((waits, None, None))
        self.last_w = {}
        self.readers = {}

    def flush(self):
        nc = self.nc
        sems = self.sems
        ops = self.ops
        self.ops = {e: [] for e in ops}
        if os.environ.get('K_STATS'):
            print("FLUSH", {e: (len(v), sum(len(w[0]) for w in v)) for e, v in ops.items()})
        with nc.Block() as block:
            def run(engname, engobj):
                for waits, fn, inc in ops[engname]:
                    for ws, wv in waits:
                        engobj.wait_ge(sems[ws], wv)
                    if fn is not None:
                        ins = fn(engobj)
                        ins.then_inc(sems[inc[0]], inc[1])

            @block.sync
            def _(e):
                run('sp', e)

            @block.tensor
            def _(e):
                run('pe', e)

            @block.scalar
            def _(e):
                run('act', e)

            @block.vector
            def _(e):
                run('dve', e)

            @block.gpsimd
            def _(e):
                run('pool', e)


def build_nc(stop_after=99, dbg=False):
    nc = bass.Bass("TRN2", target_bir_lowering=False)

    def din(name, shape, dt=F32):
        return nc.dram_tensor(name, list(shape), dt, kind="ExternalInput").ap()

    def dscr(name, shape, dt=F32):
        return nc.dram_tensor(name, list(shape), dt, kind="Internal").ap()

    x_d = din("x", [TL, D]); ctx_d = din("ctx", [TC, D])
    cc_d = din("cc", [128, 16])
    wada_d = din("w_ada", [D, 6 * D]); bada_d = din("b_ada", [1, 6 * D])
    win_d = din("w_in", [D, 2560])
    qg_d = din("qg", [1, 512]); kg_d = din("kg", [1, 128])
    cos_d = din("cosT", [TL, 512]); sin_d = din("sinS", [TL, 512])
    ident_d = din("ident", [128, 128])
    rp_d = din("rp", [64, 8 * 10])
    lmu_d = din("lmu", [128, 3])
    decup_d = din("decup", [64, 2 * 512]); iclup_d = din("iclup", [64, 2 * 512]); gateup_d = din("gateup", [128, 512])
    mk4_d = din("mk4", [128, 2 * 512]); mn1_d = din("mn1", [128, 2 * 128]); bm_d = din("bm", [128, 512])
    rst_d = din("rst", [64, 1024])
    wout_d = din("w_out", [D, D]); lnx_d = din("lnx", [2, 512])
    ln1_d = din("ln1", [2, D]); ln2_d = din("ln2", [2, D])
    rw_d = din("router_w", [D, 16])
    x1D = dscr("x1D", [TL, D]); h2D = dscr("h2D", [TL, D], BF16)
    attD = dscr("attD", [128, 4 * TL], BF16)
    wg_d = din("exp_w_gate", [16, D, D]); wu_d = din("exp_w_up", [16, D, D]); wd_d = din("exp_w_down", [16, D, D])
    iot_d = din("iot", [128, 512 + 4]); ustr_d = din("ustr", [128, 128])
    ywD = dscr("ywD", [16, 512, D], BF16)
    ydir = dscr("ydir", [2, TL, 512]); bonD = dscr("bonD", [TL, 512]); gD = dscr("gD", [TL, 512])
    out_d = nc.dram_tensor("out", [TL, D], F32, kind="ExternalOutput").ap()
    modd = dscr("modd", [2, 6 * D])
    rwT = dscr("rwT", [1792, TT])
    dbg_outs = {}

    def dout(name, shape, dt=F32):
        ap = nc.dram_tensor(name, list(shape), dt, kind="ExternalOutput").ap()
        dbg_outs[name] = ap
        return ap

    with contextlib.ExitStack() as top:
        S = Sched(nc, top)

        def sb(stack, name, shape, dt):
            return stack.enter_context(nc.sbuf_tensor("sb_" + name, list(shape), dt))

        PS = top.enter_context(nc.psum_tensor("PS", [128, 8 * 512], F32))
        P = [PS[:, i * 512:(i + 1) * 512] for i in range(8)]
        PK = ['P%d' % i for i in range(8)]
        identf = sb(top, "identf", [128, 128], F32)
        identb = sb(top, "identb", [128, 128], BF16)
        eps5 = sb(top, "eps5", [128, 1], F32)
        eps6 = sb(top, "eps6", [128, 1], F32)
        S.dma(lambda e: e.dma_start(out=identf[:], in_=ident_d[:, :]), writes=['identf'])
        S.op('dve', lambda e: e.tensor_copy(out=identb[:], in_=identf[:]), reads=['identf'], writes=['identb'])
        S.op('pool', lambda e: e.memset(eps5[:], 1e-5), writes=['eps5'])
        S.op('pool', lambda e: e.memset(eps6[:], 1e-6), writes=['eps6'])

        def ln_stats(stack_tiles, src, key_src, eps_t, eps_key):
            stats, mv, rstd, nb, kq = stack_tiles
            for cch in range(2):
                S.op('dve', lambda e, cch=cch: e.bn_stats(out=stats[:, cch, :], in_=src[:, cch * 512:(cch + 1) * 512]),
                     reads=[key_src], writes=[kq + 'stats'])
            S.op('dve', lambda e: e.bn_aggr(out=mv[:], in_=stats[:]), reads=[kq + 'stats'], writes=[kq + 'mv'])
            S.op('act', lambda e: e.activation(out=rstd[:], in_=mv[:, 1:2], func=AF.Ln, bias=eps_t[:], scale=1.0),
                 reads=[kq + 'mv', eps_key], writes=[kq + 'rstd'])
            S.op('act', lambda e: e.activation(out=rstd[:], in_=rstd[:], func=AF.Exp, scale=-0.5),
                 reads=[kq + 'rstd'], writes=[kq + 'rstd'])
            S.op('dve', lambda e: e.scalar_tensor_tensor(out=nb[:], in0=mv[:, 0:1], scalar=-1.0, in1=rstd[:],
                                                        op0=ALU.mult, op1=ALU.mult),
                 reads=[kq + 'mv', kq + 'rstd'], writes=[kq + 'nb'])

        with contextlib.ExitStack() as ph:
            cc = sb(ph, "cc", [128, 8, 2], F32)
            ccs = sb(ph, "ccs", [128, 8, 2], F32)
            wst = [sb(ph, "wst%d" % i, [128, 8, 512], F32) for i in range(2)]
            bada = sb(ph, "bada", [2, 6 * D], F32)
            modrow = sb(ph, "modrow", [2, 6 * D], F32)
            S.dma(lambda e: e.dma_start(out=cc[:].rearrange("p a b -> p (a b)"), in_=cc_d[:, :]), writes=['cc'])
            S.dma(lambda e: e.dma_start(out=bada[:], in_=bada_d.partition_broadcast(2)), writes=['bada'])
            S.op('act', lambda e: e.activation(out=ccs[:], in_=cc[:], func=AF.Silu), reads=['cc'], writes=['ccs'])
            for g in range(12):
                w = wst[g % 2]
                wk = 'wst%d' % (g % 2)
                S.dma(lambda e, w=w, g=g: e.dma_start(
                    out=w[:], in_=wada_d[:, g * 512:(g + 1) * 512].rearrange("(j p) n -> p j n", p=128)), writes=[wk])
                for j in range(8):
                    S.op('pe', lambda e, w=w, j=j: e.matmul(P[0][0:2, :], lhsT=ccs[:, j, :], rhs=w[:, j, :],
                                                           start=(j == 0), stop=(j == 7)),
                         reads=['ccs', wk], writes=['P0'])
                S.op('dve', lambda e, g=g: e.tensor_tensor(out=modrow[:, g * 512:(g + 1) * 512], in0=P[0][0:2, :],
                                                          in1=bada[:, g * 512:(g + 1) * 512], op=ALU.add),
                     reads=['P0', 'bada'], writes=['modrow'])
            for lo in (1024, 4096):
                S.op('dve', lambda e, lo=lo: e.tensor_scalar_add(out=modrow[:, lo:lo + 1024], in0=modrow[:, lo:lo + 1024],
                                                                scalar1=1.0), reads=['modrow'], writes=['modrow'])
            S.dma(lambda e: e.dma_start(out=modd[:, :], in_=modrow[:]), reads=['modrow'], writes=['modd'])
            if dbg:
                o = dout("dbg_mod", [2, 6 * D])
                S.dma(lambda e: e.dma_start(out=o[:, :], in_=modrow[:]), reads=['modrow'])
            S.barrier()
            S.flush()
        if stop_after <= 0:
            return nc, dbg_outs

        def modrow_bc(dst, row, lo):
            S.dma(lambda e: e.dma_start(out=dst[:], in_=modd[row:row + 1, lo:lo + 1024].partition_broadcast(128)),
                  reads=['modd'], writes=[dst.name if hasattr(dst, 'name') else 'x'])

        aff_all = sb(top, "aff_all", [128, 32, 16], F32)
        with contextlib.ExitStack() as phAB:
            qT_all = sb(phAB, "qT_all", [128, 4, TL], BF16)
            kTd = sb(phAB, "kTd", [128, 2, TT], BF16)
            Vx = sb(phAB, "Vx", [128, NT, 2, 80], BF16)
            attT_all = sb(phAB, "attT_all", [128, 4, TL], BF16)
            with contextlib.ExitStack() as ph:
                winb = sb(ph, "winb", [128, 8, 2560], BF16)
                wstg = [sb(ph, "wstg%d" % i, [128, 640], F32) for i in range(2)]
                sc1p = sb(ph, "sc1p", [128, D], F32); sh1 = sb(ph, "sh1", [128, D], F32)
                csc1p = sb(ph, "csc1p", [128, D], F32); csh1 = sb(ph, "csh1", [128, D], F32)
                qg = sb(ph, "qg", [128, 512], F32); kg = sb(ph, "kg", [128, 128], F32)
                for dst, nm, row, lo in ((sh1, 'sh1', 0, 0), (sc1p, 'sc1p', 0, 1024), (csh1, 'csh1', 1, 0), (csc1p, 'csc1p', 1, 1024)):
                    S.dma(lambda e, dst=dst, row=row, lo=lo: e.dma_start(
                        out=dst[:], in_=modd[row:row + 1, lo:lo + 1024].partition_broadcast(128)), reads=['modd'], writes=[nm])
                S.dma(lambda e: e.dma_start(out=qg[:], in_=qg_d.partition_broadcast(128)), writes=['qg'])
                S.dma(lambda e: e.dma_start(out=kg[:], in_=kg_d.partition_broadcast(128)), writes=['kg'])
                for jj in range(32):
                    j = jj // 4; c0 = (jj % 4) * 640
                    w = wstg[jj % 2]; wk = 'wstg%d' % (jj % 2)
                    S.dma(lambda e, w=w, j=j, c0=c0: e.dma_start(out=w[:], in_=win_d[j * 128:(j + 1) * 128, c0:c0 + 640]), writes=[wk])
                    S.op('pool' if jj % 2 else 'dve', lambda e, w=w, j=j, c0=c0: e.tensor_copy(out=winb[:, j, c0:c0 + 640], in_=w[:]),
                         reads=[wk], writes=['winb'])
                S.op('pool', lambda e: e.memset(Vx[:], 1.0), writes=['Vx'])
                xt = [sb(ph, "xt%d" % i, [128, D], F32) for i in range(2)]
                xn = sb(ph, "xn", [128, D], F32)
                hb = sb(ph, "hb", [128, D], BF16)
                hT = sb(ph, "hT", [128, 8, 512], BF16)
                stats = sb(ph, "stats", [128, 2, 6], F32); mv = sb(ph, "mv", [128, 2], F32)
                rstd = sb(ph, "rstd", [128, 1], F32); nb = sb(ph, "nb", [128, 1], F32)
                lnt = (stats, mv, rstd, nb, 'A')
                rstg = [sb(ph, "rstg%d" % i, [128, 512], F32) for i in range(2)]
                cosT = sb(ph, "cosT", [128, 512], F32); sinS = sb(ph, "sinS", [128, 512], F32)
                sq = sb(ph, "sq", [128, 512], F32)
                ssum = sb(ph, "ssum", [128, 8], F32)
                qn = sb(ph, "qn", [128, 512], F32); qa = sb(ph, "qa", [128, 512], F32)
                qbt = sb(ph, "qbt", [128, 512], F32)
                qo = sb(ph, "qo", [128, 512], BF16)
                ko = sb(ph, "ko", [128, 2, 2, 64], BF16)
                blocks = [(0, 256)] + [(256 + 512 * i, 512) for i in range(8)]
                ti = 0
                for (u0, ntok) in blocks:
                    ntile = ntok // 128
                    is_ctx = (u0 == 0)
                    for tl in range(ntile):
                        u = u0 + tl * 128
                        gt = u // 128
                        X = xt[ti % 2]; xk = 'xt%d' % (ti % 2)
                        ti += 1
                        src = ctx_d[u:u + 128, :] if is_ctx else x_d[u - TC:u - TC + 128, :]
                        S.dma(lambda e, X=X, src=src: e.dma_start(out=X[:], in_=src), writes=[xk])
                        ln_stats(lnt, X, xk, eps5, 'eps5')
                        S.op('act', lambda e, X=X: e.activation(out=xn[:], in_=X[:], func=AF.Identity, bias=nb[:], scale=rstd[:]),
                             reads=[xk, 'Anb', 'Arstd'], writes=['xn'])
                        scp, shh, k1, k2 = (csc1p, csh1, 'csc1p', 'csh1') if is_ctx else (sc1p, sh1, 'sc1p', 'sh1')
                        S.op('pool', lambda e, scp=scp: e.tensor_tensor(out=xn[:], in0=xn[:], in1=scp[:], op=ALU.mult),
                             reads=['xn', k1], writes=['xn'])
                        S.op('dve', lambda e, shh=shh: e.tensor_tensor(out=hb[:], in0=xn[:], in1=shh[:], op=ALU.add),
                             reads=['xn', k2], writes=['hb'])
                        pTb = P[0][:].bitcast(BF16)
                        for j in range(8):
                            S.op('pe', lambda e, j=j, pTb=pTb: e.transpose(out=pTb[:, j * 128:(j + 1) * 128],
                                                                      in_=hb[:, j * 128:(j + 1) * 128], identity=identb[:]),
                                 reads=['hb', 'identb'], writes=['P0'])
                        S.op('act', lambda e, tl=tl, pTb=pTb: e.copy(out=hT[:, :, tl * 128:(tl + 1) * 128],
                                                                 in_=pTb.rearrange("p (j t) -> p j t", j=8)),
                             reads=['P0'], writes=['hT'])
                        for j in range(8):
                            S.op('pe', lambda e, j=j, tl=tl: e.matmul(P[1][:, :], lhsT=hT[:, j, tl * 128:(tl + 1) * 128],
                                                                     rhs=winb[:, j, 0:512], start=(j == 0), stop=(j == 7)),
                                 reads=['hT', 'winb'], writes=['P1'])
                        for j in range(8):
                            S.op('pe', lambda e, j=j, tl=tl: e.matmul(P[2][:, 0:256], lhsT=hT[:, j, tl * 128:(tl + 1) * 128],
                                                                     rhs=winb[:, j, 512:768], start=(j == 0), stop=(j == 7)),
                                 reads=['hT', 'winb'], writes=['P2'])
                        S.op('act', lambda e, gt=gt: e.copy(out=Vx[:, gt, :, 0:64],
                                                          in_=P[2][:, 128:256].rearrange("p (a b) -> p a b", a=2)),
                             reads=['P2'], writes=['Vx'])
                        if not is_ctx:
                            ul = u - TC
                            S.dma(lambda e, ul=ul: e.dma_start(out=cosT[:], in_=cos_d[ul:ul + 128, :]), writes=['cosT'])
                            S.dma(lambda e, ul=ul: e.dma_start(out=sinS[:], in_=sin_d[ul:ul + 128, :]), writes=['sinS'])

                        def normrope(psrc, pk, nh, gain, gk, do_rope, outfn):
                            w_ = nh * 64
                            S.op('act', lambda e: e.activation(out=sq[:, 0:w_], in_=psrc, func=AF.Square),
                                 reads=[pk], writes=['sq'])
                            S.op('dve', lambda e: e.tensor_reduce(out=ssum[:, 0:nh], in_=sq[:, 0:w_].rearrange("p (h d) -> p h d", d=64),
                                                                 axis=AX.X, op=ALU.add), reads=['sq'], writes=['ssum'])
                            S.op('act', lambda e: e.activation(out=ssum[:, 0:nh], in_=ssum[:, 0:nh], func=AF.Ln, bias=eps6[:], scale=1.0 / 64),
                                 reads=['ssum', 'eps6'], writes=['ssum'])
                            S.op('act', lambda e: e.activation(out=ssum[:, 0:nh], in_=ssum[:, 0:nh], func=AF.Exp, scale=-0.5),
                                 reads=['ssum'], writes=['ssum'])
                            S.op('dve', lambda e: e.tensor_tensor(out=qn[:, 0:w_].rearrange("p (h d) -> p h d", d=64),
                                                                 in0=psrc.rearrange("p (h d) -> p h d", d=64),
                                                                 in1=ssum[:, 0:nh].unsqueeze(2).to_broadcast([128, nh, 64]), op=ALU.mult),
                                 reads=[pk, 'ssum'], writes=['qn'])
                            if not do_rope:
                                S.op('pool', lambda e: outfn(e, qn[:, 0:w_], gain[:, 0:w_], ALU.mult), reads=['qn', gk], writes=['qko'])
                                return
                            S.op('pool', lambda e: e.tensor_tensor(out=qn[:, 0:w_], in0=qn[:, 0:w_], in1=gain[:, 0:w_], op=ALU.mult),
                                 reads=['qn', gk], writes=['qn'])
                            S.op('dve', lambda e: e.tensor_tensor(out=qa[:, 0:w_], in0=qn[:, 0:w_], in1=cosT[:, 0:w_], op=ALU.mult),
                                 reads=['qn', 'cosT'], writes=['qa'])
                            qv = qn[:, 0:w_].rearrange("p (g s q) -> p g s q", s=2, q=16)
                            bv = qbt[:, 0:w_].rearrange("p (g s q) -> p g s q", s=2, q=16)
                            sv = sinS[:, 0:w_].rearrange("p (g s q) -> p g s q", s=2, q=16)
                            for s_ in range(2):
                                S.op('pool', lambda e, s_=s_: e.tensor_tensor(out=bv[:, :, s_, :], in0=qv[:, :, 1 - s_, :],
                                                                             in1=sv[:, :, s_, :], op=ALU.mult),
                                     reads=['qn', 'sinS'], writes=['qbt'])
                            S.op('dve', lambda e: outfn(e, qa[:, 0:w_], qbt[:, 0:w_], ALU.add), reads=['qa', 'qbt'], writes=['qko'])

                        if not is_ctx:
                            normrope(P[1][:, :], 'P1', 8, qg, 'qg', True,
                                     lambda e, a, b_, op: e.tensor_tensor(out=qo[:], in0=a, in1=b_, op=op))
                            pq = P[3][:].bitcast(BF16)
                            for hp in range(4):
                                S.op('pe', lambda e, hp=hp, pq=pq: e.transpose(out=pq[:, hp * 128:(hp + 1) * 128],
                                                                          in_=qo[:, hp * 128:(hp + 1) * 128], identity=identb[:]),
                                     reads=['qko', 'identb'], writes=['P3'])
                            S.op('act', lambda e, ul=ul, pq=pq: e.copy(out=qT_all[:, :, ul:ul + 128],
                                                                   in_=pq[:, 0:512].rearrange("p (j t) -> p j t", j=4)),
                                 reads=['P3'], writes=['qT_all'])

                        def kout(e, a, b_, op):
                            return e.tensor_tensor(out=ko[:, :, 0, :], in0=a.rearrange("p (h d) -> p h d", d=64),
                                                   in1=b_.rearrange("p (h d) -> p h d", d=64), op=op)
                        normrope(P[2][:, 0:128], 'P2', 2, kg, 'kg', not is_ctx, kout)
                        S.op('pool', lambda e: e.tensor_copy(out=ko[:, :, 1, :], in_=ko[:, :, 0, :]), reads=['qko'], writes=['qko'])
                        pk_ = P[3][:].bitcast(BF16)
                        for kv in range(2):
                            S.op('pe', lambda e, kv=kv, pk_=pk_: e.transpose(
                                out=pk_[:, 512 + kv * 128:512 + (kv + 1) * 128],
                                in_=ko[:, kv, :, :].rearrange("p a d -> p (a d)"), identity=identb[:]),
                                reads=['qko', 'identb'], writes=['P3'])
                        S.op('act', lambda e, u=u, pk_=pk_: e.copy(out=kTd[:, :, u:u + 128],
                                                               in_=pk_[:, 512:768].rearrange("p (j t) -> p j t", j=2)),
                             reads=['P3'], writes=['kTd'])
                    for g in range(14):
                        pb = P[4 + g % 2]; pbk = 'P%d' % (4 + g % 2)
                        for j in range(8):
                            S.op('pe', lambda e, j=j, g=g, pb=pb, ntok=ntok: e.matmul(pb[:, 0:ntok], lhsT=winb[:, j, 768 + g * 128:768 + (g + 1) * 128],
                                                                         rhs=hT[:, j, 0:ntok], start=(j == 0), stop=(j == 7)),
                                 reads=['hT', 'winb'], writes=[pbk])
                        rs = rstg[g % 2]; rk = 'rstg%d' % (g % 2)
                        S.op('act' if g % 2 else 'dve',
                             (lambda e, rs=rs, pb=pb, ntok=ntok: e.copy(out=rs[:, 0:ntok], in_=pb[:, 0:ntok])) if g % 2 else
                             (lambda e, rs=rs, pb=pb, ntok=ntok: e.tensor_copy(out=rs[:, 0:ntok], in_=pb[:, 0:ntok])),
                             reads=[pbk], writes=[rk])
                        S.dma(lambda e, rs=rs, g=g, u0=u0, ntok=ntok: e.dma_start(out=rwT[g * 128:(g + 1) * 128, u0:u0 + ntok], in_=rs[:, 0:ntok]),
                              reads=[rk], writes=['rwT'])
                if dbg:
                    o1 = dout("dbg_qT", [128, 4 * TL], BF16)
                    S.dma(lambda e: e.dma_start(out=o1[:, :], in_=qT_all[:].rearrange("p a t -> p (a t)")), reads=['qT_all'])
                    o2 = dout("dbg_kTd", [128, 2 * TT], BF16)
                    S.dma(lambda e: e.dma_start(out=o2[:, :], in_=kTd[:].rearrange("p a t -> p (a t)")), reads=['kTd'])
                    o3 = dout("dbg_rwT", [1792, TT])
                    S.dma(lambda e: e.dma_start(out=o3[:, :], in_=rwT[:, :]), reads=['rwT'])
                S.barrier()
                S.flush()
            if stop_after <= 1:
                return nc, dbg_outs
            with contextlib.ExitStack() as ph:
                pT = [sb(ph, "pT%d" % i, [128, 512], BF16) for i in range(3)]
                rec = sb(ph, "rec", [128, 8], F32)
                atok = sb(ph, "atok", [128, 8, 64], BF16)
                pi = 0

                def emit_scores(q0, st, hp, pi):
                    sb0 = 2 * (hp % 2)
                    scb = PS[:, sb0 * 512:(sb0 + 2) * 512].rearrange("p (b n) -> p b n", b=2)
                    sck = 'P%d' % sb0
                    kvh = hp // 2
                    for hh in range(2):
                        S.op('pe', lambda e, hh=hh, hp=hp, scb=scb, kvh=kvh, st=st, q0=q0: e.matmul(
                            scb[:, hh, 0:256],
                            lhsT=kTd[hh * 64:(hh + 1) * 64, kvh, st * 128:(st + 1) * 128],
                            rhs=qT_all[hh * 64:(hh + 1) * 64, hp, q0:q0 + 256], start=True, stop=True),
                            reads=['kTd', 'qT_all'], writes=[sck])
                    pt = pT[pi % 3]; ptk = 'pT%d' % (pi % 3)
                    S.op('act', lambda e, pt=pt, scb=scb: e.activation(out=pt[:].rearrange('p (b n) -> p b n', b=2), in_=scb[:, :, 0:256], func=AF.Exp, scale=0.125),
                         reads=[sck], writes=[ptk])
                    return (st, hp, pt, ptk, kvh)

                def emit_pv(st, hp, pt, ptk, kvh):
                    for hh in range(2):
                        head = 2 * hp + hh
                        for qt in range(2):
                            ab = P[4 + 2 * qt + head // 4]; abk = 'P%d' % (4 + 2 * qt + head // 4)
                            c0 = (head % 4) * 65
                            S.op('pe', lambda e, ab=ab, c0=c0, pt=pt, hh=hh, qt=qt, st=st, kvh=kvh, head=head: e.matmul(
                                ab[:, c0:c0 + 65], lhsT=pt[:, hh * 256 + qt * 128:hh * 256 + (qt + 1) * 128],
                                rhs=Vx[:, st, kvh, 0:65], start=(st == 0 and head % 4 == 0), stop=(st == NT - 1 and head % 4 == 3)),
                                reads=[ptk, 'Vx'], writes=[abk])

                for g in range(int(os.environ.get('K_NG', 16))):
                    q0 = g * 256
                    pend_ = None
                    for st in range(NT):
                        for hp in range(4):
                            cur_ = emit_scores(q0, st, hp, pi)
                            pi += 1
                            if pend_ is not None:
                                emit_pv(*pend_)
                            pend_ = cur_
                    emit_pv(*pend_)
                    for qt in range(2 if not os.environ.get('K_SKIP_NORM') else 0):
                        for half in range(2):
                            ab = P[4 + 2 * qt + half]; abk = 'P%d' % (4 + 2 * qt + half)
                            av = ab[:, 0:260].rearrange("p (h c) -> p h c", c=65)
                            S.op('dve', lambda e, av=av, half=half: e.reciprocal(out=rec[:, half * 4:(half + 1) * 4], in_=av[:, :, 64]),
                                 reads=[abk], writes=['rec'])
                            S.op('dve', lambda e, av=av, half=half: e.tensor_tensor(
                                out=atok[:, half * 4:(half + 1) * 4, :], in0=av[:, :, 0:64],
                                in1=rec[:, half * 4:(half + 1) * 4].unsqueeze(2).to_broadcast([128, 4, 64]), op=ALU.mult),
                                reads=[abk, 'rec'], writes=['atok'])
                        pa = P[0][:].bitcast(BF16)
                        if os.environ.get('K_SKIP_TR'):
                            continue
                        for hp in range(4):
                            S.op('pe', lambda e, hp=hp, pa=pa: e.transpose(
                                out=pa[:, hp * 128:(hp + 1) * 128],
                                in_=atok[:, 2 * hp:2 * hp + 2, :].rearrange("p a d -> p (a d)"), identity=identb[:]),
                                reads=['atok', 'identb'], writes=['P0'])
                        if os.environ.get('K_SKIP_CP'):
                            continue
                        S.op('dve', lambda e, pa=pa, q0=q0, qt=qt: e.tensor_copy(
                            out=attT_all[:, :, q0 + qt * 128:q0 + (qt + 1) * 128],
                            in_=pa[:, 0:512].rearrange("p (j t) -> p j t", j=4)), reads=['P0'], writes=['attT_all'])
                S.dma(lambda e: e.dma_start(out=attD[:, :], in_=attT_all[:].rearrange("p a t -> p (a t)")), reads=['attT_all'], writes=['attD'])
                if dbg:
                    o1 = dout("dbg_attT", [128, 4 * TL], BF16)
                    S.dma(lambda e: e.dma_start(out=o1[:, :], in_=attT_all[:].rearrange("p a t -> p (a t)")), reads=['attT_all'])
                S.barrier()
                S.flush()
        if stop_after <= 2:
            return nc, dbg_outs

        def TTo(eng, out, a, b_, op, R, W):
            S.op(eng, lambda e: e.tensor_tensor(out=out, in0=a, in1=b_, op=op), reads=R, writes=W)

        def CP(eng, out, in_, R, W):
            if eng == 'act':
                S.op('act', lambda e: e.copy(out=out, in_=in_), reads=R, writes=W)
            else:
                S.op(eng, lambda e: e.tensor_copy(out=out, in_=in_), reads=R, writes=W)

        def ACTF(out, in_, func, R, W, scale=1.0, bias=None):
            if bias is None:
                S.op('act', lambda e: e.activation(out=out, in_=in_, func=func, scale=scale), reads=R, writes=W)
            else:
                S.op('act', lambda e: e.activation(out=out, in_=in_, func=func, scale=scale, bias=bias), reads=R, writes=W)

        def MM(out, lhsT, rhs, R, W, start=True, stop=True):
            S.op('pe', lambda e: e.matmul(out, lhsT=lhsT, rhs=rhs, start=start, stop=stop), reads=R, writes=W)

        def TR(out, in_, idn, R, W):
            S.op('pe', lambda e: e.transpose(out=out, in_=in_, identity=idn), reads=R, writes=W)

        with contextlib.ExitStack() as ph:
            def t32(name, shape=(64, 1024)):
                return sb(ph, name, list(shape), F32)
            rp = t32("rp", (64, 8, 10)); rpd = t32("rpd", (64, 8, 8))
            lmu = t32("lmu", (128, 3)); lmd = t32("lmd", (128, 6))
            decupb = sb(ph, "decupb", [64, 2, 512], BF16); iclupb = sb(ph, "iclupb", [64, 2, 512], BF16)
            gateupb = sb(ph, "gateupb", [128, 512], BF16)
            mk4 = t32("mk4", (128, 2, 512)); mn1 = t32("mn1", (128, 2, 128)); bm = t32("bm", (128, 512))
            rst = t32("rst"); ones64 = sb(ph, "ones64", [64, 64], BF16); tiny = t32("tiny", (64, 1))
            S.dma(lambda e: e.dma_start(out=rp[:].rearrange("k h n -> k (h n)"), in_=rp_d[:, :]), writes=['rp'])
            S.dma(lambda e: e.dma_start(out=lmu[:], in_=lmu_d[:, :]), writes=['lmu'])
            S.dma(lambda e: e.dma_start(out=mk4[:].rearrange("p a n -> p (a n)"), in_=mk4_d[:, :]), writes=['mk4'])
            S.dma(lambda e: e.dma_start(out=mn1[:].rearrange("p a n -> p (a n)"), in_=mn1_d[:, :]), writes=['mn1'])
            S.dma(lambda e: e.dma_start(out=bm[:], in_=bm_d[:, :]), writes=['bm'])
            S.dma(lambda e: e.dma_start(out=rst[:], in_=rst_d[:, :]), writes=['rst'])
            S.op('pool', lambda e: e.memset(ones64[:], 1.0), writes=['ones64'])
            S.op('pool', lambda e: e.memset(tiny[:], 1e-24), writes=['tiny'])
            for i in range(3):
                S.op('dve', lambda e, i=i: e.tensor_scalar(out=rpd[:, :, 2 * i], in0=rp[:, :, i], scalar1=0.5, scalar2=None, op0=ALU.mult),
                     reads=['rp'], writes=['rpd'])
                S.op('dve', lambda e, i=i: e.tensor_scalar(out=rpd[:, :, 2 * i + 1], in0=rp[:, :, i], scalar1=-1.0, scalar2=1.0,
                                                          op0=ALU.mult, op1=ALU.add), reads=['rp'], writes=['rpd'])
                S.op('dve', lambda e, i=i: e.tensor_scalar(out=lmd[:, 2 * i:2 * i + 1], in0=lmu[:, i:i + 1], scalar1=0.5, scalar2=None, op0=ALU.mult),
                     reads=['lmu'], writes=['lmd'])
                S.op('dve', lambda e, i=i: e.tensor_scalar(out=lmd[:, 2 * i + 1:2 * i + 2], in0=lmu[:, i:i + 1], scalar1=-1.0, scalar2=1.0,
                                                          op0=ALU.mult, op1=ALU.add), reads=['lmu'], writes=['lmd'])
            S.op('dve', lambda e: e.tensor_scalar(out=rpd[:, :, 6], in0=rp[:, :, 4], scalar1=-1.0, scalar2=1.0, op0=ALU.mult, op1=ALU.add),
                 reads=['rp'], writes=['rpd'])

            def bc(t2):
                return t2.unsqueeze(2).to_broadcast([64, 8, 128])

            pin = [sb(ph, "pin%d" % i, [64, 8, 130], F32) for i in range(3)]
            plo = [sb(ph, "plo%d" % i, [128, 130], F32) for i in range(3)]
            tS = t32("tS"); xr = t32("xr"); xk = t32("xk"); xv = t32("xv")
            xlo = t32("xlo", (128, 3, 128)); twl = sb(ph, "twl", [64, 128], BF16); xalb = sb(ph, "xalb", [64, 128], BF16)
            glsb = sb(ph, "glsb", [128, 128], BF16)
            sqb = sb(ph, "sqb", [64, 1024], BF16); kk = t32("kk")
            sg = t32("sg"); ad = t32("ad"); cs = t32("cs"); ex = t32("ex"); E1 = t32("E1"); E2s = [t32("E2_0"), t32("E2_1")]; E3 = t32("E3")
            bb = t32("bb"); t1 = t32("t1"); kd = bb; bt32 = t32("bt32"); kt32 = t32("kt32"); rkb = sqb; kkk = t1; rs = E1; csb = bt32; bv = kt32
            bvt = t32("bvt", (128, 512)); gtok = t32("gtok", (128, 512)); glS = t32("glS", (128, 128))
            opTs = [{n: sb(ph, "op%d_" % q_ + n, [64, 1024], BF16) for n in ("a", "r", "b", "k", "bh", "kh", "v")} for q_ in range(2)]
            N1Ta, N1a, IN1Ta, N2a, N2Ta, IN2Ta, N4a, N4Ta, IN4Ta, IN8Ta, AakTa, ArbTa, ArkTa = [
                sb(ph, "ba%d" % i, [128, 8, 128], BF16) for i in range(13)]
            TMa = sb(ph, "TMa", [128, 8, 5, 64], BF16)
            Xa = [sb(ph, "Xa%d" % i, [128, 8, 128], BF16) for i in range(2)]
            Bbd = sb(ph, "Bbd", [128, 512], BF16); Ubd = sb(ph, "Ubd", [128, 512], BF16); Vbd = sb(ph, "Vbd", [128, 512], BF16)
            GTs = sb(ph, "GTs", [64, 512], BF16); Es = t32("Es", (64, 512)); QTs = sb(ph, "QTs", [64, 128], BF16)
            Y0s = t32("Y0s", (128, 64)); ytmp = gtok; yc = t32("yc", (128, 64))
            Yblk = t32("Yblk", (128, 8, 64))
            H = t32("H", (64, 512)); Hb = sb(ph, "Hb", [64, 512], BF16); Ht = t32("Ht", (64, 512))

            def view_hct(t):
                return t[:, :]

            def view_out(t):
                return t[:, :]

            def g16(t):
                return t[:, :].rearrange("k (g t) -> k g t", t=16)

            def chv(t, c):
                return t[:, :].rearrange("k (h c t) -> k h c t", h=8, c=8)[:, :, c, :]

            for (dst, src, nm, rows) in ((decupb, decup_d, 'decupb', 64), (iclupb, iclup_d, 'iclupb', 64), (gateupb, gateup_d, 'gateupb', 128)):
                stg_, sk_ = (bt32, 'bt32') if rows == 64 else (bvt, 'bvt')
                S.dma(lambda e, src=src, stg_=stg_: e.dma_start(out=stg_[:, :], in_=src[:, :]), writes=[sk_])
                dv = dst[:].rearrange("p a n -> p (a n)") if rows == 64 else dst[:]
                CP('dve', dv, stg_[:, :], [sk_], [nm])
            blocks = []
            nblk = int(os.environ.get('K_RBLK', 99))
            for d_ in range(2):
                cb_ = [0, 1] if d_ == 0 else [1, 0]
                lb_ = list(range(2, NT)) if d_ == 0 else list(range(NT - 1, 1, -1))
                blocks += [(d_, b_, j_ == 0) for j_, b_ in enumerate((cb_ + lb_)[:nblk])]

            def emit_prep(idx):
                d, blk, first = blocks[idx]
                opT = opTs[idx % 2]; E2 = E2s[idx % 2]; e2k = 'E2_%d' % (idx % 2); opk = 'o%d_' % (idx % 2)

                u0 = blk * 128
                is_ctx = blk < 2
                seq_lo, seq_hi = (0, TC) if is_ctx else (TC, TT)
                lo = max(u0 - 1, seq_lo); hi = min(u0 + 129, seq_hi)
                c_lo = lo - (u0 - 1); c_hi = c_lo + (hi - lo)
                for i in range(3):
                    if c_lo > 0:
                        S.op('pool', lambda e, i=i: e.memset(pin[i][:, :, 0:1], 0.0), writes=['pin%d' % i])
                    if c_hi < 130:
                        S.op('pool', lambda e, i=i: e.memset(pin[i][:, :, 129:130], 0.0), writes=['pin%d' % i])
                    S.dma(lambda e, i=i, lo=lo, hi=hi, c_lo=c_lo, c_hi=c_hi: e.dma_start(
                        out=pin[i][:, :, c_lo:c_hi],
                        in_=rwT[i * 512:(i + 1) * 512, lo:hi].rearrange("(h k) t -> k h t", k=64)), reads=['rwT'], writes=['pin%d' % i])
                for i, (r0, nr) in enumerate(((1536, 64), (1600, 64), (1664, 128))):
                    if c_lo > 0:
                        S.op('pool', lambda e, i=i: e.memset(plo[i][:, 0:1], 0.0), writes=['plo%d' % i])
                    if c_hi < 130:
                        S.op('pool', lambda e, i=i: e.memset(plo[i][:, 129:130], 0.0), writes=['plo%d' % i])
                    S.dma(lambda e, i=i, r0=r0, nr=nr, lo=lo, hi=hi, c_lo=c_lo, c_hi=c_hi: e.dma_start(
                        out=plo[i][0:nr, c_lo:c_hi], in_=rwT[r0:r0 + nr, lo:hi]), reads=['rwT'], writes=['plo%d' % i])
                for i, xo in enumerate((xr, xk, xv)):
                    xo3 = xo[:, :].rearrange("k (h t) -> k h t", h=8)
                    ts3 = tS[:, :].rearrange("k (h t) -> k h t", h=8)
                    TTo('pool', ts3, pin[i][:, :, 0:128], pin[i][:, :, 2:130], ALU.add, ['pin%d' % i], ['tS'])
                    TTo('pool', ts3, ts3, bc(rpd[:, :, 2 * i]), ALU.mult, ['tS', 'rpd'], ['tS'])
                    TTo('dve', xo3, pin[i][:, :, 1:129], bc(rpd[:, :, 2 * i + 1]), ALU.mult, ['pin%d' % i, 'rpd'], ['x%d' % i])
                    TTo('dve', xo3, xo3, ts3, ALU.add, ['x%d' % i, 'tS'], ['x%d' % i])
                for i, nr in enumerate((64, 64, 128)):
                    S.op('pool', lambda e, i=i, nr=nr: e.tensor_tensor(out=tS[0:nr, 0:128] if nr == 64 else glS[:, 0:128],
                                                                     in0=plo[i][0:nr, 0:128], in1=plo[i][0:nr, 2:130], op=ALU.add),
                         reads=['plo%d' % i], writes=['tS' if nr == 64 else 'glS'])
                    S.op('dve', lambda e, i=i, nr=nr: e.tensor_scalar(out=xlo[0:nr, i, :], in0=plo[i][0:nr, 1:129],
                                                                    scalar1=lmd[0:nr, 2 * i + 1:2 * i + 2], scalar2=None, op0=ALU.mult),
                         reads=['plo%d' % i, 'lmd'], writes=['xlo'])
                    S.op('dve', lambda e, i=i, nr=nr: e.scalar_tensor_tensor(
                        out=xlo[0:nr, i, :], in0=(tS[0:nr, 0:128] if nr == 64 else glS[:, 0:128]), scalar=lmd[0:nr, 2 * i:2 * i + 1],
                        in1=xlo[0:nr, i, :], op0=ALU.mult, op1=ALU.add),
                        reads=['tS' if nr == 64 else 'glS', 'lmd', 'xlo'], writes=['xlo'])
                ACTF(twl[:], xlo[0:64, 0, :], AF.Tanh, ['xlo'], ['twl'])
                CP('pool', xalb[:], xlo[0:64, 1, :], ['xlo'], ['xalb'])
                k3 = lambda t: t[:, :].rearrange("k (h t) -> k h t", h=8)
                TTo('pool', k3(kkk), k3(xk), bc(rp[:, :, 3]), ALU.mult, ['x1', 'rp'], ['t1'])
                ACTF(sqb[:], kkk[:], AF.Square, ['t1'], ['sqb'])
                PP = PS[0:64, 0:1024]
                for hf in range(2):
                    MM(PS[0:64, hf * 512:(hf + 1) * 512], ones64[:], sqb[:, hf * 512:(hf + 1) * 512], ['ones64', 'sqb'], ['P0'])
                ACTF(rs[:], PP, AF.Ln, ['P0', 'tiny'], ['E1'], bias=tiny[:])
                ACTF(rs[:], rs[:], AF.Exp, ['E1'], ['E1'], scale=-0.5)
                TTo('dve', kk[:], kkk[:], rs[:], ALU.mult, ['t1', 'E1'], ['kk'])
                for h in range(8):
                    MM(PS[0:64, h * 128:(h + 1) * 128], decupb[:, d, h * 64:(h + 1) * 64], twl[:], ['decupb', 'twl'], ['P0'])
                TTo('dve', k3(sg), PP.rearrange("k (h t) -> k h t", h=8), bc(rp[:, :, 6 + d]), ALU.add, ['P0', 'rp'], ['sg'])
                ACTF(sg[:], sg[:], AF.Sigmoid, ['sg'], ['sg'])
                for h in range(8):
                    MM(PS[0:64, h * 128:(h + 1) * 128], iclupb[:, d, h * 64:(h + 1) * 64], xalb[:], ['iclupb', 'xalb'], ['P0'])
                TTo('dve', k3(ad), PP.rearrange("k (h t) -> k h t", h=8), bc(rp[:, :, 8 + d]), ALU.add, ['P0', 'rp'], ['ad'])
                ACTF(ad[:], ad[:], AF.Sigmoid, ['ad'], ['ad'])
                S.op('dve', lambda e: e.tensor_tensor_scan(out=cs[:], data0=rst[:], data1=sg[:], initial=0.0, op0=ALU.mult, op1=ALU.add),
                     reads=['rst', 'sg'], writes=['cs'])
                csf = cs
                if d == 1:
                    TTo('pool', ex[:], sg[:], cs[:], ALU.subtract, ['sg', 'cs'], ['ex'])
                    csv = cs[:, :].rearrange("k (g t) -> k g t", t=16)
                    TTo('dve', csb[:, :].rearrange("k (g t) -> k g t", t=16), ex[:, :].rearrange("k (g t) -> k g t", t=16),
                       csv[:, :, 15:16].to_broadcast([64, 64, 16]), ALU.add, ['ex', 'cs'], ['bt32'])
                    csf = csb
                ACTF(E2[:], csf[:], AF.Exp, ['cs', 'bt32'], [e2k], scale=DEC_C)
                TTo('pool', ex[:], csf[:], sg[:], ALU.subtract, ['cs', 'bt32', 'sg'], ['ex'])
                ACTF(E1[:], ex[:], AF.Exp, ['ex'], ['E1'], scale=DEC_C)
                S.op('dve', lambda e: e.reciprocal(out=E3[:], in_=E2[:]), reads=[e2k], writes=['E3'])
                tsel = 15 if d == 0 else 0
                def cm(t, h):
                    return t[:, :].rearrange("k (c h t) -> k c h t", c=8, h=8)[:, :, h, :]

                def hm(t, h):
                    return t[:, :].rearrange("k (h c t) -> k h c t", h=8, c=8)[:, h, :, :]
                TTo('pool', bb[:], kk[:], ad[:], ALU.mult, ['kk', 'ad'], ['bb'])
                TTo('dve', bt32[:], bb[:], E3[:], ALU.mult, ['bb', 'E3'], ['bt32'])
                TTo('pool', k3(t1), k3(ad), bc(rp[:, :, 4]), ALU.mult, ['ad', 'rp'], ['t1'])
                TTo('dve', k3(t1), k3(t1), bc(rpd[:, :, 6]), ALU.add, ['t1', 'rpd'], ['t1'])
                TTo('pool', kd[:], xk[:], t1[:], ALU.mult, ['x1', 't1'], ['bb'])
                TTo('dve', kt32[:], kd[:], E3[:], ALU.mult, ['bb', 'E3'], ['kt32'])
                for h in range(8):
                    pch = hm(E2, h)[:, :, tsel:tsel + 1].to_broadcast([64, 8, 16])
                    S.op('dve', lambda e, h=h: e.scalar_tensor_tensor(out=cm(opT['a'], h), in0=hm(kk, h), scalar=-1.0, in1=hm(E1, h),
                                                                     op0=ALU.mult, op1=ALU.mult), reads=['kk', 'E1'], writes=[opk + 'a'])
                    TTo('pool', cm(opT['r'], h), hm(xr, h), hm(E2, h), ALU.mult, ['x0', e2k], [opk + 'r'])
                    CP('act', cm(opT['b'], h), hm(bt32, h), ['bt32'], [opk + 'b'])
                    TTo('pool', cm(opT['bh'], h), hm(bt32, h), pch, ALU.mult, ['bt32', e2k], [opk + 'bh'])
                    CP('act', cm(opT['k'], h), hm(kt32, h), ['kt32'], [opk + 'k'])
                    TTo('dve', cm(opT['kh'], h), hm(kt32, h), pch, ALU.mult, ['kt32', e2k], [opk + 'kh'])
                    CP('act', cm(opT['v'], h), hm(xv, h), ['x2'], [opk + 'v'])
                if d == 0 and not is_ctx:
                    ul = u0 - TC
                    TTo('pool', k3(t1), k3(xr), bc(rp[:, :, 5]), ALU.mult, ['x0', 'rp'], ['t1'])
                    TTo('dve', rkb[:], t1[:], xk[:], ALU.mult, ['t1', 'x1'], ['sqb'])
                    for hf in range(2):
                        MM(PS[0:64, hf * 512:(hf + 1) * 512], ones64[:], rkb[:, hf * 512:(hf + 1) * 512], ['ones64', 'sqb'], ['P0'])
                    TTo('dve', bv[:], PP, xv[:], ALU.mult, ['P0', 'x2'], ['kt32'])
                    for h in range(8):
                        TR(PS[:, 512 + h * 64:512 + (h + 1) * 64], bv[:, h * 128:(h + 1) * 128], identf[0:64, 0:64], ['kt32', 'identf'], ['P0'])
                    CP('dve', bvt[:], PS[:, 512:1024], ['P0'], ['bvt'])
                    S.dma(lambda e, ul=ul: e.dma_start(out=bonD[ul:ul + 128, :], in_=bvt[:]), reads=['bvt'], writes=['bonD'])
                    ACTF(glsb[:], xlo[:, 2, :], AF.Sigmoid, ['xlo'], ['glsb'])
                    MM(PS[:, 512:1024], glsb[:], gateupb[:], ['glsb', 'gateupb'], ['P0'])
                    CP('dve', gtok[:], PS[:, 512:1024], ['P0'], ['gtok'])
                    S.dma(lambda e, ul=ul: e.dma_start(out=gD[ul:ul + 128, :], in_=gtok[:]), reads=['gtok'], writes=['gD'])

            def emit_chunks(idx):
                d, blk, first = blocks[idx]
                opT = opTs[idx % 2]; E2 = E2s[idx % 2]; e2k = 'E2_%d' % (idx % 2); opk = 'o%d_' % (idx % 2)
                u0 = blk * 128
                is_ctx = blk < 2
                tsel = 15 if d == 0 else 0
                if first:
                    S.op('pool', lambda e: e.memset(H[:], 0.0), writes=['H'])
                    S.op('pool', lambda e: e.memset(Hb[:], 0.0), writes=['Hb'])

                PA_, PB_, PC_, PD_ = (PS[:, 0:1024], PS[:, 1024:2048], PS[:, 2048:3072], PS[:, 3072:4096])
                kPB, kPC, kPD = ['P2', 'P3'], ['P4', 'P5'], ['P6', 'P7']
                c8 = lambda ap: ap.rearrange("p (c n) -> p c n", c=8)
                def bc8(m):
                    return m.unsqueeze(1).to_broadcast([128, 8, 128])
                MSd = mk4[:, d, 0:128]; MId = mk4[:, d, 128:256]; MStd = mn1[:, d, :]
                def ch(name, c):
                    return opT[name][:, c * 128:(c + 1) * 128]
                for c in range(8):
                    MM(PB_[:, c * 128:(c + 1) * 128], ch('b', c), ch('a', c), [opk + 'b', opk + 'a'], kPB)
                for c in range(8):
                    MM(PC_[:, c * 128:(c + 1) * 128], ch('a', c), ch('b', c), [opk + 'a', opk + 'b'], kPC)
                for c in range(8):
                    MM(PD_[:, c * 128:(c + 1) * 128], ch('k', c), ch('a', c), [opk + 'k', opk + 'a'], kPD)
                TTo('dve', N1Ta[:], c8(PB_), bc8(MSd), ALU.mult, kPB + ['mk4'], ['N1Ta'])
                TTo('dve', N1a[:], c8(PC_), bc8(MStd), ALU.mult, kPC + ['mn1'], ['N1a'])
                TTo('dve', AakTa[:], c8(PD_), bc8(MSd), ALU.mult, kPD + ['mk4'], ['AakTa'])
                TTo('pool', IN1Ta[:], N1Ta[:], bc8(identb[:, :]), ALU.add, ['N1Ta', 'identb'], ['IN1Ta'])
                PBb = PB_.bitcast(BF16)
                for c in range(8):
                    for si, nm_ in ((0, 'a'), (1, 'v'), (2, 'bh'), (3, 'kh')):
                        TR(PBb[:, c * 256 + si * 64:c * 256 + (si + 1) * 64], ch(nm_, c), identb[0:64, 0:64], [opk + nm_, 'identb'], kPB)
                PBb4 = PBb.rearrange("p (c s n) -> p c s n", c=8, s=4)
                CP('dve', TMa[:, :, 0, :], PBb4[:, :, 0, :], kPB, ['TMa'])
                CP('dve', TMa[:, :, 2:5, :].rearrange("p c s n -> p c (s n)"), PBb.rearrange("p (c n) -> p c n", c=8)[:, :, 64:256], kPB, ['TMa'])
                for c in range(8):
                    MM(PC_[:, c * 128:(c + 1) * 128], N1Ta[:, c, :], N1a[:, c, :], ['N1Ta', 'N1a'], kPC)
                for c in range(8):
                    MM(PD_[:, c * 128:(c + 1) * 128], N1a[:, c, :], N1Ta[:, c, :], ['N1Ta', 'N1a'], kPD)
                CP('act', N2a[:], c8(PC_), kPC, ['N2a'])
                CP('dve', N2Ta[:], c8(PD_), kPD, ['N2Ta'])
                TTo('pool', IN2Ta[:], N2Ta[:], bc8(identb[:, :]), ALU.add, ['N2Ta', 'identb'], ['IN2Ta'])
                for c in range(8):
                    MM(PB_[:, c * 64:(c + 1) * 64], AakTa[:, c, :], TMa[:, c, 2, :], ['AakTa', 'TMa'], kPB)
                CP('dve', TMa[:, :, 1, :], PB_[:, 0:512].rearrange("p (c n) -> p c n", c=8), kPB, ['TMa'])
                for c in range(8):
                    MM(PC_[:, c * 128:(c + 1) * 128], N2Ta[:, c, :], N2a[:, c, :], ['N2Ta', 'N2a'], kPC)
                for c in range(8):
                    MM(PD_[:, c * 128:(c + 1) * 128], N2a[:, c, :], N2Ta[:, c, :], ['N2Ta', 'N2a'], kPD)
                CP('act', N4a[:], c8(PC_), kPC, ['N4a'])
                CP('dve', N4Ta[:], c8(PD_), kPD, ['N4Ta'])
                TTo('pool', IN4Ta[:], N4Ta[:], bc8(identb[:, :]), ALU.add, ['N4Ta', 'identb'], ['IN4Ta'])
                for c in range(8):
                    MM(PB_[:, c * 128:(c + 1) * 128], N4a[:, c, :], N4Ta[:, c, :], ['N4a', 'N4Ta'], kPB)
                TTo('dve', IN8Ta[:], c8(PB_), bc8(identf[:, :]), ALU.add, kPB + ['identf'], ['IN8Ta'])
                if not is_ctx:
                    for c in range(8):
                        MM(PC_[:, c * 128:(c + 1) * 128], ch('b', c), ch('r', c), [opk + 'b', opk + 'r'], kPC)
                    for c in range(8):
                        MM(PD_[:, c * 128:(c + 1) * 128], ch('k', c), ch('r', c), [opk + 'k', opk + 'r'], kPD)
                    TTo('dve', ArbTa[:], c8(PC_), bc8(MId), ALU.mult, kPC + ['mk4'], ['ArbTa'])
                    TTo('dve', ArkTa[:], c8(PD_), bc8(MId), ALU.mult, kPD + ['mk4'], ['ArkTa'])
                xsrc = None
                for li, (INa, ik) in enumerate(((IN8Ta, 'IN8Ta'), (IN4Ta, 'IN4Ta'), (IN2Ta, 'IN2Ta'), (IN1Ta, 'IN1Ta'))):
                    Pq, kq = (PB_, kPB) if li % 2 == 0 else (PC_, kPC)
                    for c in range(8):
                        rhs_ = TMa[:, c, 0:2, :].rearrange("p a n -> p (a n)") if xsrc is None else xsrc[:, c, :]
                        MM(Pq[:, c * 128:(c + 1) * 128], INa[:, c, :], rhs_, [ik, 'TMa' if xsrc is None else xk_], kq)
                    Xn = Xa[li % 2]; xk_ = 'Xa%d' % (li % 2)
                    CP('dve' if li % 2 else 'act', Xn[:], c8(Pq), kq, [xk_])
                    xsrc = Xn
                B3 = PS[:, 1536:2048]; B4 = PS[:, 2048:2560]; B5 = PS[:, 2560:3072]; B6 = PS[:, 3072:3584]; B7 = PS[:, 3584:4096]
                bm3 = bm[:, :].rearrange("p (h n) -> p h n", h=8)
                nch = int(os.environ.get('K_RCH', 8))
                for c in (list(range(8)) if d == 0 else list(range(7, -1, -1)))[:nch]:
                    Wc = xsrc[:, c, 0:64]; U0 = xsrc[:, c, 64:128]
                    Bh_ = TMa[:, c, 3, :]; Kh_ = TMa[:, c, 4, :]; Vt_ = TMa[:, c, 2, :]
                    for dst_, src_, rk_, wk_ in ((Bbd, Bh_, 'TMa', 'Bbd'), (Ubd, U0, xk_, 'Ubd'), (Vbd, Vt_, 'TMa', 'Vbd')):
                        TTo('pool', dst_[:, :].rearrange("p (h n) -> p h n", h=8), src_.unsqueeze(1).to_broadcast([128, 8, 64]), bm3, ALU.mult,
                            [rk_, 'bm'], [wk_])
                    MM(B4[0:64, :], Wc, Bbd[:], [xk_, 'Bbd'], ['P4'])
                    CP('act', GTs[:], B4[0:64, :], ['P4'], ['GTs'])
                    if not is_ctx:
                        MM(B6[0:64, 0:128], Wc, ArbTa[:, c, :], [xk_, 'ArbTa'], ['P6'])
                        TTo('dve', QTs[:], B6[0:64, 0:128], ch('r', c), ALU.add, ['P6', opk + 'r'], ['QTs'])
                        MM(B6[:, 128:192], ArbTa[:, c, :], U0, ['ArbTa', xk_], ['P6'], start=True, stop=False)
                        MM(B6[:, 128:192], ArkTa[:, c, :], Vt_, ['ArkTa', 'TMa'], ['P6'], start=False, stop=True)
                        CP('dve', Y0s[:], B6[:, 128:192], ['P6'], ['Y0s'])
                        MM(B7, QTs[:], Hb[:], ['QTs', 'Hb'], ['P7'])
                        TTo('dve', ytmp[:], B7, bm[:], ALU.mult, ['P7', 'bm'], ['gtok'])
                        S.op('dve', lambda e: e.tensor_reduce(out=yc[:], in_=ytmp[:, :].rearrange("p (h v) -> p v h", h=8), axis=AX.X, op=ALU.add),
                             reads=['gtok'], writes=['yc'])
                        TTo('pool', Yblk[:, c, :], yc[:], Y0s[:], ALU.add, ['yc', 'Y0s'], ['Yblk'])
                    MM(B3[0:64, :], Bh_, Ubd[:], ['TMa', 'Ubd'], ['P3'], start=True, stop=False)
                    MM(B3[0:64, :], Kh_, Vbd[:], ['TMa', 'Vbd'], ['P3'], start=False, stop=False)
                    for h in range(8):
                        MM(B3[0:64, h * 64:(h + 1) * 64], GTs[:, h * 64:(h + 1) * 64], Hb[:, h * 64:(h + 1) * 64], ['GTs', 'Hb'], ['P3'],
                           start=False, stop=(h == 7))
                    PCc = chv(E2, c)[:, :, tsel:tsel + 1].to_broadcast([64, 8, 64])
                    TTo('pool', Ht[:, :].rearrange("k (h v) -> k h v", h=8), H[:, :].rearrange("k (h v) -> k h v", h=8), PCc, ALU.mult,
                        ['H', e2k], ['Ht'])
                    TTo('dve', Hb[:], Ht[:], B3[0:64, :], ALU.add, ['Ht', 'P3'], ['Hb'])
                    TTo('dve', H[:], Ht[:], B3[0:64, :], ALU.add, ['Ht', 'P3'], ['H'])
                    S.drain(S.pend, (len(S.pend) + 7) // 8)
                if not is_ctx:
                    ul = u0 - TC
                    for h in range(8):
                        S.dma(lambda e, h=h, ul=ul, d=d: e.dma_start(
                            out=ydir[d, ul:ul + 128, h * 64:(h + 1) * 64].rearrange("(c t) v -> t c v", t=16),
                            in_=Yblk[h * 16:(h + 1) * 16, :, :]), reads=['Yblk'], writes=['ydir'])

            S.capture = []
            emit_prep(0)
            pend = S.capture; S.capture = None
            S.drain(pend, len(pend))
            for idx in range(len(blocks)):
                pend = []
                if idx + 1 < len(blocks):
                    S.capture = []
                    emit_prep(idx + 1)
                    pend = S.capture; S.capture = None
                S.pend = pend
                emit_chunks(idx)
                S.drain(pend, len(pend))

            if dbg:
                oy = dout("dbg_y", [2 * TL, 512]); ob = dout("dbg_bon", [TL, 512]); og = dout("dbg_g", [TL, 512])
                S.dma(lambda e: e.dma_start(out=oy[:, :], in_=ydir.rearrange("d t n -> (d t) n")), reads=['ydir'])
                S.dma(lambda e: e.dma_start(out=ob[:, :], in_=bonD[:, :]), reads=['bonD'])
                S.dma(lambda e: e.dma_start(out=og[:, :], in_=gD[:, :]), reads=['gD'])
                oH = dout("dbg_H", [64, 512])
                S.dma(lambda e: e.dma_start(out=oH[:, :], in_=H[:]), reads=['H'])
            S.barrier()
            S.flush()
        if stop_after <= 3:
            return nc, dbg_outs

        with contextlib.ExitStack() as ph:
            woutb = sb(ph, "woutb", [128, 8, D], BF16)
            attT_all = sb(ph, "attT_c", [128, 4, TL], BF16)
            S.dma(lambda e: e.dma_start(out=attT_all[:].rearrange("p a t -> p (a t)"), in_=attD[:, :]), reads=['attD'], writes=['attT_all'])
            cst = {}
            for nm, src in (("gt1", modd[0:1, 2048:3072]), ("sh2", modd[0:1, 3072:4096]), ("sc2p", modd[0:1, 4096:5120]),
                            ("ln1g", ln1_d[0:1, :]), ("ln1b", ln1_d[1:2, :])):
                cst[nm] = sb(ph, nm, [128, D], F32)
                S.dma(lambda e, nm=nm, src=src: e.dma_start(out=cst[nm][:], in_=src.partition_broadcast(128)), reads=['modd'], writes=[nm])
            for nm, row in (("lnxg", 0), ("lnxb", 1)):
                cst[nm] = sb(ph, nm, [128, 512], F32)
                S.dma(lambda e, nm=nm, row=row: e.dma_start(out=cst[nm][:], in_=lnx_d[row:row + 1, :].partition_broadcast(128)), writes=[nm])
            wst2 = [sb(ph, "wst2_%d" % i, [128, D], F32) for i in range(2)]
            for j in range(8):
                w = wst2[j % 2]; wk = 'wst2_%d' % (j % 2)
                S.dma(lambda e, w=w, j=j: e.dma_start(out=w[:], in_=wout_d[j * 128:(j + 1) * 128, :]), writes=[wk])
                CP('pool' if j % 2 else 'dve', woutb[:, j, :], w[:], [wk], ['woutb'])
            rwf = sb(ph, "rwf", [128, 8, 16], F32)
            S.dma(lambda e: e.dma_start(out=rwf[:], in_=rw_d.rearrange("(j p) n -> p j n", p=128)), writes=['rwf'])
            gneps = sb(ph, "gneps", [128, 1], F32)
            S.op('pool', lambda e: e.memset(gneps[:], 64e-5), writes=['gneps'])
            yf = sb(ph, "yf", [128, 512], F32); yb = sb(ph, "yb", [128, 512], F32)
            bon = sb(ph, "bon", [128, 512], F32); gg = sb(ph, "gg", [128, 512], F32)
            ysum = sb(ph, "ysum", [128, 512], F32); ysq = sb(ph, "ysq", [128, 512], F32)
            gst = sb(ph, "gst", [128, 8], F32); gvar = sb(ph, "gvar", [128, 8], F32)
            rwob = sb(ph, "rwob", [128, 512], BF16); rwoT = sb(ph, "rwoT", [128, 4, 128], BF16)
            xin_t = sb(ph, "xin_t", [128, D], F32); tres = sb(ph, "tres", [128, D], F32)
            x1t = sb(ph, "x1t", [128, D], F32); h2f = sb(ph, "h2f", [128, D], F32); h2b = sb(ph, "h2b", [128, D], BF16)
            h2T = sb(ph, "h2T", [128, 8, 128], F32)
            stats = sb(ph, "statsC", [128, 2, 6], F32); mv = sb(ph, "mvC", [128, 2], F32)
            rstd = sb(ph, "rstdC", [128, 1], F32); nb = sb(ph, "nbC", [128, 1], F32)
            lntC = (stats, mv, rstd, nb, 'C')
            lmax = sb(ph, "lmax", [128, 1], F32); lex = sb(ph, "lex", [128, 16], F32); lsum = sb(ph, "lsum", [128, 1], F32)

            def v8(t):
                return t[:, :].rearrange("p (h v) -> p h v", h=8)

            def b8(t):
                return t[:, :].unsqueeze(2).to_broadcast([128, 8, 64])
            for i in range(int(os.environ.get('K_CT', 32))):
                t0 = i * 128
                S.dma(lambda e, t0=t0: e.dma_start(out=yf[:], in_=ydir[0, t0:t0 + 128, :]), reads=['ydir'], writes=['yf'])
                S.dma(lambda e, t0=t0: e.dma_start(out=yb[:], in_=ydir[1, t0:t0 + 128, :]), reads=['ydir'], writes=['yb'])
                S.dma(lambda e, t0=t0: e.dma_start(out=bon[:], in_=bonD[t0:t0 + 128, :]), reads=['bonD'], writes=['bon'])
                S.dma(lambda e, t0=t0: e.dma_start(out=gg[:], in_=gD[t0:t0 + 128, :]), reads=['gD'], writes=['gg'])
                S.dma(lambda e, t0=t0: e.dma_start(out=xin_t[:], in_=x_d[t0:t0 + 128, :]), writes=['xin_t'])
                TTo('pool', ysum[:], yf[:], yb[:], ALU.add, ['yf', 'yb'], ['ysum'])
                S.op('dve', lambda e: e.tensor_reduce(out=gst[:], in_=v8(ysum), axis=AX.X, op=ALU.add), reads=['ysum'], writes=['gst'])
                S.op('dve', lambda e: e.tensor_scalar(out=gst[:], in0=gst[:], scalar1=-1.0 / 64, scalar2=None, op0=ALU.mult),
                     reads=['gst'], writes=['gst'])
                TTo('dve', v8(ysum), v8(ysum), b8(gst), ALU.add, ['ysum', 'gst'], ['ysum'])
                ACTF(ysq[:], ysum[:], AF.Square, ['ysum'], ['ysq'])
                S.op('dve', lambda e: e.tensor_reduce(out=gvar[:], in_=v8(ysq), axis=AX.X, op=ALU.add), reads=['ysq'], writes=['gvar'])
                ACTF(gvar[:], gvar[:], AF.Ln, ['gvar', 'gneps'], ['gvar'], scale=1.0 / 64, bias=gneps[:])
                ACTF(gvar[:], gvar[:], AF.Exp, ['gvar'], ['gvar'], scale=-0.5)
                TTo('dve', v8(ysum), v8(ysum), b8(gvar), ALU.mult, ['ysum', 'gvar'], ['ysum'])
                TTo('pool', ysum[:], ysum[:], cst['lnxg'][:], ALU.mult, ['ysum', 'lnxg'], ['ysum'])
                TTo('dve', ysum[:], ysum[:], cst['lnxb'][:], ALU.add, ['ysum', 'lnxb'], ['ysum'])
                TTo('pool', ysum[:], ysum[:], bon[:], ALU.add, ['ysum', 'bon'], ['ysum'])
                TTo('dve', rwob[:], ysum[:], gg[:], ALU.mult, ['ysum', 'gg'], ['rwob'])
                pr_ = P[0][:].bitcast(BF16)
                for j in range(4):
                    TR(pr_[:, j * 128:(j + 1) * 128], rwob[:, j * 128:(j + 1) * 128], identb[:], ['rwob', 'identb'], ['P0'])
                CP('dve', rwoT[:], pr_[:, 0:512].rearrange("p (j t) -> p j t", j=4), ['P0'], ['rwoT'])
                for half in range(2):
                    ob = PS[:, 1024 + half * 512:1024 + (half + 1) * 512]
                    for j in range(8):
                        lt = attT_all[:, j, t0:t0 + 128] if j < 4 else rwoT[:, j - 4, :]
                        MM(ob, lt, woutb[:, j, half * 512:(half + 1) * 512], ['attT_all', 'rwoT', 'woutb'], ['P2'], start=(j == 0), stop=(j == 7))
                TTo('dve', tres[:], PS[:, 1024:2048], cst['gt1'][:], ALU.mult, ['P2', 'gt1'], ['tres'])
                S.op('dve', lambda e: e.scalar_tensor_tensor(out=tres[:], in0=xin_t[:], scalar=ALPHA, in1=tres[:], op0=ALU.mult, op1=ALU.add),
                     reads=['xin_t', 'tres'], writes=['tres'])
                ln_stats(lntC, tres, 'tres', eps5, 'eps5')
                S.op('act', lambda e: e.activation(out=x1t[:], in_=tres[:], func=AF.Identity, bias=nb[:], scale=rstd[:]),
                     reads=['tres', 'Cnb', 'Crstd'], writes=['x1t'])
                TTo('pool', x1t[:], x1t[:], cst['ln1g'][:], ALU.mult, ['x1t', 'ln1g'], ['x1t'])
                TTo('dve', x1t[:], x1t[:], cst['ln1b'][:], ALU.add, ['x1t', 'ln1b'], ['x1t'])
                S.dma(lambda e, t0=t0: e.dma_start(out=x1D[t0:t0 + 128, :], in_=x1t[:]), reads=['x1t'], writes=['x1D'])
                ln_stats(lntC, x1t, 'x1t', eps5, 'eps5')
                S.op('act', lambda e: e.activation(out=h2f[:], in_=x1t[:], func=AF.Identity, bias=nb[:], scale=rstd[:]),
                     reads=['x1t', 'Cnb', 'Crstd'], writes=['h2f'])
                TTo('pool', h2f[:], h2f[:], cst['sc2p'][:], ALU.mult, ['h2f', 'sc2p'], ['h2f'])
                TTo('dve', h2f[:], h2f[:], cst['sh2'][:], ALU.add, ['h2f', 'sh2'], ['h2f'])
                CP('pool', h2b[:], h2f[:], ['h2f'], ['h2b'])
                S.dma(lambda e, t0=t0: e.dma_start(out=h2D[t0:t0 + 128, :], in_=h2b[:]), reads=['h2b'], writes=['h2D'])
                for j in range(8):
                    TR(PS[:, 2048 + j * 128:2048 + (j + 1) * 128], h2f[:, j * 128:(j + 1) * 128], identf[:], ['h2f', 'identf'], ['P4'])
                CP('dve', h2T[:].rearrange("p j t -> p (j t)"), PS[:, 2048:3072], ['P4'], ['h2T'])
                for j in range(8):
                    MM(PS[:, 3072:3088], h2T[:, j, :], rwf[:, j, :], ['h2T', 'rwf'], ['P6'], start=(j == 0), stop=(j == 7))
                S.op('dve', lambda e: e.tensor_reduce(out=lmax[:], in_=PS[:, 3072:3088], axis=AX.X, op=ALU.max), reads=['P6'], writes=['lmax'])
                S.op('dve', lambda e: e.tensor_scalar(out=lmax[:], in0=lmax[:], scalar1=-1.0, scalar2=None, op0=ALU.mult), reads=['lmax'], writes=['lmax'])
                ACTF(lex[:], PS[:, 3072:3088], AF.Exp, ['P6', 'lmax'], ['lex'], bias=lmax[:])
                S.op('dve', lambda e: e.tensor_reduce(out=lsum[:], in_=lex[:], axis=AX.X, op=ALU.add), reads=['lex'], writes=['lsum'])
                S.op('dve', lambda e: e.reciprocal(out=lsum[:], in_=lsum[:]), reads=['lsum'], writes=['lsum'])
                S.op('dve', lambda e, i=i: e.tensor_scalar(out=aff_all[:, i, :], in0=lex[:], scalar1=lsum[:], scalar2=None, op0=ALU.mult),
                     reads=['lex', 'lsum'], writes=['aff_all'])
            if dbg:
                o1 = dout("dbg_x1", [TL, D]); o2 = dout("dbg_aff", [128, 512])
                S.dma(lambda e: e.dma_start(out=o1[:, :], in_=x1D[:, :]), reads=['x1D'])
                S.dma(lambda e: e.dma_start(out=o2[:, :], in_=aff_all[:].rearrange("p a b -> p (a b)")), reads=['aff_all'])
            S.barrier()
            S.flush()
        if stop_after <= 4:
            return nc, dbg_outs
        posm = sb(top, "posm", [128, 32, 16], F32)
        gw = sb(top, "gw", [128, 32, 16, 2], BF16)
        onesf = sb(top, "onesf", [128, 128], F32)
        iot = sb(top, "iot", [128, 516], F32)
        S.op('pool', lambda e: e.memset(onesf[:], 1.0), writes=['onesf'])
        S.dma(lambda e: e.dma_start(out=iot[:], in_=iot_d[:, :]), writes=['iot'])

        with contextlib.ExitStack() as ph:
            lo = sb(ph, "lo", [128, 16], F32); hi = sb(ph, "hi", [128, 16], F32); mid = sb(ph, "mid", [128, 16], F32)
            cmpt = sb(ph, "cmpt", [128, 32, 16], F32); cntp = sb(ph, "cntp", [128, 16], F32); ge = sb(ph, "ge", [128, 16], F32)
            dlt = sb(ph, "dlt", [128, 16], F32)
            ustr = sb(ph, "ustr", [128, 128], F32)
            mask = sb(ph, "mask", [128, 32, 16], F32); tot = sb(ph, "tot", [128, 32, 16], F32); cum = sb(ph, "cum", [128, 32, 16], F32)
            glo = sb(ph, "glo", [128, 32, 16], F32); ghi32 = sb(ph, "ghi32", [128, 32, 16], F32)
            S.dma(lambda e: e.dma_start(out=ustr[:], in_=ustr_d[:, :]), writes=['ustr'])
            S.op('pool', lambda e: e.memset(lo[:], 0.0), writes=['lo'])
            S.op('pool', lambda e: e.memset(hi[:], 1.0), writes=['hi'])
            affv = aff_all[:, :, :]
            for it in range(30):
                TTo('dve', mid[:], lo[:], hi[:], ALU.add, ['lo', 'hi'], ['mid'])
                S.op('dve', lambda e: e.tensor_scalar(out=mid[:], in0=mid[:], scalar1=0.5, scalar2=None, op0=ALU.mult), reads=['mid'], writes=['mid'])
                TTo('dve', cmpt[:], affv, mid[:, :].unsqueeze(1).to_broadcast([128, 32, 16]), ALU.is_ge, ['aff_all', 'mid'], ['cmpt'])
                S.op('dve', lambda e: e.tensor_reduce(out=cntp[:], in_=cmpt[:].rearrange("p t e -> p e t"), axis=AX.X, op=ALU.add),
                     reads=['cmpt'], writes=['cntp'])
                MM(PS[:, 0:16], onesf[:], cntp[:], ['onesf', 'cntp'], ['P0'])
                S.op('dve', lambda e: e.tensor_scalar(out=ge[:], in0=PS[:, 0:16], scalar1=511.5, scalar2=None, op0=ALU.is_ge), reads=['P0'], writes=['ge'])
                TTo('dve', dlt[:], mid[:], lo[:], ALU.subtract, ['mid', 'lo'], ['dlt'])
                TTo('dve', dlt[:], dlt[:], ge[:], ALU.mult, ['dlt', 'ge'], ['dlt'])
                TTo('dve', lo[:], lo[:], dlt[:], ALU.add, ['lo', 'dlt'], ['lo'])
                TTo('dve', dlt[:], hi[:], mid[:], ALU.subtract, ['hi', 'mid'], ['dlt'])
                TTo('dve', dlt[:], dlt[:], ge[:], ALU.mult, ['dlt', 'ge'], ['dlt'])
                TTo('dve', hi[:], mid[:], dlt[:], ALU.add, ['mid', 'dlt'], ['hi'])
            TTo('dve', mask[:], affv, lo[:, :].unsqueeze(1).to_broadcast([128, 32, 16]), ALU.is_ge, ['aff_all', 'lo'], ['mask'])
            m2 = mask[:].rearrange("p t e -> p (t e)")
            MM(PS[:, 512:1024], ustr[:], m2, ['ustr', 'mask'], ['P1'])
            MM(PS[:, 1024:1536], onesf[:], m2, ['onesf', 'mask'], ['P2'])
            CP('dve', tot[:].rearrange("p t e -> p (t e)"), PS[:, 1024:1536], ['P2'], ['tot'])
            for e_ in range(16):
                S.op('dve', lambda e, e_=e_: e.tensor_tensor_scan(out=cum[:, :, e_], data0=onesf[:, 0:32], data1=tot[:, :, e_], initial=0.0,
                                                                 op0=ALU.mult, op1=ALU.add), reads=['tot', 'onesf'], writes=['cum'])
            TTo('dve', cum[:], cum[:], tot[:], ALU.subtract, ['cum', 'tot'], ['cum'])
            TTo('dve', cum[:].rearrange("p t e -> p (t e)"), cum[:].rearrange("p t e -> p (t e)"), PS[:, 512:1024], ALU.add, ['cum', 'P1'], ['cum'])
            S.op('dve', lambda e: e.scalar_tensor_tensor(out=posm[:], in0=cum[:], scalar=1.0, in1=mask[:], op0=ALU.add, op1=ALU.mult),
                 reads=['cum', 'mask'], writes=['posm'])
            S.op('dve', lambda e: e.tensor_scalar(out=posm[:], in0=posm[:], scalar1=-1.0, scalar2=None, op0=ALU.add), reads=['posm'], writes=['posm'])
            TTo('dve', glo[:], affv, mask[:], ALU.mult, ['aff_all', 'mask'], ['glo'])
            CP('dve', gw[:, :, :, 0], glo[:], ['glo'], ['gw'])
            CP('dve', ghi32[:], gw[:, :, :, 0], ['gw'], ['ghi32'])
            TTo('dve', gw[:, :, :, 1], glo[:], ghi32[:], ALU.subtract, ['glo', 'ghi32'], ['gw'])
            if dbg:
                o1 = dout("dbg_posm", [128, 512])
                S.dma(lambda e: e.dma_start(out=o1[:, :], in_=posm[:].rearrange("p a b -> p (a b)")), reads=['posm'])
            S.barrier()
            S.flush()
        if stop_after <= 5:
            return nc, dbg_outs

        with contextlib.ExitStack() as ph:
            h2_all = sb(ph, "h2_all", [128, 32, D], BF16)
            for i in range(32):
                S.dma(lambda e, i=i: e.dma_start(out=h2_all[:, i, :], in_=h2D[i * 128:(i + 1) * 128, :]), reads=['h2D'], writes=['h2_all'])
            OH = sb(ph, "OH", [128, 32, 512], BF16)
            xinT = sb(ph, "xinT", [128, 8, 512], BF16); hidT = sb(ph, "hidT", [128, 8, 512], BF16)
            wgb = sb(ph, "wgb", [128, 8, D], BF16); wub = sb(ph, "wub", [128, 8, D], BF16); wdb = sb(ph, "wdb", [128, 8, D], BF16)
            wsg = [sb(ph, "wsg%d" % i, [128, D], F32) for i in range(6)]
            gcs = sb(ph, "gcs", [128, 4], F32); sgt = sb(ph, "sgt", [128, 512], F32)
            ywt = sb(ph, "ywt", [128, 4, D], BF16)
            wi = 0
            for ex_ in range(int(os.environ.get('K_NE', 16))):
                for i in range(32):
                    S.op('dve' if i % 2 else 'pool', lambda e, i=i, ex_=ex_: e.tensor_scalar(
                        out=OH[:, i, :], in0=iot[:, 0:512], scalar1=posm[:, i, ex_:ex_ + 1], scalar2=None, op0=ALU.is_equal),
                        reads=['iot', 'posm'], writes=['OH'])
                for j in range(8):
                    pb = P[j % 2]; pk = 'P%d' % (j % 2)
                    for i in range(32):
                        MM(pb, h2_all[:, i, j * 128:(j + 1) * 128], OH[:, i, :], ['h2_all', 'OH'], [pk], start=(i == 0), stop=(i == 31))
                    CP('act' if j % 2 else 'dve', xinT[:, j, :], pb, [pk], ['xinT'])
                for (wsrc, wdst, wkey) in ((wg_d, wgb, 'wgb'), (wu_d, wub, 'wub'), (wd_d, wdb, 'wdb')):
                    for j in range(8):
                        w = wsg[wi % 6]; wk = 'wsg%d' % (wi % 6)
                        S.dma(lambda e, w=w, wsrc=wsrc, ex_=ex_, j=j: e.dma_start(out=w[:], in_=wsrc[ex_, j * 128:(j + 1) * 128, :]), writes=[wk])
                        CP(('dve', 'pool', 'act')[wi % 3], wdst[:, j, :], w[:], [wk], [wkey])
                        wi += 1
                gcp = PS[:, 1024:1032].rearrange("p (c k) -> p c k", k=2)
                for ct in range(4):
                    for i in range(32):
                        MM(gcp[:, ct, :], OH[:, i, ct * 128:(ct + 1) * 128], gw[:, i, ex_, :], ['OH', 'gw'], ['P2'],
                           start=(i == 0 and ct == 0), stop=(i == 31 and ct == 3))
                TTo('dve', gcs[:], gcp[:, :, 0], gcp[:, :, 1], ALU.add, ['P2'], ['gcs']) if False else None
                CP('dve', sgt[:, 0:8], PS[:, 1024:1032], ['P2'], ['sgt'])
                TTo('dve', gcs[:], sgt[:, 0:8].rearrange("p (c k) -> p c k", k=2)[:, :, 0], sgt[:, 0:8].rearrange("p (c k) -> p c k", k=2)[:, :, 1],
                    ALU.add, ['sgt'], ['gcs'])
                for fc in range(8):
                    for j in range(8):
                        MM(P[4], wgb[:, j, fc * 128:(fc + 1) * 128], xinT[:, j, :], ['wgb', 'xinT'], ['P4'], start=(j == 0), stop=(j == 7))
                    for j in range(8):
                        MM(P[5], wub[:, j, fc * 128:(fc + 1) * 128], xinT[:, j, :], ['wub', 'xinT'], ['P5'], start=(j == 0), stop=(j == 7))
                    ACTF(sgt[:], P[4], AF.Silu, ['P4'], ['sgt'])
                    TTo('dve', hidT[:, fc, :], sgt[:], P[5], ALU.mult, ['sgt', 'P5'], ['hidT'])
                for ct in range(4):
                    for half in range(2):
                        pb = P[6 + half]; pk = 'P%d' % (6 + half)
                        for fc in range(8):
                            MM(pb, hidT[:, fc, ct * 128:(ct + 1) * 128], wdb[:, fc, half * 512:(half + 1) * 512], ['hidT', 'wdb'], [pk],
                               start=(fc == 0), stop=(fc == 7))
                        S.op('act', lambda e, ct=ct, half=half, pb=pb: e.activation(out=ywt[:, ct, half * 512:(half + 1) * 512], in_=pb,
                                                                                  func=AF.Copy, scale=gcs[:, ct:ct + 1]),
                             reads=[pk, 'gcs'], writes=['ywt'])
                S.dma(lambda e, ex_=ex_: e.dma_start(out=ywD[ex_].rearrange("(c p) n -> p c n", p=128), in_=ywt[:]), reads=['ywt'], writes=['ywD'])
            if dbg:
                o1 = dout("dbg_yw", [16 * 512, D], BF16)
                S.dma(lambda e: e.dma_start(out=o1[:, :], in_=ywD.rearrange("e c n -> (e c) n")), reads=['ywD'])
            S.barrier()
            S.flush()
        if stop_after <= 6:
            return nc, dbg_outs

        with contextlib.ExitStack() as ph:
            yw_all = sb(ph, "yw_all", [128, 16, 4, D], BF16)
            for ex_ in range(16):
                S.dma(lambda e, ex_=ex_: e.dma_start(out=yw_all[:, ex_, :, :], in_=ywD[ex_].rearrange("(c p) n -> p c n", p=128)),
                      reads=['ywD'], writes=['yw_all'])
            cst = {}
            for nm, src in (("gt2", modd[0:1, 5120:6144]), ("ln2g", ln2_d[0:1, :]), ("ln2b", ln2_d[1:2, :])):
                cst[nm] = sb(ph, nm, [128, D], F32)
                S.dma(lambda e, nm=nm, src=src: e.dma_start(out=cst[nm][:], in_=src.partition_broadcast(128)), reads=['modd'], writes=[nm])
            dg = sb(ph, "dg", [128, 4, 128], F32)
            OHTs = [sb(ph, "OHT%d" % q_, [128, 4, 2048], BF16) for q_ in range(2)]
            x1ls = [sb(ph, "x1l%d" % q_, [128, D], F32) for q_ in range(2)]
            tr2s = [sb(ph, "tr2%d" % q_, [128, D], F32) for q_ in range(2)]
            xos = [sb(ph, "xo%d" % q_, [128, D], F32) for q_ in range(2)]
            stats = sb(ph, "statsF", [128, 2, 6], F32); mv = sb(ph, "mvF", [128, 2], F32)
            rstd = sb(ph, "rstdF", [128, 1], F32); nb = sb(ph, "nbF", [128, 1], F32)
            lntF = (stats, mv, rstd, nb, 'F')
            nFT = int(os.environ.get('K_FT', 32))

            def f_front(i):
                q_ = i % 2
                OHT = OHTs[q_]; kOHT = 'OHT%d' % q_
                for eg in range(4):
                    for k_ in range(4):
                        ex_ = eg * 4 + k_
                        S.op('dve' if k_ % 2 else 'pool', lambda e, k_=k_, ex_=ex_, i=i: e.tensor_scalar(
                            out=dg[:, k_, :], in0=identf[:], scalar1=posm[:, i, ex_:ex_ + 1], scalar2=None, op0=ALU.mult),
                            reads=['identf', 'posm'], writes=['dg'])
                    MM(PS[:, eg * 512:(eg + 1) * 512], onesf[:], dg[:].rearrange("p a t -> p (a t)"), ['onesf', 'dg'], ['P%d' % eg])

            def f_cmp(i):
                q_ = i % 2
                OHT = OHTs[q_]; kOHT = 'OHT%d' % q_
                for ct in range(4):
                    S.op('dve', lambda e, ct=ct, OHT=OHT: e.tensor_scalar(out=OHT[:, ct, :], in0=PS[:, 0:2048], scalar1=iot[:, 512 + ct:513 + ct],
                                                                         scalar2=None, op0=ALU.is_equal),
                         reads=['P0', 'P1', 'P2', 'P3', 'iot'], writes=[kOHT])

            def f_back(i):
                t0 = i * 128
                q_ = i % 2
                OHT = OHTs[q_]; x1l = x1ls[q_]; tr2 = tr2s[q_]; xo = xos[q_]
                kOHT = 'OHT%d' % q_; kx1l = 'x1l%d' % q_; ktr2 = 'tr2%d' % q_; kxo = 'xo%d' % q_
                S.dma(lambda e, t0=t0, x1l=x1l: e.dma_start(out=x1l[:], in_=x1D[t0:t0 + 128, :]), reads=['x1D'], writes=[kx1l])
                for half in range(2):
                    pb = P[4 + half]; pk = 'P%d' % (4 + half)
                    n = 0
                    for ex_ in range(16):
                        for ct in range(4):
                            MM(pb, OHT[:, ct, ex_ * 128:(ex_ + 1) * 128], yw_all[:, ex_, ct, half * 512:(half + 1) * 512], [kOHT, 'yw_all'], [pk],
                               start=(n == 0), stop=(n == 63))
                            n += 1
                if i + 1 < nFT:
                    f_cmp(i + 1)
                TTo('dve', tr2[:], PS[:, 2048:3072], cst['gt2'][:], ALU.mult, ['P4', 'P5', 'gt2'], [ktr2])
                S.op('dve', lambda e, tr2=tr2, x1l=x1l: e.scalar_tensor_tensor(out=tr2[:], in0=x1l[:], scalar=ALPHA, in1=tr2[:], op0=ALU.mult, op1=ALU.add),
                     reads=[kx1l, ktr2], writes=[ktr2])
                ln_stats(lntF, tr2, ktr2, eps5, 'eps5')
                S.op('act', lambda e, xo=xo, tr2=tr2: e.activation(out=xo[:], in_=tr2[:], func=AF.Identity, bias=nb[:], scale=rstd[:]),
                     reads=[ktr2, 'Fnb', 'Frstd'], writes=[kxo])
                TTo('pool', xo[:], xo[:], cst['ln2g'][:], ALU.mult, [kxo, 'ln2g'], [kxo])
                TTo('dve', xo[:], xo[:], cst['ln2b'][:], ALU.add, [kxo, 'ln2b'], [kxo])
                S.dma(lambda e, t0=t0, xo=xo: e.dma_start(out=out_d[t0:t0 + 128, :], in_=xo[:]), reads=[kxo], writes=['out'])

            f_front(0)
            f_cmp(0)
            for i in range(nFT):
                if i + 1 < nFT:
                    f_front(i + 1)
                f_back(i)
            S.barrier()
            S.flush()
    return nc, dbg_outs


def host_inputs(inp, b):
    f = np.float32
    m = {}
    m["x"] = np.ascontiguousarray(inp["x"][b], dtype=f)
    m["ctx"] = np.ascontiguousarray(inp["ctx"][b], dtype=f)
    m["cc"] = np.ascontiguousarray(np.stack([inp["c"][b].reshape(8, 128).T, inp["c_ctx"].reshape(8, 128).T], -1).reshape(128, 16), dtype=f)
    m["w_ada"] = np.ascontiguousarray(inp["w_ada"][0], dtype=f)
    m["b_ada"] = np.ascontiguousarray(inp["b_ada"][0].reshape(1, -1), dtype=f)
    m["w_in"] = np.ascontiguousarray(inp["w_in"][0], dtype=f)
    m["qg"] = np.ascontiguousarray(np.tile(inp["q_gain"][0], 8).reshape(1, 512), dtype=f)
    m["kg"] = np.ascontiguousarray(np.tile(inp["k_gain"][0], 2).reshape(1, 128), dtype=f)
    t = np.arange(TL)
    pos = np.stack([t // 64, t % 64], -1).astype(np.float32)
    inv = (10000.0 ** (-np.arange(16, dtype=np.float32) / 16)).astype(np.float32)
    ang = pos[:, :, None] * inv[None, None, :]
    cs = np.cos(ang).astype(f); sn = np.sin(ang).astype(f)
    cos2 = np.stack([cs, cs], 2).reshape(TL, 64)
    sin2 = np.stack([-sn, sn], 2).reshape(TL, 64)
    m["cosT"] = np.ascontiguousarray(np.tile(cos2, (1, 8)), dtype=f)
    m["sinS"] = np.ascontiguousarray(np.tile(sin2, (1, 8)), dtype=f)
    m["ident"] = np.eye(128, dtype=f)
    def kh(v):
        return np.asarray(v, dtype=f).reshape(8, 64).T
    mu = inp["tshift_mu"][0]
    cols = [kh(mu[0:512]), kh(mu[512:1024]), kh(mu[1024:1536]), kh(inp["k_k"][0]), kh(inp["k_a"][0]), kh(inp["r_k"][0].reshape(-1)),
            kh(inp["decay_w0"][0, 0]), kh(inp["decay_w0"][0, 1]), kh(inp["iclr_a0"][0, 0]), kh(inp["iclr_a0"][0, 1])]
    m["rp"] = np.ascontiguousarray(np.stack(cols, -1).reshape(64, 80), dtype=f)
    lmu = np.zeros((128, 3), f)
    lmu[0:64, 0] = mu[1536:1600]; lmu[0:64, 1] = mu[1600:1664]; lmu[:, 2] = mu[1664:1792]
    m["lmu"] = lmu
    m["decup"] = np.ascontiguousarray(np.concatenate([inp["decay_up"][0, 0], inp["decay_up"][0, 1]], 1), dtype=f)
    m["iclup"] = np.ascontiguousarray(np.concatenate([inp["iclr_up"][0, 0], inp["iclr_up"][0, 1]], 1), dtype=f)
    m["gateup"] = np.ascontiguousarray(inp["gate_up"][0], dtype=f)
    hh = np.repeat(np.arange(8), 16); tt = np.tile(np.arange(16), 8)
    same = hh[:, None] == hh[None, :]
    msf = (same & (tt[:, None] < tt[None, :])).astype(f); mif = (same & (tt[:, None] <= tt[None, :])).astype(f)
    msb = msf.T.copy(); mib = mif.T.copy()
    m["mk4"] = np.ascontiguousarray(np.concatenate([msf, mif, msf, mif, msb, mib, msb, mib], 1), dtype=f)
    m["mn1"] = np.ascontiguousarray(np.concatenate([msb, msf], 1), dtype=f)
    m["bm"] = np.ascontiguousarray((hh[:, None] == np.repeat(np.arange(8), 64)[None, :]).astype(f))
    rst = np.ones((64, 1024), f); rst[:, ::16] = 0.0
    m["rst"] = rst
    m["w_out"] = np.ascontiguousarray(inp["w_out"][0], dtype=f)
    m["lnx"] = np.ascontiguousarray(np.stack([inp["lnx_g"][0], inp["lnx_b"][0]]), dtype=f)
    m["ln1"] = np.ascontiguousarray(np.stack([inp["ln1_g"][0], inp["ln1_b"][0]]), dtype=f)
    m["ln2"] = np.ascontiguousarray(np.stack([inp["ln2_g"][0], inp["ln2_b"][0]]), dtype=f)
    m["router_w"] = np.ascontiguousarray(inp["router_w"][0], dtype=f)
    m["exp_w_gate"] = np.ascontiguousarray(inp["exp_w_gate"][0], dtype=f)
    m["exp_w_up"] = np.ascontiguousarray(inp["exp_w_up"][0], dtype=f)
    m["exp_w_down"] = np.ascontiguousarray(inp["exp_w_down"][0], dtype=f)
    iot = np.zeros((128, 516), f)
    iot[:, 0:512] = np.arange(512, dtype=f)[None, :]
    iot[:, 512:516] = np.arange(128, dtype=f)[:, None] + 128.0 * np.arange(4, dtype=f)[None, :]
    m["iot"] = iot
    m["ustr"] = np.triu(np.ones((128, 128), f), 1)
    return m


_NC_CACHE = {}


def kernel(**inputs):
    inp = {k: np.asarray(v) for k, v in inputs.items()}
    if "full" not in _NC_CACHE:
        _NC_CACHE["full"] = build_nc()[0]
    nc = _NC_CACHE["full"]
    in_maps = [host_inputs(inp, c // 2) for c in range(8)]
    res = run_bass_kernel_spmd(nc, in_maps, core_ids=list(range(8)))
    out = np.stack([res.results[2 * b]["out"] for b in range(4)], 0).astype(np.float32)
    return out
```

```python
import contextlib
import os
import numpy as np
import concourse.bass as bass
import concourse.mybir as mybir
from concourse.bass_utils import run_bass_kernel_spmd

F32 = mybir.dt.float32
BF16 = mybir.dt.bfloat16
AF = mybir.ActivationFunctionType
ALU = mybir.AluOpType
AX = mybir.AxisListType

D = 1024
TL = 4096
TC = 256
TT = TL + TC
NT = TT // 128
ALPHA = 2.0 ** 0.25
DEC_C = -float(np.exp(-0.5))


class Sched:
    CE = ('pe', 'act', 'dve', 'pool')

    def __init__(self, nc, stack, ndma=32):
        self.nc = nc
        self.ops = {e: [] for e in ('pe', 'act', 'dve', 'pool', 'sp')}
        self.cnt = {e: 0 for e in self.CE}
        self.last_w = {}
        self.readers = {}
        self.waited = {e: {} for e in self.ops}
        self.ndma = ndma
        self.dma_val = [0] * ndma
        self.dma_i = 0
        names = list(self.CE) + ['d%d' % i for i in range(ndma)]
        self.sems = {n: stack.enter_context(nc.semaphore('s_' + n)) for n in names}

    def _deps(self, eng, reads, writes):
        deps = {}

        def add(tok):
            if tok is None:
                return
            s, v = tok
            if deps.get(s, 0) < v:
                deps[s] = v
        for r in reads:
            add(self.last_w.get(r))
        for w in writes:
            add(self.last_w.get(w))
            for t in self.readers.get(w, ()):
                add(t)
        waits = []
        for s, v in deps.items():
            if s == eng and (eng == 'pe' or os.environ.get('K_NOSELF')):
                continue
            if self.waited[eng].get(s, 0) >= v:
                continue
            self.waited[eng][s] = v
            waits.append((s, v))
        return waits

    def _commit(self, tok, reads, writes):
        for r in reads:
            self.readers.setdefault(r, []).append(tok)
        for w in writes:
            self.last_w[w] = tok
            self.readers[w] = []

    capture = None
    pend = None

    def drain(self, lst, n):
        for _ in range(min(n, len(lst))):
            kind, a = lst.pop(0)
            (self.op if kind == 'op' else self.dma)(*a)

    def op(self, eng, fn, reads=(), writes=()):
        if self.capture is not None:
            self.capture.append(('op', (eng, fn, tuple(reads), tuple(writes))))
            return
        waits = self._deps(eng, reads, writes)
        self.cnt[eng] += 1
        tok = (eng, self.cnt[eng])
        self.ops[eng].append((waits, fn, (eng, 1)))
        self._commit(tok, reads, writes)

    def dma(self, fn, reads=(), writes=(), q='sp'):
        if self.capture is not None:
            self.capture.append(('dma', (fn, tuple(reads), tuple(writes), q)))
            return
        slot = self.dma_i % self.ndma
        self.dma_i += 1
        s = 'd%d' % slot
        waits = self._deps(q, reads, writes)
        pv = self.dma_val[slot]
        if pv > 0 and self.waited[q].get(s, 0) < pv:
            self.waited[q][s] = pv
            waits.append((s, pv))
        self.dma_val[slot] = pv + 16
        tok = (s, pv + 16)
        self.ops[q].append((waits, fn, (s, 16)))
        self._commit(tok, reads, writes)

    def barrier(self):
        allw = [(e, c) for e, c in self.cnt.items() if c > 0]
        allw += [('d%d' % i, v) for i, v in enumerate(self.dma_val) if v > 0]
        for eng in self.ops:
            waits = []
            for s, v in allw:
                if self.waited[eng].get(s, 0) >= v:
                    continue
                self.waited[eng][s] = v
                waits.append((s, v))
            if waits:
                self.ops[eng].append((waits, None, None))
        self.last_w = {}
        self.readers = {}

    def flush(self):
        nc = self.nc
        sems = self.sems
        ops = self.ops
        self.ops = {e: [] for e in ops}
        if os.environ.get('K_STATS'):
            print("FLUSH", {e: (len(v), sum(len(w[0]) for w in v)) for e, v in ops.items()})
        with nc.Block() as block:
            def run(engname, engobj):
                for waits, fn, inc in ops[engname]:
                    for ws, wv in waits:
                        engobj.wait_ge(sems[ws], wv)
                    if fn is not None:
                        ins = fn(engobj)
                        ins.then_inc(sems[inc[0]], inc[1])

            @block.sync
            def _(e):
                run('sp', e)

            @block.tensor
            def _(e):
                run('pe', e)

            @block.scalar
            def _(e):
                run('act', e)

            @block.vector
            def _(e):
                run('dve', e)

            @block.gpsimd
            def _(e):
                run('pool', e)


def build_nc(stop_after=99, dbg=False):
    nc = bass.Bass("TRN2", target_bir_lowering=False)

    def din(name, shape, dt=F32):
        return nc.dram_tensor(name, list(shape), dt, kind="ExternalInput").ap()

    def dscr(name, shape, dt=F32):
        return nc.dram_tensor(name, list(shape), dt, kind="Internal").ap()

    x_d = din("x", [TL, D]); ctx_d = din("ctx", [TC, D])
    cc_d = din("cc", [128, 16])
    wada_d = din("w_ada", [D, 6 * D]); bada_d = din("b_ada", [1, 6 * D])
    win_d = din("w_in", [D, 2560])
    qg_d = din("qg", [1, 512]); kg_d = din("kg", [1, 128])
    cos_d = din("cosT", [TL, 512]); sin_d = din("sinS", [TL, 512])
    ident_d = din("ident", [128, 128])
    rp_d = din("rp", [64, 8 * 10])
    lmu_d = din("lmu", [128, 3])
    decup_d = din("decup", [64, 2 * 512]); iclup_d = din("iclup", [64, 2 * 512]); gateup_d = din("gateup", [128, 512])
    mk4_d = din("mk4", [128, 2 * 512]); mn1_d = din("mn1", [128, 2 * 128]); bm_d = din("bm", [128, 512])
    rst_d = din("rst", [64, 1024])
    wout_d = din("w_out", [D, D]); lnx_d = din("lnx", [2, 512])
    ln1_d = din("ln1", [2, D]); ln2_d = din("ln2", [2, D])
    rw_d = din("router_w", [D, 16])
    x1D = dscr("x1D", [TL, D]); h2D = dscr("h2D", [TL, D], BF16)
    attD = dscr("attD", [128, 4 * TL], BF16)
    wg_d = din("exp_w_gate", [16, D, D]); wu_d = din("exp_w_up", [16, D, D]); wd_d = din("exp_w_down", [16, D, D])
    iot_d = din("iot", [128, 512 + 4]); ustr_d = din("ustr", [128, 128])
    ywD = dscr("ywD", [16, 512, D], BF16)
    ydir = dscr("ydir", [2, TL, 512]); bonD = dscr("bonD", [TL, 512]); gD = dscr("gD", [TL, 512])
    out_d = nc.dram_tensor("out", [TL, D], F32, kind="ExternalOutput").ap()
    modd = dscr("modd", [2, 6 * D])
    rwT = dscr("rwT", [1792, TT])
    dbg_outs = {}

    def dout(name, shape, dt=F32):
        ap = nc.dram_tensor(name, list(shape), dt, kind="ExternalOutput").ap()
        dbg_outs[name] = ap
        return ap

    with contextlib.ExitStack() as top:
        S = Sched(nc, top)

        def sb(stack, name, shape, dt):
            return stack.enter_context(nc.sbuf_tensor("sb_" + name, list(shape), dt))

        PS = top.enter_context(nc.psum_tensor("PS", [128, 8 * 512], F32))
        P = [PS[:, i * 512:(i + 1) * 512] for i in range(8)]
        PK = ['P%d' % i for i in range(8)]
        identf = sb(top, "identf", [128, 128], F32)
        identb = sb(top, "identb", [128, 128], BF16)
        eps5 = sb(top, "eps5", [128, 1], F32)
        eps6 = sb(top, "eps6", [128, 1], F32)
        S.dma(lambda e: e.dma_start(out=identf[:], in_=ident_d[:, :]), writes=['identf'])
        S.op('dve', lambda e: e.tensor_copy(out=identb[:], in_=identf[:]), reads=['identf'], writes=['identb'])
        S.op('pool', lambda e: e.memset(eps5[:], 1e-5), writes=['eps5'])
        S.op('pool', lambda e: e.memset(eps6[:], 1e-6), writes=['eps6'])

        def ln_stats(stack_tiles, src, key_src, eps_t, eps_key):
            stats, mv, rstd, nb, kq = stack_tiles
            for cch in range(2):
                S.op('dve', lambda e, cch=cch: e.bn_stats(out=stats[:, cch, :], in_=src[:, cch * 512:(cch + 1) * 512]),
                     reads=[key_src], writes=[kq + 'stats'])
            S.op('dve', lambda e: e.bn_aggr(out=mv[:], in_=stats[:]), reads=[kq + 'stats'], writes=[kq + 'mv'])
            S.op('act', lambda e: e.activation(out=rstd[:], in_=mv[:, 1:2], func=AF.Ln, bias=eps_t[:], scale=1.0),
                 reads=[kq + 'mv', eps_key], writes=[kq + 'rstd'])
            S.op('act', lambda e: e.activation(out=rstd[:], in_=rstd[:], func=AF.Exp, scale=-0.5),
                 reads=[kq + 'rstd'], writes=[kq + 'rstd'])
            S.op('dve', lambda e: e.scalar_tensor_tensor(out=nb[:], in0=mv[:, 0:1], scalar=-1.0, in1=rstd[:],
                                                        op0=ALU.mult, op1=ALU.mult),
                 reads=[kq + 'mv', kq + 'rstd'], writes=[kq + 'nb'])

        with contextlib.ExitStack() as ph:
            cc = sb(ph, "cc", [128, 8, 2], F32)
            ccs = sb(ph, "ccs", [128, 8, 2], F32)
            wst = [sb(ph, "wst%d" % i, [128, 8, 512], F32) for i in range(2)]
            bada = sb(ph, "bada", [2, 6 * D], F32)
            modrow = sb(ph, "modrow", [2, 6 * D], F32)
            S.dma(lambda e: e.dma_start(out=cc[:].rearrange("p a b -> p (a b)"), in_=cc_d[:, :]), writes=['cc'])
            S.dma(lambda e: e.dma_start(out=bada[:], in_=bada_d.partition_broadcast(2)), writes=['bada'])
            S.op('act', lambda e: e.activation(out=ccs[:], in_=cc[:], func=AF.Silu), reads=['cc'], writes=['ccs'])
            for g in range(12):
                w = wst[g % 2]
                wk = 'wst%d' % (g % 2)
                S.dma(lambda e, w=w, g=g: e.dma_start(
                    out=w[:], in_=wada_d[:, g * 512:(g + 1) * 512].rearrange("(j p) n -> p j n", p=128)), writes=[wk])
                for j in range(8):
                    S.op('pe', lambda e, w=w, j=j: e.matmul(P[0][0:2, :], lhsT=ccs[:, j, :], rhs=w[:, j, :],
                                                           start=(j == 0), stop=(j == 7)),
                         reads=['ccs', wk], writes=['P0'])
                S.op('dve', lambda e, g=g: e.tensor_tensor(out=modrow[:, g * 512:(g + 1) * 512], in0=P[0][0:2, :],
                                                          in1=bada[:, g * 512:(g + 1) * 512], op=ALU.add),
                     reads=['P0', 'bada'], writes=['modrow'])
            for lo in (1024, 4096):
                S.op('dve', lambda e, lo=lo: e.tensor_scalar_add(out=modrow[:, lo:lo + 1024], in0=modrow[:, lo:lo + 1024],
                                                                scalar1=1.0), reads=['modrow'], writes=['modrow'])
            S.dma(lambda e: e.dma_start(out=modd[:, :], in_=modrow[:]), reads=['modrow'], writes=['modd'])
            if dbg:
                o = dout("dbg_mod", [2, 6 * D])
                S.dma(lambda e: e.dma_start(out=o[:, :], in_=modrow[:]), reads=['modrow'])
            S.barrier()
            S.flush()
        if stop_after <= 0:
            return nc, dbg_outs

        def modrow_bc(dst, row, lo):
            S.dma(lambda e: e.dma_start(out=dst[:], in_=modd[row:row + 1, lo:lo + 1024].partition_broadcast(128)),
                  reads=['modd'], writes=[dst.name if hasattr(dst, 'name') else 'x'])

        aff_all = sb(top, "aff_all", [128, 32, 16], F32)
        with contextlib.ExitStack() as phAB:
            qT_all = sb(phAB, "qT_all", [128, 4, TL], BF16)
            kTd = sb(phAB, "kTd", [128, 2, TT], BF16)
            Vx = sb(phAB, "Vx", [128, NT, 2, 80], BF16)
            attT_all = sb(phAB, "attT_all", [128, 4, TL], BF16)
            with contextlib.ExitStack() as ph:
                winb = sb(ph, "winb", [128, 8, 2560], BF16)
                wstg = [sb(ph, "wstg%d" % i, [128, 640], F32) for i in range(2)]
                sc1p = sb(ph, "sc1p", [128, D], F32); sh1 = sb(ph, "sh1", [128, D], F32)
                csc1p = sb(ph, "csc1p", [128, D], F32); csh1 = sb(ph, "csh1", [128, D], F32)
                qg = sb(ph, "qg", [128, 512], F32); kg = sb(ph, "kg", [128, 128], F32)
                for dst, nm, row, lo in ((sh1, 'sh1', 0, 0), (sc1p, 'sc1p', 0, 1024), (csh1, 'csh1', 1, 0), (csc1p, 'csc1p', 1, 1024)):
                    S.dma(lambda e, dst=dst, row=row, lo=lo: e.dma_start(
                        out=dst[:], in_=modd[row:row + 1, lo:lo + 1024].partition_broadcast(128)), reads=['modd'], writes=[nm])
                S.dma(lambda e: e.dma_start(out=qg[:], in_=qg_d.partition_broadcast(128)), writes=['qg'])
                S.dma(lambda e: e.dma_start(out=kg[:], in_=kg_d.partition_broadcast(128)), writes=['kg'])
                for jj in range(32):
                    j = jj // 4; c0 = (jj % 4) * 640
                    w = wstg[jj % 2]; wk = 'wstg%d' % (jj % 2)
                    S.dma(lambda e, w=w, j=j, c0=c0: e.dma_start(out=w[:], in_=win_d[j * 128:(j + 1) * 128, c0:c0 + 640]), writes=[wk])
                    S.op('pool' if jj % 2 else 'dve', lambda e, w=w, j=j, c0=c0: e.tensor_copy(out=winb[:, j, c0:c0 + 640], in_=w[:]),
                         reads=[wk], writes=['winb'])
                S.op('pool', lambda e: e.memset(Vx[:], 1.0), writes=['Vx'])
                xt = [sb(ph, "xt%d" % i, [128, D], F32) for i in range(2)]
                xn = sb(ph, "xn", [128, D], F32)
                hb = sb(ph, "hb", [128, D], BF16)
                hT = sb(ph, "hT", [128, 8, 512], BF16)
                stats = sb(ph, "stats", [128, 2, 6], F32); mv = sb(ph, "mv", [128, 2], F32)
                rstd = sb(ph, "rstd", [128, 1], F32); nb = sb(ph, "nb", [128, 1], F32)
                lnt = (stats, mv, rstd, nb, 'A')
                rstg = [sb(ph, "rstg%d" % i, [128, 512], F32) for i in range(2)]
                cosT = sb(ph, "cosT", [128, 512], F32); sinS = sb(ph, "sinS", [128, 512], F32)
                sq = sb(ph, "sq", [128, 512], F32)
                ssum = sb(ph, "ssum", [128, 8], F32)
                qn = sb(ph, "qn", [128, 512], F32); qa = sb(ph, "qa", [128, 512], F32)
                qbt = sb(ph, "qbt", [128, 512], F32)
                qo = sb(ph, "qo", [128, 512], BF16)
                ko = sb(ph, "ko", [128, 2, 2, 64], BF16)
                blocks = [(0, 256)] + [(256 + 512 * i, 512) for i in range(8)]
                ti = 0
                for (u0, ntok) in blocks:
                    ntile = ntok // 128
                    is_ctx = (u0 == 0)
                    for tl in range(ntile):
                        u = u0 + tl * 128
                        gt = u // 128
                        X = xt[ti % 2]; xk = 'xt%d' % (ti % 2)
                        ti += 1
                        src = ctx_d[u:u + 128, :] if is_ctx else x_d[u - TC:u - TC + 128, :]
                        S.dma(lambda e, X=X, src=src: e.dma_start(out=X[:], in_=src), writes=[xk])
                        ln_stats(lnt, X, xk, eps5, 'eps5')
                        S.op('act', lambda e, X=X: e.activation(out=xn[:], in_=X[:], func=AF.Identity, bias=nb[:], scale=rstd[:]),
                             reads=[xk, 'Anb', 'Arstd'], writes=['xn'])
                        scp, shh, k1, k2 = (csc1p, csh1, 'csc1p', 'csh1') if is_ctx else (sc1p, sh1, 'sc1p', 'sh1')
                        S.op('pool', lambda e, scp=scp: e.tensor_tensor(out=xn[:], in0=xn[:], in1=scp[:], op=ALU.mult),
                             reads=['xn', k1], writes=['xn'])
                        S.op('dve', lambda e, shh=shh: e.tensor_tensor(out=hb[:], in0=xn[:], in1=shh[:], op=ALU.add),
                             reads=['xn', k2], writes=['hb'])
                        pTb = P[0][:].bitcast(BF16)
                        for j in range(8):
                            S.op('pe', lambda e, j=j, pTb=pTb: e.transpose(out=pTb[:, j * 128:(j + 1) * 128],
                                                                      in_=hb[:, j * 128:(j + 1) * 128], identity=identb[:]),
                                 reads=['hb', 'identb'], writes=['P0'])
                        S.op('act', lambda e, tl=tl, pTb=pTb: e.copy(out=hT[:, :, tl * 128:(tl + 1) * 128],
                                                                 in_=pTb.rearrange("p (j t) -> p j t", j=8)),
                             reads=['P0'], writes=['hT'])
                        for j in range(8):
                            S.op('pe', lambda e, j=j, tl=tl: e.matmul(P[1][:, :], lhsT=hT[:, j, tl * 128:(tl + 1) * 128],
                                                                     rhs=winb[:, j, 0:512], start=(j == 0), stop=(j == 7)),
                                 reads=['hT', 'winb'], writes=['P1'])
                        for j in range(8):
                            S.op('pe', lambda e, j=j, tl=tl: e.matmul(P[2][:, 0:256], lhsT=hT[:, j, tl * 128:(tl + 1) * 128],
                                                                     rhs=winb[:, j, 512:768], start=(j == 0), stop=(j == 7)),
                                 reads=['hT', 'winb'], writes=['P2'])
                        S.op('act', lambda e, gt=gt: e.copy(out=Vx[:, gt, :, 0:64],
                                                          in_=P[2][:, 128:256].rearrange("p (a b) -> p a b", a=2)),
                             reads=['P2'], writes=['Vx'])
                        if not is_ctx:
                            ul = u - TC
                            S.dma(lambda e, ul=ul: e.dma_start(out=cosT[:], in_=cos_d[ul:ul + 128, :]), writes=['cosT'])
                            S.dma(lambda e, ul=ul: e.dma_start(out=sinS[:], in_=sin_d[ul:ul + 128, :]), writes=['sinS'])

                        def normrope(psrc, pk, nh, gain, gk, do_rope, outfn):
                            w_ = nh * 64
                            S.op('act', lambda e: e.activation(out=sq[:, 0:w_], in_=psrc, func=AF.Square),
                                 reads=[pk], writes=['sq'])
                            S.op('dve', lambda e: e.tensor_reduce(out=ssum[:, 0:nh], in_=sq[:, 0:w_].rearrange("p (h d) -> p h d", d=64),
                                                                 axis=AX.X, op=ALU.add), reads=['sq'], writes=['ssum'])
                            S.op('act', lambda e: e.activation(out=ssum[:, 0:nh], in_=ssum[:, 0:nh], func=AF.Ln, bias=eps6[:], scale=1.0 / 64),
                                 reads=['ssum', 'eps6'], writes=['ssum'])
                            S.op('act', lambda e: e.activation(out=ssum[:, 0:nh], in_=ssum[:, 0:nh], func=AF.Exp, scale=-0.5),
                                 reads=['ssum'], writes=['ssum'])
                            S.op('dve', lambda e: e.tensor_tensor(out=qn[:, 0:w_].rearrange("p (h d) -> p h d", d=64),
                                                                 in0=psrc.rearrange("p (h d) -> p h d", d=64),
                                                                 in1=ssum[:, 0:nh].unsqueeze(2).to_broadcast([128, nh, 64]), op=ALU.mult),
                                 reads=[pk, 'ssum'], writes=['qn'])
                            if not do_rope:
                                S.op('pool', lambda e: outfn(e, qn[:, 0:w_], gain[:, 0:w_], ALU.mult), reads=['qn', gk], writes=['qko'])
                                return
                            S.op('pool', lambda e: e.tensor_tensor(out=qn[:, 0:w_], in0=qn[:, 0:w_], in1=gain[:, 0:w_], op=ALU.mult),
                                 reads=['qn', gk], writes=['qn'])
                            S.op('dve', lambda e: e.tensor_tensor(out=qa[:, 0:w_], in0=qn[:, 0:w_], in1=cosT[:, 0:w_], op=ALU.mult),
                                 reads=['qn', 'cosT'], writes=['qa'])
                            qv = qn[:, 0:w_].rearrange("p (g s q) -> p g s q", s=2, q=16)
                            bv = qbt[:, 0:w_].rearrange("p (g s q) -> p g s q", s=2, q=16)
                            sv = sinS[:, 0:w_].rearrange("p (g s q) -> p g s q", s=2, q=16)
                            for s_ in range(2):
                                S.op('pool', lambda e, s_=s_: e.tensor_tensor(out=bv[:, :, s_, :], in0=qv[:, :, 1 - s_, :],
                                                                             in1=sv[:, :, s_, :], op=ALU.mult),
                                     reads=['qn', 'sinS'], writes=['qbt'])
                            S.op('dve', lambda e: outfn(e, qa[:, 0:w_], qbt[:, 0:w_], ALU.add), reads=['qa', 'qbt'], writes=['qko'])

                        if not is_ctx:
                            normrope(P[1][:, :], 'P1', 8, qg, 'qg', True,
                                     lambda e, a, b_, op: e.tensor_tensor(out=qo[:], in0=a, in1=b_, op=op))
                            pq = P[3][:].bitcast(BF16)
                            for hp in range(4):
                                S.op('pe', lambda e, hp=hp, pq=pq: e.transpose(out=pq[:, hp * 128:(hp + 1) * 128],
                                                                          in_=qo[:, hp * 128:(hp + 1) * 128], identity=identb[:]),
                                     reads=['qko', 'identb'], writes=['P3'])
                            S.op('act', lambda e, ul=ul, pq=pq: e.copy(out=qT_all[:, :, ul:ul + 128],
                                                                   in_=pq[:, 0:512].rearrange("p (j t) -> p j t", j=4)),
                                 reads=['P3'], writes=['qT_all'])

                        def kout(e, a, b_, op):
                            return e.tensor_tensor(out=ko[:, :, 0, :], in0=a.rearrange("p (h d) -> p h d", d=64),
                                                   in1=b_.rearrange("p (h d) -> p h d", d=64), op=op)
                        normrope(P[2][:, 0:128], 'P2', 2, kg, 'kg', not is_ctx, kout)
                        S.op('pool', lambda e: e.tensor_copy(out=ko[:, :, 1, :], in_=ko[:, :, 0, :]), reads=['qko'], writes=['qko'])
                        pk_ = P[3][:].bitcast(BF16)
                        for kv in range(2):
                            S.op('pe', lambda e, kv=kv, pk_=pk_: e.transpose(
                                out=pk_[:, 512 + kv * 128:512 + (kv + 1) * 128],
                                in_=ko[:, kv, :, :].rearrange("p a d -> p (a d)"), identity=identb[:]),
                                reads=['qko', 'identb'], writes=['P3'])
                        S.op('act', lambda e, u=u, pk_=pk_: e.copy(out=kTd[:, :, u:u + 128],
                                                               in_=pk_[:, 512:768].rearrange("p (j t) -> p j t", j=2)),
                             reads=['P3'], writes=['kTd'])
                    for g in range(14):
                        pb = P[4 + g % 2]; pbk = 'P%d' % (4 + g % 2)
                        for j in range(8):
                            S.op('pe', lambda e, j=j, g=g, pb=pb, ntok=ntok: e.matmul(pb[:, 0:ntok], lhsT=winb[:, j, 768 + g * 128:768 + (g + 1) * 128],
                                                                         rhs=hT[:, j, 0:ntok], start=(j == 0), stop=(j == 7)),
                                 reads=['hT', 'winb'], writes=[pbk])
                        rs = rstg[g % 2]; rk = 'rstg%d' % (g % 2)
                        S.op('act' if g % 2 else 'dve',
                             (lambda e, rs=rs, pb=pb, ntok=ntok: e.copy(out=rs[:, 0:ntok], in_=pb[:, 0:ntok])) if g % 2 else
                             (lambda e, rs=rs, pb=pb, ntok=ntok: e.tensor_copy(out=rs[:, 0:ntok], in_=pb[:, 0:ntok])),
                             reads=[pbk], writes=[rk])
                        S.dma(lambda e, rs=rs, g=g, u0=u0, ntok=ntok: e.dma_start(out=rwT[g * 128:(g + 1) * 128, u0:u0 + ntok], in_=rs[:, 0:ntok]),
                              reads=[rk], writes=['rwT'])
                if dbg:
                    o1 = dout("dbg_qT", [128, 4 * TL], BF16)
                    S.dma(lambda e: e.dma_start(out=o1[:, :], in_=qT_all[:].rearrange("p a t -> p (a t)")), reads=['qT_all'])
                    o2 = dout("dbg_kTd", [128, 2 * TT], BF16)
                    S.dma(lambda e: e.dma_start(out=o2[:, :], in_=kTd[:].rearrange("p a t -> p (a t)")), reads=['kTd'])
                    o3 = dout("dbg_rwT", [1792, TT])
                    S.dma(lambda e: e.dma_start(out=o3[:, :], in_=rwT[:, :]), reads=['rwT'])
                S.barrier()
                S.flush()
            if stop_after <= 1:
                return nc, dbg_outs
            with contextlib.ExitStack() as ph:
                pT = [sb(ph, "pT%d" % i, [128, 512], BF16) for i in range(3)]
                rec = sb(ph, "rec", [128, 8], F32)
                atok = sb(ph, "atok", [128, 8, 64], BF16)
                pi = 0

                def emit_scores(q0, st, hp, pi):
                    sb0 = 2 * (hp % 2)
                    scb = PS[:, sb0 * 512:(sb0 + 2) * 512].rearrange("p (b n) -> p b n", b=2)
                    sck = 'P%d' % sb0
                    kvh = hp // 2
                    for hh in range(2):
                        S.op('pe', lambda e, hh=hh, hp=hp, scb=scb, kvh=kvh, st=st, q0=q0: e.matmul(
                            scb[:, hh, 0:256],
                            lhsT=kTd[hh * 64:(hh + 1) * 64, kvh, st * 128:(st + 1) * 128],
                            rhs=qT_all[hh * 64:(hh + 1) * 64, hp, q0:q0 + 256], start=True, stop=True),
                            reads=['kTd', 'qT_all'], writes=[sck])
                    pt = pT[pi % 3]; ptk = 'pT%d' % (pi % 3)
                    S.op('act', lambda e, pt=pt, scb=scb: e.activation(out=pt[:].rearrange('p (b n) -> p b n', b=2), in_=scb[:, :, 0:256], func=AF.Exp, scale=0.125),
                         reads=[sck], writes=[ptk])
                    return (st, hp, pt, ptk, kvh)

                def emit_pv(st, hp, pt, ptk, kvh):
                    for hh in range(2):
                        head = 2 * hp + hh
                        for qt in range(2):
                            ab = P[4 + 2 * qt + head // 4]; abk = 'P%d' % (4 + 2 * qt + head // 4)
                            c0 = (head % 4) * 65
                            S.op('pe', lambda e, ab=ab, c0=c0, pt=pt, hh=hh, qt=qt, st=st, kvh=kvh, head=head: e.matmul(
                                ab[:, c0:c0 + 65], lhsT=pt[:, hh * 256 + qt * 128:hh * 256 + (qt + 1) * 128],
                                rhs=Vx[:, st, kvh, 0:65], start=(st == 0 and head % 4 == 0), stop=(st == NT - 1 and head % 4 == 3)),
                                reads=[ptk, 'Vx'], writes=[abk])

                for g in range(int(os.environ.get('K_NG', 16))):
                    q0 = g * 256
                    pend_ = None
                    for st in range(NT):
                        for hp in range(4):
                            cur_ = emit_scores(q0, st, hp, pi)
                            pi += 1
                            if pend_ is not None:
                                emit_pv(*pend_)
                            pend_ = cur_
                    emit_pv(*pend_)
                    for qt in range(2 if not os.environ.get('K_SKIP_NORM') else 0):
                        for half in range(2):
                            ab = P[4 + 2 * qt + half]; abk = 'P%d' % (4 + 2 * qt + half)
                            av = ab[:, 0:260].rearrange("p (h c) -> p h c", c=65)
                            S.op('dve', lambda e, av=av, half=half: e.reciprocal(out=rec[:, half * 4:(half + 1) * 4], in_=av[:, :, 64]),
                                 reads=[abk], writes=['rec'])
                            S.op('dve', lambda e, av=av, half=half: e.tensor_tensor(
                                out=atok[:, half * 4:(half + 1) * 4, :], in0=av[:, :, 0:64],
                                in1=rec[:, half * 4:(half + 1) * 4].unsqueeze(2).to_broadcast([128, 4, 64]), op=ALU.mult),
                                reads=[abk, 'rec'], writes=['atok'])
                        pa = P[0][:].bitcast(BF16)
                        if os.environ.get('K_SKIP_TR'):
                            continue
                        for hp in range(4):
                            S.op('pe', lambda e, hp=hp, pa=pa: e.transpose(
                                out=pa[:, hp * 128:(hp + 1) * 128],
                                in_=atok[:, 2 * hp:2 * hp + 2, :].rearrange("p a d -> p (a d)"), identity=identb[:]),
                                reads=['atok', 'identb'], writes=['P0'])
                        if os.environ.get('K_SKIP_CP'):
                            continue
                        S.op('dve', lambda e, pa=pa, q0=q0, qt=qt: e.tensor_copy(
                            out=attT_all[:, :, q0 + qt * 128:q0 + (qt + 1) * 128],
                            in_=pa[:, 0:512].rearrange("p (j t) -> p j t", j=4)), reads=['P0'], writes=['attT_all'])
                S.dma(lambda e: e.dma_start(out=attD[:, :], in_=attT_all[:].rearrange("p a t -> p (a t)")), reads=['attT_all'], writes=['attD'])
                if dbg:
                    o1 = dout("dbg_attT", [128, 4 * TL], BF16)
                    S.dma(lambda e: e.dma_start(out=o1[:, :], in_=attT_all[:].rearrange("p a t -> p (a t)")), reads=['attT_all'])
                S.barrier()
                S.flush()
        if stop_after <= 2:
            return nc, dbg_outs

        def TTo(eng, out, a, b_, op, R, W):
            S.op(eng, lambda e: e.tensor_tensor(out=out, in0=a, in1=b_, op=op), reads=R, writes=W)

        def CP(eng, out, in_, R, W):
            if eng == 'act':
                S.op('act', lambda e: e.copy(out=out, in_=in_), reads=R, writes=W)
            else:
                S.op(eng, lambda e: e.tensor_copy(out=out, in_=in_), reads=R, writes=W)

        def ACTF(out, in_, func, R, W, scale=1.0, bias=None):
            if bias is None:
                S.op('act', lambda e: e.activation(out=out, in_=in_, func=func, scale=scale), reads=R, writes=W)
            else:
                S.op('act', lambda e: e.activation(out=out, in_=in_, func=func, scale=scale, bias=bias), reads=R, writes=W)

        def MM(out, lhsT, rhs, R, W, start=True, stop=True):
            S.op('pe', lambda e: e.matmul(out, lhsT=lhsT, rhs=rhs, start=start, stop=stop), reads=R, writes=W)

        def TR(out, in_, idn, R, W):
            S.op('pe', lambda e: e.transpose(out=out, in_=in_, identity=idn), reads=R, writes=W)

        with contextlib.ExitStack() as ph:
            def t32(name, shape=(64, 1024)):
                return sb(ph, name, list(shape), F32)
            rp = t32("rp", (64, 8, 10)); rpd = t32("rpd", (64, 8, 8))
            lmu = t32("lmu", (128, 3)); lmd = t32("lmd", (128, 6))
            decupb = sb(ph, "decupb", [64, 2, 512], BF16); iclupb = sb(ph, "iclupb", [64, 2, 512], BF16)
            gateupb = sb(ph, "gateupb", [128, 512], BF16)
            mk4 = t32("mk4", (128, 2, 512)); mn1 = t32("mn1", (128, 2, 128)); bm = t32("bm", (128, 512))
            rst = t32("rst"); ones64 = sb(ph, "ones64", [64, 64], BF16); tiny = t32("tiny", (64, 1))
            S.dma(lambda e: e.dma_start(out=rp[:].rearrange("k h n -> k (h n)"), in_=rp_d[:, :]), writes=['rp'])
            S.dma(lambda e: e.dma_start(out=lmu[:], in_=lmu_d[:, :]), writes=['lmu'])
            S.dma(lambda e: e.dma_start(out=mk4[:].rearrange("p a n -> p (a n)"), in_=mk4_d[:, :]), writes=['mk4'])
            S.dma(lambda e: e.dma_start(out=mn1[:].rearrange("p a n -> p (a n)"), in_=mn1_d[:, :]), writes=['mn1'])
            S.dma(lambda e: e.dma_start(out=bm[:], in_=bm_d[:, :]), writes=['bm'])
            S.dma(lambda e: e.dma_start(out=rst[:], in_=rst_d[:, :]), writes=['rst'])
            S.op('pool', lambda e: e.memset(ones64[:], 1.0), writes=['ones64'])
            S.op('pool', lambda e: e.memset(tiny[:], 1e-24), writes=['tiny'])
            for i in range(3):
                S.op('dve', lambda e, i=i: e.tensor_scalar(out=rpd[:, :, 2 * i], in0=rp[:, :, i], scalar1=0.5, scalar2=None, op0=ALU.mult),
                     reads=['rp'], writes=['rpd'])
                S.op('dve', lambda e, i=i: e.tensor_scalar(out=rpd[:, :, 2 * i + 1], in0=rp[:, :, i], scalar1=-1.0, scalar2=1.0,
                                                          op0=ALU.mult, op1=ALU.add), reads=['rp'], writes=['rpd'])
                S.op('dve', lambda e, i=i: e.tensor_scalar(out=lmd[:, 2 * i:2 * i + 1], in0=lmu[:, i:i + 1], scalar1=0.5, scalar2=None, op0=ALU.mult),
                     reads=['lmu'], writes=['lmd'])
                S.op('dve', lambda e, i=i: e.tensor_scalar(out=lmd[:, 2 * i + 1:2 * i + 2], in0=lmu[:, i:i + 1], scalar1=-1.0, scalar2=1.0,
                                                          op0=ALU.mult, op1=ALU.add), reads=['lmu'], writes=['lmd'])
            S.op('dve', lambda e: e.tensor_scalar(out=rpd[:, :, 6], in0=rp[:, :, 4], scalar1=-1.0, scalar2=1.0, op0=ALU.mult, op1=ALU.add),
                 reads=['rp'], writes=['rpd'])

            def bc(t2):
                return t2.unsqueeze(2).to_broadcast([64, 8, 128])

            pin = [sb(ph, "pin%d" % i, [64, 8, 130], F32) for i in range(3)]
            plo = [sb(ph, "plo%d" % i, [128, 130], F32) for i in range(3)]
            tS = t32("tS"); xr = t32("xr"); xk = t32("xk"); xv = t32("xv")
            xlo = t32("xlo", (128, 3, 128)); twl = sb(ph, "twl", [64, 128], BF16); xalb = sb(ph, "xalb", [64, 128], BF16)
            glsb = sb(ph, "glsb", [128, 128], BF16)
            sqb = sb(ph, "sqb", [64, 1024], BF16); kk = t32("kk")
            sg = t32("sg"); ad = t32("ad"); cs = t32("cs"); ex = t32("ex"); E1 = t32("E1"); E2s = [t32("E2_0"), t32("E2_1")]; E3 = t32("E3")
            bb = t32("bb"); t1 = t32("t1"); kd = bb; bt32 = t32("bt32"); kt32 = t32("kt32"); rkb = sqb; kkk = t1; rs = E1; csb = bt32; bv = kt32
            bvt = t32("bvt", (128, 512)); gtok = t32("gtok", (128, 512)); glS = t32("glS", (128, 128))
            opTs = [{n: sb(ph, "op%d_" % q_ + n, [64, 1024], BF16) for n in ("a", "r", "b", "k", "bh", "kh", "v")} for q_ in range(2)]
            N1Ta, N1a, IN1Ta, N2a, N2Ta, IN2Ta, N4a, N4Ta, IN4Ta, IN8Ta, AakTa, ArbTa, ArkTa = [
                sb(ph, "ba%d" % i, [128, 8, 128], BF16) for i in range(13)]
            TMa = sb(ph, "TMa", [128, 8, 5, 64], BF16)
            Xa = [sb(ph, "Xa%d" % i, [128, 8, 128], BF16) for i in range(2)]
            Bbd = sb(ph, "Bbd", [128, 512], BF16); Ubd = sb(ph, "Ubd", [128, 512], BF16); Vbd = sb(ph, "Vbd", [128, 512], BF16)
            GTs = sb(ph, "GTs", [64, 512], BF16); Es = t32("Es", (64, 512)); QTs = sb(ph, "QTs", [64, 128], BF16)
            Y0s = t32("Y0s", (128, 64)); ytmp = gtok; yc = t32("yc", (128, 64))
            Yblk = t32("Yblk", (128, 8, 64))
            H = t32("H", (64, 512)); Hb = sb(ph, "Hb", [64, 512], BF16); Ht = t32("Ht", (64, 512))

            def view_hct(t):
                return t[:, :]

            def view_out(t):
                return t[:, :]

            def g16(t):
                return t[:, :].rearrange("k (g t) -> k g t", t=16)

            def chv(t, c):
                return t[:, :].rearrange("k (h c t) -> k h c t", h=8, c=8)[:, :, c, :]

            for (dst, src, nm, rows) in ((decupb, decup_d, 'decupb', 64), (iclupb, iclup_d, 'iclupb', 64), (gateupb, gateup_d, 'gateupb', 128)):
                stg_, sk_ = (bt32, 'bt32') if rows == 64 else (bvt, 'bvt')
                S.dma(lambda e, src=src, stg_=stg_: e.dma_start(out=stg_[:, :], in_=src[:, :]), writes=[sk_])
                dv = dst[:].rearrange("p a n -> p (a n)") if rows == 64 else dst[:]
                CP('dve', dv, stg_[:, :], [sk_], [nm])
            blocks = []
            nblk = int(os.environ.get('K_RBLK', 99))
            for d_ in range(2):
                cb_ = [0, 1] if d_ == 0 else [1, 0]
                lb_ = list(range(2, NT)) if d_ == 0 else list(range(NT - 1, 1, -1))
                blocks += [(d_, b_, j_ == 0) for j_, b_ in enumerate((cb_ + lb_)[:nblk])]

            def emit_prep(idx):
                d, blk, first = blocks[idx]
                opT = opTs[idx % 2]; E2 = E2s[idx % 2]; e2k = 'E2_%d' % (idx % 2); opk = 'o%d_' % (idx % 2)

                u0 = blk * 128
                is_ctx = blk < 2
                seq_lo, seq_hi = (0, TC) if is_ctx else (TC, TT)
                lo = max(u0 - 1, seq_lo); hi = min(u0 + 129, seq_hi)
                c_lo = lo - (u0 - 1); c_hi = c_lo + (hi - lo)
                for i in range(3):
                    if c_lo > 0:
                        S.op('pool', lambda e, i=i: e.memset(pin[i][:, :, 0:1], 0.0), writes=['pin%d' % i])
                    if c_hi < 130:
                        S.op('pool', lambda e, i=i: e.memset(pin[i][:, :, 129:130], 0.0), writes=['pin%d' % i])
                    S.dma(lambda e, i=i, lo=lo, hi=hi, c_lo=c_lo, c_hi=c_hi: e.dma_start(
                        out=pin[i][:, :, c_lo:c_hi],
                        in_=rwT[i * 512:(i + 1) * 512, lo:hi].rearrange("(h k) t -> k h t", k=64)), reads=['rwT'], writes=['pin%d' % i])
                for i, (r0, nr) in enumerate(((1536, 64), (1600, 64), (1664, 128))):
                    if c_lo > 0:
                        S.op('pool', lambda e, i=i: e.memset(plo[i][:, 0:1], 0.0), writes=['plo%d' % i])
                    if c_hi < 130:
                        S.op('pool', lambda e, i=i: e.memset(plo[i][:, 129:130], 0.0), writes=['plo%d' % i])
                    S.dma(lambda e, i=i, r0=r0, nr=nr, lo=lo, hi=hi, c_lo=c_lo, c_hi=c_hi: e.dma_start(
                        out=plo[i][0:nr, c_lo:c_hi], in_=rwT[r0:r0 + nr, lo:hi]), reads=['rwT'], writes=['plo%d' % i])
                for i, xo in enumerate((xr, xk, xv)):
                    xo3 = xo[:, :].rearrange("k (h t) -> k h t", h=8)
                    ts3 = tS[:, :].rearrange("k (h t) -> k h t", h=8)
                    TTo('pool', ts3, pin[i][:, :, 0:128], pin[i][:, :, 2:130], ALU.add, ['pin%d' % i], ['tS'])
                    TTo('pool', ts3, ts3, bc(rpd[:, :, 2 * i]), ALU.mult, ['tS', 'rpd'], ['tS'])
                    TTo('dve', xo3, pin[i][:, :, 1:129], bc(rpd[:, :, 2 * i + 1]), ALU.mult, ['pin%d' % i, 'rpd'], ['x%d' % i])
                    TTo('dve', xo3, xo3, ts3, ALU.add, ['x%d' % i, 'tS'], ['x%d' % i])
                for i, nr in enumerate((64, 64, 128)):
                    S.op('pool', lambda e, i=i, nr=nr: e.tensor_tensor(out=tS[0:nr, 0:128] if nr == 64 else glS[:, 0:128],
                                                                     in0=plo[i][0:nr, 0:128], in1=plo[i][0:nr, 2:130], op=ALU.add),
                         reads=['plo%d' % i], writes=['tS' if nr == 64 else 'glS'])
                    S.op('dve', lambda e, i=i, nr=nr: e.tensor_scalar(out=xlo[0:nr, i, :], in0=plo[i][0:nr, 1:129],
                                                                    scalar1=lmd[0:nr, 2 * i + 1:2 * i + 2], scalar2=None, op0=ALU.mult),
                         reads=['plo%d' % i, 'lmd'], writes=['xlo'])
                    S.op('dve', lambda e, i=i, nr=nr: e.scalar_tensor_tensor(
                        out=xlo[0:nr, i, :], in0=(tS[0:nr, 0:128] if nr == 64 else glS[:, 0:128]), scalar=lmd[0:nr, 2 * i:2 * i + 1],
                        in1=xlo[0:nr, i, :], op0=ALU.mult, op1=ALU.add),
                        reads=['tS' if nr == 64 else 'glS', 'lmd', 'xlo'], writes=['xlo'])
                ACTF(twl[:], xlo[0:64, 0, :], AF.Tanh, ['xlo'], ['twl'])
                CP('pool', xalb[:], xlo[0:64, 1, :], ['xlo'], ['xalb'])
                k3 = lambda t: t[:, :].rearrange("k (h t) -> k h t", h=8)
                TTo('pool', k3(kkk), k3(xk), bc(rp[:, :, 3]), ALU.mult, ['x1', 'rp'], ['t1'])
                ACTF(sqb[:], kkk[:], AF.Square, ['t1'], ['sqb'])
                PP = PS[0:64, 0:1024]
                for hf in range(2):
                    MM(PS[0:64, hf * 512:(hf + 1) * 512], ones64[:], sqb[:, hf * 512:(hf + 1) * 512], ['ones64', 'sqb'], ['P0'])
                ACTF(rs[:], PP, AF.Ln, ['P0', 'tiny'], ['E1'], bias=tiny[:])
                ACTF(rs[:], rs[:], AF.Exp, ['E1'], ['E1'], scale=-0.5)
                TTo('dve', kk[:], kkk[:], rs[:], ALU.mult, ['t1', 'E1'], ['kk'])
                for h in range(8):
                    MM(PS[0:64, h * 128:(h + 1) * 128], decupb[:, d, h * 64:(h + 1) * 64], twl[:], ['decupb', 'twl'], ['P0'])
                TTo('dve', k3(sg), PP.rearrange("k (h t) -> k h t", h=8), bc(rp[:, :, 6 + d]), ALU.add, ['P0', 'rp'], ['sg'])
                ACTF(sg[:], sg[:], AF.Sigmoid, ['sg'], ['sg'])
                for h in range(8):
                    MM(PS[0:64, h * 128:(h + 1) * 128], iclupb[:, d, h * 64:(h + 1) * 64], xalb[:], ['iclupb', 'xalb'], ['P0'])
                TTo('dve', k3(ad), PP.rearrange("k (h t) -> k h t", h=8), bc(rp[:, :, 8 + d]), ALU.add, ['P0', 'rp'], ['ad'])
                ACTF(ad[:], ad[:], AF.Sigmoid, ['ad'], ['ad'])
                S.op('dve', lambda e: e.tensor_tensor_scan(out=cs[:], data0=rst[:], data1=sg[:], initial=0.0, op0=ALU.mult, op1=ALU.add),
                     reads=['rst', 'sg'], writes=['cs'])
                csf = cs
                if d == 1:
                    TTo('pool', ex[:], sg[:], cs[:], ALU.subtract, ['sg', 'cs'], ['ex'])
                    csv = cs[:, :].rearrange("k (g t) -> k g t", t=16)
                    TTo('dve', csb[:, :].rearrange("k (g t) -> k g t", t=16), ex[:, :].rearrange("k (g t) -> k g t", t=16),
                       csv[:, :, 15:16].to_broadcast([64, 64, 16]), ALU.add, ['ex', 'cs'], ['bt32'])
                    csf = csb
                ACTF(E2[:], csf[:], AF.Exp, ['cs', 'bt32'], [e2k], scale=DEC_C)
                TTo('pool', ex[:], csf[:], sg[:], ALU.subtract, ['cs', 'bt32', 'sg'], ['ex'])
                ACTF(E1[:], ex[:], AF.Exp, ['ex'], ['E1'], scale=DEC_C)
                S.op('dve', lambda e: e.reciprocal(out=E3[:], in_=E2[:]), reads=[e2k], writes=['E3'])
                tsel = 15 if d == 0 else 0
                def cm(t, h):
                    return t[:, :].rearrange("k (c h t) -> k c h t", c=8, h=8)[:, :, h, :]

                def hm(t, h):
                    return t[:, :].rearrange("k (h c t) -> k h c t", h=8, c=8)[:, h, :, :]
                TTo('pool', bb[:], kk[:], ad[:], ALU.mult, ['kk', 'ad'], ['bb'])
                TTo('dve', bt32[:], bb[:], E3[:], ALU.mult, ['bb', 'E3'], ['bt32'])
                TTo('pool', k3(t1), k3(ad), bc(rp[:, :, 4]), ALU.mult, ['ad', 'rp'], ['t1'])
                TTo('dve', k3(t1), k3(t1), bc(rpd[:, :, 6]), ALU.add, ['t1', 'rpd'], ['t1'])
                TTo('pool', kd[:], xk[:], t1[:], ALU.mult, ['x1', 't1'], ['bb'])
                TTo('dve', kt32[:], kd[:], E3[:], ALU.mult, ['bb', 'E3'], ['kt32'])
                for h in range(8):
                    pch = hm(E2, h)[:, :, tsel:tsel + 1].to_broadcast([64, 8, 16])
                    S.op('dve', lambda e, h=h: e.scalar_tensor_tensor(out=cm(opT['a'], h), in0=hm(kk, h), scalar=-1.0, in1=hm(E1, h),
                                                                     op0=ALU.mult, op1=ALU.mult), reads=['kk', 'E1'], writes=[opk + 'a'])
                    TTo('pool', cm(opT['r'], h), hm(xr, h), hm(E2, h), ALU.mult, ['x0', e2k], [opk + 'r'])
                    CP('act', cm(opT['b'], h), hm(bt32, h), ['bt32'], [opk + 'b'])
                    TTo('pool', cm(opT['bh'], h), hm(bt32, h), pch, ALU.mult, ['bt32', e2k], [opk + 'bh'])
                    CP('act', cm(opT['k'], h), hm(kt32, h), ['kt32'], [opk + 'k'])
                    TTo('dve', cm(opT['kh'], h), hm(kt32, h), pch, ALU.mult, ['kt32', e2k], [opk + 'kh'])
                    CP('act', cm(opT['v'], h), hm(xv, h), ['x2'], [opk + 'v'])
                if d == 0 and not is_ctx:
                    ul = u0 - TC
                    TTo('pool', k3(t1), k3(xr), bc(rp[:, :, 5]), ALU.mult, ['x0', 'rp'], ['t1'])
                    TTo('dve', rkb[:], t1[:], xk[:], ALU.mult, ['t1', 'x1'], ['sqb'])
                    for hf in range(2):
                        MM(PS[0:64, hf * 512:(hf + 1) * 512], ones64[:], rkb[:, hf * 512:(hf + 1) * 512], ['ones64', 'sqb'], ['P0'])
                    TTo('dve', bv[:], PP, xv[:], ALU.mult, ['P0', 'x2'], ['kt32'])
                    for h in range(8):
                        TR(PS[:, 512 + h * 64:512 + (h + 1) * 64], bv[:, h * 128:(h + 1) * 128], identf[0:64, 0:64], ['kt32', 'identf'], ['P0'])
                    CP('dve', bvt[:], PS[:, 512:1024], ['P0'], ['bvt'])
                    S.dma(lambda e, ul=ul: e.dma_start(out=bonD[ul:ul + 128, :], in_=bvt[:]), reads=['bvt'], writes=['bonD'])
                    ACTF(glsb[:], xlo[:, 2, :], AF.Sigmoid, ['xlo'], ['glsb'])
                    MM(PS[:, 512:1024], glsb[:], gateupb[:], ['glsb', 'gateupb'], ['P0'])
                    CP('dve', gtok[:], PS[:, 512:1024], ['P0'], ['gtok'])
                    S.dma(lambda e, ul=ul: e.dma_start(out=gD[ul:ul + 128, :], in_=gtok[:]), reads=['gtok'], writes=['gD'])

            def emit_chunks(idx):
                d, blk, first = blocks[idx]
                opT = opTs[idx % 2]; E2 = E2s[idx % 2]; e2k = 'E2_%d' % (idx % 2); opk = 'o%d_' % (idx % 2)
                u0 = blk * 128
                is_ctx = blk < 2
                tsel = 15 if d == 0 else 0
                if first:
                    S.op('pool', lambda e: e.memset(H[:], 0.0), writes=['H'])
                    S.op('pool', lambda e: e.memset(Hb[:], 0.0), writes=['Hb'])

                PA_, PB_, PC_, PD_ = (PS[:, 0:1024], PS[:, 1024:2048], PS[:, 2048:3072], PS[:, 3072:4096])
                kPB, kPC, kPD = ['P2', 'P3'], ['P4', 'P5'], ['P6', 'P7']
                c8 = lambda ap: ap.rearrange("p (c n) -> p c n", c=8)
                def bc8(m):
                    return m.unsqueeze(1).to_broadcast([128, 8, 128])
                MSd = mk4[:, d, 0:128]; MId = mk4[:, d, 128:256]; MStd = mn1[:, d, :]
                def ch(name, c):
                    return opT[name][:, c * 128:(c + 1) * 128]
                for c in range(8):
                    MM(PB_[:, c * 128:(c + 1) * 128], ch('b', c), ch('a', c), [opk + 'b', opk + 'a'], kPB)
                for c in range(8):
                    MM(PC_[:, c * 128:(c + 1) * 128], ch('a', c), ch('b', c), [opk + 'a', opk + 'b'], kPC)
                for c in range(8):
                    MM(PD_[:, c * 128:(c + 1) * 128], ch('k', c), ch('a', c), [opk + 'k', opk + 'a'], kPD)
                TTo('dve', N1Ta[:], c8(PB_), bc8(MSd), ALU.mult, kPB + ['mk4'], ['N1Ta'])
                TTo('dve', N1a[:], c8(PC_), bc8(MStd), ALU.mult, kPC + ['mn1'], ['N1a'])
                TTo('dve', AakTa[:], c8(PD_), bc8(MSd), ALU.mult, kPD + ['mk4'], ['AakTa'])
                TTo('pool', IN1Ta[:], N1Ta[:], bc8(identb[:, :]), ALU.add, ['N1Ta', 'identb'], ['IN1Ta'])
                PBb = PB_.bitcast(BF16)
                for c in range(8):
                    for si, nm_ in ((0, 'a'), (1, 'v'), (2, 'bh'), (3, 'kh')):
                        TR(PBb[:, c * 256 + si * 64:c * 256 + (si + 1) * 64], ch(nm_, c), identb[0:64, 0:64], [opk + nm_, 'identb'], kPB)
                PBb4 = PBb.rearrange("p (c s n) -> p c s n", c=8, s=4)
                CP('dve', TMa[:, :, 0, :], PBb4[:, :, 0, :], kPB, ['TMa'])
                CP('dve', TMa[:, :, 2:5, :].rearrange("p c s n -> p c (s n)"), PBb.rearrange("p (c n) -> p c n", c=8)[:, :, 64:256], kPB, ['TMa'])
                for c in range(8):
                    MM(PC_[:, c * 128:(c + 1) * 128], N1Ta[:, c, :], N1a[:, c, :], ['N1Ta', 'N1a'], kPC)
                for c in range(8):
                    MM(PD_[:, c * 128:(c + 1) * 128], N1a[:, c, :], N1Ta[:, c, :], ['N1Ta', 'N1a'], kPD)
                CP('act', N2a[:], c8(PC_), kPC, ['N2a'])
                CP('dve', N2Ta[:], c8(PD_), kPD, ['N2Ta'])
                TTo('pool', IN2Ta[:], N2Ta[:], bc8(identb[:, :]), ALU.add, ['N2Ta', 'identb'], ['IN2Ta'])
                for c in range(8):
                    MM(PB_[:, c * 64:(c + 1) * 64], AakTa[:, c, :], TMa[:, c, 2, :], ['AakTa', 'TMa'], kPB)
                CP('dve', TMa[:, :, 1, :], PB_[:, 0:512].rearrange("p (c n) -> p c n", c=8), kPB, ['TMa'])
                for c in range(8):
                    MM(PC_[:, c * 128:(c + 1) * 128], N2Ta[:, c, :], N2a[:, c, :], ['N2Ta', 'N2a'], kPC)
                for c in range(8):
                    MM(PD_[:, c * 128:(c + 1) * 128], N2a[:, c, :], N2Ta[:, c, :], ['N2Ta', 'N2a'], kPD)
                CP('act', N4a[:], c8(PC_), kPC, ['N4a'])
                CP('dve', N4Ta[:], c8(PD_), kPD, ['N4Ta'])
                TTo('pool', IN4Ta[:], N4Ta[:], bc8(identb[:, :]), ALU.add, ['N4Ta', 'identb'], ['IN4Ta'])
                for c in range(8):
                    MM(PB_[:, c * 128:(c + 1) * 128], N4a[:, c, :], N4Ta[:, c, :], ['N4a', 'N4Ta'], kPB)
                TTo('dve', IN8Ta[:], c8(PB_), bc8(identf[:, :]), ALU.add, kPB + ['identf'], ['IN8Ta'])
                if not is_ctx:
                    for c in range(8):
                        MM(PC_[:, c * 128:(c + 1) * 128], ch('b', c), ch('r', c), [opk + 'b', opk + 'r'], kPC)
                    for c in range(8):
                        MM(PD_[:, c * 128:(c + 1) * 128], ch('k', c), ch('r', c), [opk + 'k', opk + 'r'], kPD)
                    TTo('dve', ArbTa[:], c8(PC_), bc8(MId), ALU.mult, kPC + ['mk4'], ['ArbTa'])
                    TTo('dve', ArkTa[:], c8(PD_), bc8(MId), ALU.mult, kPD + ['mk4'], ['ArkTa'])
                xsrc = None
                for li, (INa, ik) in enumerate(((IN8Ta, 'IN8Ta'), (IN4Ta, 'IN4Ta'), (IN2Ta, 'IN2Ta'), (IN1Ta, 'IN1Ta'))):
                    Pq, kq = (PB_, kPB) if li % 2 == 0 else (PC_, kPC)
                    for c in range(8):
                        rhs_ = TMa[:, c, 0:2, :].rearrange("p a n -> p (a n)") if xsrc is None else xsrc[:, c, :]
                        MM(Pq[:, c * 128:(c + 1) * 128], INa[:, c, :], rhs_, [ik, 'TMa' if xsrc is None else xk_], kq)
                    Xn = Xa[li % 2]; xk_ = 'Xa%d' % (li % 2)
                    CP('dve' if li % 2 else 'act', Xn[:], c8(Pq), kq, [xk_])
                    xsrc = Xn
                B3 = PS[:, 1536:2048]; B4 = PS[:, 2048:2560]; B5 = PS[:, 2560:3072]; B6 = PS[:, 3072:3584]; B7 = PS[:, 3584:4096]
                bm3 = bm[:, :].rearrange("p (h n) -> p h n", h=8)
                nch = int(os.environ.get('K_RCH', 8))
                for c in (list(range(8)) if d == 0 else list(range(7, -1, -1)))[:nch]:
                    Wc = xsrc[:, c, 0:64]; U0 = xsrc[:, c, 64:128]
                    Bh_ = TMa[:, c, 3, :]; Kh_ = TMa[:, c, 4, :]; Vt_ = TMa[:, c, 2, :]
                    for dst_, src_, rk_, wk_ in ((Bbd, Bh_, 'TMa', 'Bbd'), (Ubd, U0, xk_, 'Ubd'), (Vbd, Vt_, 'TMa', 'Vbd')):
                        TTo('pool', dst_[:, :].rearrange("p (h n) -> p h n", h=8), src_.unsqueeze(1).to_broadcast([128, 8, 64]), bm3, ALU.mult,
                            [rk_, 'bm'], [wk_])
                    MM(B4[0:64, :], Wc, Bbd[:], [xk_, 'Bbd'], ['P4'])
                    CP('act', GTs[:], B4[0:64, :], ['P4'], ['GTs'])
                    if not is_ctx:
                        MM(B6[0:64, 0:128], Wc, ArbTa[:, c, :], [xk_, 'ArbTa'], ['P6'])
                        TTo('dve', QTs[:], B6[0:64, 0:128], ch('r', c), ALU.add, ['P6', opk + 'r'], ['QTs'])
                        MM(B6[:, 128:192], ArbTa[:, c, :], U0, ['ArbTa', xk_], ['P6'], start=True, stop=False)
                        MM(B6[:, 128:192], ArkTa[:, c, :], Vt_, ['ArkTa', 'TMa'], ['P6'], start=False, stop=True)
                        CP('dve', Y0s[:], B6[:, 128:192], ['P6'], ['Y0s'])
                        MM(B7, QTs[:], Hb[:], ['QTs', 'Hb'], ['P7'])
                        TTo('dve', ytmp[:], B7, bm[:], ALU.mult, ['P7', 'bm'], ['gtok'])
                        S.op('dve', lambda e: e.tensor_reduce(out=yc[:], in_=ytmp[:, :].rearrange("p (h v) -> p v h", h=8), axis=AX.X, op=ALU.add),
                             reads=['gtok'], writes=['yc'])
                        TTo('pool', Yblk[:, c, :], yc[:], Y0s[:], ALU.add, ['yc', 'Y0s'], ['Yblk'])
                    MM(B3[0:64, :], Bh_, Ubd[:], ['TMa', 'Ubd'], ['P3'], start=True, stop=False)
                    MM(B3[0:64, :], Kh_, Vbd[:], ['TMa', 'Vbd'], ['P3'], start=False, stop=False)
                    for h in range(8):
                        MM(B3[0:64, h * 64:(h + 1) * 64], GTs[:, h * 64:(h + 1) * 64], Hb[:, h * 64:(h + 1) * 64], ['GTs', 'Hb'], ['P3'],
                           start=False, stop=(h == 7))
                    PCc = chv(E2, c)[:, :, tsel:tsel + 1].to_broadcast([64, 8, 64])
                    TTo('pool', Ht[:, :].rearrange("k (h v) -> k h v", h=8), H[:, :].rearrange("k (h v) -> k h v", h=8), PCc, ALU.mult,
                        ['H', e2k], ['Ht'])
                    TTo('dve', Hb[:], Ht[:], B3[0:64, :], ALU.add, ['Ht', 'P3'], ['Hb'])
                    TTo('dve', H[:], Ht[:], B3[0:64, :], ALU.add, ['Ht', 'P3'], ['H'])
                    S.drain(S.pend, (len(S.pend) + 7) // 8)
                if not is_ctx:
                    ul = u0 - TC
                    for h in range(8):
                        S.dma(lambda e, h=h, ul=ul, d=d: e.dma_start(
                            out=ydir[d, ul:ul + 128, h * 64:(h + 1) * 64].rearrange("(c t) v -> t c v", t=16),
                            in_=Yblk[h * 16:(h + 1) * 16, :, :]), reads=['Yblk'], writes=['ydir'])

            S.capture = []
            emit_prep(0)
            pend = S.capture; S.capture = None
            S.drain(pend, len(pend))
            for idx in range(len(blocks)):
                pend = []
                if idx + 1 < len(blocks):
                    S.capture = []
                    emit_prep(idx + 1)
                    pend = S.capture; S.capture = None
                S.pend = pend
                emit_chunks(idx)
                S.drain(pend, len(pend))

            if dbg:
                oy = dout("dbg_y", [2 * TL, 512]); ob = dout("dbg_bon", [TL, 512]); og = dout("dbg_g", [TL, 512])
                S.dma(lambda e: e.dma_start(out=oy[:, :], in_=ydir.rearrange("d t n -> (d t) n")), reads=['ydir'])
                S.dma(lambda e: e.dma_start(out=ob[:, :], in_=bonD[:, :]), reads=['bonD'])
                S.dma(lambda e: e.dma_start(out=og[:, :], in_=gD[:, :]), reads=['gD'])
                oH = dout("dbg_H", [64, 512])
                S.dma(lambda e: e.dma_start(out=oH[:, :], in_=H[:]), reads=['H'])
            S.barrier()
            S.flush()
        if stop_after <= 3:
            return nc, dbg_outs

        with contextlib.ExitStack() as ph:
            woutb = sb(ph, "woutb", [128, 8, D], BF16)
            attT_all = sb(ph, "attT_c", [128, 4, TL], BF16)
            S.dma(lambda e: e.dma_start(out=attT_all[:].rearrange("p a t -> p (a t)"), in_=attD[:, :]), reads=['attD'], writes=['attT_all'])
            cst = {}
            for nm, src in (("gt1", modd[0:1, 2048:3072]), ("sh2", modd[0:1, 3072:4096]), ("sc2p", modd[0:1, 4096:5120]),
                            ("ln1g", ln1_d[0:1, :]), ("ln1b", ln1_d[1:2, :])):
                cst[nm] = sb(ph, nm, [128, D], F32)
                S.dma(lambda e, nm=nm, src=src: e.dma_start(out=cst[nm][:], in_=src.partition_broadcast(128)), reads=['modd'], writes=[nm])
            for nm, row in (("lnxg", 0), ("lnxb", 1)):
                cst[nm] = sb(ph, nm, [128, 512], F32)
                S.dma(lambda e, nm=nm, row=row: e.dma_start(out=cst[nm][:], in_=lnx_d[row:row + 1, :].partition_broadcast(128)), writes=[nm])
            wst2 = [sb(ph, "wst2_%d" % i, [128, D], F32) for i in range(2)]
            for j in range(8):
                w = wst2[j % 2]; wk = 'wst2_%d' % (j % 2)
                S.dma(lambda e, w=w, j=j: e.dma_start(out=w[:], in_=wout_d[j * 128:(j + 1) * 128, :]), writes=[wk])
                CP('pool' if j % 2 else 'dve', woutb[:, j, :], w[:], [wk], ['woutb'])
            rwf = sb(ph, "rwf", [128, 8, 16], F32)
            S.dma(lambda e: e.dma_start(out=rwf[:], in_=rw_d.rearrange("(j p) n -> p j n", p=128)), writes=['rwf'])
            gneps = sb(ph, "gneps", [128, 1], F32)
            S.op('pool', lambda e: e.memset(gneps[:], 64e-5), writes=['gneps'])
            yf = sb(ph, "yf", [128, 512], F32); yb = sb(ph, "yb", [128, 512], F32)
            bon = sb(ph, "bon", [128, 512], F32); gg = sb(ph, "gg", [128, 512], F32)
            ysum = sb(ph, "ysum", [128, 512], F32); ysq = sb(ph, "ysq", [128, 512], F32)
            gst = sb(ph, "gst", [128, 8], F32); gvar = sb(ph, "gvar", [128, 8], F32)
            rwob = sb(ph, "rwob", [128, 512], BF16); rwoT = sb(ph, "rwoT", [128, 4, 128], BF16)
            xin_t = sb(ph, "xin_t", [128, D], F32); tres = sb(ph, "tres", [128, D], F32)
            x1t = sb(ph, "x1t", [128, D], F32); h2f = sb(ph, "h2f", [128, D], F32); h2b = sb(ph, "h2b", [128, D], BF16)
            h2T = sb(ph, "h2T", [128, 8, 128], F32)
            stats = sb(ph, "statsC", [128, 2, 6], F32); mv = sb(ph, "mvC", [128, 2], F32)
            rstd = sb(ph, "rstdC", [128, 1], F32); nb = sb(ph, "nbC", [128, 1], F32)
            lntC = (stats, mv, rstd, nb, 'C')
            lmax = sb(ph, "lmax", [128, 1], F32); lex = sb(ph, "lex", [128, 16], F32); lsum = sb(ph, "lsum", [128, 1], F32)

            def v8(t):
                return t[:, :].rearrange("p (h v) -> p h v", h=8)

            def b8(t):
                return t[:, :].unsqueeze(2).to_broadcast([128, 8, 64])
            for i in range(int(os.environ.get('K_CT', 32))):
                t0 = i * 128
                S.dma(lambda e, t0=t0: e.dma_start(out=yf[:], in_=ydir[0, t0:t0 + 128, :]), reads=['ydir'], writes=['yf'])
                S.dma(lambda e, t0=t0: e.dma_start(out=yb[:], in_=ydir[1, t0:t0 + 128, :]), reads=['ydir'], writes=['yb'])
                S.dma(lambda e, t0=t0: e.dma_start(out=bon[:], in_=bonD[t0:t0 + 128, :]), reads=['bonD'], writes=['bon'])
                S.dma(lambda e, t0=t0: e.dma_start(out=gg[:], in_=gD[t0:t0 + 128, :]), reads=['gD'], writes=['gg'])
                S.dma(lambda e, t0=t0: e.dma_start(out=xin_t[:], in_=x_d[t0:t0 + 128, :]), writes=['xin_t'])
                TTo('pool', ysum[:], yf[:], yb[:], ALU.add, ['yf', 'yb'], ['ysum'])
                S.op('dve', lambda e: e.tensor_reduce(out=gst[:], in_=v8(ysum), axis=AX.X, op=ALU.add), reads=['ysum'], writes=['gst'])
                S.op('dve', lambda e: e.tensor_scalar(out=gst[:], in0=gst[:], scalar1=-1.0 / 64, scalar2=None, op0=ALU.mult),
                     reads=['gst'], writes=['gst'])
                TTo('dve', v8(ysum), v8(ysum), b8(gst), ALU.add, ['ysum', 'gst'], ['ysum'])
                ACTF(ysq[:], ysum[:], AF.Square, ['ysum'], ['ysq'])
                S.op('dve', lambda e: e.tensor_reduce(out=gvar[:], in_=v8(ysq), axis=AX.X, op=ALU.add), reads=['ysq'], writes=['gvar'])
                ACTF(gvar[:], gvar[:], AF.Ln, ['gvar', 'gneps'], ['gvar'], scale=1.0 / 64, bias=gneps[:])
                ACTF(gvar[:], gvar[:], AF.Exp, ['gvar'], ['gvar'], scale=-0.5)
                TTo('dve', v8(ysum), v8(ysum), b8(gvar), ALU.mult, ['ysum', 'gvar'], ['ysum'])
                TTo('pool', ysum[:], ysum[:], cst['lnxg'][:], ALU.mult, ['ysum', 'lnxg'], ['ysum'])
                TTo('dve', ysum[:], ysum[:], cst['lnxb'][:], ALU.add, ['ysum', 'lnxb'], ['ysum'])
                TTo('pool', ysum[:], ysum[:], bon[:], ALU.add, ['ysum', 'bon'], ['ysum'])
                TTo('dve', rwob[:], ysum[:], gg[:], ALU.mult, ['ysum', 'gg'], ['rwob'])
                pr_ = P[0][:].bitcast(BF16)
                for j in range(4):
                    TR(pr_[:, j * 128:(j + 1) * 128], rwob[:, j * 128:(j + 1) * 128], identb[:], ['rwob', 'identb'], ['P0'])
                CP('dve', rwoT[:], pr_[:, 0:512].rearrange("p (j t) -> p j t", j=4), ['P0'], ['rwoT'])
                for half in range(2):
                    ob = PS[:, 1024 + half * 512:1024 + (half + 1) * 512]
                    for j in range(8):
                        lt = attT_all[:, j, t0:t0 + 128] if j < 4 else rwoT[:, j - 4, :]
                        MM(ob, lt, woutb[:, j, half * 512:(half + 1) * 512], ['attT_all', 'rwoT', 'woutb'], ['P2'], start=(j == 0), stop=(j == 7))
                TTo('dve', tres[:], PS[:, 1024:2048], cst['gt1'][:], ALU.mult, ['P2', 'gt1'], ['tres'])
                S.op('dve', lambda e: e.scalar_tensor_tensor(out=tres[:], in0=xin_t[:], scalar=ALPHA, in1=tres[:], op0=ALU.mult, op1=ALU.add),
                     reads=['xin_t', 'tres'], writes=['tres'])
                ln_stats(lntC, tres, 'tres', eps5, 'eps5')
                S.op('act', lambda e: e.activation(out=x1t[:], in_=tres[:], func=AF.Identity, bias=nb[:], scale=rstd[:]),
                     reads=['tres', 'Cnb', 'Crstd'], writes=['x1t'])
                TTo('pool', x1t[:], x1t[:], cst['ln1g'][:], ALU.mult, ['x1t', 'ln1g'], ['x1t'])
                TTo('dve', x1t[:], x1t[:], cst['ln1b'][:], ALU.add, ['x1t', 'ln1b'], ['x1t'])
                S.dma(lambda e, t0=t0: e.dma_start(out=x1D[t0:t0 + 128, :], in_=x1t[:]), reads=['x1t'], writes=['x1D'])
                ln_stats(lntC, x1t, 'x1t', eps5, 'eps5')
                S.op('act', lambda e: e.activation(out=h2f[:], in_=x1t[:], func=AF.Identity, bias=nb[:], scale=rstd[:]),
                     reads=['x1t', 'Cnb', 'Crstd'], writes=['h2f'])
                TTo('pool', h2f[:], h2f[:], cst['sc2p'][:], ALU.mult, ['h2f', 'sc2p'], ['h2f'])
                TTo('dve', h2f[:], h2f[:], cst['sh2'][:], ALU.add, ['h2f', 'sh2'], ['h2f'])
                CP('pool', h2b[:], h2f[:], ['h2f'], ['h2b'])
                S.dma(lambda e, t0=t0: e.dma_start(out=h2D[t0:t0 + 128, :], in_=h2b[:]), reads=['h2b'], writes=['h2D'])
                for j in range(8):
                    TR(PS[:, 2048 + j * 128:2048 + (j + 1) * 128], h2f[:, j * 128:(j + 1) * 128], identf[:], ['h2f', 'identf'], ['P4'])
                CP('dve', h2T[:].rearrange("p j t -> p (j t)"), PS[:, 2048:3072], ['P4'], ['h2T'])
                for j in range(8):
                    MM(PS[:, 3072:3088], h2T[:, j, :], rwf[:, j, :], ['h2T', 'rwf'], ['P6'], start=(j == 0), stop=(j == 7))
                S.op('dve', lambda e: e.tensor_reduce(out=lmax[:], in_=PS[:, 3072:3088], axis=AX.X, op=ALU.max), reads=['P6'], writes=['lmax'])
                S.op('dve', lambda e: e.tensor_scalar(out=lmax[:], in0=lmax[:], scalar1=-1.0, scalar2=None, op0=ALU.mult), reads=['lmax'], writes=['lmax'])
                ACTF(lex[:], PS[:, 3072:3088], AF.Exp, ['P6', 'lmax'], ['lex'], bias=lmax[:])
                S.op('dve', lambda e: e.tensor_reduce(out=lsum[:], in_=lex[:], axis=AX.X, op=ALU.add), reads=['lex'], writes=['lsum'])
                S.op('dve', lambda e: e.reciprocal(out=lsum[:], in_=lsum[:]), reads=['lsum'], writes=['lsum'])
                S.op('dve', lambda e, i=i: e.tensor_scalar(out=aff_all[:, i, :], in0=lex[:], scalar1=lsum[:], scalar2=None, op0=ALU.mult),
                     reads=['lex', 'lsum'], writes=['aff_all'])
            if dbg:
                o1 = dout("dbg_x1", [TL, D]); o2 = dout("dbg_aff", [128, 512])
                S.dma(lambda e: e.dma_start(out=o1[:, :], in_=x1D[:, :]), reads=['x1D'])
                S.dma(lambda e: e.dma_start(out=o2[:, :], in_=aff_all[:].rearrange("p a b -> p (a b)")), reads=['aff_all'])
            S.barrier()
            S.flush()
        if stop_after <= 4:
            return nc, dbg_outs
        posm = sb(top, "posm", [128, 32, 16], F32)
        gw = sb(top, "gw", [128, 32, 16, 2], BF16)
        onesf = sb(top, "onesf", [128, 128], F32)
        iot = sb(top, "iot", [128, 516], F32)
        S.op('pool', lambda e: e.memset(onesf[:], 1.0), writes=['onesf'])
        S.dma(lambda e: e.dma_start(out=iot[:], in_=iot_d[:, :]), writes=['iot'])

        with contextlib.ExitStack() as ph:
            lo = sb(ph, "lo", [128, 16], F32); hi = sb(ph, "hi", [128, 16], F32); mid = sb(ph, "mid", [128, 16], F32)
            cmpt = sb(ph, "cmpt", [128, 32, 16], F32); cntp = sb(ph, "cntp", [128, 16], F32); ge = sb(ph, "ge", [128, 16], F32)
            dlt = sb(ph, "dlt", [128, 16], F32)
            ustr = sb(ph, "ustr", [128, 128], F32)
            mask = sb(ph, "mask", [128, 32, 16], F32); tot = sb(ph, "tot", [128, 32, 16], F32); cum = sb(ph, "cum", [128, 32, 16], F32)
            glo = sb(ph, "glo", [128, 32, 16], F32); ghi32 = sb(ph, "ghi32", [128, 32, 16], F32)
            S.dma(lambda e: e.dma_start(out=ustr[:], in_=ustr_d[:, :]), writes=['ustr'])
            S.op('pool', lambda e: e.memset(lo[:], 0.0), writes=['lo'])
            S.op('pool', lambda e: e.memset(hi[:], 1.0), writes=['hi'])
            affv = aff_all[:, :, :]
            for it in range(30):
                TTo('dve', mid[:], lo[:], hi[:], ALU.add, ['lo', 'hi'], ['mid'])
                S.op('dve', lambda e: e.tensor_scalar(out=mid[:], in0=mid[:], scalar1=0.5, scalar2=None, op0=ALU.mult), reads=['mid'], writes=['mid'])
                TTo('dve', cmpt[:], affv, mid[:, :].unsqueeze(1).to_broadcast([128, 32, 16]), ALU.is_ge, ['aff_all', 'mid'], ['cmpt'])
                S.op('dve', lambda e: e.tensor_reduce(out=cntp[:], in_=cmpt[:].rearrange("p t e -> p e t"), axis=AX.X, op=ALU.add),
                     reads=['cmpt'], writes=['cntp'])
                MM(PS[:, 0:16], onesf[:], cntp[:], ['onesf', 'cntp'], ['P0'])
                S.op('dve', lambda e: e.tensor_scalar(out=ge[:], in0=PS[:, 0:16], scalar1=511.5, scalar2=None, op0=ALU.is_ge), reads=['P0'], writes=['ge'])
                TTo('dve', dlt[:], mid[:], lo[:], ALU.subtract, ['mid', 'lo'], ['dlt'])
                TTo('dve', dlt[:], dlt[:], ge[:], ALU.mult, ['dlt', 'ge'], ['dlt'])
                TTo('dve', lo[:], lo[:], dlt[:], ALU.add, ['lo', 'dlt'], ['lo'])
                TTo('dve', dlt[:], hi[:], mid[:], ALU.subtract, ['hi', 'mid'], ['dlt'])
                TTo('dve', dlt[:], dlt[:], ge[:], ALU.mult, ['dlt', 'ge'], ['dlt'])
                TTo('dve', hi[:], mid[:], dlt[:], ALU.add, ['mid', 'dlt'], ['hi'])
            TTo('dve', mask[:], affv, lo[:, :].unsqueeze(1).to_broadcast([128, 32, 16]), ALU.is_ge, ['aff_all', 'lo'], ['mask'])
            m2 = mask[:].rearrange("p t e -> p (t e)")
            MM(PS[:, 512:1024], ustr[:], m2, ['ustr', 'mask'], ['P1'])
            MM(PS[:, 1024:1536], onesf[:], m2, ['onesf', 'mask'], ['P2'])
            CP('dve', tot[:].rearrange("p t e -> p (t e)"), PS[:, 1024:1536], ['P2'], ['tot'])
            for e_ in range(16):
                S.op('dve', lambda e, e_=e_: e.tensor_tensor_scan(out=cum[:, :, e_], data0=onesf[:, 0:32], data1=tot[:, :, e_], initial=0.0,
                                                                 op0=ALU.mult, op1=ALU.add), reads=['tot', 'onesf'], writes=['cum'])
            TTo('dve', cum[:], cum[:], tot[:], ALU.subtract, ['cum', 'tot'], ['cum'])
            TTo('dve', cum[:].rearrange("p t e -> p (t e)"), cum[:].rearrange("p t e -> p (t e)"), PS[:, 512:1024], ALU.add, ['cum', 'P1'], ['cum'])
            S.op('dve', lambda e: e.scalar_tensor_tensor(out=posm[:], in0=cum[:], scalar=1.0, in1=mask[:], op0=ALU.add, op1=ALU.mult),
                 reads=['cum', 'mask'], writes=['posm'])
            S.op('dve', lambda e: e.tensor_scalar(out=posm[:], in0=posm[:], scalar1=-1.0, scalar2=None, op0=ALU.add), reads=['posm'], writes=['posm'])
            TTo('dve', glo[:], affv, mask[:], ALU.mult, ['aff_all', 'mask'], ['glo'])
            CP('dve', gw[:, :, :, 0], glo[:], ['glo'], ['gw'])
            CP('dve', ghi32[:], gw[:, :, :, 0], ['gw'], ['ghi32'])
            TTo('dve', gw[:, :, :, 1], glo[:], ghi32[:], ALU.subtract, ['glo', 'ghi32'], ['gw'])
            if dbg:
                o1 = dout("dbg_posm", [128, 512])
                S.dma(lambda e: e.dma_start(out=o1[:, :], in_=posm[:].rearrange("p a b -> p (a b)")), reads=['posm'])
            S.barrier()
            S.flush()
        if stop_after <= 5:
            return nc, dbg_outs

        with contextlib.ExitStack() as ph:
            h2_all = sb(ph, "h2_all", [128, 32, D], BF16)
            for i in range(32):
                S.dma(lambda e, i=i: e.dma_start(out=h2_all[:, i, :], in_=h2D[i * 128:(i + 1) * 128, :]), reads=['h2D'], writes=['h2_all'])
            OH = sb(ph, "OH", [128, 32, 512], BF16)
            xinT = sb(ph, "xinT", [128, 8, 512], BF16); hidT = sb(ph, "hidT", [128, 8, 512], BF16)
            wgb = sb(ph, "wgb", [128, 8, D], BF16); wub = sb(ph, "wub", [128, 8, D], BF16); wdb = sb(ph, "wdb", [128, 8, D], BF16)
            wsg = [sb(ph, "wsg%d" % i, [128, D], F32) for i in range(6)]
            gcs = sb(ph, "gcs", [128, 4], F32); sgt = sb(ph, "sgt", [128, 512], F32); sgts = [sgt, sb(ph, "sgt1", [128, 512], F32)]
            ywt = sb(ph, "ywt", [128, 4, D], BF16)
            wi = 0
            for ex_ in range(int(os.environ.get('K_NE', 16))):
                for i in range(32):
                    S.op('dve' if i % 2 else 'pool', lambda e, i=i, ex_=ex_: e.tensor_scalar(
                        out=OH[:, i, :], in0=iot[:, 0:512], scalar1=posm[:, i, ex_:ex_ + 1], scalar2=None, op0=ALU.is_equal),
                        reads=['iot', 'posm'], writes=['OH'])
                for j in range(8):
                    pb = P[j % 2]; pk = 'P%d' % (j % 2)
                    for i in range(32):
                        MM(pb, h2_all[:, i, j * 128:(j + 1) * 128], OH[:, i, :], ['h2_all', 'OH'], [pk], start=(i == 0), stop=(i == 31))
                    CP('act' if j % 2 else 'dve', xinT[:, j, :], pb, [pk], ['xinT'])
                for (wsrc, wdst, wkey) in ((wg_d, wgb, 'wgb'), (wu_d, wub, 'wub'), (wd_d, wdb, 'wdb')):
                    for j in range(8):
                        w = wsg[wi % 6]; wk = 'wsg%d' % (wi % 6)
                        S.dma(lambda e, w=w, wsrc=wsrc, ex_=ex_, j=j: e.dma_start(out=w[:], in_=wsrc[ex_, j * 128:(j + 1) * 128, :]), writes=[wk])
                        CP(('dve', 'pool', 'act')[wi % 3], wdst[:, j, :], w[:], [wk], [wkey])
                        wi += 1
                gcp = PS[:, 1024:1032].rearrange("p (c k) -> p c k", k=2)
                for ct in range(4):
                    for i in range(32):
                        MM(gcp[:, ct, :], OH[:, i, ct * 128:(ct + 1) * 128], gw[:, i, ex_, :], ['OH', 'gw'], ['P2'],
                           start=(i == 0 and ct == 0), stop=(i == 31 and ct == 3))
                TTo('dve', gcs[:], gcp[:, :, 0], gcp[:, :, 1], ALU.add, ['P2'], ['gcs']) if False else None
                CP('dve', sgt[:, 0:8], PS[:, 1024:1032], ['P2'], ['sgt0'])
                TTo('dve', gcs[:], sgt[:, 0:8].rearrange("p (c k) -> p c k", k=2)[:, :, 0], sgt[:, 0:8].rearrange("p (c k) -> p c k", k=2)[:, :, 1],
                    ALU.add, ['sgt0'], ['gcs'])
                for fc in range(8):
                    bg_ = 4 - 2 * (fc % 2); bu_ = 5 - 2 * (fc % 2)
                    sgt_ = sgts[fc % 2]; sgk_ = 'sgt%d' % (fc % 2)
                    for j in range(8):
                        MM(P[bg_], wgb[:, j, fc * 128:(fc + 1) * 128], xinT[:, j, :], ['wgb', 'xinT'], ['P%d' % bg_], start=(j == 0), stop=(j == 7))
                    for j in range(8):
                        MM(P[bu_], wub[:, j, fc * 128:(fc + 1) * 128], xinT[:, j, :], ['wub', 'xinT'], ['P%d' % bu_], start=(j == 0), stop=(j == 7))
                    ACTF(sgt_[:], P[bg_], AF.Silu, ['P%d' % bg_], [sgk_])
                    TTo('dve', hidT[:, fc, :], sgt_[:], P[bu_], ALU.mult, [sgk_, 'P%d' % bu_], ['hidT'])
                for ct in range(4):
                    for half in range(2):
                        pb = P[6 + half]; pk = 'P%d' % (6 + half)
                        for fc in range(8):
                            MM(pb, hidT[:, fc, ct * 128:(ct + 1) * 128], wdb[:, fc, half * 512:(half + 1) * 512], ['hidT', 'wdb'], [pk],
                               start=(fc == 0), stop=(fc == 7))
                        S.op('act', lambda e, ct=ct, half=half, pb=pb: e.activation(out=ywt[:, ct, half * 512:(half + 1) * 512], in_=pb,
                                                                                  func=AF.Copy, scale=gcs[:, ct:ct + 1]),
                             reads=[pk, 'gcs'], writes=['ywt'])
                S.dma(lambda e, ex_=ex_: e.dma_start(out=ywD[ex_].rearrange("(c p) n -> p c n", p=128), in_=ywt[:]), reads=['ywt'], writes=['ywD'])
            if dbg:
                o1 = dout("dbg_yw", [16 * 512, D], BF16)
                S.dma(lambda e: e.dma_start(out=o1[:, :], in_=ywD.rearrange("e c n -> (e c) n")), reads=['ywD'])
            S.barrier()
            S.flush()
        if stop_after <= 6:
            return nc, dbg_outs

        with contextlib.ExitStack() as ph:
            yw_all = sb(ph, "yw_all", [128, 16, 4, D], BF16)
            for ex_ in range(16):
                S.dma(lambda e, ex_=ex_: e.dma_start(out=yw_all[:, ex_, :, :], in_=ywD[ex_].rearrange("(c p) n -> p c n", p=128)),
                      reads=['ywD'], writes=['yw_all'])
            cst = {}
            for nm, src in (("gt2", modd[0:1, 5120:6144]), ("ln2g", ln2_d[0:1, :]), ("ln2b", ln2_d[1:2, :])):
                cst[nm] = sb(ph, nm, [128, D], F32)
                S.dma(lambda e, nm=nm, src=src: e.dma_start(out=cst[nm][:], in_=src.partition_broadcast(128)), reads=['modd'], writes=[nm])
            dg = sb(ph, "dg", [128, 4, 128], F32)
            OHTs = [sb(ph, "OHT%d" % q_, [128, 4, 2048], BF16) for q_ in range(2)]
            x1ls = [sb(ph, "x1l%d" % q_, [128, D], F32) for q_ in range(2)]
            tr2s = [sb(ph, "tr2%d" % q_, [128, D], F32) for q_ in range(2)]
            xos = [sb(ph, "xo%d" % q_, [128, D], F32) for q_ in range(2)]
            stats = sb(ph, "statsF", [128, 2, 6], F32); mv = sb(ph, "mvF", [128, 2], F32)
            rstd = sb(ph, "rstdF", [128, 1], F32); nb = sb(ph, "nbF", [128, 1], F32)
            lntF = (stats, mv, rstd, nb, 'F')
            nFT = int(os.environ.get('K_FT', 32))

            def f_front(i):
                q_ = i % 2
                OHT = OHTs[q_]; kOHT = 'OHT%d' % q_
                for eg in range(4):
                    for k_ in range(4):
                        ex_ = eg * 4 + k_
                        S.op('dve' if k_ % 2 else 'pool', lambda e, k_=k_, ex_=ex_, i=i: e.tensor_scalar(
                            out=dg[:, k_, :], in0=identf[:], scalar1=posm[:, i, ex_:ex_ + 1], scalar2=None, op0=ALU.mult),
                            reads=['identf', 'posm'], writes=['dg'])
                    MM(PS[:, eg * 512:(eg + 1) * 512], onesf[:], dg[:].rearrange("p a t -> p (a t)"), ['onesf', 'dg'], ['P%d' % eg])

            def f_cmp(i):
                q_ = i % 2
                OHT = OHTs[q_]; kOHT = 'OHT%d' % q_
                for ct in range(4):
                    S.op('dve', lambda e, ct=ct, OHT=OHT: e.tensor_scalar(out=OHT[:, ct, :], in0=PS[:, 0:2048], scalar1=iot[:, 512 + ct:513 + ct],
                                                                         scalar2=None, op0=ALU.is_equal),
                         reads=['P0', 'P1', 'P2', 'P3', 'iot'], writes=[kOHT])

            def f_back(i):
                t0 = i * 128
                q_ = i % 2
                OHT = OHTs[q_]; x1l = x1ls[q_]; tr2 = tr2s[q_]; xo = xos[q_]
                kOHT = 'OHT%d' % q_; kx1l = 'x1l%d' % q_; ktr2 = 'tr2%d' % q_; kxo = 'xo%d' % q_
                S.dma(lambda e, t0=t0, x1l=x1l: e.dma_start(out=x1l[:], in_=x1D[t0:t0 + 128, :]), reads=['x1D'], writes=[kx1l])
                for half in range(2):
                    pb = P[4 + half]; pk = 'P%d' % (4 + half)
                    n = 0
                    for ex_ in range(16):
                        for ct in range(4):
                            MM(pb, OHT[:, ct, ex_ * 128:(ex_ + 1) * 128], yw_all[:, ex_, ct, half * 512:(half + 1) * 512], [kOHT, 'yw_all'], [pk],
                               start=(n == 0), stop=(n == 63))
                            n += 1
                if i + 1 < nFT:
                    f_cmp(i + 1)
                TTo('dve', tr2[:], PS[:, 2048:3072], cst['gt2'][:], ALU.mult, ['P4', 'P5', 'gt2'], [ktr2])
                S.op('dve', lambda e, tr2=tr2, x1l=x1l: e.scalar_tensor_tensor(out=tr2[:], in0=x1l[:], scalar=ALPHA, in1=tr2[:], op0=ALU.mult, op1=ALU.add),
                     reads=[kx1l, ktr2], writes=[ktr2])
                ln_stats(lntF, tr2, ktr2, eps5, 'eps5')
                S.op('act', lambda e, xo=xo, tr2=tr2: e.activation(out=xo[:], in_=tr2[:], func=AF.Identity, bias=nb[:], scale=rstd[:]),
                     reads=[ktr2, 'Fnb', 'Frstd'], writes=[kxo])
                TTo('pool', xo[:], xo[:], cst['ln2g'][:], ALU.mult, [kxo, 'ln2g'], [kxo])
                TTo('dve', xo[:], xo[:], cst['ln2b'][:], ALU.add, [kxo, 'ln2b'], [kxo])
                S.dma(lambda e, t0=t0, xo=xo: e.dma_start(out=out_d[t0:t0 + 128, :], in_=xo[:]), reads=[kxo], writes=['out'])

            f_front(0)
            f_cmp(0)
            for i in range(nFT):
                if i + 1 < nFT:
                    f_front(i + 1)
                f_back(i)
            S.barrier()
            S.flush()
    return nc, dbg_outs


def host_inputs(inp, b):
    f = np.float32
    m = {}
    m["x"] = np.ascontiguousarray(inp["x"][b], dtype=f)
    m["ctx"] = np.ascontiguousarray(inp["ctx"][b], dtype=f)
    m["cc"] = np.ascontiguousarray(np.stack([inp["c"][b].reshape(8, 128).T, inp["c_ctx"].reshape(8, 128).T], -1).reshape(128, 16), dtype=f)
    m["w_ada"] = np.ascontiguousarray(inp["w_ada"][0], dtype=f)
    m["b_ada"] = np.ascontiguousarray(inp["b_ada"][0].reshape(1, -1), dtype=f)
    m["w_in"] = np.ascontiguousarray(inp["w_in"][0], dtype=f)
    m["qg"] = np.ascontiguousarray(np.tile(inp["q_gain"][0], 8).reshape(1, 512), dtype=f)
    m["kg"] = np.ascontiguousarray(np.tile(inp["k_gain"][0], 2).reshape(1, 128), dtype=f)
    t = np.arange(TL)
    pos = np.stack([t // 64, t % 64], -1).astype(np.float32)
    inv = (10000.0 ** (-np.arange(16, dtype=np.float32) / 16)).astype(np.float32)
    ang = pos[:, :, None] * inv[None, None, :]
    cs = np.cos(ang).astype(f); sn = np.sin(ang).astype(f)
    cos2 = np.stack([cs, cs], 2).reshape(TL, 64)
    sin2 = np.stack([-sn, sn], 2).reshape(TL, 64)
    m["cosT"] = np.ascontiguousarray(np.tile(cos2, (1, 8)), dtype=f)
    m["sinS"] = np.ascontiguousarray(np.tile(sin2, (1, 8)), dtype=f)
    m["ident"] = np.eye(128, dtype=f)
    def kh(v):
        return np.asarray(v, dtype=f).reshape(8, 64).T
    mu = inp["tshift_mu"][0]
    cols = [kh(mu[0:512]), kh(mu[512:1024]), kh(mu[1024:1536]), kh(inp["k_k"][0]), kh(inp["k_a"][0]), kh(inp["r_k"][0].reshape(-1)),
            kh(inp["decay_w0"][0, 0]), kh(inp["decay_w0"][0, 1]), kh(inp["iclr_a0"][0, 0]), kh(inp["iclr_a0"][0, 1])]
    m["rp"] = np.ascontiguousarray(np.stack(cols, -1).reshape(64, 80), dtype=f)
    lmu = np.zeros((128, 3), f)
    lmu[0:64, 0] = mu[1536:1600]; lmu[0:64, 1] = mu[1600:1664]; lmu[:, 2] = mu[1664:1792]
    m["lmu"] = lmu
    m["decup"] = np.ascontiguousarray(np.concatenate([inp["decay_up"][0, 0], inp["decay_up"][0, 1]], 1), dtype=f)
    m["iclup"] = np.ascontiguousarray(np.concatenate([inp["iclr_up"][0, 0], inp["iclr_up"][0, 1]], 1), dtype=f)
    m["gateup"] = np.ascontiguousarray(inp["gate_up"][0], dtype=f)
    hh = np.repeat(np.arange(8), 16); tt = np.tile(np.arange(16), 8)
    same = hh[:, None] == hh[None, :]
    msf = (same & (tt[:, None] < tt[None, :])).astype(f); mif = (same & (tt[:, None] <= tt[None, :])).astype(f)
    msb = msf.T.copy(); mib = mif.T.copy()
    m["mk4"] = np.ascontiguousarray(np.concatenate([msf, mif, msf, mif, msb, mib, msb, mib], 1), dtype=f)
    m["mn1"] = np.ascontiguousarray(np.concatenate([msb, msf], 1), dtype=f)
    m["bm"] = np.ascontiguousarray((hh[:, None] == np.repeat(np.arange(8), 64)[None, :]).astype(f))
    rst = np.ones((64, 1024), f); rst[:, ::16] = 0.0
    m["rst"] = rst
    m["w_out"] = np.ascontiguousarray(inp["w_out"][0], dtype=f)
    m["lnx"] = np.ascontiguousarray(np.stack([inp["lnx_g"][0], inp["lnx_b"][0]]), dtype=f)
    m["ln1"] = np.ascontiguousarray(np.stack([inp["ln1_g"][0], inp["ln1_b"][0]]), dtype=f)
    m["ln2"] = np.ascontiguousarray(np.stack([inp["ln2_g"][0], inp["ln2_b"][0]]), dtype=f)
    m["router_w"] = np.ascontiguousarray(inp["router_w"][0], dtype=f)
    m["exp_w_gate"] = np.ascontiguousarray(inp["exp_w_gate"][0], dtype=f)
    m["exp_w_up"] = np.ascontiguousarray(inp["exp_w_up"][0], dtype=f)
    m["exp_w_down"] = np.ascontiguousarray(inp["exp_w_down"][0], dtype=f)
    iot = np.zeros((128, 516), f)
    iot[:, 0:512] = np.arange(512, dtype=f)[None, :]
    iot[:, 512:516] = np.arange(128, dtype=f)[:, None] + 128.0 * np.arange(4, dtype=f)[None, :]
    m["iot"] = iot
    m["ustr"] = np.triu(np.ones((128, 128), f), 1)
    return m


_NC_CACHE = {}


def kernel(**inputs):
    inp = {k: np.asarray(v) for k, v in inputs.items()}
    if "full" not in _NC_CACHE:
        _NC_CACHE["full"] = build_nc()[0]
    nc = _NC_CACHE["full"]
    in_maps = [host_inputs(inp, c // 2) for c in range(8)]
    res = run_bass_kernel_spmd(nc, in_maps, core_ids=list(range(8)))
    out = np.stack([res.results[2 * b]["out"] for b in range(4)], 0).astype(np.float32)
    return out
```

```python
import contextlib
import os
import numpy as np
import concourse.bass as bass
import concourse.mybir as mybir
from concourse.bass_utils import run_bass_kernel_spmd

F32 = mybir.dt.float32
BF16 = mybir.dt.bfloat16
AF = mybir.ActivationFunctionType
ALU = mybir.AluOpType
AX = mybir.AxisListType

D = 1024
TL = 4096
TC = 256
TT = TL + TC
NT = TT // 128
ALPHA = 2.0 ** 0.25
DEC_C = -float(np.exp(-0.5))


class Sched:
    CE = ('pe', 'act', 'dve', 'pool')

    def __init__(self, nc, stack, ndma=32):
        self.nc = nc
        self.ops = {e: [] for e in ('pe', 'act', 'dve', 'pool', 'sp')}
        self.cnt = {e: 0 for e in self.CE}
        self.last_w = {}
        self.readers = {}
        self.waited = {e: {} for e in self.ops}
        self.ndma = ndma
        self.dma_val = [0] * ndma
        self.dma_i = 0
        names = list(self.CE) + ['d%d' % i for i in range(ndma)]
        self.sems = {n: stack.enter_context(nc.semaphore('s_' + n)) for n in names}

    def _deps(self, eng, reads, writes):
        deps = {}

        def add(tok):
            if tok is None:
                return
            s, v = tok
            if deps.get(s, 0) < v:
                deps[s] = v
        for r in reads:
            add(self.last_w.get(r))
        for w in writes:
            add(self.last_w.get(w))
            for t in self.readers.get(w, ()):
                add(t)
        waits = []
        for s, v in deps.items():
            if s == eng and (eng == 'pe' or os.environ.get('K_NOSELF')):
                continue
            if self.waited[eng].get(s, 0) >= v:
                continue
            self.waited[eng][s] = v
            waits.append((s, v))
        return waits

    def _commit(self, tok, reads, writes):
        for r in reads:
            self.readers.setdefault(r, []).append(tok)
        for w in writes:
            self.last_w[w] = tok
            self.readers[w] = []

    capture = None
    pend = None

    def drain(self, lst, n):
        for _ in range(min(n, len(lst))):
            kind, a = lst.pop(0)
            (self.op if kind == 'op' else self.dma)(*a)

    def op(self, eng, fn, reads=(), writes=()):
        if self.capture is not None:
            self.capture.append(('op', (eng, fn, tuple(reads), tuple(writes))))
            return
        waits = self._deps(eng, reads, writes)
        self.cnt[eng] += 1
        tok = (eng, self.cnt[eng])
        self.ops[eng].append((waits, fn, (eng, 1)))
        self._commit(tok, reads, writes)

    def dma(self, fn, reads=(), writes=(), q='sp'):
        if self.capture is not None:
            self.capture.append(('dma', (fn, tuple(reads), tuple(writes), q)))
            return
        slot = self.dma_i % self.ndma
        self.dma_i += 1
        s = 'd%d' % slot
        waits = self._deps(q, reads, writes)
        pv = self.dma_val[slot]
        if pv > 0 and self.waited[q].get(s, 0) < pv:
            self.waited[q][s] = pv
            waits.append((s, pv))
        self.dma_val[slot] = pv + 16
        tok = (s, pv + 16)
        self.ops[q].append((waits, fn, (s, 16)))
        self._commit(tok, reads, writes)

    def barrier(self):
        allw = [(e, c) for e, c in self.cnt.items() if c > 0]
        allw += [('d%d' % i, v) for i, v in enumerate(self.dma_val) if v > 0]
        for eng in self.ops:
            waits = []
            for s, v in allw:
                if self.waited[eng].get(s, 0) >= v:
                    continue
                self.waited[eng][s] = v
                waits.append((s, v))
            if waits:
                self.ops[eng].append((waits, None, None))
        self.last_w = {}
        self.readers = {}

    def flush(self):
        nc = self.nc
        sems = self.sems
        ops = self.ops
        self.ops = {e: [] for e in ops}
        if os.environ.get('K_STATS'):
            print("FLUSH", {e: (len(v), sum(len(w[0]) for w in v)) for e, v in ops.items()})
        with nc.Block() as block:
            def run(engname, engobj):
                for waits, fn, inc in ops[engname]:
                    for ws, wv in waits:
                        engobj.wait_ge(sems[ws], wv)
                    if fn is not None:
                        ins = fn(engobj)
                        ins.then_inc(sems[inc[0]], inc[1])

            @block.sync
            def _(e):
                run('sp', e)

            @block.tensor
            def _(e):
                run('pe', e)

            @block.scalar
            def _(e):
                run('act', e)

            @block.vector
            def _(e):
                run('dve', e)

            @block.gpsimd
            def _(e):
                run('pool', e)


def build_nc(stop_after=99, dbg=False):
    nc = bass.Bass("TRN2", target_bir_lowering=False)

    def din(name, shape, dt=F32):
        return nc.dram_tensor(name, list(shape), dt, kind="ExternalInput").ap()

    def dscr(name, shape, dt=F32):
        return nc.dram_tensor(name, list(shape), dt, kind="Internal").ap()

    x_d = din("x", [TL, D]); ctx_d = din("ctx", [TC, D])
    cc_d = din("cc", [128, 16])
    wada_d = din("w_ada", [D, 6 * D]); bada_d = din("b_ada", [1, 6 * D])
    win_d = din("w_in", [D, 2560])
    qg_d = din("qg", [1, 512]); kg_d = din("kg", [1, 128])
    cos_d = din("cosT", [TL, 512]); sin_d = din("sinS", [TL, 512])
    ident_d = din("ident", [128, 128])
    rp_d = din("rp", [64, 8 * 10])
    lmu_d = din("lmu", [128, 3])
    decup_d = din("decup", [64, 2 * 512]); iclup_d = din("iclup", [64, 2 * 512]); gateup_d = din("gateup", [128, 512])
    mk4_d = din("mk4", [128, 2 * 512]); mn1_d = din("mn1", [128, 2 * 128]); bm_d = din("bm", [128, 512])
    rst_d = din("rst", [64, 1024])
    wout_d = din("w_out", [D, D]); lnx_d = din("lnx", [2, 512])
    ln1_d = din("ln1", [2, D]); ln2_d = din("ln2", [2, D])
    rw_d = din("router_w", [D, 16])
    x1D = dscr("x1D", [TL, D]); h2D = dscr("h2D", [TL, D], BF16)
    attD = dscr("attD", [128, 4 * TL], BF16)
    wg_d = din("exp_w_gate", [16, D, D]); wu_d = din("exp_w_up", [16, D, D]); wd_d = din("exp_w_down", [16, D, D])
    iot_d = din("iot", [128, 512 + 4]); ustr_d = din("ustr", [128, 128])
    ywD = dscr("ywD", [16, 512, D], BF16)
    ydir = dscr("ydir", [2, TL, 512]); bonD = dscr("bonD", [TL, 512]); gD = dscr("gD", [TL, 512])
    out_d = nc.dram_tensor("out", [TL, D], F32, kind="ExternalOutput").ap()
    modd = dscr("modd", [2, 6 * D])
    rwT = dscr("rwT", [1792, TT])
    dbg_outs = {}

    def dout(name, shape, dt=F32):
        ap = nc.dram_tensor(name, list(shape), dt, kind="ExternalOutput").ap()
        dbg_outs[name] = ap
        return ap

    with contextlib.ExitStack() as top:
        S = Sched(nc, top)

        def sb(stack, name, shape, dt):
            return stack.enter_context(nc.sbuf_tensor("sb_" + name, list(shape), dt))

        PS = top.enter_context(nc.psum_tensor("PS", [128, 8 * 512], F32))
        P = [PS[:, i * 512:(i + 1) * 512] for i in range(8)]
        PK = ['P%d' % i for i in range(8)]
        identf = sb(top, "identf", [128, 128], F32)
        identb = sb(top, "identb", [128, 128], BF16)
        eps5 = sb(top, "eps5", [128, 1], F32)
        eps6 = sb(top, "eps6", [128, 1], F32)
        S.dma(lambda e: e.dma_start(out=identf[:], in_=ident_d[:, :]), writes=['identf'])
        S.op('dve', lambda e: e.tensor_copy(out=identb[:], in_=identf[:]), reads=['identf'], writes=['identb'])
        S.op('pool', lambda e: e.memset(eps5[:], 1e-5), writes=['eps5'])
        S.op('pool', lambda e: e.memset(eps6[:], 1e-6), writes=['eps6'])

        def ln_stats(stack_tiles, src, key_src, eps_t, eps_key):
            stats, mv, rstd, nb, kq = stack_tiles
            for cch in range(2):
                S.op('dve', lambda e, cch=cch: e.bn_stats(out=stats[:, cch, :], in_=src[:, cch * 512:(cch + 1) * 512]),
                     reads=[key_src], writes=[kq + 'stats'])
            S.op('dve', lambda e: e.bn_aggr(out=mv[:], in_=stats[:]), reads=[kq + 'stats'], writes=[kq + 'mv'])
            S.op('act', lambda e: e.activation(out=rstd[:], in_=mv[:, 1:2], func=AF.Ln, bias=eps_t[:], scale=1.0),
                 reads=[kq + 'mv', eps_key], writes=[kq + 'rstd'])
            S.op('act', lambda e: e.activation(out=rstd[:], in_=rstd[:], func=AF.Exp, scale=-0.5),
                 reads=[kq + 'rstd'], writes=[kq + 'rstd'])
            S.op('dve', lambda e: e.scalar_tensor_tensor(out=nb[:], in0=mv[:, 0:1], scalar=-1.0, in1=rstd[:],
                                                        op0=ALU.mult, op1=ALU.mult),
                 reads=[kq + 'mv', kq + 'rstd'], writes=[kq + 'nb'])

        with contextlib.ExitStack() as ph:
            cc = sb(ph, "cc", [128, 8, 2], F32)
            ccs = sb(ph, "ccs", [128, 8, 2], F32)
            wst = [sb(ph, "wst%d" % i, [128, 8, 512], F32) for i in range(2)]
            bada = sb(ph, "bada", [2, 6 * D], F32)
            modrow = sb(ph, "modrow", [2, 6 * D], F32)
            S.dma(lambda e: e.dma_start(out=cc[:].rearrange("p a b -> p (a b)"), in_=cc_d[:, :]), writes=['cc'])
            S.dma(lambda e: e.dma_start(out=bada[:], in_=bada_d.partition_broadcast(2)), writes=['bada'])
            S.op('act', lambda e: e.activation(out=ccs[:], in_=cc[:], func=AF.Silu), reads=['cc'], writes=['ccs'])
            for g in range(12):
                w = wst[g % 2]
                wk = 'wst%d' % (g % 2)
                S.dma(lambda e, w=w, g=g: e.dma_start(
                    out=w[:], in_=wada_d[:, g * 512:(g + 1) * 512].rearrange("(j p) n -> p j n", p=128)), writes=[wk])
                for j in range(8):
                    S.op('pe', lambda e, w=w, j=j: e.matmul(P[0][0:2, :], lhsT=ccs[:, j, :], rhs=w[:, j, :],
                                                           start=(j == 0), stop=(j == 7)),
                         reads=['ccs', wk], writes=['P0'])
                S.op('dve', lambda e, g=g: e.tensor_tensor(out=modrow[:, g * 512:(g + 1) * 512], in0=P[0][0:2, :],
                                                          in1=bada[:, g * 512:(g + 1) * 512], op=ALU.add),
                     reads=['P0', 'bada'], writes=['modrow'])
            for lo in (1024, 4096):
                S.op('dve', lambda e, lo=lo: e.tensor_scalar_add(out=modrow[:, lo:lo + 1024], in0=modrow[:, lo:lo + 1024],
                                                                scalar1=1.0), reads=['modrow'], writes=['modrow'])
            S.dma(lambda e: e.dma_start(out=modd[:, :], in_=modrow[:]), reads=['modrow'], writes=['modd'])
            if dbg:
                o = dout("dbg_mod", [2, 6 * D])
                S.dma(lambda e: e.dma_start(out=o[:, :], in_=modrow[:]), reads=['modrow'])
            S.barrier()
            S.flush()
        if stop_after <= 0:
            return nc, dbg_outs

        def modrow_bc(dst, row, lo):
            S.dma(lambda e: e.dma_start(out=dst[:], in_=modd[row:row + 1, lo:lo + 1024].partition_broadcast(128)),
                  reads=['modd'], writes=[dst.name if hasattr(dst, 'name') else 'x'])

        aff_all = sb(top, "aff_all", [128, 32, 16], F32)
        with contextlib.ExitStack() as phAB:
            qT_all = sb(phAB, "qT_all", [128, 4, TL], BF16)
            kTd = sb(phAB, "kTd", [128, 2, TT], BF16)
            Vx = sb(phAB, "Vx", [128, NT, 2, 80], BF16)
            attT_all = sb(phAB, "attT_all", [128, 4, TL], BF16)
            with contextlib.ExitStack() as ph:
                winb = sb(ph, "winb", [128, 8, 2560], BF16)
                wstg = [sb(ph, "wstg%d" % i, [128, 640], F32) for i in range(2)]
                sc1p = sb(ph, "sc1p", [128, D], F32); sh1 = sb(ph, "sh1", [128, D], F32)
                csc1p = sb(ph, "csc1p", [128, D], F32); csh1 = sb(ph, "csh1", [128, D], F32)
                qg = sb(ph, "qg", [128, 512], F32); kg = sb(ph, "kg", [128, 128], F32)
                for dst, nm, row, lo in ((sh1, 'sh1', 0, 0), (sc1p, 'sc1p', 0, 1024), (csh1, 'csh1', 1, 0), (csc1p, 'csc1p', 1, 1024)):
                    S.dma(lambda e, dst=dst, row=row, lo=lo: e.dma_start(
                        out=dst[:], in_=modd[row:row + 1, lo:lo + 1024].partition_broadcast(128)), reads=['modd'], writes=[nm])
                S.dma(lambda e: e.dma_start(out=qg[:], in_=qg_d.partition_broadcast(128)), writes=['qg'])
                S.dma(lambda e: e.dma_start(out=kg[:], in_=kg_d.partition_broadcast(128)), writes=['kg'])
                for jj in range(32):
                    j = jj // 4; c0 = (jj % 4) * 640
                    w = wstg[jj % 2]; wk = 'wstg%d' % (jj % 2)
                    S.dma(lambda e, w=w, j=j, c0=c0: e.dma_start(out=w[:], in_=win_d[j * 128:(j + 1) * 128, c0:c0 + 640]), writes=[wk])
                    S.op('pool' if jj % 2 else 'dve', lambda e, w=w, j=j, c0=c0: e.tensor_copy(out=winb[:, j, c0:c0 + 640], in_=w[:]),
                         reads=[wk], writes=['winb'])
                S.op('pool', lambda e: e.memset(Vx[:], 1.0), writes=['Vx'])
                xt = [sb(ph, "xt%d" % i, [128, D], F32) for i in range(2)]
                xn = sb(ph, "xn", [128, D], F32)
                hb = sb(ph, "hb", [128, D], BF16)
                hT = sb(ph, "hT", [128, 8, 512], BF16)
                stats = sb(ph, "stats", [128, 2, 6], F32); mv = sb(ph, "mv", [128, 2], F32)
                rstd = sb(ph, "rstd", [128, 1], F32); nb = sb(ph, "nb", [128, 1], F32)
                lnt = (stats, mv, rstd, nb, 'A')
                rstg = [sb(ph, "rstg%d" % i, [128, 512], F32) for i in range(2)]
                cosT = sb(ph, "cosT", [128, 512], F32); sinS = sb(ph, "sinS", [128, 512], F32)
                sq = sb(ph, "sq", [128, 512], F32)
                ssum = sb(ph, "ssum", [128, 8], F32)
                qn = sb(ph, "qn", [128, 512], F32); qa = sb(ph, "qa", [128, 512], F32)
                qbt = sb(ph, "qbt", [128, 512], F32)
                qo = sb(ph, "qo", [128, 512], BF16)
                ko = sb(ph, "ko", [128, 2, 2, 64], BF16)
                blocks = [(0, 256)] + [(256 + 512 * i, 512) for i in range(8)]
                ti = 0
                for (u0, ntok) in blocks:
                    ntile = ntok // 128
                    is_ctx = (u0 == 0)
                    for tl in range(ntile):
                        u = u0 + tl * 128
                        gt = u // 128
                        X = xt[ti % 2]; xk = 'xt%d' % (ti % 2)
                        ti += 1
                        src = ctx_d[u:u + 128, :] if is_ctx else x_d[u - TC:u - TC + 128, :]
                        S.dma(lambda e, X=X, src=src: e.dma_start(out=X[:], in_=src), writes=[xk])
                        ln_stats(lnt, X, xk, eps5, 'eps5')
                        S.op('act', lambda e, X=X: e.activation(out=xn[:], in_=X[:], func=AF.Identity, bias=nb[:], scale=rstd[:]),
                             reads=[xk, 'Anb', 'Arstd'], writes=['xn'])
                        scp, shh, k1, k2 = (csc1p, csh1, 'csc1p', 'csh1') if is_ctx else (sc1p, sh1, 'sc1p', 'sh1')
                        S.op('pool', lambda e, scp=scp: e.tensor_tensor(out=xn[:], in0=xn[:], in1=scp[:], op=ALU.mult),
                             reads=['xn', k1], writes=['xn'])
                        S.op('dve', lambda e, shh=shh: e.tensor_tensor(out=hb[:], in0=xn[:], in1=shh[:], op=ALU.add),
                             reads=['xn', k2], writes=['hb'])
                        pTb = P[0][:].bitcast(BF16)
                        for j in range(8):
                            S.op('pe', lambda e, j=j, pTb=pTb: e.transpose(out=pTb[:, j * 128:(j + 1) * 128],
                                                                      in_=hb[:, j * 128:(j + 1) * 128], identity=identb[:]),
                                 reads=['hb', 'identb'], writes=['P0'])
                        S.op('act', lambda e, tl=tl, pTb=pTb: e.copy(out=hT[:, :, tl * 128:(tl + 1) * 128],
                                                                 in_=pTb.rearrange("p (j t) -> p j t", j=8)),
                             reads=['P0'], writes=['hT'])
                        for j in range(8):
                            S.op('pe', lambda e, j=j, tl=tl: e.matmul(P[1][:, :], lhsT=hT[:, j, tl * 128:(tl + 1) * 128],
                                                                     rhs=winb[:, j, 0:512], start=(j == 0), stop=(j == 7)),
                                 reads=['hT', 'winb'], writes=['P1'])
                        for j in range(8):
                            S.op('pe', lambda e, j=j, tl=tl: e.matmul(P[2][:, 0:256], lhsT=hT[:, j, tl * 128:(tl + 1) * 128],
                                                                     rhs=winb[:, j, 512:768], start=(j == 0), stop=(j == 7)),
                                 reads=['hT', 'winb'], writes=['P2'])
                        S.op('act', lambda e, gt=gt: e.copy(out=Vx[:, gt, :, 0:64],
                                                          in_=P[2][:, 128:256].rearrange("p (a b) -> p a b", a=2)),
                             reads=['P2'], writes=['Vx'])
                        if not is_ctx:
                            ul = u - TC
                            S.dma(lambda e, ul=ul: e.dma_start(out=cosT[:], in_=cos_d[ul:ul + 128, :]), writes=['cosT'])
                            S.dma(lambda e, ul=ul: e.dma_start(out=sinS[:], in_=sin_d[ul:ul + 128, :]), writes=['sinS'])

                        def normrope(psrc, pk, nh, gain, gk, do_rope, outfn):
                            w_ = nh * 64
                            S.op('act', lambda e: e.activation(out=sq[:, 0:w_], in_=psrc, func=AF.Square),
                                 reads=[pk], writes=['sq'])
                            S.op('dve', lambda e: e.tensor_reduce(out=ssum[:, 0:nh], in_=sq[:, 0:w_].rearrange("p (h d) -> p h d", d=64),
                                                                 axis=AX.X, op=ALU.add), reads=['sq'], writes=['ssum'])
                            S.op('act', lambda e: e.activation(out=ssum[:, 0:nh], in_=ssum[:, 0:nh], func=AF.Ln, bias=eps6[:], scale=1.0 / 64),
                                 reads=['ssum', 'eps6'], writes=['ssum'])
                            S.op('act', lambda e: e.activation(out=ssum[:, 0:nh], in_=ssum[:, 0:nh], func=AF.Exp, scale=-0.5),
                                 reads=['ssum'], writes=['ssum'])
                            S.op('dve', lambda e: e.tensor_tensor(out=qn[:, 0:w_].rearrange("p (h d) -> p h d", d=64),
                                                                 in0=psrc.rearrange("p (h d) -> p h d", d=64),
                                                                 in1=ssum[:, 0:nh].unsqueeze(2).to_broadcast([128, nh, 64]), op=ALU.mult),
                                 reads=[pk, 'ssum'], writes=['qn'])
                            if not do_rope:
                                S.op('pool', lambda e: outfn(e, qn[:, 0:w_], gain[:, 0:w_], ALU.mult), reads=['qn', gk], writes=['qko'])
                                return
                            S.op('pool', lambda e: e.tensor_tensor(out=qn[:, 0:w_], in0=qn[:, 0:w_], in1=gain[:, 0:w_], op=ALU.mult),
                                 reads=['qn', gk], writes=['qn'])
                            S.op('dve', lambda e: e.tensor_tensor(out=qa[:, 0:w_], in0=qn[:, 0:w_], in1=cosT[:, 0:w_], op=ALU.mult),
                                 reads=['qn', 'cosT'], writes=['qa'])
                            qv = qn[:, 0:w_].rearrange("p (g s q) -> p g s q", s=2, q=16)
                            bv = qbt[:, 0:w_].rearrange("p (g s q) -> p g s q", s=2, q=16)
                            sv = sinS[:, 0:w_].rearrange("p (g s q) -> p g s q", s=2, q=16)
                            for s_ in range(2):
                                S.op('pool', lambda e, s_=s_: e.tensor_tensor(out=bv[:, :, s_, :], in0=qv[:, :, 1 - s_, :],
                                                                             in1=sv[:, :, s_, :], op=ALU.mult),
                                     reads=['qn', 'sinS'], writes=['qbt'])
                            S.op('dve', lambda e: outfn(e, qa[:, 0:w_], qbt[:, 0:w_], ALU.add), reads=['qa', 'qbt'], writes=['qko'])

                        if not is_ctx:
                            normrope(P[1][:, :], 'P1', 8, qg, 'qg', True,
                                     lambda e, a, b_, op: e.tensor_tensor(out=qo[:], in0=a, in1=b_, op=op))
                            pq = P[3][:].bitcast(BF16)
                            for hp in range(4):
                                S.op('pe', lambda e, hp=hp, pq=pq: e.transpose(out=pq[:, hp * 128:(hp + 1) * 128],
                                                                          in_=qo[:, hp * 128:(hp + 1) * 128], identity=identb[:]),
                                     reads=['qko', 'identb'], writes=['P3'])
                            S.op('act', lambda e, ul=ul, pq=pq: e.copy(out=qT_all[:, :, ul:ul + 128],
                                                                   in_=pq[:, 0:512].rearrange("p (j t) -> p j t", j=4)),
                                 reads=['P3'], writes=['qT_all'])

                        def kout(e, a, b_, op):
                            return e.tensor_tensor(out=ko[:, :, 0, :], in0=a.rearrange("p (h d) -> p h d", d=64),
                                                   in1=b_.rearrange("p (h d) -> p h d", d=64), op=op)
                        normrope(P[2][:, 0:128], 'P2', 2, kg, 'kg', not is_ctx, kout)
                        S.op('pool', lambda e: e.tensor_copy(out=ko[:, :, 1, :], in_=ko[:, :, 0, :]), reads=['qko'], writes=['qko'])
                        pk_ = P[3][:].bitcast(BF16)
                        for kv in range(2):
                            S.op('pe', lambda e, kv=kv, pk_=pk_: e.transpose(
                                out=pk_[:, 512 + kv * 128:512 + (kv + 1) * 128],
                                in_=ko[:, kv, :, :].rearrange("p a d -> p (a d)"), identity=identb[:]),
                                reads=['qko', 'identb'], writes=['P3'])
                        S.op('act', lambda e, u=u, pk_=pk_: e.copy(out=kTd[:, :, u:u + 128],
                                                               in_=pk_[:, 512:768].rearrange("p (j t) -> p j t", j=2)),
                             reads=['P3'], writes=['kTd'])
                    for g in range(14):
                        pb = P[4 + g % 2]; pbk = 'P%d' % (4 + g % 2)
                        for j in range(8):
                            S.op('pe', lambda e, j=j, g=g, pb=pb, ntok=ntok: e.matmul(pb[:, 0:ntok], lhsT=winb[:, j, 768 + g * 128:768 + (g + 1) * 128],
                                                                         rhs=hT[:, j, 0:ntok], start=(j == 0), stop=(j == 7)),
                                 reads=['hT', 'winb'], writes=[pbk])
                        rs = rstg[g % 2]; rk = 'rstg%d' % (g % 2)
                        S.op('act' if g % 2 else 'dve',
                             (lambda e, rs=rs, pb=pb, ntok=ntok: e.copy(out=rs[:, 0:ntok], in_=pb[:, 0:ntok])) if g % 2 else
                             (lambda e, rs=rs, pb=pb, ntok=ntok: e.tensor_copy(out=rs[:, 0:ntok], in_=pb[:, 0:ntok])),
                             reads=[pbk], writes=[rk])
                        S.dma(lambda e, rs=rs, g=g, u0=u0, ntok=ntok: e.dma_start(out=rwT[g * 128:(g + 1) * 128, u0:u0 + ntok], in_=rs[:, 0:ntok]),
                              reads=[rk], writes=['rwT'])
                if dbg:
                    o1 = dout("dbg_qT", [128, 4 * TL], BF16)
                    S.dma(lambda e: e.dma_start(out=o1[:, :], in_=qT_all[:].rearrange("p a t -> p (a t)")), reads=['qT_all'])
                    o2 = dout("dbg_kTd", [128, 2 * TT], BF16)
                    S.dma(lambda e: e.dma_start(out=o2[:, :], in_=kTd[:].rearrange("p a t -> p (a t)")), reads=['kTd'])
                    o3 = dout("dbg_rwT", [1792, TT])
                    S.dma(lambda e: e.dma_start(out=o3[:, :], in_=rwT[:, :]), reads=['rwT'])
                S.barrier()
                S.flush()
            if stop_after <= 1:
                return nc, dbg_outs
            with contextlib.ExitStack() as ph:
                pT = [sb(ph, "pT%d" % i, [128, 512], BF16) for i in range(3)]
                rec = sb(ph, "rec", [128, 8], F32)
                atok = sb(ph, "atok", [128, 8, 64], BF16)
                pi = 0

                def emit_scores(q0, st, hp, pi):
                    sb0 = 2 * (hp % 2)
                    scb = PS[:, sb0 * 512:(sb0 + 2) * 512].rearrange("p (b n) -> p b n", b=2)
                    sck = 'P%d' % sb0
                    kvh = hp // 2
                    for hh in range(2):
                        S.op('pe', lambda e, hh=hh, hp=hp, scb=scb, kvh=kvh, st=st, q0=q0: e.matmul(
                            scb[:, hh, 0:256],
                            lhsT=kTd[hh * 64:(hh + 1) * 64, kvh, st * 128:(st + 1) * 128],
                            rhs=qT_all[hh * 64:(hh + 1) * 64, hp, q0:q0 + 256], start=True, stop=True),
                            reads=['kTd', 'qT_all'], writes=[sck])
                    pt = pT[pi % 3]; ptk = 'pT%d' % (pi % 3)
                    S.op('act', lambda e, pt=pt, scb=scb: e.activation(out=pt[:].rearrange('p (b n) -> p b n', b=2), in_=scb[:, :, 0:256], func=AF.Exp, scale=0.125),
                         reads=[sck], writes=[ptk])
                    return (st, hp, pt, ptk, kvh)

                def emit_pv(st, hp, pt, ptk, kvh):
                    for hh in range(2):
                        head = 2 * hp + hh
                        for qt in range(2):
                            ab = P[4 + 2 * qt + head // 4]; abk = 'P%d' % (4 + 2 * qt + head // 4)
                            c0 = (head % 4) * 65
                            S.op('pe', lambda e, ab=ab, c0=c0, pt=pt, hh=hh, qt=qt, st=st, kvh=kvh, head=head: e.matmul(
                                ab[:, c0:c0 + 65], lhsT=pt[:, hh * 256 + qt * 128:hh * 256 + (qt + 1) * 128],
                                rhs=Vx[:, st, kvh, 0:65], start=(st == 0 and head % 4 == 0), stop=(st == NT - 1 and head % 4 == 3)),
                                reads=[ptk, 'Vx'], writes=[abk])

                for g in range(int(os.environ.get('K_NG', 16))):
                    q0 = g * 256
                    pend_ = None
                    for st in range(NT):
                        for hp in range(4):
                            cur_ = emit_scores(q0, st, hp, pi)
                            pi += 1
                            if pend_ is not None:
                                emit_pv(*pend_)
                            pend_ = cur_
                    emit_pv(*pend_)
                    for qt in range(2 if not os.environ.get('K_SKIP_NORM') else 0):
                        for half in range(2):
                            ab = P[4 + 2 * qt + half]; abk = 'P%d' % (4 + 2 * qt + half)
                            av = ab[:, 0:260].rearrange("p (h c) -> p h c", c=65)
                            S.op('dve', lambda e, av=av, half=half: e.reciprocal(out=rec[:, half * 4:(half + 1) * 4], in_=av[:, :, 64]),
                                 reads=[abk], writes=['rec'])
                            S.op('dve', lambda e, av=av, half=half: e.tensor_tensor(
                                out=atok[:, half * 4:(half + 1) * 4, :], in0=av[:, :, 0:64],
                                in1=rec[:, half * 4:(half + 1) * 4].unsqueeze(2).to_broadcast([128, 4, 64]), op=ALU.mult),
                                reads=[abk, 'rec'], writes=['atok'])
                        pa = P[0][:].bitcast(BF16)
                        if os.environ.get('K_SKIP_TR'):
                            continue
                        for hp in range(4):
                            S.op('pe', lambda e, hp=hp, pa=pa: e.transpose(
                                out=pa[:, hp * 128:(hp + 1) * 128],
                                in_=atok[:, 2 * hp:2 * hp + 2, :].rearrange("p a d -> p (a d)"), identity=identb[:]),
                                reads=['atok', 'identb'], writes=['P0'])
                        if os.environ.get('K_SKIP_CP'):
                            continue
                        S.op('dve', lambda e, pa=pa, q0=q0, qt=qt: e.tensor_copy(
                            out=attT_all[:, :, q0 + qt * 128:q0 + (qt + 1) * 128],
                            in_=pa[:, 0:512].rearrange("p (j t) -> p j t", j=4)), reads=['P0'], writes=['attT_all'])
                S.dma(lambda e: e.dma_start(out=attD[:, :], in_=attT_all[:].rearrange("p a t -> p (a t)")), reads=['attT_all'], writes=['attD'])
                if dbg:
                    o1 = dout("dbg_attT", [128, 4 * TL], BF16)
                    S.dma(lambda e: e.dma_start(out=o1[:, :], in_=attT_all[:].rearrange("p a t -> p (a t)")), reads=['attT_all'])
                S.barrier()
                S.flush()
        if stop_after <= 2:
            return nc, dbg_outs

        def TTo(eng, out, a, b_, op, R, W):
            S.op(eng, lambda e: e.tensor_tensor(out=out, in0=a, in1=b_, op=op), reads=R, writes=W)

        def CP(eng, out, in_, R, W):
            if eng == 'act':
                S.op('act', lambda e: e.copy(out=out, in_=in_), reads=R, writes=W)
            else:
                S.op(eng, lambda e: e.tensor_copy(out=out, in_=in_), reads=R, writes=W)

        def ACTF(out, in_, func, R, W, scale=1.0, bias=None):
            if bias is None:
                S.op('act', lambda e: e.activation(out=out, in_=in_, func=func, scale=scale), reads=R, writes=W)
            else:
                S.op('act', lambda e: e.activation(out=out, in_=in_, func=func, scale=scale, bias=bias), reads=R, writes=W)

        def MM(out, lhsT, rhs, R, W, start=True, stop=True):
            S.op('pe', lambda e: e.matmul(out, lhsT=lhsT, rhs=rhs, start=start, stop=stop), reads=R, writes=W)

        def TR(out, in_, idn, R, W):
            S.op('pe', lambda e: e.transpose(out=out, in_=in_, identity=idn), reads=R, writes=W)

        with contextlib.ExitStack() as ph:
            def t32(name, shape=(64, 1024)):
                return sb(ph, name, list(shape), F32)
            rp = t32("rp", (64, 8, 10)); rpd = t32("rpd", (64, 8, 8))
            lmu = t32("lmu", (128, 3)); lmd = t32("lmd", (128, 6))
            decupb = sb(ph, "decupb", [64, 2, 512], BF16); iclupb = sb(ph, "iclupb", [64, 2, 512], BF16)
            gateupb = sb(ph, "gateupb", [128, 512], BF16)
            mk4 = t32("mk4", (128, 2, 512)); mn1 = t32("mn1", (128, 2, 128)); bm = t32("bm", (128, 512))
            rst = t32("rst"); ones64 = sb(ph, "ones64", [64, 64], BF16); tiny = t32("tiny", (64, 1))
            S.dma(lambda e: e.dma_start(out=rp[:].rearrange("k h n -> k (h n)"), in_=rp_d[:, :]), writes=['rp'])
            S.dma(lambda e: e.dma_start(out=lmu[:], in_=lmu_d[:, :]), writes=['lmu'])
            S.dma(lambda e: e.dma_start(out=mk4[:].rearrange("p a n -> p (a n)"), in_=mk4_d[:, :]), writes=['mk4'])
            S.dma(lambda e: e.dma_start(out=mn1[:].rearrange("p a n -> p (a n)"), in_=mn1_d[:, :]), writes=['mn1'])
            S.dma(lambda e: e.dma_start(out=bm[:], in_=bm_d[:, :]), writes=['bm'])
            S.dma(lambda e: e.dma_start(out=rst[:], in_=rst_d[:, :]), writes=['rst'])
            S.op('pool', lambda e: e.memset(ones64[:], 1.0), writes=['ones64'])
            S.op('pool', lambda e: e.memset(tiny[:], 1e-24), writes=['tiny'])
            for i in range(3):
                S.op('dve', lambda e, i=i: e.tensor_scalar(out=rpd[:, :, 2 * i], in0=rp[:, :, i], scalar1=0.5, scalar2=None, op0=ALU.mult),
                     reads=['rp'], writes=['rpd'])
                S.op('dve', lambda e, i=i: e.tensor_scalar(out=rpd[:, :, 2 * i + 1], in0=rp[:, :, i], scalar1=-1.0, scalar2=1.0,
                                                          op0=ALU.mult, op1=ALU.add), reads=['rp'], writes=['rpd'])
                S.op('dve', lambda e, i=i: e.tensor_scalar(out=lmd[:, 2 * i:2 * i + 1], in0=lmu[:, i:i + 1], scalar1=0.5, scalar2=None, op0=ALU.mult),
                     reads=['lmu'], writes=['lmd'])
                S.op('dve', lambda e, i=i: e.tensor_scalar(out=lmd[:, 2 * i + 1:2 * i + 2], in0=lmu[:, i:i + 1], scalar1=-1.0, scalar2=1.0,
                                                          op0=ALU.mult, op1=ALU.add), reads=['lmu'], writes=['lmd'])
            S.op('dve', lambda e: e.tensor_scalar(out=rpd[:, :, 6], in0=rp[:, :, 4], scalar1=-1.0, scalar2=1.0, op0=ALU.mult, op1=ALU.add),
                 reads=['rp'], writes=['rpd'])

            def bc(t2):
                return t2.unsqueeze(2).to_broadcast([64, 8, 128])

            pin = [sb(ph, "pin%d" % i, [64, 8, 130], F32) for i in range(3)]
            plo = [sb(ph, "plo%d" % i, [128, 130], F32) for i in range(3)]
            tS = t32("tS"); xr = t32("xr"); xk = t32("xk"); xv = t32("xv")
            xlo = t32("xlo", (128, 3, 128)); twl = sb(ph, "twl", [64, 128], BF16); xalb = sb(ph, "xalb", [64, 128], BF16)
            glsb = sb(ph, "glsb", [128, 128], BF16)
            sqb = sb(ph, "sqb", [64, 1024], BF16); kk = t32("kk")
            sg = t32("sg"); ad = t32("ad"); cs = t32("cs"); ex = t32("ex"); E1 = t32("E1"); E2s = [t32("E2_0"), t32("E2_1")]; E3 = t32("E3")
            bb = t32("bb"); t1 = t32("t1"); kd = bb; bt32 = t32("bt32"); kt32 = t32("kt32"); rkb = sqb; kkk = t1; rs = E1; csb = bt32; bv = kt32
            bvt = t32("bvt", (128, 512)); gtok = t32("gtok", (128, 512)); glS = t32("glS", (128, 128))
            opTs = [{n: sb(ph, "op%d_" % q_ + n, [64, 1024], BF16) for n in ("a", "r", "b", "k", "bh", "kh", "v")} for q_ in range(2)]
            N1Ta, N1a, IN1Ta, N2a, N2Ta, IN2Ta, N4a, N4Ta, IN4Ta, IN8Ta, AakTa, ArbTa, ArkTa = [
                sb(ph, "ba%d" % i, [128, 8, 128], BF16) for i in range(13)]
            TMa = sb(ph, "TMa", [128, 8, 5, 64], BF16)
            Xa = [sb(ph, "Xa%d" % i, [128, 8, 128], BF16) for i in range(2)]
            Bbd = sb(ph, "Bbd", [128, 512], BF16); Ubd = sb(ph, "Ubd", [128, 512], BF16); Vbd = sb(ph, "Vbd", [128, 512], BF16)
            GTs = sb(ph, "GTs", [64, 512], BF16); Es = t32("Es", (64, 512)); QTs = sb(ph, "QTs", [64, 128], BF16)
            Y0s = t32("Y0s", (128, 64)); ytmp = gtok; yc = t32("yc", (128, 64))
            Yblk = t32("Yblk", (128, 8, 64))
            H = t32("H", (64, 512)); Hb = sb(ph, "Hb", [64, 512], BF16); Ht = t32("Ht", (64, 512))

            def view_hct(t):
                return t[:, :]

            def view_out(t):
                return t[:, :]

            def g16(t):
                return t[:, :].rearrange("k (g t) -> k g t", t=16)

            def chv(t, c):
                return t[:, :].rearrange("k (h c t) -> k h c t", h=8, c=8)[:, :, c, :]

            for (dst, src, nm, rows) in ((decupb, decup_d, 'decupb', 64), (iclupb, iclup_d, 'iclupb', 64), (gateupb, gateup_d, 'gateupb', 128)):
                stg_, sk_ = (bt32, 'bt32') if rows == 64 else (bvt, 'bvt')
                S.dma(lambda e, src=src, stg_=stg_: e.dma_start(out=stg_[:, :], in_=src[:, :]), writes=[sk_])
                dv = dst[:].rearrange("p a n -> p (a n)") if rows == 64 else dst[:]
                CP('dve', dv, stg_[:, :], [sk_], [nm])
            blocks = []
            nblk = int(os.environ.get('K_RBLK', 99))
            for d_ in range(2):
                cb_ = [0, 1] if d_ == 0 else [1, 0]
                lb_ = list(range(2, NT)) if d_ == 0 else list(range(NT - 1, 1, -1))
                blocks += [(d_, b_, j_ == 0) for j_, b_ in enumerate((cb_ + lb_)[:nblk])]

            def emit_prep(idx):
                d, blk, first = blocks[idx]
                opT = opTs[idx % 2]; E2 = E2s[idx % 2]; e2k = 'E2_%d' % (idx % 2); opk = 'o%d_' % (idx % 2)

                u0 = blk * 128
                is_ctx = blk < 2
                seq_lo, seq_hi = (0, TC) if is_ctx else (TC, TT)
                lo = max(u0 - 1, seq_lo); hi = min(u0 + 129, seq_hi)
                c_lo = lo - (u0 - 1); c_hi = c_lo + (hi - lo)
                for i in range(3):
                    if c_lo > 0:
                        S.op('pool', lambda e, i=i: e.memset(pin[i][:, :, 0:1], 0.0), writes=['pin%d' % i])
                    if c_hi < 130:
                        S.op('pool', lambda e, i=i: e.memset(pin[i][:, :, 129:130], 0.0), writes=['pin%d' % i])
                    S.dma(lambda e, i=i, lo=lo, hi=hi, c_lo=c_lo, c_hi=c_hi: e.dma_start(
                        out=pin[i][:, :, c_lo:c_hi],
                        in_=rwT[i * 512:(i + 1) * 512, lo:hi].rearrange("(h k) t -> k h t", k=64)), reads=['rwT'], writes=['pin%d' % i])
                for i, (r0, nr) in enumerate(((1536, 64), (1600, 64), (1664, 128))):
                    if c_lo > 0:
                        S.op('pool', lambda e, i=i: e.memset(plo[i][:, 0:1], 0.0), writes=['plo%d' % i])
                    if c_hi < 130:
                        S.op('pool', lambda e, i=i: e.memset(plo[i][:, 129:130], 0.0), writes=['plo%d' % i])
                    S.dma(lambda e, i=i, r0=r0, nr=nr, lo=lo, hi=hi, c_lo=c_lo, c_hi=c_hi: e.dma_start(
                        out=plo[i][0:nr, c_lo:c_hi], in_=rwT[r0:r0 + nr, lo:hi]), reads=['rwT'], writes=['plo%d' % i])
                for i, xo in enumerate((xr, xk, xv)):
                    xo3 = xo[:, :].rearrange("k (h t) -> k h t", h=8)
                    ts3 = tS[:, :].rearrange("k (h t) -> k h t", h=8)
                    TTo('pool', ts3, pin[i][:, :, 0:128], pin[i][:, :, 2:130], ALU.add, ['pin%d' % i], ['tS'])
                    TTo('pool', ts3, ts3, bc(rpd[:, :, 2 * i]), ALU.mult, ['tS', 'rpd'], ['tS'])
                    TTo('dve', xo3, pin[i][:, :, 1:129], bc(rpd[:, :, 2 * i + 1]), ALU.mult, ['pin%d' % i, 'rpd'], ['x%d' % i])
                    TTo('dve', xo3, xo3, ts3, ALU.add, ['x%d' % i, 'tS'], ['x%d' % i])
                for i, nr in enumerate((64, 64, 128)):
                    S.op('pool', lambda e, i=i, nr=nr: e.tensor_tensor(out=tS[0:nr, 0:128] if nr == 64 else glS[:, 0:128],
                                                                     in0=plo[i][0:nr, 0:128], in1=plo[i][0:nr, 2:130], op=ALU.add),
                         reads=['plo%d' % i], writes=['tS' if nr == 64 else 'glS'])
                    S.op('dve', lambda e, i=i, nr=nr: e.tensor_scalar(out=xlo[0:nr, i, :], in0=plo[i][0:nr, 1:129],
                                                                    scalar1=lmd[0:nr, 2 * i + 1:2 * i + 2], scalar2=None, op0=ALU.mult),
                         reads=['plo%d' % i, 'lmd'], writes=['xlo'])
                    S.op('dve', lambda e, i=i, nr=nr: e.scalar_tensor_tensor(
                        out=xlo[0:nr, i, :], in0=(tS[0:nr, 0:128] if nr == 64 else glS[:, 0:128]), scalar=lmd[0:nr, 2 * i:2 * i + 1],
                        in1=xlo[0:nr, i, :], op0=ALU.mult, op1=ALU.add),
                        reads=['tS' if nr == 64 else 'glS', 'lmd', 'xlo'], writes=['xlo'])
                ACTF(twl[:], xlo[0:64, 0, :], AF.Tanh, ['xlo'], ['twl'])
                CP('pool', xalb[:], xlo[0:64, 1, :], ['xlo'], ['xalb'])
                k3 = lambda t: t[:, :].rearrange("k (h t) -> k h t", h=8)
                TTo('pool', k3(kkk), k3(xk), bc(rp[:, :, 3]), ALU.mult, ['x1', 'rp'], ['t1'])
                ACTF(sqb[:], kkk[:], AF.Square, ['t1'], ['sqb'])
                PP = PS[0:64, 0:1024]
                for hf in range(2):
                    MM(PS[0:64, hf * 512:(hf + 1) * 512], ones64[:], sqb[:, hf * 512:(hf + 1) * 512], ['ones64', 'sqb'], ['P0'])
                ACTF(rs[:], PP, AF.Ln, ['P0', 'tiny'], ['E1'], bias=tiny[:])
                ACTF(rs[:], rs[:], AF.Exp, ['E1'], ['E1'], scale=-0.5)
                TTo('dve', kk[:], kkk[:], rs[:], ALU.mult, ['t1', 'E1'], ['kk'])
                for h in range(8):
                    MM(PS[0:64, h * 128:(h + 1) * 128], decupb[:, d, h * 64:(h + 1) * 64], twl[:], ['decupb', 'twl'], ['P0'])
                TTo('dve', k3(sg), PP.rearrange("k (h t) -> k h t", h=8), bc(rp[:, :, 6 + d]), ALU.add, ['P0', 'rp'], ['sg'])
                ACTF(sg[:], sg[:], AF.Sigmoid, ['sg'], ['sg'])
                for h in range(8):
                    MM(PS[0:64, h * 128:(h + 1) * 128], iclupb[:, d, h * 64:(h + 1) * 64], xalb[:], ['iclupb', 'xalb'], ['P0'])
                TTo('dve', k3(ad), PP.rearrange("k (h t) -> k h t", h=8), bc(rp[:, :, 8 + d]), ALU.add, ['P0', 'rp'], ['ad'])
                ACTF(ad[:], ad[:], AF.Sigmoid, ['ad'], ['ad'])
                S.op('dve', lambda e: e.tensor_tensor_scan(out=cs[:], data0=rst[:], data1=sg[:], initial=0.0, op0=ALU.mult, op1=ALU.add),
                     reads=['rst', 'sg'], writes=['cs'])
                csf = cs
                if d == 1:
                    TTo('pool', ex[:], sg[:], cs[:], ALU.subtract, ['sg', 'cs'], ['ex'])
                    csv = cs[:, :].rearrange("k (g t) -> k g t", t=16)
                    TTo('dve', csb[:, :].rearrange("k (g t) -> k g t", t=16), ex[:, :].rearrange("k (g t) -> k g t", t=16),
                       csv[:, :, 15:16].to_broadcast([64, 64, 16]), ALU.add, ['ex', 'cs'], ['bt32'])
                    csf = csb
                ACTF(E2[:], csf[:], AF.Exp, ['cs', 'bt32'], [e2k], scale=DEC_C)
                TTo('pool', ex[:], csf[:], sg[:], ALU.subtract, ['cs', 'bt32', 'sg'], ['ex'])
                ACTF(E1[:], ex[:], AF.Exp, ['ex'], ['E1'], scale=DEC_C)
                S.op('dve', lambda e: e.reciprocal(out=E3[:], in_=E2[:]), reads=[e2k], writes=['E3'])
                tsel = 15 if d == 0 else 0
                def cm(t, h):
                    return t[:, :].rearrange("k (c h t) -> k c h t", c=8, h=8)[:, :, h, :]

                def hm(t, h):
                    return t[:, :].rearrange("k (h c t) -> k h c t", h=8, c=8)[:, h, :, :]
                TTo('pool', bb[:], kk[:], ad[:], ALU.mult, ['kk', 'ad'], ['bb'])
                TTo('dve', bt32[:], bb[:], E3[:], ALU.mult, ['bb', 'E3'], ['bt32'])
                TTo('pool', k3(t1), k3(ad), bc(rp[:, :, 4]), ALU.mult, ['ad', 'rp'], ['t1'])
                TTo('dve', k3(t1), k3(t1), bc(rpd[:, :, 6]), ALU.add, ['t1', 'rpd'], ['t1'])
                TTo('pool', kd[:], xk[:], t1[:], ALU.mult, ['x1', 't1'], ['bb'])
                TTo('dve', kt32[:], kd[:], E3[:], ALU.mult, ['bb', 'E3'], ['kt32'])
                for h in range(8):
                    pch = hm(E2, h)[:, :, tsel:tsel + 1].to_broadcast([64, 8, 16])
                    S.op('dve', lambda e, h=h: e.scalar_tensor_tensor(out=cm(opT['a'], h), in0=hm(kk, h), scalar=-1.0, in1=hm(E1, h),
                                                                     op0=ALU.mult, op1=ALU.mult), reads=['kk', 'E1'], writes=[opk + 'a'])
                    TTo('pool', cm(opT['r'], h), hm(xr, h), hm(E2, h), ALU.mult, ['x0', e2k], [opk + 'r'])
                    CP('act', cm(opT['b'], h), hm(bt32, h), ['bt32'], [opk + 'b'])
                    TTo('pool', cm(opT['bh'], h), hm(bt32, h), pch, ALU.mult, ['bt32', e2k], [opk + 'bh'])
                    CP('act', cm(opT['k'], h), hm(kt32, h), ['kt32'], [opk + 'k'])
                    TTo('dve', cm(opT['kh'], h), hm(kt32, h), pch, ALU.mult, ['kt32', e2k], [opk + 'kh'])
                    CP('act', cm(opT['v'], h), hm(xv, h), ['x2'], [opk + 'v'])
                if d == 0 and not is_ctx:
                    ul = u0 - TC
                    TTo('pool', k3(t1), k3(xr), bc(rp[:, :, 5]), ALU.mult, ['x0', 'rp'], ['t1'])
                    TTo('dve', rkb[:], t1[:], xk[:], ALU.mult, ['t1', 'x1'], ['sqb'])
                    for hf in range(2):
                        MM(PS[0:64, hf * 512:(hf + 1) * 512], ones64[:], rkb[:, hf * 512:(hf + 1) * 512], ['ones64', 'sqb'], ['P0'])
                    TTo('dve', bv[:], PP, xv[:], ALU.mult, ['P0', 'x2'], ['kt32'])
                    for h in range(8):
                        TR(PS[:, 512 + h * 64:512 + (h + 1) * 64], bv[:, h * 128:(h + 1) * 128], identf[0:64, 0:64], ['kt32', 'identf'], ['P0'])
                    CP('dve', bvt[:], PS[:, 512:1024], ['P0'], ['bvt'])
                    S.dma(lambda e, ul=ul: e.dma_start(out=bonD[ul:ul + 128, :], in_=bvt[:]), reads=['bvt'], writes=['bonD'])
                    ACTF(glsb[:], xlo[:, 2, :], AF.Sigmoid, ['xlo'], ['glsb'])
                    MM(PS[:, 512:1024], glsb[:], gateupb[:], ['glsb', 'gateupb'], ['P0'])
                    CP('dve', gtok[:], PS[:, 512:1024], ['P0'], ['gtok'])
                    S.dma(lambda e, ul=ul: e.dma_start(out=gD[ul:ul + 128, :], in_=gtok[:]), reads=['gtok'], writes=['gD'])

            def emit_chunks(idx):
                d, blk, first = blocks[idx]
                opT = opTs[idx % 2]; E2 = E2s[idx % 2]; e2k = 'E2_%d' % (idx % 2); opk = 'o%d_' % (idx % 2)
                u0 = blk * 128
                is_ctx = blk < 2
                tsel = 15 if d == 0 else 0
                if first:
                    S.op('pool', lambda e: e.memset(H[:], 0.0), writes=['H'])
                    S.op('pool', lambda e: e.memset(Hb[:], 0.0), writes=['Hb'])

                PA_, PB_, PC_, PD_ = (PS[:, 0:1024], PS[:, 1024:2048], PS[:, 2048:3072], PS[:, 3072:4096])
                kPB, kPC, kPD = ['P2', 'P3'], ['P4', 'P5'], ['P6', 'P7']
                c8 = lambda ap: ap.rearrange("p (c n) -> p c n", c=8)
                def bc8(m):
                    return m.unsqueeze(1).to_broadcast([128, 8, 128])
                MSd = mk4[:, d, 0:128]; MId = mk4[:, d, 128:256]; MStd = mn1[:, d, :]
                def ch(name, c):
                    return opT[name][:, c * 128:(c + 1) * 128]
                for c in range(8):
                    MM(PB_[:, c * 128:(c + 1) * 128], ch('b', c), ch('a', c), [opk + 'b', opk + 'a'], kPB)
                for c in range(8):
                    MM(PC_[:, c * 128:(c + 1) * 128], ch('a', c), ch('b', c), [opk + 'a', opk + 'b'], kPC)
                for c in range(8):
                    MM(PD_[:, c * 128:(c + 1) * 128], ch('k', c), ch('a', c), [opk + 'k', opk + 'a'], kPD)
                TTo('dve', N1Ta[:], c8(PB_), bc8(MSd), ALU.mult, kPB + ['mk4'], ['N1Ta'])
                TTo('dve', N1a[:], c8(PC_), bc8(MStd), ALU.mult, kPC + ['mn1'], ['N1a'])
                TTo('dve', AakTa[:], c8(PD_), bc8(MSd), ALU.mult, kPD + ['mk4'], ['AakTa'])
                TTo('pool', IN1Ta[:], N1Ta[:], bc8(identb[:, :]), ALU.add, ['N1Ta', 'identb'], ['IN1Ta'])
                PBb = PB_.bitcast(BF16)
                for c in range(8):
                    for si, nm_ in ((0, 'a'), (1, 'v'), (2, 'bh'), (3, 'kh')):
                        TR(PBb[:, c * 256 + si * 64:c * 256 + (si + 1) * 64], ch(nm_, c), identb[0:64, 0:64], [opk + nm_, 'identb'], kPB)
                PBb4 = PBb.rearrange("p (c s n) -> p c s n", c=8, s=4)
                CP('dve', TMa[:, :, 0, :], PBb4[:, :, 0, :], kPB, ['TMa'])
                CP('dve', TMa[:, :, 2:5, :].rearrange("p c s n -> p c (s n)"), PBb.rearrange("p (c n) -> p c n", c=8)[:, :, 64:256], kPB, ['TMa'])
                for c in range(8):
                    MM(PC_[:, c * 128:(c + 1) * 128], N1Ta[:, c, :], N1a[:, c, :], ['N1Ta', 'N1a'], kPC)
                for c in range(8):
                    MM(PD_[:, c * 128:(c + 1) * 128], N1a[:, c, :], N1Ta[:, c, :], ['N1Ta', 'N1a'], kPD)
                CP('act', N2a[:], c8(PC_), kPC, ['N2a'])
                CP('dve', N2Ta[:], c8(PD_), kPD, ['N2Ta'])
                TTo('pool', IN2Ta[:], N2Ta[:], bc8(identb[:, :]), ALU.add, ['N2Ta', 'identb'], ['IN2Ta'])
                for c in range(8):
                    MM(PB_[:, c * 64:(c + 1) * 64], AakTa[:, c, :], TMa[:, c, 2, :], ['AakTa', 'TMa'], kPB)
                CP('dve', TMa[:, :, 1, :], PB_[:, 0:512].rearrange("p (c n) -> p c n", c=8), kPB, ['TMa'])
                for c in range(8):
                    MM(PC_[:, c * 128:(c + 1) * 128], N2Ta[:, c, :], N2a[:, c, :], ['N2Ta', 'N2a'], kPC)
                for c in range(8):
                    MM(PD_[:, c * 128:(c + 1) * 128], N2a[:, c, :], N2Ta[:, c, :], ['N2Ta', 'N2a'], kPD)
                CP('act', N4a[:], c8(PC_), kPC, ['N4a'])
                CP('dve', N4Ta[:], c8(PD_), kPD, ['N4Ta'])
                TTo('pool', IN4Ta[:], N4Ta[:], bc8(identb[:, :]), ALU.add, ['N4Ta', 'identb'], ['IN4Ta'])
                for c in range(8):
                    MM(PB_[:, c * 128:(c + 1) * 128], N4a[:, c, :], N4Ta[:, c, :], ['N4a', 'N4Ta'], kPB)
                TTo('dve', IN8Ta[:], c8(PB_), bc8(identf[:, :]), ALU.add, kPB + ['identf'], ['IN8Ta'])
                if not is_ctx:
                    for c in range(8):
                        MM(PC_[:, c * 128:(c + 1) * 128], ch('b', c), ch('r', c), [opk + 'b', opk + 'r'], kPC)
                    for c in range(8):
                        MM(PD_[:, c * 128:(c + 1) * 128], ch('k', c), ch('r', c), [opk + 'k', opk + 'r'], kPD)
                    TTo('dve', ArbTa[:], c8(PC_), bc8(MId), ALU.mult, kPC + ['mk4'], ['ArbTa'])
                    TTo('dve', ArkTa[:], c8(PD_), bc8(MId), ALU.mult, kPD + ['mk4'], ['ArkTa'])
                xsrc = None
                for li, (INa, ik) in enumerate(((IN8Ta, 'IN8Ta'), (IN4Ta, 'IN4Ta'), (IN2Ta, 'IN2Ta'), (IN1Ta, 'IN1Ta'))):
                    Pq, kq = (PB_, kPB) if li % 2 == 0 else (PC_, kPC)
                    for c in range(8):
                        rhs_ = TMa[:, c, 0:2, :].rearrange("p a n -> p (a n)") if xsrc is None else xsrc[:, c, :]
                        MM(Pq[:, c * 128:(c + 1) * 128], INa[:, c, :], rhs_, [ik, 'TMa' if xsrc is None else xk_], kq)
                    Xn = Xa[li % 2]; xk_ = 'Xa%d' % (li % 2)
                    CP('dve' if li % 2 else 'act', Xn[:], c8(Pq), kq, [xk_])
                    xsrc = Xn
                B3 = PS[:, 1536:2048]; B4 = PS[:, 2048:2560]; B5 = PS[:, 2560:3072]; B6 = PS[:, 3072:3584]; B7 = PS[:, 3584:4096]
                bm3 = bm[:, :].rearrange("p (h n) -> p h n", h=8)
                nch = int(os.environ.get('K_RCH', 8))
                for c in (list(range(8)) if d == 0 else list(range(7, -1, -1)))[:nch]:
                    Wc = xsrc[:, c, 0:64]; U0 = xsrc[:, c, 64:128]
                    Bh_ = TMa[:, c, 3, :]; Kh_ = TMa[:, c, 4, :]; Vt_ = TMa[:, c, 2, :]
                    for dst_, src_, rk_, wk_ in ((Bbd, Bh_, 'TMa', 'Bbd'), (Ubd, U0, xk_, 'Ubd'), (Vbd, Vt_, 'TMa', 'Vbd')):
                        TTo('pool', dst_[:, :].rearrange("p (h n) -> p h n", h=8), src_.unsqueeze(1).to_broadcast([128, 8, 64]), bm3, ALU.mult,
                            [rk_, 'bm'], [wk_])
                    MM(B4[0:64, :], Wc, Bbd[:], [xk_, 'Bbd'], ['P4'])
                    CP('act', GTs[:], B4[0:64, :], ['P4'], ['GTs'])
                    if not is_ctx:
                        MM(B6[0:64, 0:128], Wc, ArbTa[:, c, :], [xk_, 'ArbTa'], ['P6'])
                        TTo('dve', QTs[:], B6[0:64, 0:128], ch('r', c), ALU.add, ['P6', opk + 'r'], ['QTs'])
                        MM(B6[:, 128:192], ArbTa[:, c, :], U0, ['ArbTa', xk_], ['P6'], start=True, stop=False)
                        MM(B6[:, 128:192], ArkTa[:, c, :], Vt_, ['ArkTa', 'TMa'], ['P6'], start=False, stop=True)
                        CP('dve', Y0s[:], B6[:, 128:192], ['P6'], ['Y0s'])
                        MM(B7, QTs[:], Hb[:], ['QTs', 'Hb'], ['P7'])
                        TTo('dve', ytmp[:], B7, bm[:], ALU.mult, ['P7', 'bm'], ['gtok'])
                        S.op('dve', lambda e: e.tensor_reduce(out=yc[:], in_=ytmp[:, :].rearrange("p (h v) -> p v h", h=8), axis=AX.X, op=ALU.add),
                             reads=['gtok'], writes=['yc'])
                        TTo('pool', Yblk[:, c, :], yc[:], Y0s[:], ALU.add, ['yc', 'Y0s'], ['Yblk'])
                    MM(B3[0:64, :], Bh_, Ubd[:], ['TMa', 'Ubd'], ['P3'], start=True, stop=False)
                    MM(B3[0:64, :], Kh_, Vbd[:], ['TMa', 'Vbd'], ['P3'], start=False, stop=False)
                    for h in range(8):
                        MM(B3[0:64, h * 64:(h + 1) * 64], GTs[:, h * 64:(h + 1) * 64], Hb[:, h * 64:(h + 1) * 64], ['GTs', 'Hb'], ['P3'],
                           start=False, stop=(h == 7))
                    PCc = chv(E2, c)[:, :, tsel:tsel + 1].to_broadcast([64, 8, 64])
                    TTo('pool', Ht[:, :].rearrange("k (h v) -> k h v", h=8), H[:, :].rearrange("k (h v) -> k h v", h=8), PCc, ALU.mult,
                        ['H', e2k], ['Ht'])
                    TTo('dve', Hb[:], Ht[:], B3[0:64, :], ALU.add, ['Ht', 'P3'], ['Hb'])
                    TTo('dve', H[:], Ht[:], B3[0:64, :], ALU.add, ['Ht', 'P3'], ['H'])
                    S.drain(S.pend, (len(S.pend) + 7) // 8)
                if not is_ctx:
                    ul = u0 - TC
                    for h in range(8):
                        S.dma(lambda e, h=h, ul=ul, d=d: e.dma_start(
                            out=ydir[d, ul:ul + 128, h * 64:(h + 1) * 64].rearrange("(c t) v -> t c v", t=16),
                            in_=Yblk[h * 16:(h + 1) * 16, :, :]), reads=['Yblk'], writes=['ydir'])

            S.capture = []
            emit_prep(0)
            pend = S.capture; S.capture = None
            S.drain(pend, len(pend))
            for idx in range(len(blocks)):
                pend = []
                if idx + 1 < len(blocks):
                    S.capture = []
                    emit_prep(idx + 1)
                    pend = S.capture; S.capture = None
                S.pend = pend
                emit_chunks(idx)
                S.drain(pend, len(pend))

            if dbg:
                oy = dout("dbg_y", [2 * TL, 512]); ob = dout("dbg_bon", [TL, 512]); og = dout("dbg_g", [TL, 512])
                S.dma(lambda e: e.dma_start(out=oy[:, :], in_=ydir.rearrange("d t n -> (d t) n")), reads=['ydir'])
                S.dma(lambda e: e.dma_start(out=ob[:, :], in_=bonD[:, :]), reads=['bonD'])
                S.dma(lambda e: e.dma_start(out=og[:, :], in_=gD[:, :]), reads=['gD'])
                oH = dout("dbg_H", [64, 512])
                S.dma(lambda e: e.dma_start(out=oH[:, :], in_=H[:]), reads=['H'])
            S.barrier()
            S.flush()
        if stop_after <= 3:
            return nc, dbg_outs

        with contextlib.ExitStack() as ph:
            woutb = sb(ph, "woutb", [128, 8, D], BF16)
            attT_all = sb(ph, "attT_c", [128, 4, TL], BF16)
            S.dma(lambda e: e.dma_start(out=attT_all[:].rearrange("p a t -> p (a t)"), in_=attD[:, :]), reads=['attD'], writes=['attT_all'])
            cst = {}
            for nm, src in (("gt1", modd[0:1, 2048:3072]), ("sh2", modd[0:1, 3072:4096]), ("sc2p", modd[0:1, 4096:5120]),
                            ("ln1g", ln1_d[0:1, :]), ("ln1b", ln1_d[1:2, :])):
                cst[nm] = sb(ph, nm, [128, D], F32)
                S.dma(lambda e, nm=nm, src=src: e.dma_start(out=cst[nm][:], in_=src.partition_broadcast(128)), reads=['modd'], writes=[nm])
            for nm, row in (("lnxg", 0), ("lnxb", 1)):
                cst[nm] = sb(ph, nm, [128, 512], F32)
                S.dma(lambda e, nm=nm, row=row: e.dma_start(out=cst[nm][:], in_=lnx_d[row:row + 1, :].partition_broadcast(128)), writes=[nm])
            wst2 = [sb(ph, "wst2_%d" % i, [128, D], F32) for i in range(2)]
            for j in range(8):
                w = wst2[j % 2]; wk = 'wst2_%d' % (j % 2)
                S.dma(lambda e, w=w, j=j: e.dma_start(out=w[:], in_=wout_d[j * 128:(j + 1) * 128, :]), writes=[wk])
                CP('pool' if j % 2 else 'dve', woutb[:, j, :], w[:], [wk], ['woutb'])
            rwf = sb(ph, "rwf", [128, 8, 16], F32)
            S.dma(lambda e: e.dma_start(out=rwf[:], in_=rw_d.rearrange("(j p) n -> p j n", p=128)), writes=['rwf'])
            gneps = sb(ph, "gneps", [128, 1], F32)
            S.op('pool', lambda e: e.memset(gneps[:], 64e-5), writes=['gneps'])
            yf = sb(ph, "yf", [128, 512], F32); yb = sb(ph, "yb", [128, 512], F32)
            bon = sb(ph, "bon", [128, 512], F32); gg = sb(ph, "gg", [128, 512], F32)
            ysum = sb(ph, "ysum", [128, 512], F32); ysq = sb(ph, "ysq", [128, 512], F32)
            gst = sb(ph, "gst", [128, 8], F32); gvar = sb(ph, "gvar", [128, 8], F32)
            rwob = sb(ph, "rwob", [128, 512], BF16); rwoT = sb(ph, "rwoT", [128, 4, 128], BF16)
            xin_t = sb(ph, "xin_t", [128, D], F32); tres = sb(ph, "tres", [128, D], F32)
            x1t = sb(ph, "x1t", [128, D], F32); h2f = sb(ph, "h2f", [128, D], F32); h2b = sb(ph, "h2b", [128, D], BF16)
            h2T = sb(ph, "h2T", [128, 8, 128], F32)
            stats = sb(ph, "statsC", [128, 2, 6], F32); mv = sb(ph, "mvC", [128, 2], F32)
            rstd = sb(ph, "rstdC", [128, 1], F32); nb = sb(ph, "nbC", [128, 1], F32)
            lntC = (stats, mv, rstd, nb, 'C')
            lmax = sb(ph, "lmax", [128, 1], F32); lex = sb(ph, "lex", [128, 16], F32); lsum = sb(ph, "lsum", [128, 1], F32)

            def v8(t):
                return t[:, :].rearrange("p (h v) -> p h v", h=8)

            def b8(t):
                return t[:, :].unsqueeze(2).to_broadcast([128, 8, 64])
            for i in range(int(os.environ.get('K_CT', 32))):
                t0 = i * 128
                S.dma(lambda e, t0=t0: e.dma_start(out=yf[:], in_=ydir[0, t0:t0 + 128, :]), reads=['ydir'], writes=['yf'])
                S.dma(lambda e, t0=t0: e.dma_start(out=yb[:], in_=ydir[1, t0:t0 + 128, :]), reads=['ydir'], writes=['yb'])
                S.dma(lambda e, t0=t0: e.dma_start(out=bon[:], in_=bonD[t0:t0 + 128, :]), reads=['bonD'], writes=['bon'])
                S.dma(lambda e, t0=t0: e.dma_start(out=gg[:], in_=gD[t0:t0 + 128, :]), reads=['gD'], writes=['gg'])
                S.dma(lambda e, t0=t0: e.dma_start(out=xin_t[:], in_=x_d[t0:t0 + 128, :]), writes=['xin_t'])
                TTo('pool', ysum[:], yf[:], yb[:], ALU.add, ['yf', 'yb'], ['ysum'])
                S.op('dve', lambda e: e.tensor_reduce(out=gst[:], in_=v8(ysum), axis=AX.X, op=ALU.add), reads=['ysum'], writes=['gst'])
                S.op('dve', lambda e: e.tensor_scalar(out=gst[:], in0=gst[:], scalar1=-1.0 / 64, scalar2=None, op0=ALU.mult),
                     reads=['gst'], writes=['gst'])
                TTo('dve', v8(ysum), v8(ysum), b8(gst), ALU.add, ['ysum', 'gst'], ['ysum'])
                ACTF(ysq[:], ysum[:], AF.Square, ['ysum'], ['ysq'])
                S.op('dve', lambda e: e.tensor_reduce(out=gvar[:], in_=v8(ysq), axis=AX.X, op=ALU.add), reads=['ysq'], writes=['gvar'])
                ACTF(gvar[:], gvar[:], AF.Ln, ['gvar', 'gneps'], ['gvar'], scale=1.0 / 64, bias=gneps[:])
                ACTF(gvar[:], gvar[:], AF.Exp, ['gvar'], ['gvar'], scale=-0.5)
                TTo('dve', v8(ysum), v8(ysum), b8(gvar), ALU.mult, ['ysum', 'gvar'], ['ysum'])
                TTo('pool', ysum[:], ysum[:], cst['lnxg'][:], ALU.mult, ['ysum', 'lnxg'], ['ysum'])
                TTo('dve', ysum[:], ysum[:], cst['lnxb'][:], ALU.add, ['ysum', 'lnxb'], ['ysum'])
                TTo('pool', ysum[:], ysum[:], bon[:], ALU.add, ['ysum', 'bon'], ['ysum'])
                TTo('dve', rwob[:], ysum[:], gg[:], ALU.mult, ['ysum', 'gg'], ['rwob'])
                pr_ = P[0][:].bitcast(BF16)
                for j in range(4):
                    TR(pr_[:, j * 128:(j + 1) * 128], rwob[:, j * 128:(j + 1) * 128], identb[:], ['rwob', 'identb'], ['P0'])
                CP('dve', rwoT[:], pr_[:, 0:512].rearrange("p (j t) -> p j t", j=4), ['P0'], ['rwoT'])
                for half in range(2):
                    ob = PS[:, 1024 + half * 512:1024 + (half + 1) * 512]
                    for j in range(8):
                        lt = attT_all[:, j, t0:t0 + 128] if j < 4 else rwoT[:, j - 4, :]
                        MM(ob, lt, woutb[:, j, half * 512:(half + 1) * 512], ['attT_all', 'rwoT', 'woutb'], ['P2'], start=(j == 0), stop=(j == 7))
                TTo('dve', tres[:], PS[:, 1024:2048], cst['gt1'][:], ALU.mult, ['P2', 'gt1'], ['tres'])
                S.op('dve', lambda e: e.scalar_tensor_tensor(out=tres[:], in0=xin_t[:], scalar=ALPHA, in1=tres[:], op0=ALU.mult, op1=ALU.add),
                     reads=['xin_t', 'tres'], writes=['tres'])
                ln_stats(lntC, tres, 'tres', eps5, 'eps5')
                S.op('act', lambda e: e.activation(out=x1t[:], in_=tres[:], func=AF.Identity, bias=nb[:], scale=rstd[:]),
                     reads=['tres', 'Cnb', 'Crstd'], writes=['x1t'])
                TTo('pool', x1t[:], x1t[:], cst['ln1g'][:], ALU.mult, ['x1t', 'ln1g'], ['x1t'])
                TTo('dve', x1t[:], x1t[:], cst['ln1b'][:], ALU.add, ['x1t', 'ln1b'], ['x1t'])
                S.dma(lambda e, t0=t0: e.dma_start(out=x1D[t0:t0 + 128, :], in_=x1t[:]), reads=['x1t'], writes=['x1D'])
                ln_stats(lntC, x1t, 'x1t', eps5, 'eps5')
                S.op('act', lambda e: e.activation(out=h2f[:], in_=x1t[:], func=AF.Identity, bias=nb[:], scale=rstd[:]),
                     reads=['x1t', 'Cnb', 'Crstd'], writes=['h2f'])
                TTo('pool', h2f[:], h2f[:], cst['sc2p'][:], ALU.mult, ['h2f', 'sc2p'], ['h2f'])
                TTo('dve', h2f[:], h2f[:], cst['sh2'][:], ALU.add, ['h2f', 'sh2'], ['h2f'])
                CP('pool', h2b[:], h2f[:], ['h2f'], ['h2b'])
                S.dma(lambda e, t0=t0: e.dma_start(out=h2D[t0:t0 + 128, :], in_=h2b[:]), reads=['h2b'], writes=['h2D'])
                for j in range(8):
                    TR(PS[:, 2048 + j * 128:2048 + (j + 1) * 128], h2f[:, j * 128:(j + 1) * 128], identf[:], ['h2f', 'identf'], ['P4'])
                CP('dve', h2T[:].rearrange("p j t -> p (j t)"), PS[:, 2048:3072], ['P4'], ['h2T'])
                for j in range(8):
                    MM(PS[:, 3072:3088], h2T[:, j, :], rwf[:, j, :], ['h2T', 'rwf'], ['P6'], start=(j == 0), stop=(j == 7))
                S.op('dve', lambda e: e.tensor_reduce(out=lmax[:], in_=PS[:, 3072:3088], axis=AX.X, op=ALU.max), reads=['P6'], writes=['lmax'])
                S.op('dve', lambda e: e.tensor_scalar(out=lmax[:], in0=lmax[:], scalar1=-1.0, scalar2=None, op0=ALU.mult), reads=['lmax'], writes=['lmax'])
                ACTF(lex[:], PS[:, 3072:3088], AF.Exp, ['P6', 'lmax'], ['lex'], bias=lmax[:])
                S.op('dve', lambda e: e.tensor_reduce(out=lsum[:], in_=lex[:], axis=AX.X, op=ALU.add), reads=['lex'], writes=['lsum'])
                S.op('dve', lambda e: e.reciprocal(out=lsum[:], in_=lsum[:]), reads=['lsum'], writes=['lsum'])
                S.op('dve', lambda e, i=i: e.tensor_scalar(out=aff_all[:, i, :], in0=lex[:], scalar1=lsum[:], scalar2=None, op0=ALU.mult),
                     reads=['lex', 'lsum'], writes=['aff_all'])
            if dbg:
                o1 = dout("dbg_x1", [TL, D]); o2 = dout("dbg_aff", [128, 512])
                S.dma(lambda e: e.dma_start(out=o1[:, :], in_=x1D[:, :]), reads=['x1D'])
                S.dma(lambda e: e.dma_start(out=o2[:, :], in_=aff_all[:].rearrange("p a b -> p (a b)")), reads=['aff_all'])
            S.barrier()
            S.flush()
        if stop_after <= 4:
            return nc, dbg_outs
        posm = sb(top, "posm", [128, 32, 16], F32)
        gw = sb(top, "gw", [128, 32, 16, 2], BF16)
        onesf = sb(top, "onesf", [128, 128], F32)
        iot = sb(top, "iot", [128, 516], F32)
        S.op('pool', lambda e: e.memset(onesf[:], 1.0), writes=['onesf'])
        S.dma(lambda e: e.dma_start(out=iot[:], in_=iot_d[:, :]), writes=['iot'])

        with contextlib.ExitStack() as ph:
            lo = sb(ph, "lo", [128, 16], F32); hi = sb(ph, "hi", [128, 16], F32); mid = sb(ph, "mid", [128, 16], F32)
            cmpt = sb(ph, "cmpt", [128, 32, 16], F32); cntp = sb(ph, "cntp", [128, 16], F32); ge = sb(ph, "ge", [128, 16], F32)
            dlt = sb(ph, "dlt", [128, 16], F32)
            ustr = sb(ph, "ustr", [128, 128], F32)
            mask = sb(ph, "mask", [128, 32, 16], F32); tot = sb(ph, "tot", [128, 32, 16], F32); cum = sb(ph, "cum", [128, 32, 16], F32)
            glo = sb(ph, "glo", [128, 32, 16], F32); ghi32 = sb(ph, "ghi32", [128, 32, 16], F32)
            S.dma(lambda e: e.dma_start(out=ustr[:], in_=ustr_d[:, :]), writes=['ustr'])
            S.op('pool', lambda e: e.memset(lo[:], 0.0), writes=['lo'])
            S.op('pool', lambda e: e.memset(hi[:], 1.0), writes=['hi'])
            affv = aff_all[:, :, :]
            for it in range(26):
                TTo('dve', mid[:], lo[:], hi[:], ALU.add, ['lo', 'hi'], ['mid'])
                S.op('dve', lambda e: e.tensor_scalar(out=mid[:], in0=mid[:], scalar1=0.5, scalar2=None, op0=ALU.mult), reads=['mid'], writes=['mid'])
                TTo('dve', cmpt[:], affv, mid[:, :].unsqueeze(1).to_broadcast([128, 32, 16]), ALU.is_ge, ['aff_all', 'mid'], ['cmpt'])
                S.op('dve', lambda e: e.tensor_reduce(out=cntp[:], in_=cmpt[:].rearrange("p t e -> p e t"), axis=AX.X, op=ALU.add),
                     reads=['cmpt'], writes=['cntp'])
                MM(PS[:, 0:16], onesf[:], cntp[:], ['onesf', 'cntp'], ['P0'])
                S.op('dve', lambda e: e.tensor_scalar(out=ge[:], in0=PS[:, 0:16], scalar1=511.5, scalar2=None, op0=ALU.is_ge), reads=['P0'], writes=['ge'])
                TTo('dve', dlt[:], mid[:], lo[:], ALU.subtract, ['mid', 'lo'], ['dlt'])
                TTo('dve', dlt[:], dlt[:], ge[:], ALU.mult, ['dlt', 'ge'], ['dlt'])
                TTo('dve', lo[:], lo[:], dlt[:], ALU.add, ['lo', 'dlt'], ['lo'])
                TTo('dve', dlt[:], hi[:], mid[:], ALU.subtract, ['hi', 'mid'], ['dlt'])
                TTo('dve', dlt[:], dlt[:], ge[:], ALU.mult, ['dlt', 'ge'], ['dlt'])
                TTo('dve', hi[:], mid[:], dlt[:], ALU.add, ['mid', 'dlt'], ['hi'])
            TTo('dve', mask[:], affv, lo[:, :].unsqueeze(1).to_broadcast([128, 32, 16]), ALU.is_ge, ['aff_all', 'lo'], ['mask'])
            m2 = mask[:].rearrange("p t e -> p (t e)")
            MM(PS[:, 512:1024], ustr[:], m2, ['ustr', 'mask'], ['P1'])
            MM(PS[:, 1024:1536], onesf[:], m2, ['onesf', 'mask'], ['P2'])
            CP('dve', tot[:].rearrange("p t e -> p (t e)"), PS[:, 1024:1536], ['P2'], ['tot'])
            for e_ in range(16):
                S.op('dve', lambda e, e_=e_: e.tensor_tensor_scan(out=cum[:, :, e_], data0=onesf[:, 0:32], data1=tot[:, :, e_], initial=0.0,
                                                                 op0=ALU.mult, op1=ALU.add), reads=['tot', 'onesf'], writes=['cum'])
            TTo('dve', cum[:], cum[:], tot[:], ALU.subtract, ['cum', 'tot'], ['cum'])
            TTo('dve', cum[:].rearrange("p t e -> p (t e)"), cum[:].rearrange("p t e -> p (t e)"), PS[:, 512:1024], ALU.add, ['cum', 'P1'], ['cum'])
            S.op('dve', lambda e: e.scalar_tensor_tensor(out=posm[:], in0=cum[:], scalar=1.0, in1=mask[:], op0=ALU.add, op1=ALU.mult),
                 reads=['cum', 'mask'], writes=['posm'])
            S.op('dve', lambda e: e.tensor_scalar(out=posm[:], in0=posm[:], scalar1=-1.0, scalar2=None, op0=ALU.add), reads=['posm'], writes=['posm'])
            TTo('dve', glo[:], affv, mask[:], ALU.mult, ['aff_all', 'mask'], ['glo'])
            CP('dve', gw[:, :, :, 0], glo[:], ['glo'], ['gw'])
            CP('dve', ghi32[:], gw[:, :, :, 0], ['gw'], ['ghi32'])
            TTo('dve', gw[:, :, :, 1], glo[:], ghi32[:], ALU.subtract, ['glo', 'ghi32'], ['gw'])
            if dbg:
                o1 = dout("dbg_posm", [128, 512])
                S.dma(lambda e: e.dma_start(out=o1[:, :], in_=posm[:].rearrange("p a b -> p (a b)")), reads=['posm'])
            S.barrier()
            S.flush()
        if stop_after <= 5:
            return nc, dbg_outs

        with contextlib.ExitStack() as ph:
            h2_all = sb(ph, "h2_all", [128, 32, D], BF16)
            for i in range(32):
                S.dma(lambda e, i=i: e.dma_start(out=h2_all[:, i, :], in_=h2D[i * 128:(i + 1) * 128, :]), reads=['h2D'], writes=['h2_all'])
            OH = sb(ph, "OH", [128, 32, 512], BF16)
            xinT = sb(ph, "xinT", [128, 8, 512], BF16); hidT = sb(ph, "hidT", [128, 8, 512], BF16)
            wgb = sb(ph, "wgb", [128, 8, D], BF16); wub = sb(ph, "wub", [128, 8, D], BF16); wdb = sb(ph, "wdb", [128, 8, D], BF16)
            wsg = [sb(ph, "wsg%d" % i, [128, D], F32) for i in range(6)]
            gcs = sb(ph, "gcs", [128, 4], F32); sgt = sb(ph, "sgt", [128, 512], F32); sgts = [sgt, sb(ph, "sgt1", [128, 512], F32)]
            ywt = sb(ph, "ywt", [128, 4, D], BF16)
            wi = 0
            for ex_ in range(int(os.environ.get('K_NE', 16))):
                for i in range(32):
                    S.op('dve' if i % 2 else 'pool', lambda e, i=i, ex_=ex_: e.tensor_scalar(
                        out=OH[:, i, :], in0=iot[:, 0:512], scalar1=posm[:, i, ex_:ex_ + 1], scalar2=None, op0=ALU.is_equal),
                        reads=['iot', 'posm'], writes=['OH'])
                for j in range(8):
                    pb = P[j % 2]; pk = 'P%d' % (j % 2)
                    for i in range(32):
                        MM(pb, h2_all[:, i, j * 128:(j + 1) * 128], OH[:, i, :], ['h2_all', 'OH'], [pk], start=(i == 0), stop=(i == 31))
                    CP('act' if j % 2 else 'dve', xinT[:, j, :], pb, [pk], ['xinT'])
                for (wsrc, wdst, wkey) in ((wg_d, wgb, 'wgb'), (wu_d, wub, 'wub'), (wd_d, wdb, 'wdb')):
                    for j in range(8):
                        w = wsg[wi % 6]; wk = 'wsg%d' % (wi % 6)
                        S.dma(lambda e, w=w, wsrc=wsrc, ex_=ex_, j=j: e.dma_start(out=w[:], in_=wsrc[ex_, j * 128:(j + 1) * 128, :]), writes=[wk])
                        CP(('dve', 'pool', 'act')[wi % 3], wdst[:, j, :], w[:], [wk], [wkey])
                        wi += 1
                gcp = PS[:, 1024:1032].rearrange("p (c k) -> p c k", k=2)
                for ct in range(4):
                    for i in range(32):
                        MM(gcp[:, ct, :], OH[:, i, ct * 128:(ct + 1) * 128], gw[:, i, ex_, :], ['OH', 'gw'], ['P2'],
                           start=(i == 0 and ct == 0), stop=(i == 31 and ct == 3))
                TTo('dve', gcs[:], gcp[:, :, 0], gcp[:, :, 1], ALU.add, ['P2'], ['gcs']) if False else None
                CP('dve', sgt[:, 0:8], PS[:, 1024:1032], ['P2'], ['sgt0'])
                TTo('dve', gcs[:], sgt[:, 0:8].rearrange("p (c k) -> p c k", k=2)[:, :, 0], sgt[:, 0:8].rearrange("p (c k) -> p c k", k=2)[:, :, 1],
                    ALU.add, ['sgt0'], ['gcs'])
                for fc in range(8):
                    bg_ = 4 - 2 * (fc % 2); bu_ = 5 - 2 * (fc % 2)
                    sgt_ = sgts[fc % 2]; sgk_ = 'sgt%d' % (fc % 2)
                    for j in range(8):
                        MM(P[bg_], wgb[:, j, fc * 128:(fc + 1) * 128], xinT[:, j, :], ['wgb', 'xinT'], ['P%d' % bg_], start=(j == 0), stop=(j == 7))
                    for j in range(8):
                        MM(P[bu_], wub[:, j, fc * 128:(fc + 1) * 128], xinT[:, j, :], ['wub', 'xinT'], ['P%d' % bu_], start=(j == 0), stop=(j == 7))
                    ACTF(sgt_[:], P[bg_], AF.Silu, ['P%d' % bg_], [sgk_])
                    TTo('dve', hidT[:, fc, :], sgt_[:], P[bu_], ALU.mult, [sgk_, 'P%d' % bu_], ['hidT'])
                for ct in range(4):
                    for half in range(2):
                        pb = P[6 + half]; pk = 'P%d' % (6 + half)
                        for fc in range(8):
                            MM(pb, hidT[:, fc, ct * 128:(ct + 1) * 128], wdb[:, fc, half * 512:(half + 1) * 512], ['hidT', 'wdb'], [pk],
                               start=(fc == 0), stop=(fc == 7))
                        S.op('act', lambda e, ct=ct, half=half, pb=pb: e.activation(out=ywt[:, ct, half * 512:(half + 1) * 512], in_=pb,
                                                                                  func=AF.Copy, scale=gcs[:, ct:ct + 1]),
                             reads=[pk, 'gcs'], writes=['ywt'])
                S.dma(lambda e, ex_=ex_: e.dma_start(out=ywD[ex_].rearrange("(c p) n -> p c n", p=128), in_=ywt[:]), reads=['ywt'], writes=['ywD'])
            if dbg:
                o1 = dout("dbg_yw", [16 * 512, D], BF16)
                S.dma(lambda e: e.dma_start(out=o1[:, :], in_=ywD.rearrange("e c n -> (e c) n")), reads=['ywD'])
            S.barrier()
            S.flush()
        if stop_after <= 6:
            return nc, dbg_outs

        with contextlib.ExitStack() as ph:
            yw_all = sb(ph, "yw_all", [128, 16, 4, D], BF16)
            for ex_ in range(16):
                S.dma(lambda e, ex_=ex_: e.dma_start(out=yw_all[:, ex_, :, :], in_=ywD[ex_].rearrange("(c p) n -> p c n", p=128)),
                      reads=['ywD'], writes=['yw_all'])
            cst = {}
            for nm, src in (("gt2", modd[0:1, 5120:6144]), ("ln2g", ln2_d[0:1, :]), ("ln2b", ln2_d[1:2, :])):
                cst[nm] = sb(ph, nm, [128, D], F32)
                S.dma(lambda e, nm=nm, src=src: e.dma_start(out=cst[nm][:], in_=src.partition_broadcast(128)), reads=['modd'], writes=[nm])
            dg = sb(ph, "dg", [128, 4, 128], F32)
            OHTs = [sb(ph, "OHT%d" % q_, [128, 4, 2048], BF16) for q_ in range(2)]
            x1ls = [sb(ph, "x1l%d" % q_, [128, D], F32) for q_ in range(2)]
            tr2s = [sb(ph, "tr2%d" % q_, [128, D], F32) for q_ in range(2)]
            xos = [sb(ph, "xo%d" % q_, [128, D], F32) for q_ in range(2)]
            stats = sb(ph, "statsF", [128, 2, 6], F32); mv = sb(ph, "mvF", [128, 2], F32)
            rstd = sb(ph, "rstdF", [128, 1], F32); nb = sb(ph, "nbF", [128, 1], F32)
            lntF = (stats, mv, rstd, nb, 'F')
            nFT = int(os.environ.get('K_FT', 32))

            def f_front(i):
                q_ = i % 2
                OHT = OHTs[q_]; kOHT = 'OHT%d' % q_
                for eg in range(4):
                    for k_ in range(4):
                        ex_ = eg * 4 + k_
                        S.op('dve' if k_ % 2 else 'pool', lambda e, k_=k_, ex_=ex_, i=i: e.tensor_scalar(
                            out=dg[:, k_, :], in0=identf[:], scalar1=posm[:, i, ex_:ex_ + 1], scalar2=None, op0=ALU.mult),
                            reads=['identf', 'posm'], writes=['dg'])
                    MM(PS[:, eg * 512:(eg + 1) * 512], onesf[:], dg[:].rearrange("p a t -> p (a t)"), ['onesf', 'dg'], ['P%d' % eg])

            def f_cmp(i):
                q_ = i % 2
                OHT = OHTs[q_]; kOHT = 'OHT%d' % q_
                for ct in range(4):
                    S.op('dve', lambda e, ct=ct, OHT=OHT: e.tensor_scalar(out=OHT[:, ct, :], in0=PS[:, 0:2048], scalar1=iot[:, 512 + ct:513 + ct],
                                                                         scalar2=None, op0=ALU.is_equal),
                         reads=['P0', 'P1', 'P2', 'P3', 'iot'], writes=[kOHT])

            def f_back(i):
                t0 = i * 128
                q_ = i % 2
                OHT = OHTs[q_]; x1l = x1ls[q_]; tr2 = tr2s[q_]; xo = xos[q_]
                kOHT = 'OHT%d' % q_; kx1l = 'x1l%d' % q_; ktr2 = 'tr2%d' % q_; kxo = 'xo%d' % q_
                S.dma(lambda e, t0=t0, x1l=x1l: e.dma_start(out=x1l[:], in_=x1D[t0:t0 + 128, :]), reads=['x1D'], writes=[kx1l])
                for half in range(2):
                    pb = P[4 + half]; pk = 'P%d' % (4 + half)
                    n = 0
                    for ex_ in range(16):
                        for ct in range(4):
                            MM(pb, OHT[:, ct, ex_ * 128:(ex_ + 1) * 128], yw_all[:, ex_, ct, half * 512:(half + 1) * 512], [kOHT, 'yw_all'], [pk],
                               start=(n == 0), stop=(n == 63))
                            n += 1
                if i + 1 < nFT:
                    f_cmp(i + 1)
                TTo('dve', tr2[:], PS[:, 2048:3072], cst['gt2'][:], ALU.mult, ['P4', 'P5', 'gt2'], [ktr2])
                S.op('dve', lambda e, tr2=tr2, x1l=x1l: e.scalar_tensor_tensor(out=tr2[:], in0=x1l[:], scalar=ALPHA, in1=tr2[:], op0=ALU.mult, op1=ALU.add),
                     reads=[kx1l, ktr2], writes=[ktr2])
                ln_stats(lntF, tr2, ktr2, eps5, 'eps5')
                S.op('act', lambda e, xo=xo, tr2=tr2: e.activation(out=xo[:], in_=tr2[:], func=AF.Identity, bias=nb[:], scale=rstd[:]),
                     reads=[ktr2, 'Fnb', 'Frstd'], writes=[kxo])
                TTo('pool', xo[:], xo[:], cst['ln2g'][:], ALU.mult, [kxo, 'ln2g'], [kxo])
                TTo('dve', xo[:], xo[:], cst['ln2b'][:], ALU.add, [kxo, 'ln2b'], [kxo])
                S.dma(lambda e, t0=t0, xo=xo: e.dma_start(out=out_d[t0:t0 + 128, :], in_=xo[:]), reads=[kxo], writes=['out'])

            f_front(0)
            f_cmp(0)
            for i in range(nFT):
                if i + 1 < nFT:
                    f_front(i + 1)
                f_back(i)
            S.barrier()
            S.flush()
    return nc, dbg_outs


def host_inputs(inp, b):
    f = np.float32
    m = {}
    m["x"] = np.ascontiguousarray(inp["x"][b], dtype=f)
    m["ctx"] = np.ascontiguousarray(inp["ctx"][b], dtype=f)
    m["cc"] = np.ascontiguousarray(np.stack([inp["c"][b].reshape(8, 128).T, inp["c_ctx"].reshape(8, 128).T], -1).reshape(128, 16), dtype=f)
    m["w_ada"] = np.ascontiguousarray(inp["w_ada"][0], dtype=f)
    m["b_ada"] = np.ascontiguousarray(inp["b_ada"][0].reshape(1, -1), dtype=f)
    m["w_in"] = np.ascontiguousarray(inp["w_in"][0], dtype=f)
    m["qg"] = np.ascontiguousarray(np.tile(inp["q_gain"][0], 8).reshape(1, 512), dtype=f)
    m["kg"] = np.ascontiguousarray(np.tile(inp["k_gain"][0], 2).reshape(1, 128), dtype=f)
    t = np.arange(TL)
    pos = np.stack([t // 64, t % 64], -1).astype(np.float32)
    inv = (10000.0 ** (-np.arange(16, dtype=np.float32) / 16)).astype(np.float32)
    ang = pos[:, :, None] * inv[None, None, :]
    cs = np.cos(ang).astype(f); sn = np.sin(ang).astype(f)
    cos2 = np.stack([cs, cs], 2).reshape(TL, 64)
    sin2 = np.stack([-sn, sn], 2).reshape(TL, 64)
    m["cosT"] = np.ascontiguousarray(np.tile(cos2, (1, 8)), dtype=f)
    m["sinS"] = np.ascontiguousarray(np.tile(sin2, (1, 8)), dtype=f)
    m["ident"] = np.eye(128, dtype=f)
    def kh(v):
        return np.asarray(v, dtype=f).reshape(8, 64).T
    mu = inp["tshift_mu"][0]
    cols = [kh(mu[0:512]), kh(mu[512:1024]), kh(mu[1024:1536]), kh(inp["k_k"][0]), kh(inp["k_a"][0]), kh(inp["r_k"][0].reshape(-1)),
            kh(inp["decay_w0"][0, 0]), kh(inp["decay_w0"][0, 1]), kh(inp["iclr_a0"][0, 0]), kh(inp["iclr_a0"][0, 1])]
    m["rp"] = np.ascontiguousarray(np.stack(cols, -1).reshape(64, 80), dtype=f)
    lmu = np.zeros((128, 3), f)
    lmu[0:64, 0] = mu[1536:1600]; lmu[0:64, 1] = mu[1600:1664]; lmu[:, 2] = mu[1664:1792]
    m["lmu"] = lmu
    m["decup"] = np.ascontiguousarray(np.concatenate([inp["decay_up"][0, 0], inp["decay_up"][0, 1]], 1), dtype=f)
    m["iclup"] = np.ascontiguousarray(np.concatenate([inp["iclr_up"][0, 0], inp["iclr_up"][0, 1]], 1), dtype=f)
    m["gateup"] = np.ascontiguousarray(inp["gate_up"][0], dtype=f)
    hh = np.repeat(np.arange(8), 16); tt = np.tile(np.arange(16), 8)
    same = hh[:, None] == hh[None, :]
    msf = (same & (tt[:, None] < tt[None, :])).astype(f); mif = (same & (tt[:, None] <= tt[None, :])).astype(f)
    msb = msf.T.copy(); mib = mif.T.copy()
    m["mk4"] = np.ascontiguousarray(np.concatenate([msf, mif, msf, mif, msb, mib, msb, mib], 1), dtype=f)
    m["mn1"] = np.ascontiguousarray(np.concatenate([msb, msf], 1), dtype=f)
    m["bm"] = np.ascontiguousarray((hh[:, None] == np.repeat(np.arange(8), 64)[None, :]).astype(f))
    rst = np.ones((64, 1024), f); rst[:, ::16] = 0.0
    m["rst"] = rst
    m["w_out"] = np.ascontiguousarray(inp["w_out"][0], dtype=f)
    m["lnx"] = np.ascontiguousarray(np.stack([inp["lnx_g"][0], inp["lnx_b"][0]]), dtype=f)
    m["ln1"] = np.ascontiguousarray(np.stack([inp["ln1_g"][0], inp["ln1_b"][0]]), dtype=f)
    m["ln2"] = np.ascontiguousarray(np.stack([inp["ln2_g"][0], inp["ln2_b"][0]]), dtype=f)
    m["router_w"] = np.ascontiguousarray(inp["router_w"][0], dtype=f)
    m["exp_w_gate"] = np.ascontiguousarray(inp["exp_w_gate"][0], dtype=f)
    m["exp_w_up"] = np.ascontiguousarray(inp["exp_w_up"][0], dtype=f)
    m["exp_w_down"] = np.ascontiguousarray(inp["exp_w_down"][0], dtype=f)
    iot = np.zeros((128, 516), f)
    iot[:, 0:512] = np.arange(512, dtype=f)[None, :]
    iot[:, 512:516] = np.arange(128, dtype=f)[:, None] + 128.0 * np.arange(4, dtype=f)[None, :]
    m["iot"] = iot
    m["ustr"] = np.triu(np.ones((128, 128), f), 1)
    return m


_NC_CACHE = {}


def kernel(**inputs):
    inp = {k: np.asarray(v) for k, v in inputs.items()}
    if "full" not in _NC_CACHE:
        _NC_CACHE["full"] = build_nc()[0]
    nc = _NC_CACHE["full"]
    in_maps = [host_inputs(inp, c // 2) for c in range(8)]
    res = run_bass_kernel_spmd(nc, in_maps, core_ids=list(range(8)))
    out = np.stack([res.results[2 * b]["out"] for b in range(4)], 0).astype(np.float32)
    return out
```
